# Optimizing a Trainium2 kernel written in Bass

```python
import jax, jax.numpy as jnp
from jax import lax
import numpy as np


D_MODEL = 1024
BATCH = 8
SEQ = 2048
DEPTH = 2

N_EVEN = (DEPTH + 1) // 2
N_ODD = DEPTH // 2
D_FF = 2816
EPS = 1e-6

RWKV_HEADS = 8
RWKV_HD = 64
RWKV_DIM = RWKV_HEADS * RWKV_HD
LORA_W = 64
LORA_A = 64
LORA_G = 128
RWKV_COLS = 3 * RWKV_DIM + LORA_W + LORA_A + LORA_G
RWKV_SPLITS = (RWKV_DIM, 2 * RWKV_DIM, 3 * RWKV_DIM, 3 * RWKV_DIM + LORA_W, 3 * RWKV_DIM + LORA_W + LORA_A)
LNX_EPS = 64e-5

MOBA_HEADS = 8
MOBA_HD = 64
MOBA_DIM = MOBA_HEADS * MOBA_HD
MOBA_BLOCK = 256
MOBA_TOPK = 3
Q_CHUNK = 16
MOBA_COLS = 3 * MOBA_DIM

EVEN_IN = RWKV_COLS + MOBA_COLS
EVEN_MIX = RWKV_DIM + MOBA_DIM

HG_HEADS = 8
HG_DK = 128
HG_DV = 128
HG_FDIM = HG_HEADS * HG_DK
HG_VDIM = HG_HEADS * HG_DV
HG_CHUNK = 64
ODD_IN = 2 * HG_FDIM + 2 * HG_VDIM

kernel_name = 'hybrid_rwkv7_moba_hgrn2_macaron'


def rmsnorm(x, g):
    xf = x.astype(jnp.float32)
    y = xf * lax.rsqrt(jnp.mean(xf * xf, axis=-1, keepdims=True) + EPS)
    return (y * g).astype(x.dtype)


def swiglu(h, w_gate, w_up, w_down):
    return (jax.nn.silu(h @ w_gate) * (h @ w_up)) @ w_down


def _shift_prev(t):
    return jnp.pad(t, ((0, 0), (1, 0), (0, 0)))[:, :-1]


def _heads(t, n_heads):
    return t.reshape(t.shape[0], t.shape[1], n_heads, -1)


def rwkv7_mix(p, mu, w0, w2, a0, a2, g2, k_k, k_a, r_k, lnx_w, lnx_b):
    bsz, seq, _ = p.shape
    f32 = jnp.float32
    p = p + (_shift_prev(p) - p) * mu
    r, k, v, w_lr, a_lr, g_lr = jnp.split(p, RWKV_SPLITS, axis=-1)
    w_raw = -jax.nn.softplus(-(w0 + jnp.tanh(w_lr) @ w2)) - 0.5
    decay = jnp.exp(-jnp.exp(w_raw.astype(f32)))
    a = jax.nn.sigmoid(a0 + a_lr @ a2)
    g = jax.nn.sigmoid(g_lr) @ g2
    kk = _heads(k * k_k, RWKV_HEADS).astype(f32)
    kk = kk * lax.rsqrt(jnp.maximum(jnp.sum(kk * kk, axis=-1, keepdims=True), 1e-24))
    k = k * (1.0 + (a - 1.0) * k_a)
    rh, kh, vh, ah, wh = [_heads(t, RWKV_HEADS).astype(f32) for t in (r, k, v, a, decay)]

    def step(state, inp):
        r_t, w_t, k_t, v_t, kk_t, a_t = inp
        sa = jnp.einsum('bhvk,bhk->bhv', state, -kk_t)
        state = (state * w_t[:, :, None, :]
                 + jnp.einsum('bhv,bhk->bhvk', sa, kk_t * a_t)
                 + jnp.einsum('bhv,bhk->bhvk', v_t, k_t))
        return state, jnp.einsum('bhvk,bhk->bhv', state, r_t)

    tm = lambda t: jnp.moveaxis(t, 1, 0)
    s0 = jnp.zeros((bsz, RWKV_HEADS, RWKV_HD, RWKV_HD), f32)
    _, y = lax.scan(step, s0, (tm(rh), tm(wh), tm(kh), tm(vh), tm(kk), tm(ah)))
    y = jnp.moveaxis(y, 0, 1)
    mean = jnp.mean(y, axis=-1, keepdims=True)
    var = jnp.mean(jnp.square(y - mean), axis=-1, keepdims=True)
    y = ((y - mean) * lax.rsqrt(var + LNX_EPS)).reshape(bsz, seq, RWKV_DIM) * lnx_w + lnx_b
    bonus = jnp.sum(rh * kh * r_k, axis=-1, keepdims=True) * vh
    y = y + bonus.reshape(bsz, seq, RWKV_DIM)
    return (y * g).astype(p.dtype)


def moba_mix(p, slopes):
    bsz, seq, _ = p.shape
    f32 = jnp.float32
    q, k, v = [jnp.moveaxis(_heads(t, MOBA_HEADS), 2, 1) for t in jnp.split(p, 3, axis=-1)]
    q = q * (MOBA_HD ** -0.5)
    n_blk = -(-seq // MOBA_BLOCK)
    pad = n_blk * MOBA_BLOCK - seq
    padding = ((0, 0), (0, 0), (0, pad), (0, 0))
    kb = jnp.pad(k, padding).reshape(bsz, MOBA_HEADS, n_blk, MOBA_BLOCK, MOBA_HD)
    vb = jnp.pad(v, padding).reshape(bsz, MOBA_HEADS, n_blk, MOBA_BLOCK, MOBA_HD)
    pos = jnp.arange(seq, dtype=jnp.int32)
    own = pos // MOBA_BLOCK
    own_idx = jnp.broadcast_to(own[:, None], (bsz, MOBA_HEADS, seq, 1))
    n_sel = min(MOBA_TOPK, n_blk - 1)
    if n_sel > 0:
        k_mean = jnp.mean(kb, axis=3)
        gate = jnp.einsum('bhsd,bhnd->bhsn', q, k_mean).astype(f32)
        fully_past = jnp.arange(n_blk, dtype=jnp.int32)[None, :] < own[:, None]
        gate = jnp.where(fully_past, gate, -jnp.inf)
        _, top_idx = lax.top_k(gate, n_sel)
        top_idx = top_idx.astype(jnp.int32)
        idx = jnp.concatenate([top_idx, own_idx], axis=-1)
        valid = jnp.concatenate([top_idx < own[:, None], jnp.ones(own_idx.shape, bool)], axis=-1)
    else:
        idx = own_idx
        valid = jnp.ones(own_idx.shape, bool)

    n_chunk = seq // Q_CHUNK

    def to_chunks(t):
        return jnp.moveaxis(t.reshape(t.shape[:2] + (n_chunk, Q_CHUNK) + t.shape[3:]), 2, 0)

    b_i = jnp.arange(bsz)[:, None, None, None]
    h_i = jnp.arange(MOBA_HEADS)[None, :, None, None]
    offs = jnp.arange(MOBA_BLOCK, dtype=jnp.int32)

    def attend(args):
        q_c, idx_c, valid_c, pos_c = args
        k_g = kb[b_i, h_i, idx_c]
        v_g = vb[b_i, h_i, idx_c]
        s = jnp.einsum('bhqd,bhqnkd->bhqnk', q_c, k_g).astype(f32)
        dist = pos_c[:, None, None] - (idx_c[..., None] * MOBA_BLOCK + offs)
        s = s - slopes[:, None, None, None] * dist.astype(f32)
        s = jnp.where(valid_c[..., None] & (dist >= 0), s, -jnp.inf)
        pr = jax.nn.softmax(s.reshape(s.shape[:3] + (-1,)), axis=-1).reshape(s.shape)
        return jnp.einsum('bhqnk,bhqnkd->bhqd', pr.astype(v_g.dtype), v_g)

    o = lax.map(attend, (to_chunks(q), to_chunks(idx), to_chunks(valid), pos.reshape(n_chunk, Q_CHUNK)))
    o = jnp.moveaxis(o, 0, 2).reshape(bsz, MOBA_HEADS, seq, MOBA_HD)
    return jnp.moveaxis(o, 1, 2).reshape(bsz, seq, MOBA_DIM).astype(p.dtype)


def hgrn2_mix(p, lb, norm_w):
    bsz, seq, _ = p.shape
    f32 = jnp.float32
    q, f_raw, i, g = jnp.split(p, 4, axis=-1)
    f_raw = f_raw.astype(f32)
    log_f = jnp.log(lb + (1.0 - lb) * jax.nn.sigmoid(f_raw))
    k = (1.0 - lb) * jax.nn.sigmoid(-f_raw)
    n_chunk = seq // HG_CHUNK

    def chunks(t):
        t = t.astype(f32).reshape(bsz, n_chunk, HG_CHUNK, HG_HEADS, -1)
        return jnp.transpose(t, (1, 0, 3, 2, 4))

    causal = jnp.tril(jnp.ones((HG_CHUNK, HG_CHUNK), bool))[:, :, None]

    def step(state, inp):
        q_c, k_c, v_c, lf_c = inp
        b = jnp.cumsum(lf_c, axis=2)
        decay = jnp.exp(jnp.where(causal, b[:, :, :, None, :] - b[:, :, None, :, :], -jnp.inf))
        scores = jnp.einsum('bhtk,bhtsk,bhsk->bhts', q_c, decay, k_c)
        o = (jnp.einsum('bhts,bhsv->bhtv', scores, v_c)
             + jnp.einsum('bhtk,bhkv->bhtv', q_c * jnp.exp(b), state))
        b_last = b[:, :, -1:, :]
        state = (jnp.exp(b_last[:, :, 0, :])[..., None] * state
                 + jnp.einsum('bhsk,bhsv->bhkv', k_c * jnp.exp(b_last - b), v_c))
        return state, o

    s0 = jnp.zeros((bsz, HG_HEADS, HG_DK, HG_DV), f32)
    _, o = lax.scan(step, s0, (chunks(q), chunks(k), chunks(i), chunks(log_f)))
    o = jnp.transpose(o, (1, 0, 3, 2, 4)).reshape(bsz, seq, HG_HEADS, HG_DV)
    o = o * lax.rsqrt(jnp.mean(o * o, axis=-1, keepdims=True) + EPS)
    o = o.reshape(bsz, seq, HG_VDIM) * norm_w * jax.nn.sigmoid(g.astype(f32))
    return o.astype(p.dtype)


def setup_inputs(seed: int = 0) -> dict:
    key = jax.random.key(seed)
    ks = jax.random.split(key, 32)
    f32 = jnp.float32
    nrm = lambda k, shape, scale: scale * jax.random.normal(k, shape, f32)
    D, F = D_MODEL, D_FF
    return {
        'x': jax.random.normal(ks[0], (BATCH, SEQ, D), f32),
        'norm_g': 1.0 + nrm(ks[1], (DEPTH, 3, D), 0.02),
        'ffn1_wg': nrm(ks[2], (DEPTH, D, F), D ** -0.5),
        'ffn1_wu': nrm(ks[3], (DEPTH, D, F), D ** -0.5),
        'ffn1_wd': nrm(ks[4], (DEPTH, F, D), F ** -0.5),
        'ffn2_wg': nrm(ks[5], (DEPTH, D, F), D ** -0.5),
        'ffn2_wu': nrm(ks[6], (DEPTH, D, F), D ** -0.5),
        'ffn2_wd': nrm(ks[7], (DEPTH, F, D), F ** -0.5),
        'ev_w_in': nrm(ks[8], (N_EVEN, D, EVEN_IN), D ** -0.5),
        'ev_w_out': nrm(ks[9], (N_EVEN, EVEN_MIX, D), EVEN_MIX ** -0.5),
        'rw_mu': jax.random.uniform(ks[10], (N_EVEN, RWKV_COLS), f32),
        'rw_w0': jax.random.uniform(ks[11], (N_EVEN, RWKV_DIM), f32, -5.0, 1.0),
        'rw_w2': nrm(ks[12], (N_EVEN, LORA_W, RWKV_DIM), 0.1),
        'rw_a0': nrm(ks[13], (N_EVEN, RWKV_DIM), 0.1),
        'rw_a2': nrm(ks[14], (N_EVEN, LORA_A, RWKV_DIM), LORA_A ** -0.5),
        'rw_g2': nrm(ks[15], (N_EVEN, LORA_G, RWKV_DIM), LORA_G ** -0.5),
        'rw_k_k': 0.85 + nrm(ks[16], (N_EVEN, RWKV_DIM), 0.05),
        'rw_k_a': 1.0 + nrm(ks[17], (N_EVEN, RWKV_DIM), 0.05),
        'rw_r_k': nrm(ks[18], (N_EVEN, RWKV_HEADS, RWKV_HD), 0.1),
        'rw_lnx_w': 1.0 + nrm(ks[19], (N_EVEN, RWKV_DIM), 0.02),
        'rw_lnx_b': nrm(ks[20], (N_EVEN, RWKV_DIM), 0.02),
        'od_w_in': nrm(ks[21], (N_ODD, D, ODD_IN), D ** -0.5),
        'od_w_out': nrm(ks[22], (N_ODD, HG_VDIM, D), HG_VDIM ** -0.5),
        'hg_norm_w': 1.0 + nrm(ks[23], (N_ODD, HG_VDIM), 0.02),
        'hg_lb_logits': nrm(ks[24], (DEPTH, HG_FDIM), 0.1),
        'final_g': 1.0 + nrm(ks[25], (D,), 0.02),
    }


def reference(x, norm_g, ffn1_wg, ffn1_wu, ffn1_wd, ffn2_wg, ffn2_wu, ffn2_wd,
              ev_w_in, ev_w_out, rw_mu, rw_w0, rw_w2, rw_a0, rw_a2, rw_g2, rw_k_k, rw_k_a,
              rw_r_k, rw_lnx_w, rw_lnx_b, od_w_in, od_w_out, hg_norm_w, hg_lb_logits, final_g):
    f32 = jnp.float32
    slopes = jnp.exp2(-8.0 * jnp.arange(1, MOBA_HEADS + 1, dtype=f32) / MOBA_HEADS)
    lb_sm = jax.nn.softmax(hg_lb_logits.astype(f32), axis=0)
    lb_table = jnp.cumsum(lb_sm, axis=0) - lb_sm[0]
    for l in range(DEPTH):
        x = x + 0.5 * swiglu(rmsnorm(x, norm_g[l, 0]), ffn1_wg[l], ffn1_wu[l], ffn1_wd[l])
        h = rmsnorm(x, norm_g[l, 1])
        if l % 2 == 0:
            e = l // 2
            p = h @ ev_w_in[e]
            y_a = rwkv7_mix(p[..., :RWKV_COLS], rw_mu[e], rw_w0[e], rw_w2[e], rw_a0[e], rw_a2[e],
                            rw_g2[e], rw_k_k[e], rw_k_a[e], rw_r_k[e], rw_lnx_w[e], rw_lnx_b[e])
            y_b = moba_mix(p[..., RWKV_COLS:], slopes)
            y = jnp.concatenate([y_a, y_b], axis=-1) @ ev_w_out[e]
        else:
            o = l // 2
            p = h @ od_w_in[o]
            y = hgrn2_mix(p, lb_table[l], hg_norm_w[o]) @ od_w_out[o]
        x = x + y.astype(x.dtype)
        x = x + 0.5 * swiglu(rmsnorm(x, norm_g[l, 2]), ffn2_wg[l], ffn2_wu[l], ffn2_wd[l])
    return rmsnorm(x, final_g)
```

```python
import numpy as np
from contextlib import ExitStack
import concourse.bass as bass
import concourse.mybir as mybir
from concourse.bass_utils import run_bass_kernel_spmd

F32 = mybir.dt.float32
F32R = mybir.dt.float32r
AF = mybir.ActivationFunctionType
ALU = mybir.AluOpType
AX = mybir.AxisListType

D = 1024
S = 2048
FF = 2816
NFC = 22
EPS = 1e-6


class Buf:
    __slots__ = ("lw", "rd", "name")

    def __init__(self, name=""):
        self.lw = None
        self.rd = []
        self.name = name


class Op:
    __slots__ = ("eng", "fn", "deps", "needed", "sem", "val", "is_dma", "prev_dma")

    def __init__(self, eng, fn):
        self.eng = eng
        self.fn = fn
        self.deps = []
        self.needed = False
        self.sem = None
        self.val = 0
        self.is_dma = False
        self.prev_dma = None


ENGS = ("pe", "dve", "act", "pool", "sp")


class Prog:
    NSLOT = 6

    def __init__(self):
        self.ops = {e: [] for e in ENGS}
        self.fence_deps = []
        self.last = {e: None for e in ENGS}
        self.dma_slots = {e: [] for e in ENGS}
        self.dma_count = {e: 0 for e in ENGS}
        self.all_dma_last = {}

    def _collect(self, op, R, W):
        deps = []
        for b in R:
            if b.lw is not None:
                deps.append(b.lw)
        for b in W:
            if b.lw is not None:
                deps.append(b.lw)
            deps.extend(b.rd)
        deps.extend(self.fence_deps)
        seen = set()
        for d in deps:
            if id(d) in seen or d is op:
                continue
            seen.add(id(d))
            if d.eng == "pe" and op.eng == "pe" and not d.is_dma and not op.is_dma:
                continue
            op.deps.append(d)
            d.needed = True
        for b in W:
            b.lw = op
            b.rd = []
        for b in R:
            b.rd.append(op)

    def op(self, eng, fn, R=(), W=()):
        o = Op(eng, fn)
        self._collect(o, R, W)
        self.ops[eng].append(o)
        self.last[eng] = o
        return o

    def dma(self, q, out, in_, R=(), W=()):
        o = Op(q, lambda e: e.dma_start(out=out, in_=in_))
        o.is_dma = True
        o.needed = True
        k = self.dma_count[q]
        self.dma_count[q] += 1
        slot = k % self.NSLOT
        o.sem = ("dma", q, slot)
        o.val = 16 * (k // self.NSLOT + 1)
        slots = self.dma_slots[q]
        if len(slots) <= slot:
            slots.append(None)
        o.prev_dma = slots[slot]
        slots[slot] = o
        self._collect(o, R, W)
        self.ops[q].append(o)
        self.all_dma_last[(q, slot)] = o
        return o

    def fence(self):
        deps = [o for o in self.last.values() if o is not None]
        deps += list(self.all_dma_last.values())
        for d in deps:
            d.needed = True
        self.fence_deps = deps

    def emit(self, nc, es, final_waits):
        sems = {}
        for e in ENGS:
            sems[("eng", e)] = es.enter_context(nc.semaphore("s_" + e))
            for sl in range(len(self.dma_slots[e])):
                sems[("dma", e, sl)] = es.enter_context(nc.semaphore("d_%s_%d" % (e, sl)))
        for e in ENGS:
            cnt = 0
            for o in self.ops[e]:
                if o.is_dma:
                    continue
                o.sem = ("eng", e)
                if o.needed:
                    cnt += 1
                    o.val = cnt
        handles = {"pe": "tensor", "dve": "vector", "act": "scalar", "pool": "gpsimd", "sp": "sync"}
        block = es.enter_context(nc.Block())

        def make(e):
            def body(eng):
                seen = {}
                for o in self.ops[e]:
                    waits = list(o.deps)
                    if o.is_dma and o.prev_dma is not None:
                        waits.append(o.prev_dma)
                    for d in waits:
                        if seen.get(d.sem, 0) < d.val:
                            eng.wait_ge(sems[d.sem], d.val)
                            seen[d.sem] = d.val
                    ins = o.fn(eng)
                    if o.is_dma:
                        ins.then_inc(sems[o.sem], 16)
                    elif o.needed:
                        ins.then_inc(sems[o.sem], 1)
                if e == "sp":
                    for d in final_waits:
                        if seen.get(d.sem, 0) < d.val:
                            eng.wait_ge(sems[d.sem], d.val)
                            seen[d.sem] = d.val
            return body

        for e in ENGS:
            getattr(block, handles[e])(make(e))


def r32(ap):
    return ap


class K:
    def __init__(self, stage):
        self.stage = stage
        self.nc = bass.Bass("TRN2", target_bir_lowering=False)
        self.P = Prog()
        self.es = ExitStack()
        self.wq = 0

    def dram_in(self, name, shape, dt=F32):
        return self.nc.dram_tensor(name, list(shape), dt, kind="ExternalInput").ap()

    def sb(self, name, shape, dt=F32):
        return self.es.enter_context(self.nc.sbuf_tensor(name, list(shape), dt))

    def ps(self, name, shape, dt=F32):
        return self.es.enter_context(self.nc.psum_tensor(name, list(shape), dt))


ALL_STAGES = ("f00", "rwkv", "moba", "f01", "f10", "hgrn", "f11", "final")


def build(stage=ALL_STAGES):
    k = K(stage)
    nc, P, es = k.nc, k.P, k.es
    with es:
        _build(k)
    return nc


def _build(k):
    nc, P = k.nc, k.P
    stage = k.stage
    xT_d = k.dram_in("xT", [128, 8, S])
    pv_d = k.dram_in("pvec", [128, NPV])
    wg_d = [[k.dram_in("wg%d%d" % (l, f), [NFC, 128, 8, 128]) for f in range(2)] for l in range(2)]
    wu_d = [[k.dram_in("wu%d%d" % (l, f), [NFC, 128, 8, 128]) for f in range(2)] for l in range(2)]
    wd_d = [[k.dram_in("wd%d%d" % (l, f), [8, 128, NFC, 128]) for f in range(2)] for l in range(2)]
    odwin_d = k.dram_in("odwin", [32, 128, 8, 128])
    odwout_d = k.dram_in("odwout", [8, 128, 8, 128])
    rwin_d = k.dram_in("rwin", [24, 128, 8, 64])
    rwlo_d = k.dram_in("rwlo", [2, 128, 8, 128])
    rww2_d = k.dram_in("rww2", [64, 512])
    rwa2_d = k.dram_in("rwa2", [64, 512])
    rwg2_d = k.dram_in("rwg2", [128, 512])
    rwout_d = k.dram_in("rwout", [8, 64, 8, 128])
    mbin_d = k.dram_in("mbin", [12, 128, 8, 128])
    mbout_d = k.dram_in("mbout", [8, 64, 8, 128])
    mbkaug_d = k.dram_in("mbkaug", [10, S])
    mbqc_d = k.dram_in("mbqc", [8, 2, S])
    mscr_d = nc.dram_tensor("mscr", [128, 8, S], F32).ap()
    mscr_b = [Buf() for g in range(8)]
    dbg_d = nc.dram_tensor("dbg", [64, 32, 1024], F32, kind="ExternalOutput").ap() if "dbg" in stage else None
    dbg_ops = []
    out_d = nc.dram_tensor("outT", [128, 8, S], F32, kind="ExternalOutput").ap()

    xT = k.sb("xT_sb", [128, 8, S])
    xb = [[Buf("x%d_%d" % (c, g)) for g in range(4)] for c in range(8)]
    pv = k.sb("pv_sb", [128, NPV])
    pvb = Buf("pv")
    ones = k.sb("ones", [128, 128])
    onesb = Buf("ones")
    epsb_t = k.sb("epsc", [128, 4])
    epsb = Buf("eps")
    ARENA = 32800
    arena = k.sb("arena", [128, ARENA])

    pbank = [k.ps("pb%d" % i, [128, 512]) for i in range(8)]
    pbb = [Buf("pb%d" % i) for i in range(8)]

    P.op("pool", lambda e: e.memset(ones[:], 1.0), W=[onesb])
    P.op("pool", lambda e: e.memset(epsb_t[:, 0:1], EPS), W=[epsb])
    P.dma("sp", pv[:], pv_d[:], W=[pvb])
    for c in range(8):
        for g in range(4):
            P.dma("sp", xT[:, c, g * 512:(g + 1) * 512], xT_d[:, c, g * 512:(g + 1) * 512], W=[xb[c][g]])

    ident = k.sb("ident", [128, 128])
    mask4t = k.sb("mask4", [128, 512])
    mask4 = mask4t[:].rearrange("p (c m) -> p c m", c=4)
    resetm = k.sb("resetm", [128, 512])
    lbt = k.sb("lbt", [128, 16])
    cb = Buf("consts")
    lbb = Buf("lb")

    def setup_consts(e):
        e.memset(ident[:], 0.0)
        e.affine_select(out=ident[:], in_=ones[:], pattern=[[1, 128]], compare_op=ALU.is_equal, fill=0.0, base=0, channel_multiplier=-1)
        for i in range(4):
            e.affine_select(out=mask4t[:, i * 128:(i + 1) * 128], in_=ones[:], pattern=[[1, 128]], compare_op=ALU.is_ge, fill=0.0,
                            base=0, channel_multiplier=-1)
            e.memset(mask4t[0:64, i * 128 + 64:(i + 1) * 128], 0.0)
        e.memset(resetm[:], 1.0)
        ins = None
        for i in range(8):
            ins = e.memset(resetm[:, i * 64:i * 64 + 1], 0.0)
        return ins
    P.op("pool", setup_consts, R=[onesb], W=[cb])
    P.op("dve", lambda e: e.tensor_tensor(out=lbt[:, 0:8], in0=pv[:, PV_LBZ + 8:PV_LBZ + 16], in1=pv[:, PV_LBZ:PV_LBZ + 8], op=ALU.subtract),
         R=[pvb], W=[lbb])
    P.op("act", lambda e: e.activation(out=lbt[:, 0:8], in_=lbt[:, 0:8], func=AF.Sigmoid), R=[lbb], W=[lbb])
    P.op("dve", lambda e: e.tensor_scalar(out=lbt[:, 8:16], in0=lbt[:, 0:8], scalar1=-1.0, scalar2=1.0, op0=ALU.mult, op1=ALU.add),
         R=[lbb], W=[lbb])

    mk = k.sb("rwmask", [64, 4 * 512])
    mLs = mk[:, 0:512].rearrange("p (h t) -> p h t", h=8)
    mUs = mk[:, 512:1024].rearrange("p (h t) -> p h t", h=8)
    mUi = mk[:, 1024:1536].rearrange("p (h t) -> p h t", h=8)
    id8 = mk[:, 1536:2048].rearrange("p (h t) -> p h t", h=8)
    omka = k.sb("omka", [64, 8])
    cb2 = Buf("consts2")

    def setup2(e):
        o3 = ones[0:64, :].rearrange("p (a b) -> p a b", a=2)
        e.memset(mk[:], 1.0)
        e.affine_select(out=mLs, in_=mLs, pattern=[[0, 8], [-1, 64]], compare_op=ALU.is_gt, fill=0.0, base=0, channel_multiplier=1)
        e.affine_select(out=mUs, in_=mUs, pattern=[[0, 8], [1, 64]], compare_op=ALU.is_gt, fill=0.0, base=0, channel_multiplier=-1)
        e.affine_select(out=mUi, in_=mUi, pattern=[[0, 8], [1, 64]], compare_op=ALU.is_ge, fill=0.0, base=0, channel_multiplier=-1)
        return e.affine_select(out=id8, in_=id8, pattern=[[0, 8], [1, 64]], compare_op=ALU.is_equal, fill=0.0, base=0, channel_multiplier=-1)
    P.op("pool", setup2, W=[cb2])
    P.op("dve", lambda e: e.tensor_scalar(out=omka[:], in0=pv[0:64, PV_RW + 48:PV_RW + 56], scalar1=-1.0, scalar2=1.0, op0=ALU.mult, op1=ALU.add),
         R=[pvb], W=[cb2])
    P.op("pool", lambda e: e.memset(epsb_t[:, 1:2], 64e-5), W=[epsb])

    def rstd_group(g, rstd_ap, rstd_buf, sq_ap, sq_buf, bank, ndiv=1024.0):
        P.op("act", lambda e: e.activation(out=sq_ap, in_=xT[:, :, g * 512:(g + 1) * 512], func=AF.Square),
             R=[xb[c][g] for c in range(8)], W=[sq_buf])

        def mm(e):
            ins = None
            for c in range(8):
                ins = e.matmul(pbank[bank][:], ones[:], sq_ap[:, c, :], start=(c == 0), stop=(c == 7))
            return ins
        P.op("pe", mm, R=[sq_buf, onesb], W=[pbb[bank]])
        P.op("act", lambda e: e.activation(out=rstd_ap, in_=pbank[bank][:], func=AF.Ln, bias=epsb_t[:, 0:1], scale=1.0 / ndiv),
             R=[pbb[bank], epsb], W=[rstd_buf])
        P.op("act", lambda e: e.activation(out=rstd_ap, in_=rstd_ap, func=AF.Exp, scale=-0.5),
             R=[rstd_buf], W=[rstd_buf])

    def ffn(l, which):
        f = 0 if which == 0 else 1
        gcol = PV_NORMG + (l * 3 + which) * 8
        o = 0
        hT = arena[:, o:o + 8192].rearrange("p (c t) -> p c t", c=8); o += 8192
        act = arena[:, o:o + 11264].rearrange("p (c t) -> p c t", c=11); o += 11264
        sq = arena[:, o:o + 4096].rearrange("p (c t) -> p c t", c=8); o += 4096
        rstd = arena[:, o:o + 512]; o += 512
        sg = [arena[:, o + i * 512:o + (i + 1) * 512] for i in range(2)]; o += 1024
        wgb = [arena[:, o + i * 1024:o + (i + 1) * 1024].rearrange("p (c m) -> p c m", c=8) for i in range(2)]; o += 2048
        wub = [arena[:, o + i * 1024:o + (i + 1) * 1024].rearrange("p (c m) -> p c m", c=8) for i in range(2)]; o += 2048
        wdb = [arena[:, o + i * 1408:o + (i + 1) * 1408].rearrange("p (c m) -> p c m", c=11) for i in range(2)]; o += 2816
        assert o <= ARENA
        hb = [[Buf() for g in range(2)] for c in range(8)]
        actb = [[Buf() for g in range(2)] for c in range(11)]
        sqb, rstdb = Buf(), Buf()
        sgb = [Buf(), Buf()]
        wgbb = [Buf(), Buf()]
        wubb = [Buf(), Buf()]
        wdbb = [Buf(), Buf()]
        wi = 0
        wdi = 0
        pi = 0
        for tb in range(2):
            for gg in range(2):
                g = tb * 2 + gg
                rstd_group(g, rstd, rstdb, sq, sqb, 7)
                for c in range(8):
                    P.op("dve", lambda e, c=c, g=g, gg=gg: e.scalar_tensor_tensor(
                        out=r32(hT[:, c, gg * 512:(gg + 1) * 512]), in0=xT[:, c, g * 512:(g + 1) * 512],
                        scalar=pv[:, gcol + c:gcol + c + 1], in1=rstd, op0=ALU.mult, op1=ALU.mult),
                        R=[xb[c][g], rstdb, pvb], W=[hb[c][gg]])
            for fh in range(2):
                for fl in range(11):
                    fc = fh * 11 + fl
                    s = wi % 2
                    wi += 1
                    P.dma("sp", r32(wgb[s]), wg_d[l][f][fc], W=[wgbb[s]])
                    P.dma("sp", r32(wub[s]), wu_d[l][f][fc], W=[wubb[s]])
                    for gg in range(2):
                        bg, bu = (pi % 2) * 2, (pi % 2) * 2 + 1
                        pi += 1

                        def mmg(e, s=s, gg=gg, bg=bg):
                            ins = None
                            for c in range(8):
                                ins = e.matmul(pbank[bg][:], r32(wgb[s][:, c, :]), r32(hT[:, c, gg * 512:(gg + 1) * 512]),
                                               start=(c == 0), stop=(c == 7))
                            return ins

                        def mmu(e, s=s, gg=gg, bu=bu):
                            ins = None
                            for c in range(8):
                                ins = e.matmul(pbank[bu][:], r32(wub[s][:, c, :]), r32(hT[:, c, gg * 512:(gg + 1) * 512]),
                                               start=(c == 0), stop=(c == 7))
                            return ins
                        P.op("pe", mmg, R=[wgbb[s]] + [hb[c][gg] for c in range(8)], W=[pbb[bg]])
                        P.op("pe", mmu, R=[wubb[s]] + [hb[c][gg] for c in range(8)], W=[pbb[bu]])
                        P.op("act", lambda e, gg=gg, bg=bg: e.activation(out=sg[gg], in_=pbank[bg][:], func=AF.Silu),
                             R=[pbb[bg]], W=[sgb[gg]])
                        P.op("dve", lambda e, gg=gg, bu=bu, fl=fl: e.tensor_tensor(
                            out=r32(act[:, fl, gg * 512:(gg + 1) * 512]), in0=pbank[bu][:], in1=sg[gg], op=ALU.mult),
                            R=[pbb[bu], sgb[gg]], W=[actb[fl][gg]])
                for dc in range(8):
                    s = wdi % 2
                    wdi += 1
                    P.dma("sp", r32(wdb[s]), wd_d[l][f][dc][:, fh * 11:(fh + 1) * 11, :], W=[wdbb[s]])
                    for gg in range(2):
                        g = tb * 2 + gg
                        bo = 4 + (pi % 2)
                        pi += 1

                        def mmd(e, s=s, gg=gg, bo=bo):
                            ins = None
                            for fl in range(11):
                                ins = e.matmul(pbank[bo][:], r32(wdb[s][:, fl, :]), r32(act[:, fl, gg * 512:(gg + 1) * 512]),
                                               start=(fl == 0), stop=(fl == 10))
                            return ins
                        P.op("pe", mmd, R=[wdbb[s]] + [actb[fl][gg] for fl in range(11)], W=[pbb[bo]])
                        P.op("dve", lambda e, dc=dc, g=g, bo=bo: e.scalar_tensor_tensor(
                            out=xT[:, dc, g * 512:(g + 1) * 512], in0=pbank[bo][:], scalar=0.5,
                            in1=xT[:, dc, g * 512:(g + 1) * 512], op0=ALU.mult, op1=ALU.add),
                            R=[pbb[bo], xb[dc][g]], W=[xb[dc][g]])
        P.fence()

    def final_out(norm):
        o = 0
        sq = arena[:, o:o + 4096].rearrange("p (c t) -> p c t", c=8); o += 4096
        rstd = arena[:, o:o + 512]; o += 512
        ob = [arena[:, o + i * 4096:o + (i + 1) * 4096].rearrange("p (c t) -> p c t", c=8) for i in range(2)]; o += 8192
        sqb, rstdb = Buf(), Buf()
        obb = [Buf(), Buf()]
        outs = []
        for g in range(4):
            s = g % 2
            if norm:
                rstd_group(g, rstd, rstdb, sq, sqb, 7)
                for c in range(8):
                    P.op("dve", lambda e, c=c, g=g, s=s: e.scalar_tensor_tensor(
                        out=ob[s][:, c, :], in0=xT[:, c, g * 512:(g + 1) * 512],
                        scalar=pv[:, PV_FINALG + c:PV_FINALG + c + 1], in1=rstd, op0=ALU.mult, op1=ALU.mult),
                        R=[xb[c][g], rstdb, pvb], W=[obb[s]])
                outs.append(P.dma("sp", out_d[:, :, g * 512:(g + 1) * 512], ob[s], R=[obb[s]]))
            else:
                outs.append(P.dma("sp", out_d[:, :, g * 512:(g + 1) * 512], xT[:, :, g * 512:(g + 1) * 512],
                                  R=[xb[c][g] for c in range(8)]))
        return outs


    def hgrn():
        A = Arena(arena, ARENA)
        hT = A.take(4096).rearrange("p (c t) -> p c t", c=8)
        hb = [Buf() for c in range(8)]
        wt = [[A.take(1024).rearrange("p (c m) -> p c m", c=8) for j in range(4)] for s_ in range(2)]
        wtb = [[Buf() for j in range(4)] for s_ in range(2)]
        names = ["qT", "fT", "lf", "kT", "bT", "eb", "qe", "ke", "e2", "sgT", "oTs", "sqo", "rs2", "tmp"]
        T = {n: A.take(512) for n in names}
        TB = {n: Buf(n) for n in names}
        ke2tm = A.take(512).rearrange("p (c m) -> p c m", c=4); ke2tmb = Buf()
        vtm = A.take(512).rearrange("p (c m) -> p c m", c=4); vtmb = Buf()
        scs = A.take(512).rearrange("p (c m) -> p c m", c=4); scsb = Buf()
        ebl = A.take(8); eblb = Buf()
        state = [A.take(1024).rearrange("p (h v) -> p h v", h=8) for i in range(2)]
        stb = [[Buf() for h in range(8)] for i in range(2)]
        yT = A.take(4096).rearrange("p (c t) -> p c t", c=8)
        yb = [Buf() for h in range(8)]
        wo = [A.take(1024).rearrange("p (c m) -> p c m", c=8) for i in range(1)]
        wob = [Buf()]
        sq = A.take(4096).rearrange("p (c t) -> p c t", c=8); sqb = Buf()
        rstd = A.take(512); rstdb = Buf()
        gcol = PV_NORMG + (1 * 3 + 1) * 8
        scur = [0] * 8
        for h in range(8):
            P.op("pool", lambda e, h=h: e.memset(state[0][:, h, :], 0.0), W=[stb[0][h]])
        wi = 0
        woi = 0
        for g in range(4):
            rstd_group(g, rstd, rstdb, sq, sqb, 7)
            for c in range(8):
                P.op("dve", lambda e, c=c, g=g: e.scalar_tensor_tensor(
                    out=hT[:, c, :], in0=xT[:, c, g * 512:(g + 1) * 512],
                    scalar=pv[:, gcol + c:gcol + c + 1], in1=rstd, op0=ALU.mult, op1=ALU.mult),
                    R=[xb[c][g], rstdb, pvb], W=[hb[c]])
            for h in range(8):
                s_ = wi % 2
                wi += 1
                for j in range(4):
                    P.dma("sp", wt[s_][j], odwin_d[j * 8 + h], W=[wtb[s_][j]])

                def proj(j, bank, s_=s_):
                    def mm(e):
                        ins = None
                        for c in range(8):
                            ins = e.matmul(pbank[bank][:], wt[s_][j][:, c, :], hT[:, c, :], start=(c == 0), stop=(c == 7))
                        return ins
                    P.op("pe", mm, R=[wtb[s_][j]] + hb, W=[pbb[bank]])
                proj(0, 0)
                P.op("act", lambda e: e.activation(out=T["qT"], in_=pbank[0][:], func=AF.Copy), R=[pbb[0]], W=[TB["qT"]])
                proj(1, 1)
                P.op("act", lambda e: e.activation(out=T["fT"], in_=pbank[1][:], func=AF.Sigmoid), R=[pbb[1]], W=[TB["fT"]])
                P.op("dve", lambda e, h=h: e.tensor_scalar(out=T["fT"], in0=T["fT"], scalar1=lbt[:, 8 + h:9 + h], scalar2=lbt[:, h:h + 1],
                                                           op0=ALU.mult, op1=ALU.add), R=[TB["fT"], lbb], W=[TB["fT"]])
                P.op("act", lambda e: e.activation(out=T["lf"], in_=T["fT"], func=AF.Ln), R=[TB["fT"]], W=[TB["lf"]])
                P.op("dve", lambda e: e.tensor_scalar(out=T["kT"], in0=T["fT"], scalar1=-1.0, scalar2=1.0, op0=ALU.mult, op1=ALU.add),
                     R=[TB["fT"]], W=[TB["kT"]])
                P.op("dve", lambda e: e.tensor_tensor_scan(out=T["bT"], data0=resetm[:], data1=T["lf"], initial=0.0, op0=ALU.mult, op1=ALU.add),
                     R=[TB["lf"], cb], W=[TB["bT"]])
                P.op("act", lambda e: e.activation(out=T["eb"], in_=T["bT"], func=AF.Exp), R=[TB["bT"]], W=[TB["eb"]])
                P.op("dve", lambda e: e.tensor_tensor(out=T["qe"], in0=T["qT"], in1=T["eb"], op=ALU.mult), R=[TB["qT"], TB["eb"]], W=[TB["qe"]])
                P.op("act", lambda e: e.activation(out=T["eb"], in_=T["bT"], func=AF.Exp, scale=-1.0), R=[TB["bT"], TB["qe"]], W=[TB["eb"]])
                P.op("dve", lambda e: e.tensor_tensor(out=T["ke"], in0=T["kT"], in1=T["eb"], op=ALU.mult), R=[TB["kT"], TB["eb"]], W=[TB["ke"]])

                def e2f(e):
                    ins = None
                    for ci in range(8):
                        ins = e.activation(out=T["e2"][:, ci * 64:(ci + 1) * 64], in_=T["bT"][:, ci * 64:(ci + 1) * 64], func=AF.Exp,
                                           scale=-1.0, bias=T["bT"][:, ci * 64 + 63:ci * 64 + 64])
                    return ins
                P.op("act", e2f, R=[TB["bT"]], W=[TB["e2"]])
                P.op("dve", lambda e: e.tensor_tensor(out=T["e2"], in0=T["e2"], in1=T["kT"], op=ALU.mult), R=[TB["kT"], TB["e2"]], W=[TB["e2"]])
                P.op("act", lambda e: e.activation(out=ebl, in_=T["bT"].rearrange("p (c t) -> p c t", t=64)[:, :, 63], func=AF.Exp),
                     R=[TB["bT"]], W=[eblb])

                def tr(e):
                    ins = None
                    for tt in range(4):
                        ins = e.transpose(pbank[2][:, tt * 128:(tt + 1) * 128], T["e2"][:, tt * 128:(tt + 1) * 128], ident[:])
                    return ins
                P.op("pe", tr, R=[TB["e2"], cb], W=[pbb[2]])
                P.op("act", lambda e: e.activation(out=ke2tm, in_=pbank[2][:].rearrange("p (c m) -> p c m", c=4), func=AF.Copy),
                     R=[pbb[2]], W=[ke2tmb])

                def vproj(e, s_=s_):
                    ins = None
                    for tt in range(4):
                        for c in range(8):
                            ins = e.matmul(pbank[3][:, tt * 128:(tt + 1) * 128], hT[:, c, tt * 128:(tt + 1) * 128], wt[s_][2][:, c, :],
                                           start=(c == 0), stop=(c == 7))
                    return ins
                P.op("pe", vproj, R=[wtb[s_][2]] + hb, W=[pbb[3]])
                P.op("dve", lambda e: e.tensor_copy(out=vtm, in_=pbank[3][:].rearrange("p (c m) -> p c m", c=4)), R=[pbb[3]], W=[vtmb])
                proj(3, 0)
                P.op("act", lambda e: e.activation(out=T["sgT"], in_=pbank[0][:], func=AF.Sigmoid), R=[pbb[0]], W=[TB["sgT"]])

                def scm(e):
                    ins = None
                    for p_ in range(4):
                        ins = e.matmul(pbank[4][:, p_ * 128:(p_ + 1) * 128], T["ke"][:, p_ * 128:(p_ + 1) * 128],
                                       T["qe"][:, p_ * 128:(p_ + 1) * 128], start=True, stop=True)
                    return ins
                P.op("pe", scm, R=[TB["ke"], TB["qe"]], W=[pbb[4]])
                P.op("dve", lambda e: e.tensor_tensor(out=scs, in0=pbank[4][:].rearrange("p (c m) -> p c m", c=4), in1=mask4, op=ALU.mult),
                     R=[pbb[4], cb], W=[scsb])
                for p_ in range(4):
                    for half in range(2):
                        ci = p_ * 2 + half
                        sc = scur[h]
                        cs = p_ * 128 + half * 64

                        def omm(e, p_=p_, half=half, sc=sc, cs=cs, h=h):
                            if half == 0:
                                e.matmul(pbank[5][:, p_ * 128:(p_ + 1) * 128], vtm[:, p_, :], scs[:, p_, :], start=True, stop=False)
                            return e.matmul(pbank[5][:, cs:cs + 64], state[sc][:, h, :], T["qe"][:, cs:cs + 64], start=False, stop=(half == 1))
                        P.op("pe", omm, R=[vtmb, scsb, stb[sc][h], TB["qe"]], W=[pbb[5]])
                        r0 = half * 64
                        P.op("pe", lambda e, p_=p_, r0=r0: e.matmul(pbank[6][:, 0:128], ke2tm[r0:r0 + 64, p_, :], vtm[r0:r0 + 64, p_, :],
                                                                     start=True, stop=True), R=[ke2tmb, vtmb], W=[pbb[6]])
                        P.op("dve", lambda e, sc=sc, h=h, ci=ci: e.scalar_tensor_tensor(
                            out=state[1 - sc][:, h, :], in0=state[sc][:, h, :], scalar=ebl[:, ci:ci + 1], in1=pbank[6][:, 0:128],
                            op0=ALU.mult, op1=ALU.add), R=[stb[sc][h], eblb, pbb[6]], W=[stb[1 - sc][h]])
                        scur[h] = 1 - sc
                P.op("act", lambda e: e.activation(out=T["oTs"], in_=pbank[5][:], func=AF.Copy), R=[pbb[5]], W=[TB["oTs"]])
                P.op("act", lambda e: e.activation(out=T["sqo"], in_=pbank[5][:], func=AF.Square), R=[pbb[5]], W=[TB["sqo"]])
                P.op("pe", lambda e: e.matmul(pbank[7][:], ones[:], T["sqo"], start=True, stop=True), R=[TB["sqo"], onesb], W=[pbb[7]])
                P.op("act", lambda e: e.activation(out=T["rs2"], in_=pbank[7][:], func=AF.Ln, bias=epsb_t[:, 0:1], scale=1.0 / 128.0),
                     R=[pbb[7], epsb], W=[TB["rs2"]])
                P.op("act", lambda e: e.activation(out=T["rs2"], in_=T["rs2"], func=AF.Exp, scale=-0.5), R=[TB["rs2"]], W=[TB["rs2"]])
                P.op("dve", lambda e, h=h: e.scalar_tensor_tensor(out=T["tmp"], in0=T["oTs"], scalar=pv[:, PV_HGNW + h:PV_HGNW + h + 1],
                                                                  in1=T["rs2"], op0=ALU.mult, op1=ALU.mult),
                     R=[TB["oTs"], TB["rs2"], pvb], W=[TB["tmp"]])
                P.op("dve", lambda e, h=h: e.tensor_tensor(out=yT[:, h, :], in0=T["tmp"], in1=T["sgT"], op=ALU.mult),
                     R=[TB["tmp"], TB["sgT"]], W=[yb[h]])
            for dc in range(8):
                s2 = 0
                P.dma("sp", wo[s2], odwout_d[dc], W=[wob[s2]])

                def mo(e, s2=s2):
                    ins = None
                    for h in range(8):
                        ins = e.matmul(pbank[7][:], wo[s2][:, h, :], yT[:, h, :], start=(h == 0), stop=(h == 7))
                    return ins
                P.op("pe", mo, R=[wob[s2]] + yb, W=[pbb[7]])
                P.op("dve", lambda e, dc=dc, g=g: e.tensor_tensor(out=xT[:, dc, g * 512:(g + 1) * 512], in0=pbank[7][:],
                                                                  in1=xT[:, dc, g * 512:(g + 1) * 512], op=ALU.add),
                     R=[pbb[7], xb[dc][g]], W=[xb[dc][g]])
        P.fence()


    def rwkv():
        G = 128
        NG = S // G
        A = Arena(arena, ARENA)
        hT = A.take(8 * (G + 1)).rearrange("p (c t) -> p c t", c=8); hb = Buf()
        sq = A.take(8 * G).rearrange("p (c t) -> p c t", c=8); sqb = Buf()
        rstd = A.take(G); rstdb = Buf()
        wrkv = [A.take(512).rearrange("p (c m) -> p c m", c=8) for i in range(3)]
        wrkvb = [Buf() for i in range(3)]
        wlo = [A.take(1024).rearrange("p (c m) -> p c m", c=8) for i in range(2)]
        wlob = [Buf(), Buf()]
        w2a2 = A.take(512); g2 = A.take(512); wconst = Buf()
        praw = [A.take(G + 1) for i in range(2)]; prawb = [Buf(), Buf()]
        dtmp = [A.take(G) for i in range(2)]; dtmpb = [Buf(), Buf()]
        tw = A.take(G); twb = Buf()
        sg = A.take(G); sgb = Buf()
        QN = ["r", "k", "v", "lw", "a", "kk", "Lw", "e", "at", "bt", "kt", "BhT", "KhT"]
        Q = {n: A.take(8 * G).rearrange("p (h t) -> p h t", h=8) for n in QN}
        QB = {n: Buf(n) for n in QN}
        CN = ["P0", "P1", "PT0", "PT1", "TT", "AakT", "ArbT", "ArkT", "Vtm", "Bhtm", "Khtm", "Xs", "Us", "H0", "H1"]
        Cc = {n: A.take(512).rearrange("p (h t) -> p h t", h=8) for n in CN}
        CB = {n: Buf(n) for n in CN}
        gC = A.take(16).rearrange("p (h c) -> p h c", h=8); gCb = Buf()
        wo = [A.take(1024).rearrange("p (h m) -> p h m", h=8) for i in range(2)]; wob = [Buf(), Buf()]
        gcol = PV_NORMG + (0 * 3 + 1) * 8
        MU, W0, A0, KK, KA, OMKA, RK, LNW, LNB = PV_RW, PV_RW + 24, PV_RW + 32, PV_RW + 40, PV_RW + 48, PV_RW + 56, PV_RW + 64, PV_RW + 72, PV_RW + 80
        MUWA, MUG = PV_RW + 88, PV_RW + 89
        o64 = ones[0:64, 0:64]

        P.dma("sp", w2a2[0:64, :], rww2_d[:, :], W=[wconst])
        P.dma("sp", w2a2[64:128, :], rwa2_d[:, :], W=[wconst])
        P.dma("sp", g2, rwg2_d[:, :], W=[wconst])
        P.op("pool", lambda e: e.memset(Cc["H0"][0:64], 0.0), W=[CB["H0"]])
        P.op("pool", lambda e: e.memset(hT[:, :, 0:1], 0.0), W=[hb])
        hcur = 0
        woi = 0
        pi = 0
        def do_group(g):
            nonlocal hcur, woi, pi
            t0 = g * G
            g4 = t0 // 512
            xg = [xb[c][g4] for c in range(8)]
            if g > 0:
                P.op("dve", lambda e: e.tensor_copy(out=hT[:, :, 0:1], in_=hT[:, :, G:G + 1]), R=[hb], W=[hb])
            P.op("act", lambda e, t0=t0: e.activation(out=sq, in_=xT[:, :, t0:t0 + G], func=AF.Square), R=xg, W=[sqb])

            def mmn(e):
                ins = None
                for c in range(8):
                    ins = e.matmul(pbank[7][:, 0:G], ones[:], sq[:, c, :], start=(c == 0), stop=(c == 7))
                return ins
            P.op("pe", mmn, R=[sqb, onesb], W=[pbb[7]])
            P.op("act", lambda e: e.activation(out=rstd, in_=pbank[7][:, 0:G], func=AF.Ln, bias=epsb_t[:, 0:1], scale=1.0 / 1024.0),
                 R=[pbb[7], epsb], W=[rstdb])
            P.op("act", lambda e: e.activation(out=rstd, in_=rstd, func=AF.Exp, scale=-0.5), R=[rstdb], W=[rstdb])

            def hnorm(e, t0=t0):
                ins = None
                for c in range(8):
                    ins = e.scalar_tensor_tensor(out=hT[:, c, 1:G + 1], in0=xT[:, c, t0:t0 + G], scalar=pv[:, gcol + c:gcol + c + 1],
                                                 in1=rstd, op0=ALU.mult, op1=ALU.mult)
                return ins
            P.op("dve", hnorm, R=xg + [rstdb, pvb, hb], W=[hb])
            for h in range(8):
                for qi, qn in enumerate(("r", "k", "v")):
                    P.dma("sp", wrkv[qi], rwin_d[qi * 8 + h], W=[wrkvb[qi]])
                    bank = pi % 2
                    sl = pi % 2
                    pi += 1

                    def mm(e, qi=qi, bank=bank):
                        ins = None
                        for c in range(8):
                            ins = e.matmul(pbank[bank][0:64, 0:G + 1], wrkv[qi][:, c, :], hT[:, c, :], start=(c == 0), stop=(c == 7))
                        return ins
                    P.op("pe", mm, R=[wrkvb[qi], hb], W=[pbb[bank]])
                    P.op("act", lambda e, bank=bank, sl=sl: e.activation(out=praw[sl][0:64, :], in_=pbank[bank][0:64, 0:G + 1], func=AF.Copy),
                         R=[pbb[bank]], W=[prawb[sl]])
                    P.op("dve", lambda e, sl=sl: e.tensor_tensor(out=dtmp[sl][0:64, :], in0=praw[sl][0:64, 0:G], in1=praw[sl][0:64, 1:G + 1],
                                                                 op=ALU.subtract), R=[prawb[sl]], W=[dtmpb[sl]])
                    P.op("dve", lambda e, sl=sl, qn=qn, qi=qi, h=h: e.scalar_tensor_tensor(
                        out=Q[qn][0:64, h, :], in0=dtmp[sl][0:64, :], scalar=pv[0:64, MU + qi * 8 + h:MU + qi * 8 + h + 1],
                        in1=praw[sl][0:64, 1:G + 1], op0=ALU.mult, op1=ALU.add), R=[dtmpb[sl], prawb[sl], pvb], W=[QB[qn]])
            for j in range(2):
                P.dma("sp", wlo[j], rwlo_d[j], W=[wlob[j]])
                bank = pi % 2
                sl = pi % 2
                pi += 1

                def mml(e, j=j, bank=bank):
                    ins = None
                    for c in range(8):
                        ins = e.matmul(pbank[bank][:, 0:G + 1], wlo[j][:, c, :], hT[:, c, :], start=(c == 0), stop=(c == 7))
                    return ins
                P.op("pe", mml, R=[wlob[j], hb], W=[pbb[bank]])
                P.op("act", lambda e, bank=bank, sl=sl: e.activation(out=praw[sl], in_=pbank[bank][:, 0:G + 1], func=AF.Copy),
                     R=[pbb[bank]], W=[prawb[sl]])
                P.op("dve", lambda e, sl=sl: e.tensor_tensor(out=dtmp[sl], in0=praw[sl][:, 0:G], in1=praw[sl][:, 1:G + 1], op=ALU.subtract),
                     R=[prawb[sl]], W=[dtmpb[sl]])
                dst, dstb = (tw, twb) if j == 0 else (sg, sgb)
                mcol = MUWA if j == 0 else MUG
                P.op("dve", lambda e, sl=sl, dst=dst, mcol=mcol: e.scalar_tensor_tensor(
                    out=dst, in0=dtmp[sl], scalar=pv[:, mcol:mcol + 1], in1=praw[sl][:, 1:G + 1], op0=ALU.mult, op1=ALU.add),
                    R=[dtmpb[sl], prawb[sl], pvb], W=[dstb])
            P.op("act", lambda e: e.activation(out=tw[0:64, :], in_=tw[0:64, :], func=AF.Tanh), R=[twb], W=[twb])
            P.op("act", lambda e: e.activation(out=sg, in_=sg, func=AF.Sigmoid), R=[sgb], W=[sgb])
            for half in range(2):
                def mmw(e, half=half):
                    ins = None
                    for hh in range(4):
                        h = half * 4 + hh
                        ins = e.matmul(pbank[2][0:64, hh * G:(hh + 1) * G], w2a2[0:64, h * 64:(h + 1) * 64], tw[0:64, :], start=True, stop=True)
                    return ins
                P.op("pe", mmw, R=[wconst, twb], W=[pbb[2]])

                def sw(e, half=half):
                    ins = None
                    for hh in range(4):
                        h = half * 4 + hh
                        ins = e.activation(out=Q["lw"][0:64, h, :], in_=pbank[2][0:64, hh * G:(hh + 1) * G], func=AF.Sigmoid,
                                           bias=pv[0:64, W0 + h:W0 + h + 1])
                    return ins
                P.op("act", sw, R=[pbb[2], pvb], W=[QB["lw"]])

                def mma(e, half=half):
                    ins = None
                    for hh in range(4):
                        h = half * 4 + hh
                        ins = e.matmul(pbank[3][0:64, hh * G:(hh + 1) * G], w2a2[64:128, h * 64:(h + 1) * 64], tw[64:128, :], start=True, stop=True)
                    return ins
                P.op("pe", mma, R=[wconst, twb], W=[pbb[3]])

                def sa(e, half=half):
                    ins = None
                    for hh in range(4):
                        h = half * 4 + hh
                        ins = e.activation(out=Q["a"][0:64, h, :], in_=pbank[3][0:64, hh * G:(hh + 1) * G], func=AF.Sigmoid,
                                           bias=pv[0:64, A0 + h:A0 + h + 1])
                    return ins
                P.op("act", sa, R=[pbb[3], pvb], W=[QB["a"]])
            P.op("dve", lambda e: e.tensor_scalar(out=Q["lw"][0:64], in0=Q["lw"][0:64], scalar1=-0.6065306597126334, scalar2=None, op0=ALU.mult),
                 R=[QB["lw"]], W=[QB["lw"]])
            def kk1(e):
                ins = None
                for h in range(8):
                    ins = e.tensor_scalar(out=Q["kk"][0:64, h, :], in0=Q["k"][0:64, h, :], scalar1=pv[0:64, KK + h:KK + h + 1], scalar2=None, op0=ALU.mult)
                return ins
            P.op("dve", kk1, R=[QB["k"], pvb], W=[QB["kk"]])
            P.op("act", lambda e: e.activation(out=Q["e"][0:64], in_=Q["kk"][0:64], func=AF.Square), R=[QB["kk"]], W=[QB["e"]])
            for half in range(2):
                P.op("pe", lambda e, half=half: e.matmul(pbank[2][0:64, :], o64, Q["e"][0:64, half * 4:(half + 1) * 4, :], start=True, stop=True),
                     R=[QB["e"], onesb], W=[pbb[2]])
                P.op("dve", lambda e, half=half: e.tensor_scalar(out=Q["e"][0:64, half * 4:(half + 1) * 4, :],
                                                                 in0=pbank[2][0:64, :].rearrange("p (h t) -> p h t", h=4),
                                                                 scalar1=1e-24, scalar2=None, op0=ALU.max), R=[pbb[2], QB["e"]], W=[QB["e"]])
            P.op("act", lambda e: e.activation(out=Q["e"][0:64], in_=Q["e"][0:64], func=AF.Ln), R=[QB["e"]], W=[QB["e"]])
            P.op("act", lambda e: e.activation(out=Q["e"][0:64], in_=Q["e"][0:64], func=AF.Exp, scale=-0.5), R=[QB["e"]], W=[QB["e"]])
            P.op("dve", lambda e: e.tensor_tensor(out=Q["kk"][0:64], in0=Q["kk"][0:64], in1=Q["e"][0:64], op=ALU.mult),
                 R=[QB["kk"], QB["e"]], W=[QB["kk"]])
            def km1(e):
                ins = None
                for h in range(8):
                    ins = e.tensor_scalar(out=Q["e"][0:64, h, :], in0=Q["a"][0:64, h, :], scalar1=pv[0:64, KA + h:KA + h + 1],
                                          scalar2=omka[0:64, h:h + 1], op0=ALU.mult, op1=ALU.add)
                return ins
            P.op("dve", km1, R=[QB["a"], pvb, cb2], W=[QB["e"]])
            P.op("dve", lambda e: e.tensor_tensor(out=Q["k"][0:64], in0=Q["k"][0:64], in1=Q["e"][0:64], op=ALU.mult),
                 R=[QB["k"], QB["e"]], W=[QB["k"]])
            P.op("dve", lambda e: e.tensor_tensor(out=Q["a"][0:64], in0=Q["a"][0:64], in1=Q["kk"][0:64], op=ALU.mult),
                 R=[QB["a"], QB["kk"]], W=[QB["a"]])
            def bn1(e):
                ins = None
                for h in range(8):
                    ins = e.scalar_tensor_tensor(out=Q["e"][0:64, h, :], in0=Q["r"][0:64, h, :], scalar=pv[0:64, RK + h:RK + h + 1],
                                                 in1=Q["k"][0:64, h, :], op0=ALU.mult, op1=ALU.mult)
                return ins
            P.op("dve", bn1, R=[QB["r"], QB["k"], pvb], W=[QB["e"]])
            def scn(e):
                ins = None
                for h in range(8):
                    ins = e.tensor_tensor_scan(out=Q["Lw"][0:64, h, :], data0=resetm[0:64, 0:G], data1=Q["lw"][0:64, h, :], initial=0.0,
                                               op0=ALU.mult, op1=ALU.add)
                return ins
            P.op("dve", scn, R=[QB["lw"], cb], W=[QB["Lw"]])
            P.op("dve", lambda e: e.tensor_tensor(out=Q["lw"][0:64], in0=Q["Lw"][0:64], in1=Q["lw"][0:64], op=ALU.subtract),
                 R=[QB["Lw"], QB["lw"]], W=[QB["lw"]])
            for half in range(2):
                P.op("pe", lambda e, half=half: e.matmul(pbank[3][0:64, :], o64, Q["e"][0:64, half * 4:(half + 1) * 4, :], start=True, stop=True),
                     R=[QB["e"], onesb], W=[pbb[3]])
                P.op("dve", lambda e, half=half: e.tensor_tensor(out=Q["BhT"][0:64, half * 4:(half + 1) * 4, :],
                                                                 in0=pbank[3][0:64, :].rearrange("p (h t) -> p h t", h=4),
                                                                 in1=Q["v"][0:64, half * 4:(half + 1) * 4, :], op=ALU.mult),
                     R=[pbb[3], QB["v"]], W=[QB["BhT"]])
            P.op("act", lambda e: e.activation(out=Q["e"][0:64], in_=Q["lw"][0:64], func=AF.Exp), R=[QB["lw"]], W=[QB["e"]])
            P.op("dve", lambda e: e.scalar_tensor_tensor(out=Q["at"][0:64], in0=Q["kk"][0:64], scalar=-1.0, in1=Q["e"][0:64], op0=ALU.mult, op1=ALU.mult),
                 R=[QB["kk"], QB["e"]], W=[QB["at"]])
            P.op("act", lambda e: e.activation(out=Q["lw"][0:64], in_=Q["BhT"][0:64], func=AF.Copy), R=[QB["BhT"], QB["e"]], W=[QB["lw"]])
            P.op("act", lambda e: e.activation(out=Q["e"][0:64], in_=Q["Lw"][0:64], func=AF.Exp), R=[QB["Lw"], QB["at"]], W=[QB["e"]])
            P.op("dve", lambda e: e.tensor_tensor(out=Q["r"][0:64], in0=Q["r"][0:64], in1=Q["e"][0:64], op=ALU.mult), R=[QB["r"], QB["e"]], W=[QB["r"]])
            P.op("act", lambda e: e.activation(out=Q["e"][0:64], in_=Q["Lw"][0:64], func=AF.Exp, scale=-1.0), R=[QB["Lw"], QB["r"]], W=[QB["e"]])
            P.op("dve", lambda e: e.tensor_tensor(out=Q["bt"][0:64], in0=Q["a"][0:64], in1=Q["e"][0:64], op=ALU.mult), R=[QB["a"], QB["e"]], W=[QB["bt"]])
            P.op("dve", lambda e: e.tensor_tensor(out=Q["kt"][0:64], in0=Q["k"][0:64], in1=Q["e"][0:64], op=ALU.mult), R=[QB["k"], QB["e"]], W=[QB["kt"]])
            def eld(e):
                ins = None
                for h in range(8):
                    for ci in range(G // 64):
                        ins = e.activation(out=Q["e"][0:64, h, ci * 64:(ci + 1) * 64], in_=Q["Lw"][0:64, h, ci * 64:(ci + 1) * 64], func=AF.Exp,
                                           scale=-1.0, bias=Q["Lw"][0:64, h, ci * 64 + 63:ci * 64 + 64])
                return ins
            P.op("act", eld, R=[QB["Lw"], QB["bt"], QB["kt"]], W=[QB["e"]])
            P.op("act", lambda e: e.activation(out=gC[0:64], in_=Q["Lw"][0:64].rearrange("p h (c t) -> p h c t", t=64)[:, :, :, 63], func=AF.Exp),
                 R=[QB["Lw"]], W=[gCb])
            P.op("dve", lambda e: e.tensor_tensor(out=Q["BhT"][0:64], in0=Q["a"][0:64], in1=Q["e"][0:64], op=ALU.mult),
                 R=[QB["a"], QB["e"], QB["lw"]], W=[QB["BhT"]])
            P.op("dve", lambda e: e.tensor_tensor(out=Q["KhT"][0:64], in0=Q["k"][0:64], in1=Q["e"][0:64], op=ALU.mult),
                 R=[QB["k"], QB["e"]], W=[QB["KhT"]])
            if dbg_d is not None and g == 0:
                for i_, n_ in enumerate(["r", "k", "v", "lw", "a", "kk", "Lw", "at", "bt", "kt", "BhT", "KhT"]):
                    dbg_ops.append(P.dma("sp", dbg_d[:, i_, :].rearrange("p (h t) -> p h t", h=8), Q[n_][0:64], R=[QB[n_]]))
            def do_chunk(ci):
                nonlocal hcur
                cs = ci * 64

                def amat(bank, ln, rn, dst, mask, eng):
                    def mm(e):
                        ins = None
                        for h in range(8):
                            ins = e.matmul(pbank[bank][0:64, h * 64:(h + 1) * 64], Q[ln][0:64, h, cs:cs + 64], Q[rn][0:64, h, cs:cs + 64],
                                           start=True, stop=True)
                        return ins
                    P.op("pe", mm, R=[QB[ln], QB[rn]], W=[pbb[bank]])
                    P.op(eng, lambda e: e.tensor_tensor(out=Cc[dst][0:64], in0=pbank[bank][0:64, :].rearrange("p (h t) -> p h t", h=8),
                                                        in1=mask, op=ALU.mult), R=[pbb[bank], cb2], W=[CB[dst]])
                amat(2, "at", "bt", "P0", mLs, "dve")
                amat(3, "bt", "at", "PT0", mUs, "dve")
                amat(2, "kt", "at", "AakT", mUs, "dve")
                amat(3, "bt", "r", "ArbT", mUi, "dve")
                amat(2, "kt", "r", "ArkT", mUi, "dve")
                P.op("dve", lambda e: e.tensor_tensor(out=Cc["TT"][0:64], in0=Cc["PT0"][0:64], in1=id8, op=ALU.add), R=[CB["PT0"], cb2], W=[CB["TT"]])
                for src, dst in (("v", "Vtm"), ("BhT", "Bhtm"), ("KhT", "Khtm")):
                    def trp(e, src=src):
                        ins = None
                        for h in range(8):
                            ins = e.transpose(pbank[4][0:64, h * 64:(h + 1) * 64], Q[src][0:64, h, cs:cs + 64], ident[0:64, 0:64])
                        return ins
                    P.op("pe", trp, R=[QB[src], cb], W=[pbb[4]])
                    P.op("act", lambda e, dst=dst: e.activation(out=Cc[dst][0:64], in_=pbank[4][0:64, :].rearrange("p (h t) -> p h t", h=8), func=AF.Copy),
                         R=[pbb[4]], W=[CB[dst]])
                pc = 0
                for lev in range(1, 6):
                    Pn, Pp = "P%d" % (1 - pc), "P%d" % pc
                    PTn, PTp = "PT%d" % (1 - pc), "PT%d" % pc

                    def sqm(e, Pp=Pp, PTp=PTp):
                        ins = None
                        for h in range(8):
                            ins = e.matmul(pbank[2][0:64, h * 64:(h + 1) * 64], Cc[PTp][0:64, h, :], Cc[Pp][0:64, h, :], start=True, stop=True)
                        return ins
                    P.op("pe", sqm, R=[CB[Pp], CB[PTp]], W=[pbb[2]])
                    P.op("act", lambda e, Pn=Pn: e.activation(out=Cc[Pn][0:64], in_=pbank[2][0:64, :].rearrange("p (h t) -> p h t", h=8), func=AF.Copy),
                         R=[pbb[2]], W=[CB[Pn]])
                    if lev < 5:
                        def sqt(e, Pp=Pp, PTp=PTp):
                            ins = None
                            for h in range(8):
                                ins = e.matmul(pbank[3][0:64, h * 64:(h + 1) * 64], Cc[Pp][0:64, h, :], Cc[PTp][0:64, h, :], start=True, stop=True)
                            return ins
                        P.op("pe", sqt, R=[CB[Pp], CB[PTp]], W=[pbb[3]])
                        P.op("dve", lambda e, PTn=PTn: e.tensor_copy(out=Cc[PTn][0:64], in_=pbank[3][0:64, :].rearrange("p (h t) -> p h t", h=8)),
                             R=[pbb[3]], W=[CB[PTn]])

                    def ttm(e, Pn=Pn):
                        ins = None
                        for h in range(8):
                            ins = e.matmul(pbank[5][0:64, h * 64:(h + 1) * 64], Cc[Pn][0:64, h, :], Cc["TT"][0:64, h, :], start=True, stop=True)
                        return ins
                    P.op("pe", ttm, R=[CB[Pn], CB["TT"]], W=[pbb[5]])
                    P.op("dve", lambda e: e.tensor_tensor(out=Cc["TT"][0:64], in0=pbank[5][0:64, :].rearrange("p (h t) -> p h t", h=8),
                                                          in1=Cc["TT"][0:64], op=ALU.add), R=[pbb[5], CB["TT"]], W=[CB["TT"]])
                    pc = 1 - pc
                Hc, Hn = "H%d" % hcur, "H%d" % (1 - hcur)

                def xmm(e, Hc=Hc):
                    ins = None
                    for h in range(8):
                        e.matmul(pbank[6][0:64, h * 64:(h + 1) * 64], Q["at"][0:64, h, cs:cs + 64], Cc[Hc][0:64, h, :], start=True, stop=False)
                        ins = e.matmul(pbank[6][0:64, h * 64:(h + 1) * 64], Cc["AakT"][0:64, h, :], Cc["Vtm"][0:64, h, :], start=False, stop=True)
                    return ins
                P.op("pe", xmm, R=[QB["at"], CB[Hc], CB["AakT"], CB["Vtm"]], W=[pbb[6]])
                P.op("act", lambda e: e.activation(out=Cc["Xs"][0:64], in_=pbank[6][0:64, :].rearrange("p (h t) -> p h t", h=8), func=AF.Copy),
                     R=[pbb[6]], W=[CB["Xs"]])

                def umm(e):
                    ins = None
                    for h in range(8):
                        ins = e.matmul(pbank[6][0:64, h * 64:(h + 1) * 64], Cc["TT"][0:64, h, :], Cc["Xs"][0:64, h, :], start=True, stop=True)
                    return ins
                P.op("pe", umm, R=[CB["TT"], CB["Xs"]], W=[pbb[6]])
                P.op("act", lambda e: e.activation(out=Cc["Us"][0:64], in_=pbank[6][0:64, :].rearrange("p (h t) -> p h t", h=8), func=AF.Copy),
                     R=[pbb[6]], W=[CB["Us"]])

                def ymm(e, Hc=Hc):
                    ins = None
                    for h in range(8):
                        e.matmul(pbank[5][0:64, h * 64:(h + 1) * 64], Cc[Hc][0:64, h, :], Q["r"][0:64, h, cs:cs + 64], start=True, stop=False)
                        e.matmul(pbank[5][0:64, h * 64:(h + 1) * 64], Cc["Us"][0:64, h, :], Cc["ArbT"][0:64, h, :], start=False, stop=False)
                        ins = e.matmul(pbank[5][0:64, h * 64:(h + 1) * 64], Cc["Vtm"][0:64, h, :], Cc["ArkT"][0:64, h, :], start=False, stop=True)
                    return ins
                P.op("pe", ymm, R=[CB[Hc], QB["r"], CB["Us"], CB["ArbT"], CB["Vtm"], CB["ArkT"]], W=[pbb[5]])
                P.op("act", lambda e: e.activation(out=Q["Lw"][0:64, :, cs:cs + 64], in_=pbank[5][0:64, :].rearrange("p (h t) -> p h t", h=8), func=AF.Copy),
                     R=[pbb[5], gCb], W=[QB["Lw"]])

                def hmm(e):
                    ins = None
                    for h in range(8):
                        e.matmul(pbank[6][0:64, h * 64:(h + 1) * 64], Cc["Bhtm"][0:64, h, :], Cc["Us"][0:64, h, :], start=True, stop=False)
                        ins = e.matmul(pbank[6][0:64, h * 64:(h + 1) * 64], Cc["Khtm"][0:64, h, :], Cc["Vtm"][0:64, h, :], start=False, stop=True)
                    return ins
                P.op("pe", hmm, R=[CB["Bhtm"], CB["Us"], CB["Khtm"], CB["Vtm"]], W=[pbb[6]])

                def hup(e, Hc=Hc, Hn=Hn, ci=ci):
                    ins = None
                    for h in range(8):
                        ins = e.scalar_tensor_tensor(out=Cc[Hn][0:64, h, :], in0=Cc[Hc][0:64, h, :], scalar=gC[0:64, h, ci:ci + 1],
                                                     in1=pbank[6][0:64, h * 64:(h + 1) * 64], op0=ALU.mult, op1=ALU.add)
                    return ins
                P.op("dve", hup, R=[CB[Hc], gCb, pbb[6]], W=[CB[Hn]])
                hcur = 1 - hcur
                if dbg_d is not None and g == 0 and ci == 0:
                    for i_, n_ in enumerate(["P0", "PT0", "TT", "AakT", "ArbT", "ArkT", "Vtm", "Bhtm", "Khtm", "Xs", "Us", Hn]):
                        dbg_ops.append(P.dma("sp", dbg_d[:, 12 + i_, 0:512].rearrange("p (h t) -> p h t", h=8), Cc[n_][0:64], R=[CB[n_]]))
            for ci_ in range(G // 64):
                do_chunk(ci_)
            if dbg_d is not None and g == 0:
                dbg_ops.append(P.dma("sp", dbg_d[:, 24, :].rearrange("p (h t) -> p h t", h=8), Q["Lw"][0:64], R=[QB["Lw"]]))
            yr = Q["Lw"]; yrb = QB["Lw"]
            cen = Q["at"]; cenb = QB["at"]
            for half in range(2):
                P.op("pe", lambda e, half=half: e.matmul(pbank[2][0:64, :], o64, yr[0:64, half * 4:(half + 1) * 4, :], start=True, stop=True),
                     R=[yrb, onesb], W=[pbb[2]])
                P.op("dve", lambda e, half=half: e.scalar_tensor_tensor(out=cen[0:64, half * 4:(half + 1) * 4, :],
                                                                        in0=pbank[2][0:64, :].rearrange("p (h t) -> p h t", h=4), scalar=-1.0 / 64.0,
                                                                        in1=yr[0:64, half * 4:(half + 1) * 4, :], op0=ALU.mult, op1=ALU.add),
                     R=[pbb[2], yrb, CB["H0"], CB["H1"]], W=[cenb])
            P.op("act", lambda e: e.activation(out=Q["e"][0:64], in_=cen[0:64], func=AF.Square), R=[cenb, QB["BhT"], QB["KhT"]], W=[QB["e"]])
            for half in range(2):
                P.op("pe", lambda e, half=half: e.matmul(pbank[3][0:64, :], o64, Q["e"][0:64, half * 4:(half + 1) * 4, :], start=True, stop=True),
                     R=[QB["e"], onesb], W=[pbb[3]])
                P.op("act", lambda e, half=half: e.activation(out=Q["bt"][0:64, half * 4:(half + 1) * 4, :],
                                                               in_=pbank[3][0:64, :].rearrange("p (h t) -> p h t", h=4), func=AF.Ln,
                                                               bias=epsb_t[0:64, 1:2], scale=1.0 / 64.0), R=[pbb[3], epsb], W=[QB["bt"]])
            P.op("act", lambda e: e.activation(out=Q["bt"][0:64], in_=Q["bt"][0:64], func=AF.Exp, scale=-0.5), R=[QB["bt"]], W=[QB["bt"]])
            P.op("dve", lambda e: e.tensor_tensor(out=cen[0:64], in0=cen[0:64], in1=Q["bt"][0:64], op=ALU.mult), R=[cenb, QB["bt"]], W=[cenb])

            def lnx(e):
                ins = None
                for h in range(8):
                    ins = e.tensor_scalar(out=cen[0:64, h, :], in0=cen[0:64, h, :], scalar1=pv[0:64, LNW + h:LNW + h + 1],
                                          scalar2=pv[0:64, LNB + h:LNB + h + 1], op0=ALU.mult, op1=ALU.add)
                return ins
            P.op("dve", lnx, R=[cenb, pvb], W=[cenb])
            P.op("dve", lambda e: e.tensor_tensor(out=cen[0:64], in0=cen[0:64], in1=Q["lw"][0:64], op=ALU.add), R=[cenb, QB["lw"]], W=[cenb])
            for half in range(2):
                def gmm(e, half=half):
                    ins = None
                    for hh in range(4):
                        h = half * 4 + hh
                        ins = e.matmul(pbank[2][0:64, hh * G:(hh + 1) * G], g2[:, h * 64:(h + 1) * 64], sg, start=True, stop=True)
                    return ins
                P.op("pe", gmm, R=[wconst, sgb], W=[pbb[2]])
                P.op("dve", lambda e, half=half: e.tensor_tensor(out=Q["kt"][0:64, half * 4:(half + 1) * 4, :], in0=pbank[2][0:64, :].rearrange("p (h t) -> p h t", h=4),
                                                                 in1=cen[0:64, half * 4:(half + 1) * 4, :], op=ALU.mult),
                     R=[pbb[2], cenb], W=[QB["kt"]])
            yf = Q["kt"]; yfb = QB["kt"]
            for dh in range(2):
                for dd in range(4):
                    dc = dh * 4 + dd
                    s2 = woi % 2
                    woi += 1
                    P.dma("sp", wo[s2][0:64], rwout_d[dc], W=[wob[s2]])

                    def mo(e, s2=s2, dd=dd, dh=dh):
                        ins = None
                        for h in range(8):
                            ins = e.matmul(pbank[7][:, dd * G:(dd + 1) * G], wo[s2][0:64, h, :], yf[0:64, h, :], start=(h == 0), stop=(h == 7))
                        return ins
                    P.op("pe", mo, R=[wob[s2], yfb], W=[pbb[7]])
                P.op("dve", lambda e, dh=dh, t0=t0: e.tensor_tensor(out=xT[:, dh * 4:(dh + 1) * 4, t0:t0 + G],
                                                                    in0=pbank[7][:, 0:4 * G].rearrange("p (c t) -> p c t", c=4),
                                                                    in1=xT[:, dh * 4:(dh + 1) * 4, t0:t0 + G], op=ALU.add),
                     R=[pbb[7]] + [xb[c][g4] for c in range(dh * 4, dh * 4 + 4)], W=[xb[c][g4] for c in range(dh * 4, dh * 4 + 4)])
        for g_ in range(NG):
            do_group(g_)
        P.fence()


    def moba():
        G = 256
        NEG = 240000.0
        A = Arena(arena, ARENA)
        kT = A.take(8192).rearrange("p (c t) -> p c t", c=4)
        kTb = [[Buf() for g in range(8)] for c in range(4)]
        Va = A.take(16 * 8 * 65).rearrange("p (k h d) -> p k h d", k=16, h=8)
        Vab = [Buf() for kt in range(16)]
        hT = A.take(2048).rearrange("p (c t) -> p c t", c=8); hb = Buf()
        sq = A.take(2048).rearrange("p (c t) -> p c t", c=8); sqb = Buf()
        rstd = A.take(256); rstdb = Buf()
        wt = [A.take(1024).rearrange("p (c m) -> p c m", c=8) for i in range(2)]; wtb = [Buf(), Buf()]
        qT = A.take(1024).rearrange("p (c t) -> p c t", c=4); qTb = [Buf() for c in range(4)]
        ksum = A.take(32).rearrange("p (c j) -> p c j", c=4); ksb = Buf()
        gsel = A.take(16).rearrange("p (a j) -> p a j", a=2); gselb = Buf()
        m8 = A.take(16).rearrange("p (a j) -> p a j", a=2); m8b = Buf()
        negm = A.take(16).rearrange("p (a j) -> p a j", a=2); negmb = Buf()
        qaug = [A.take(256) for i in range(2)]; qaugb = [Buf(), Buf()]
        kaug = A.take(2048); kaugb = Buf()
        PT = [A.take(256) for i in range(2)]; PTb = [Buf() for i in range(2)]
        stmp = [A.take(256) for i in range(1)]; stmpb = [Buf()]
        oa = A.take(256); oab = Buf()
        rden = A.take(256); rdenb = Buf()
        oh = A.take(2048).rearrange("p (h t) -> p h t", h=8); ohb = [Buf() for h in range(8)]
        wo = [A.take(1024).rearrange("p (h m) -> p h m", h=8) for i in range(1)]; wob = [Buf()]
        stg = [A.take(512).rearrange("p (c t) -> p c t", c=2) for i in range(2)]; stgb = [Buf(), Buf()]
        ncA = A.take(256); ncB = A.take(256); sel65 = A.take(64); mcb = Buf()
        gcol = PV_NORMG + (0 * 3 + 1) * 8
        cnt = {"w": 0, "p": 0, "pt": 0, "st": 0, "wo": 0, "qa": 0}

        def setup(e):
            e.memset(ncA[:], 0.0)
            e.memset(ncB[:], 0.0)
            e.affine_select(out=ncA[:], in_=ncA[:], pattern=[[1, 256]], compare_op=ALU.is_ge, fill=-NEG, base=0, channel_multiplier=-1)
            e.affine_select(out=ncB[:], in_=ncB[:], pattern=[[1, 256]], compare_op=ALU.is_ge, fill=-NEG, base=-128, channel_multiplier=-1)
            e.memset(sel65[0:64, :], 0.0)
            e.memset(sel65[64:65, :], 1.0)
            return e.memset(Va[:, :, :, 64:65], 1.0)
        P.op("pool", setup, W=[mcb] + Vab)
        P.dma("sp", kaug[0:10, :], mbkaug_d[:, :], W=[kaugb])

        def proj_fm(widx, dst_ap, dst_bufs):
            s_ = cnt["w"] % 2
            cnt["w"] += 1
            bank = cnt["p"] % 2
            cnt["p"] += 1
            P.dma("sp", wt[s_], mbin_d[widx], W=[wtb[s_]])

            def mm(e):
                ins = None
                for c in range(8):
                    ins = e.matmul(pbank[bank][:, 0:G], wt[s_][:, c, :], hT[:, c, :], start=(c == 0), stop=(c == 7))
                return ins
            P.op("pe", mm, R=[wtb[s_], hb], W=[pbb[bank]])
            P.op("act", lambda e: e.activation(out=dst_ap, in_=pbank[bank][:, 0:G], func=AF.Copy), R=[pbb[bank]], W=dst_bufs)

        def proj_v(g, hp):
            s_ = cnt["w"] % 2
            cnt["w"] += 1
            bank = cnt["p"] % 2
            cnt["p"] += 1
            P.dma("sp", wt[s_], mbin_d[8 + hp], W=[wtb[s_]])

            def mm(e):
                ins = None
                for tt in range(2):
                    for c in range(8):
                        ins = e.matmul(pbank[bank][:, tt * 128:(tt + 1) * 128], hT[:, c, tt * 128:(tt + 1) * 128], wt[s_][:, c, :],
                                       start=(c == 0), stop=(c == 7))
                return ins
            P.op("pe", mm, R=[wtb[s_], hb], W=[pbb[bank]])
            P.op("dve", lambda e: e.tensor_copy(out=Va[:, 2 * g:2 * g + 2, 2 * hp:2 * hp + 2, 0:64],
                                                in_=pbank[bank][:, 0:256].rearrange("p (k h d) -> p k h d", k=2, h=2)),
                 R=[pbb[bank]], W=[Vab[2 * g], Vab[2 * g + 1]])

        def do_head(g, h):
            hp, ph = h // 2, h % 2
            r0 = ph * 64
            ob = g
            qs = cnt["qa"] % 2
            cnt["qa"] += 1
            qa = qaug[qs]
            qab = qaugb[qs]
            P.dma("sp", qa[8:10, :], mbqc_d[h, :, g * G:(g + 1) * G], W=[qab])
            if ob >= 4:
                def gmm(e):
                    ins = None
                    for tt in range(2):
                        ins = e.matmul(pbank[2][:, tt * 8:(tt + 1) * 8], qT[r0:r0 + 64, hp, tt * 128:(tt + 1) * 128], ksum[r0:r0 + 64, hp, :],
                                       start=True, stop=True)
                    return ins
                P.op("pe", gmm, R=[qTb[hp], ksb], W=[pbb[2]])
                P.op("pool", lambda e: e.memset(gsel, -1e30), W=[gselb])
                P.op("dve", lambda e: e.tensor_copy(out=gsel[:, :, 0:ob], in_=pbank[2][:, 0:16].rearrange("p (a j) -> p a j", a=2)[:, :, 0:ob]),
                     R=[pbb[2]], W=[gselb])

                def mx(e):
                    e.max(out=m8[:, 0, :], in_=gsel[:, 0, :])
                    return e.max(out=m8[:, 1, :], in_=gsel[:, 1, :])
                P.op("dve", mx, R=[gselb], W=[m8b])

                def ng(e):
                    ins = None
                    for tt in range(2):
                        ins = e.tensor_scalar(out=negm[:, tt, :], in0=gsel[:, tt, :], scalar1=m8[:, tt, 2:3], scalar2=1.0, op0=ALU.is_ge, op1=ALU.subtract)
                    return ins
                P.op("dve", ng, R=[gselb, m8b], W=[negmb])
                P.op("dve", lambda e: e.memset(negm[:, :, ob:8], 0.0), R=[], W=[negmb])

                def trn(e):
                    ins = None
                    for tt in range(2):
                        ins = e.transpose(pbank[2][0:8, 128 + tt * 128:128 + (tt + 1) * 128], negm[:, tt, :], ident[:])
                    return ins
                P.op("pe", trn, R=[negmb, cb], W=[pbb[2]])
                P.op("act", lambda e: e.activation(out=qa[0:8, :], in_=pbank[2][0:8, 128:384], func=AF.Copy), R=[pbb[2]], W=[qab])
            else:
                P.op("pool", lambda e: e.memset(qa[0:8, :], 0.0), W=[qab])
            nkt = 2 * ob + 2
            for kt in range(nkt):
                diag = kt - 2 * ob
                c0 = 128 if diag == 1 else 0
                n = G - c0
                sb_ = 3 + (cnt["st"] % 2)
                cnt["st"] += 1
                pti = cnt["pt"] % 2
                cnt["pt"] += 1

                def smm(e, kt=kt, c0=c0, sb_=sb_):
                    e.matmul(pbank[sb_][:, c0:G], kT[r0:r0 + 64, hp, kt * 128:(kt + 1) * 128], qT[r0:r0 + 64, hp, c0:G], start=True, stop=False)
                    return e.matmul(pbank[sb_][:, c0:G], kaug[0:10, kt * 128:(kt + 1) * 128], qa[0:10, c0:G], start=False, stop=True)
                P.op("pe", smm, R=[kTb[hp][kt // 2], qTb[hp], kaugb, qab], W=[pbb[sb_]])
                if diag >= 0:
                    nc_ = ncA if diag == 0 else ncB
                    si = 0
                    P.op("dve", lambda e, c0=c0, sb_=sb_, nc_=nc_, si=si: e.tensor_tensor(out=stmp[si][:, c0:G], in0=pbank[sb_][:, c0:G], in1=nc_[:, c0:G], op=ALU.add),
                         R=[pbb[sb_], mcb], W=[stmpb[si]])
                    P.op("act", lambda e, c0=c0, pti=pti, si=si: e.activation(out=PT[pti][:, c0:G], in_=stmp[si][:, c0:G], func=AF.Exp, scale=0.125),
                         R=[stmpb[si]], W=[PTb[pti]])
                else:
                    P.op("act", lambda e, c0=c0, pti=pti, sb_=sb_: e.activation(out=PT[pti][:, c0:G], in_=pbank[sb_][:, c0:G], func=AF.Exp, scale=0.125),
                         R=[pbb[sb_]], W=[PTb[pti]])
                P.op("pe", lambda e, kt=kt, c0=c0, pti=pti: e.matmul(pbank[5][0:65, c0:G], Va[:, kt, h, :], PT[pti][:, c0:G], start=(kt == 0), stop=(kt == nkt - 1)),
                     R=[Vab[kt], PTb[pti]], W=[pbb[5]])
            P.op("act", lambda e: e.activation(out=oa[0:65, :], in_=pbank[5][0:65, 0:G], func=AF.Copy), R=[pbb[5]], W=[oab])
            P.op("pe", lambda e: e.matmul(pbank[6][0:64, 0:G], sel65[0:65, :], oa[0:65, :], start=True, stop=True), R=[oab, mcb], W=[pbb[6]])
            P.op("dve", lambda e: e.reciprocal(out=rden[0:64, :], in_=pbank[6][0:64, 0:G]), R=[pbb[6]], W=[rdenb])
            P.op("dve", lambda e: e.tensor_tensor(out=oh[0:64, h, :], in0=oa[0:64, :], in1=rden[0:64, :], op=ALU.mult), R=[oab, rdenb], W=[ohb[h]])

        def do_group(g):
            t0 = g * G
            g4 = t0 // 512
            xg = [xb[c][g4] for c in range(8)]
            P.op("act", lambda e: e.activation(out=sq, in_=xT[:, :, t0:t0 + G], func=AF.Square), R=xg, W=[sqb])

            def mmn(e):
                ins = None
                for c in range(8):
                    ins = e.matmul(pbank[7][:, 0:G], ones[:], sq[:, c, :], start=(c == 0), stop=(c == 7))
                return ins
            P.op("pe", mmn, R=[sqb, onesb], W=[pbb[7]])
            P.op("act", lambda e: e.activation(out=rstd, in_=pbank[7][:, 0:G], func=AF.Ln, bias=epsb_t[:, 0:1], scale=1.0 / 1024.0),
                 R=[pbb[7], epsb], W=[rstdb])
            P.op("act", lambda e: e.activation(out=rstd, in_=rstd, func=AF.Exp, scale=-0.5), R=[rstdb], W=[rstdb])

            def hnorm(e):
                ins = None
                for c in range(8):
                    ins = e.scalar_tensor_tensor(out=hT[:, c, :], in0=xT[:, c, t0:t0 + G], scalar=pv[:, gcol + c:gcol + c + 1],
                                                 in1=rstd, op0=ALU.mult, op1=ALU.mult)
                return ins
            P.op("dve", hnorm, R=xg + [rstdb, pvb], W=[hb])
            for hp in range(4):
                proj_fm(hp, qT[:, hp, :], [qTb[hp]])
                proj_fm(4 + hp, kT[:, hp, t0:t0 + G], [kTb[hp][g]])
                proj_v(g, hp)
            P.op("dve", lambda e: e.tensor_reduce(out=ksum[:, :, g], in_=kT[:, :, t0:t0 + G], axis=AX.X, op=ALU.add),
                 R=[kTb[hp][g] for hp in range(4)], W=[ksb])
            for h in range(8):
                do_head(g, h)
            if dbg_d is not None and g == 0:
                dbg_ops.append(P.dma("sp", dbg_d[:, 0:2, :].rearrange("p a (h t) -> p (a h) t", h=4), oh[0:64], R=ohb))
                dbg_ops.append(P.dma("sp", dbg_d[:, 2, 0:256], qT[0:64, 0, :], R=qTb))
                dbg_ops.append(P.dma("sp", dbg_d[:, 3, 0:256], kT[0:64, 0, 0:256], R=[kTb[0][0]]))
                dbg_ops.append(P.dma("sp", dbg_d[:, 4:6, :].rearrange("p a (h d) -> p (a h) d", h=4)[:, :, 0:65], Va[0:64, 0, :, :], R=[Vab[0]]))
                dbg_ops.append(P.dma("sp", dbg_d[:, 6, 0:256], oa[0:64, :], R=[oab]))
                dbg_ops.append(P.dma("sp", dbg_d[:, 7, 0:256], rden[0:64, :], R=[rdenb]))
                dbg_ops.append(P.dma("sp", dbg_d[:, 8, 0:256], PT[0][0:64, :], R=[PTb[0]]))
                dbg_ops.append(P.dma("sp", dbg_d[:, 9, 0:256], PT[1][0:64, :], R=[PTb[1]]))
                dbg_ops.append(P.dma("sp", dbg_d[:, 10, 0:256], ncA[0:64, :], R=[mcb]))
                dbg_ops.append(P.dma("sp", dbg_d[0:10, 11, 0:256], qaug[0][0:10, :], R=[qaugb[0]]))
                dbg_ops.append(P.dma("sp", dbg_d[0:10, 12, 0:256], kaug[0:10, 0:256], R=[kaugb]))
            for dh in range(4):
                for dd in range(2):
                    dc = dh * 2 + dd
                    s2 = 0
                    P.dma("sp", wo[s2][0:64], mbout_d[dc], W=[wob[s2]])

                    def mo(e, s2=s2, dd=dd):
                        ins = None
                        for h in range(8):
                            ins = e.matmul(pbank[7][:, dd * G:(dd + 1) * G], wo[s2][0:64, h, :], oh[0:64, h, :], start=(h == 0), stop=(h == 7))
                        return ins
                    P.op("pe", mo, R=[wob[s2]] + ohb, W=[pbb[7]])
                si = cnt["wo"] % 2
                cnt["wo"] += 1
                P.op("act", lambda e, si=si: e.activation(out=stg[si], in_=pbank[7][:, 0:2 * G].rearrange("p (c t) -> p c t", c=2), func=AF.Copy),
                     R=[pbb[7]], W=[stgb[si]])
                P.dma("sp", mscr_d[:, dh * 2:(dh + 1) * 2, t0:t0 + G], stg[si], R=[stgb[si]], W=[mscr_b[g]])
        for g_ in range(8):
            do_group(g_)
        P.fence()

    def moba_add():
        A = Arena(arena, ARENA)
        tb = [A.take(4096).rearrange("p (c t) -> p c t", c=8) for i in range(2)]
        tbb = [Buf(), Buf()]
        for g in range(4):
            si = g % 2
            P.dma("sp", tb[si], mscr_d[:, :, g * 512:(g + 1) * 512], R=[mscr_b[2 * g], mscr_b[2 * g + 1]], W=[tbb[si]])
            P.op("dve", lambda e, g=g, si=si: e.tensor_tensor(out=xT[:, :, g * 512:(g + 1) * 512], in0=xT[:, :, g * 512:(g + 1) * 512],
                                                              in1=tb[si], op=ALU.add),
                 R=[tbb[si]] + [xb[c][g] for c in range(8)], W=[xb[c][g] for c in range(8)])
        P.fence()

    P.fence()
    st = stage
    if "f00" in st:
        ffn(0, 0)
    if "moba" in st:
        moba()
    if "rwkv" in st:
        rwkv()
    if "moba" in st:
        moba_add()
    if "f01" in st:
        ffn(0, 2)
    if "f10" in st:
        ffn(1, 0)
    if "hgrn" in st:
        hgrn()
    if "f11" in st:
        ffn(1, 2)
    outs = final_out("final" in st)
    P.emit(nc, k.es, outs + dbg_ops)


PV_NORMG = 0
PV_FINALG = 48
PV_HGNW = 56
PV_LBZ = 64
PV_RW = 80
NPV = 176


class Arena:
    def __init__(self, ap, size):
        self.ap, self.o, self.size = ap, 0, size

    def take(self, n):
        a = self.ap[:, self.o:self.o + n]
        self.o += n
        assert self.o <= self.size, self.o
        return a


def _tile_w_in(w, ncols):
    n = ncols // 128
    return np.ascontiguousarray(w.reshape(8, 128, n, 128).transpose(2, 1, 0, 3))


def _prep_shared(inp):
    sh = {}
    for l in range(2):
        for f, nm in enumerate(("ffn1", "ffn2")):
            sh["wg%d%d" % (l, f)] = _tile_w_in(inp[nm + "_wg"][l], FF)
            sh["wu%d%d" % (l, f)] = _tile_w_in(inp[nm + "_wu"][l], FF)
            wd = inp[nm + "_wd"][l]
            sh["wd%d%d" % (l, f)] = np.ascontiguousarray(wd.reshape(NFC, 128, 8, 128).transpose(2, 1, 0, 3))
    pv = np.zeros((128, NPV), np.float32)
    ng = inp["norm_g"].reshape(6, 8, 128)
    pv[:, PV_NORMG:PV_NORMG + 48] = ng.transpose(2, 0, 1).reshape(128, 48)
    pv[:, PV_FINALG:PV_FINALG + 8] = inp["final_g"].reshape(8, 128).T
    pv[:, PV_HGNW:PV_HGNW + 8] = inp["hg_norm_w"][0].reshape(8, 128).T
    pv[:, PV_LBZ:PV_LBZ + 16] = inp["hg_lb_logits"].reshape(2, 8, 128).transpose(2, 0, 1).reshape(128, 16)
    h64 = lambda v: np.asarray(v).reshape(8, 64).T
    mu = inp["rw_mu"][0]
    pv[0:64, PV_RW:PV_RW + 24] = mu[0:1536].reshape(24, 64).T
    pv[0:64, PV_RW + 24:PV_RW + 32] = h64(inp["rw_w0"][0])
    pv[0:64, PV_RW + 32:PV_RW + 40] = h64(inp["rw_a0"][0])
    pv[0:64, PV_RW + 40:PV_RW + 48] = h64(inp["rw_k_k"][0])
    pv[0:64, PV_RW + 48:PV_RW + 56] = h64(inp["rw_k_a"][0])
    pv[0:64, PV_RW + 64:PV_RW + 72] = h64(inp["rw_r_k"][0])
    pv[0:64, PV_RW + 72:PV_RW + 80] = h64(inp["rw_lnx_w"][0])
    pv[0:64, PV_RW + 80:PV_RW + 88] = h64(inp["rw_lnx_b"][0])
    pv[:, PV_RW + 88] = mu[1536:1664]
    pv[:, PV_RW + 89] = mu[1664:1792]
    wi = inp["ev_w_in"][0]
    sh["rwin"] = np.ascontiguousarray(wi[:, 0:1536].reshape(8, 128, 24, 64).transpose(2, 1, 0, 3))
    sh["rwlo"] = _tile_w_in(wi[:, 1536:1792], 256)
    sh["rww2"] = np.ascontiguousarray(inp["rw_w2"][0])
    sh["rwa2"] = np.ascontiguousarray(inp["rw_a2"][0])
    sh["rwg2"] = np.ascontiguousarray(inp["rw_g2"][0])
    wo_ = inp["ev_w_out"][0]
    sh["rwout"] = np.ascontiguousarray(wo_[0:512].reshape(8, 64, 8, 128).transpose(2, 1, 0, 3))
    sh["pvec"] = pv
    sh["odwin"] = _tile_w_in(inp["od_w_in"][0], 4096)
    sh["odwout"] = _tile_w_in(inp["od_w_out"][0], 1024)
    sh["mbin"] = _tile_w_in(wi[:, 1792:3328], 1536)
    sh["mbout"] = np.ascontiguousarray(wo_[512:1024].reshape(8, 64, 8, 128).transpose(2, 1, 0, 3))
    pos = np.arange(S, dtype=np.float32)
    kaug = np.zeros((10, S), np.float32)
    for j in range(8):
        kaug[j, j * 256:(j + 1) * 256] = 240000.0
    kaug[8] = pos
    kaug[9] = 1.0
    sh["mbkaug"] = kaug
    slopes = np.exp2(-np.arange(1, 9, dtype=np.float32))
    qc = np.zeros((8, 2, S), np.float32)
    qc[:, 0, :] = 8.0 * slopes[:, None]
    qc[:, 1, :] = -8.0 * slopes[:, None] * pos[None, :]
    sh["mbqc"] = qc
    return sh


_NC_CACHE = {}


def run(inputs, stage=ALL_STAGES, ncores=8, trace=False):
    stage = tuple(stage)
    import time
    t0 = time.time()
    inp = {k_: np.asarray(v, dtype=np.float32) for k_, v in inputs.items()}
    sh = _prep_shared(inp)
    t1 = time.time()
    if stage not in _NC_CACHE:
        _NC_CACHE[stage] = build(stage)
    t2 = time.time()
    print("[kernel] prep %.1fs build %.1fs" % (t1 - t0, t2 - t1), flush=True)
    nc = _NC_CACHE[stage]
    in_maps = []
    for b in range(ncores):
        m = dict(sh)
        m["xT"] = np.ascontiguousarray(inp["x"][b].T.reshape(8, 128, S).transpose(1, 0, 2))
        in_maps.append(m)
    t3 = time.time()
    res = run_bass_kernel_spmd(nc, in_maps, core_ids=list(range(ncores)), trace=trace)
    print("[kernel] run %.1fs" % (time.time() - t3), flush=True)
    outs = []
    for b in range(ncores):
        o = np.asarray(res.results[b]["outT"])
        outs.append(o.transpose(1, 0, 2).reshape(D, S).T)
    if "dbg" in stage:
        np.save("dbg_out.npy", np.asarray(res.results[0]["dbg"]))
    return np.stack(outs).astype(np.float32), res


def kernel(**inputs):
    out, _ = run(inputs)
    return out
```

```python
import numpy as np
from contextlib import ExitStack
import concourse.bass as bass
import concourse.mybir as mybir
from concourse.bass_utils import run_bass_kernel_spmd

F32 = mybir.dt.float32
F32R = mybir.dt.float32r
BF16 = mybir.dt.bfloat16
AF = mybir.ActivationFunctionType
ALU = mybir.AluOpType
AX = mybir.AxisListType

D = 1024
S = 2048
FF = 2816
NFC = 22
EPS = 1e-6


class Buf:
    __slots__ = ("lw", "rd", "name")

    def __init__(self, name=""):
        self.lw = None
        self.rd = []
        self.name = name


class Op:
    __slots__ = ("eng", "fn", "deps", "needed", "sem", "val", "is_dma", "prev_dma")

    def __init__(self, eng, fn):
        self.eng = eng
        self.fn = fn
        self.deps = []
        self.needed = False
        self.sem = None
        self.val = 0
        self.is_dma = False
        self.prev_dma = None


ENGS = ("pe", "dve", "act", "pool", "sp")


class Prog:
    NSLOT = 6

    def __init__(self):
        self.ops = {e: [] for e in ENGS}
        self.fence_deps = []
        self.last = {e: None for e in ENGS}
        self.dma_slots = {e: [] for e in ENGS}
        self.dma_count = {e: 0 for e in ENGS}
        self.all_dma_last = {}

    def _collect(self, op, R, W):
        deps = []
        for b in R:
            if b.lw is not None:
                deps.append(b.lw)
        for b in W:
            if b.lw is not None:
                deps.append(b.lw)
            deps.extend(b.rd)
        deps.extend(self.fence_deps)
        seen = set()
        for d in deps:
            if id(d) in seen or d is op:
                continue
            seen.add(id(d))
            if d.eng == "pe" and op.eng == "pe" and not d.is_dma and not op.is_dma:
                continue
            op.deps.append(d)
            d.needed = True
        for b in W:
            b.lw = op
            b.rd = []
        for b in R:
            b.rd.append(op)

    def op(self, eng, fn, R=(), W=()):
        o = Op(eng, fn)
        self._collect(o, R, W)
        self.ops[eng].append(o)
        self.last[eng] = o
        return o

    def dma(self, q, out, in_, R=(), W=()):
        o = Op(q, lambda e: e.dma_start(out=out, in_=in_))
        o.is_dma = True
        o.needed = True
        k = self.dma_count[q]
        self.dma_count[q] += 1
        slot = k % self.NSLOT
        o.sem = ("dma", q, slot)
        o.val = 16 * (k // self.NSLOT + 1)
        slots = self.dma_slots[q]
        if len(slots) <= slot:
            slots.append(None)
        o.prev_dma = slots[slot]
        slots[slot] = o
        self._collect(o, R, W)
        self.ops[q].append(o)
        self.all_dma_last[(q, slot)] = o
        return o

    def fence(self):
        deps = [o for o in self.last.values() if o is not None]
        deps += list(self.all_dma_last.values())
        for d in deps:
            d.needed = True
        self.fence_deps = deps

    def emit(self, nc, es, final_waits):
        sems = {}
        for e in ENGS:
            sems[("eng", e)] = es.enter_context(nc.semaphore("s_" + e))
            for sl in range(len(self.dma_slots[e])):
                sems[("dma", e, sl)] = es.enter_context(nc.semaphore("d_%s_%d" % (e, sl)))
        for e in ENGS:
            cnt = 0
            for o in self.ops[e]:
                if o.is_dma:
                    continue
                o.sem = ("eng", e)
                if o.needed:
                    cnt += 1
                    o.val = cnt
        handles = {"pe": "tensor", "dve": "vector", "act": "scalar", "pool": "gpsimd", "sp": "sync"}
        block = es.enter_context(nc.Block())

        def make(e):
            def body(eng):
                seen = {}
                for o in self.ops[e]:
                    waits = list(o.deps)
                    if o.is_dma and o.prev_dma is not None:
                        waits.append(o.prev_dma)
                    for d in waits:
                        if seen.get(d.sem, 0) < d.val:
                            eng.wait_ge(sems[d.sem], d.val)
                            seen[d.sem] = d.val
                    ins = o.fn(eng)
                    if o.is_dma:
                        ins.then_inc(sems[o.sem], 16)
                    elif o.needed:
                        ins.then_inc(sems[o.sem], 1)
                if e == "sp":
                    for d in final_waits:
                        if seen.get(d.sem, 0) < d.val:
                            eng.wait_ge(sems[d.sem], d.val)
                            seen[d.sem] = d.val
            return body

        for e in ENGS:
            getattr(block, handles[e])(make(e))


def r32(ap):
    return ap


class K:
    def __init__(self, stage):
        self.stage = stage
        self.nc = bass.Bass("TRN2", target_bir_lowering=False)
        self.P = Prog()
        self.es = ExitStack()
        self.wq = 0

    def dram_in(self, name, shape, dt=F32):
        return self.nc.dram_tensor(name, list(shape), dt, kind="ExternalInput").ap()

    def sb(self, name, shape, dt=F32):
        return self.es.enter_context(self.nc.sbuf_tensor(name, list(shape), dt))

    def ps(self, name, shape, dt=F32):
        return self.es.enter_context(self.nc.psum_tensor(name, list(shape), dt))


ALL_STAGES = ("f00", "rwkv", "moba", "f01", "f10", "hgrn", "f11", "final")


def build(stage=ALL_STAGES):
    k = K(stage)
    nc, P, es = k.nc, k.P, k.es
    with es:
        _build(k)
    return nc


def _build(k):
    nc, P = k.nc, k.P
    stage = k.stage
    xT_d = k.dram_in("xT", [128, 8, S])
    pv_d = k.dram_in("pvec", [128, NPV])
    wg_d = [[k.dram_in("wg%d%d" % (l, f), [NFC, 128, 8, 128]) for f in range(2)] for l in range(2)]
    wu_d = [[k.dram_in("wu%d%d" % (l, f), [NFC, 128, 8, 128]) for f in range(2)] for l in range(2)]
    wd_d = [[k.dram_in("wd%d%d" % (l, f), [8, 128, NFC, 128]) for f in range(2)] for l in range(2)]
    odwin_d = k.dram_in("odwin", [32, 128, 8, 128])
    odwout_d = k.dram_in("odwout", [8, 128, 8, 128])
    rwin_d = k.dram_in("rwin", [24, 128, 8, 64])
    rwlo_d = k.dram_in("rwlo", [2, 128, 8, 128])
    rww2_d = k.dram_in("rww2", [64, 512])
    rwa2_d = k.dram_in("rwa2", [64, 512])
    rwg2_d = k.dram_in("rwg2", [128, 512])
    rwout_d = k.dram_in("rwout", [8, 64, 8, 128])
    mbin_d = k.dram_in("mbin", [12, 128, 8, 128])
    mbout_d = k.dram_in("mbout", [8, 64, 8, 128])
    mbkaug_d = k.dram_in("mbkaug", [10, S])
    mbqc_d = k.dram_in("mbqc", [8, 2, S])
    mscr_d = nc.dram_tensor("mscr", [128, 8, S], F32).ap()
    mscr_b = [Buf() for g in range(8)]
    dbg_d = nc.dram_tensor("dbg", [64, 32, 1024], F32, kind="ExternalOutput").ap() if "dbg" in stage else None
    dbg_ops = []
    out_d = nc.dram_tensor("outT", [128, 8, S], F32, kind="ExternalOutput").ap()

    xT = k.sb("xT_sb", [128, 8, S])
    xb = [[Buf("x%d_%d" % (c, g)) for g in range(4)] for c in range(8)]
    pv = k.sb("pv_sb", [128, NPV])
    pvb = Buf("pv")
    ones = k.sb("ones", [128, 128])
    onesb = Buf("ones")
    epsb_t = k.sb("epsc", [128, 4])
    epsb = Buf("eps")
    ARENA = 32800
    arena = k.sb("arena", [128, ARENA])

    pbank = [k.ps("pb%d" % i, [128, 512]) for i in range(8)]
    pbb = [Buf("pb%d" % i) for i in range(8)]

    P.op("pool", lambda e: e.memset(ones[:], 1.0), W=[onesb])
    P.op("pool", lambda e: e.memset(epsb_t[:, 0:1], EPS), W=[epsb])
    P.dma("sp", pv[:], pv_d[:], W=[pvb])
    for c in range(8):
        for g in range(4):
            P.dma("sp", xT[:, c, g * 512:(g + 1) * 512], xT_d[:, c, g * 512:(g + 1) * 512], W=[xb[c][g]])

    ident = k.sb("ident", [128, 128])
    mask4t = k.sb("mask4", [128, 512])
    mask4 = mask4t[:].rearrange("p (c m) -> p c m", c=4)
    resetm = k.sb("resetm", [128, 512])
    lbt = k.sb("lbt", [128, 16])
    cb = Buf("consts")
    lbb = Buf("lb")

    def setup_consts(e):
        e.memset(ident[:], 0.0)
        e.affine_select(out=ident[:], in_=ones[:], pattern=[[1, 128]], compare_op=ALU.is_equal, fill=0.0, base=0, channel_multiplier=-1)
        for i in range(4):
            e.affine_select(out=mask4t[:, i * 128:(i + 1) * 128], in_=ones[:], pattern=[[1, 128]], compare_op=ALU.is_ge, fill=0.0,
                            base=0, channel_multiplier=-1)
            e.memset(mask4t[0:64, i * 128 + 64:(i + 1) * 128], 0.0)
        e.memset(resetm[:], 1.0)
        ins = None
        for i in range(8):
            ins = e.memset(resetm[:, i * 64:i * 64 + 1], 0.0)
        return ins
    P.op("pool", setup_consts, R=[onesb], W=[cb])
    P.op("dve", lambda e: e.tensor_tensor(out=lbt[:, 0:8], in0=pv[:, PV_LBZ + 8:PV_LBZ + 16], in1=pv[:, PV_LBZ:PV_LBZ + 8], op=ALU.subtract),
         R=[pvb], W=[lbb])
    P.op("act", lambda e: e.activation(out=lbt[:, 0:8], in_=lbt[:, 0:8], func=AF.Sigmoid), R=[lbb], W=[lbb])
    P.op("dve", lambda e: e.tensor_scalar(out=lbt[:, 8:16], in0=lbt[:, 0:8], scalar1=-1.0, scalar2=1.0, op0=ALU.mult, op1=ALU.add),
         R=[lbb], W=[lbb])

    mk = k.sb("rwmask", [64, 4 * 512])
    mLs = mk[:, 0:512].rearrange("p (h t) -> p h t", h=8)
    mUs = mk[:, 512:1024].rearrange("p (h t) -> p h t", h=8)
    mUi = mk[:, 1024:1536].rearrange("p (h t) -> p h t", h=8)
    id8 = mk[:, 1536:2048].rearrange("p (h t) -> p h t", h=8)
    omka = k.sb("omka", [64, 8])
    cb2 = Buf("consts2")

    def setup2(e):
        o3 = ones[0:64, :].rearrange("p (a b) -> p a b", a=2)
        e.memset(mk[:], 1.0)
        e.affine_select(out=mLs, in_=mLs, pattern=[[0, 8], [-1, 64]], compare_op=ALU.is_gt, fill=0.0, base=0, channel_multiplier=1)
        e.affine_select(out=mUs, in_=mUs, pattern=[[0, 8], [1, 64]], compare_op=ALU.is_gt, fill=0.0, base=0, channel_multiplier=-1)
        e.affine_select(out=mUi, in_=mUi, pattern=[[0, 8], [1, 64]], compare_op=ALU.is_ge, fill=0.0, base=0, channel_multiplier=-1)
        return e.affine_select(out=id8, in_=id8, pattern=[[0, 8], [1, 64]], compare_op=ALU.is_equal, fill=0.0, base=0, channel_multiplier=-1)
    P.op("pool", setup2, W=[cb2])
    P.op("dve", lambda e: e.tensor_scalar(out=omka[:], in0=pv[0:64, PV_RW + 48:PV_RW + 56], scalar1=-1.0, scalar2=1.0, op0=ALU.mult, op1=ALU.add),
         R=[pvb], W=[cb2])
    P.op("pool", lambda e: e.memset(epsb_t[:, 1:2], 64e-5), W=[epsb])

    def rstd_group(g, rstd_ap, rstd_buf, sq_ap, sq_buf, bank, ndiv=1024.0):
        P.op("act", lambda e: e.activation(out=sq_ap, in_=xT[:, :, g * 512:(g + 1) * 512], func=AF.Square),
             R=[xb[c][g] for c in range(8)], W=[sq_buf])

        def mm(e):
            ins = None
            for c in range(8):
                ins = e.matmul(pbank[bank][:], ones[:], sq_ap[:, c, :], start=(c == 0), stop=(c == 7))
            return ins
        P.op("pe", mm, R=[sq_buf, onesb], W=[pbb[bank]])
        P.op("act", lambda e: e.activation(out=rstd_ap, in_=pbank[bank][:], func=AF.Ln, bias=epsb_t[:, 0:1], scale=1.0 / ndiv),
             R=[pbb[bank], epsb], W=[rstd_buf])
        P.op("act", lambda e: e.activation(out=rstd_ap, in_=rstd_ap, func=AF.Exp, scale=-0.5),
             R=[rstd_buf], W=[rstd_buf])

    def ffn(l, which):
        f = 0 if which == 0 else 1
        gcol = PV_NORMG + (l * 3 + which) * 8
        A = Arena(arena, ARENA)
        hT = A.take(8192).bitcast(BF16).rearrange("p (c t) -> p c t", c=8)
        act = A.take(11264).bitcast(BF16).rearrange("p (c t) -> p c t", c=11)
        sq = arena[:, 8192:8192 + 4096].rearrange("p (c t) -> p c t", c=8)
        rstd = A.take(512)
        sg = [A.take(512) for i in range(2)]
        NW = 3
        wgb = [A.take(512).bitcast(BF16).rearrange("p (c m) -> p c m", c=8) for i in range(NW)]
        wub = [A.take(512).bitcast(BF16).rearrange("p (c m) -> p c m", c=8) for i in range(NW)]
        wdb = [A.take(704).bitcast(BF16).rearrange("p (c m) -> p c m", c=11) for i in range(2)]
        wgs = [A.take(1024).rearrange("p (c m) -> p c m", c=8) for i in range(2)]
        wus = [A.take(1024).rearrange("p (c m) -> p c m", c=8) for i in range(2)]
        wds = [A.take(1408).rearrange("p (c m) -> p c m", c=11) for i in range(2)]
        wgsb = [Buf(), Buf()]; wusb = [Buf(), Buf()]; wdsb = [Buf(), Buf()]
        hb = [[Buf() for g in range(4)] for c in range(8)]
        actb = [[Buf() for g in range(4)] for c in range(11)]
        sqb, rstdb = Buf(), Buf()
        sgb = [Buf(), Buf()]
        wgbb = [Buf() for i in range(NW)]
        wubb = [Buf() for i in range(NW)]
        wdbb = [Buf(), Buf()]
        cnt = {"w": 0, "wd": 0, "p": 0, "s": 0, "ws": 0}
        for g in range(4):
            rstd_group(g, rstd, rstdb, sq, sqb, 7)
            for c in range(8):
                P.op("dve", lambda e, c=c, g=g: e.scalar_tensor_tensor(
                    out=hT[:, c, g * 512:(g + 1) * 512], in0=xT[:, c, g * 512:(g + 1) * 512],
                    scalar=pv[:, gcol + c:gcol + c + 1], in1=rstd, op0=ALU.mult, op1=ALU.mult),
                    R=[xb[c][g], rstdb, pvb], W=[hb[c][g]])
        P.fence()

        def phase_a(fc, fl):
            s = cnt["w"] % NW
            cnt["w"] += 1
            ss_ = cnt["ws"] % 2
            cnt["ws"] += 1
            P.dma("sp", wgs[ss_], wg_d[l][f][fc], W=[wgsb[ss_]])
            P.dma("sp", wus[ss_], wu_d[l][f][fc], W=[wusb[ss_]])
            P.op("pool", lambda e: e.tensor_copy(out=wgb[s], in_=wgs[ss_]), R=[wgsb[ss_]], W=[wgbb[s]])
            P.op("pool", lambda e: e.tensor_copy(out=wub[s], in_=wus[ss_]), R=[wusb[ss_]], W=[wubb[s]])
            def a_group(g):
                bg, bu = (cnt["p"] % 2) * 2, (cnt["p"] % 2) * 2 + 1
                cnt["p"] += 1
                si = cnt["s"] % 2
                cnt["s"] += 1

                def mmg(e):
                    ins = None
                    for c in range(8):
                        ins = e.matmul(pbank[bg][:], wgb[s][:, c, :], hT[:, c, g * 512:(g + 1) * 512], start=(c == 0), stop=(c == 7))
                    return ins

                def mmu(e):
                    ins = None
                    for c in range(8):
                        ins = e.matmul(pbank[bu][:], wub[s][:, c, :], hT[:, c, g * 512:(g + 1) * 512], start=(c == 0), stop=(c == 7))
                    return ins
                P.op("pe", mmg, R=[wgbb[s]] + [hb[c][g] for c in range(8)], W=[pbb[bg]])
                P.op("pe", mmu, R=[wubb[s]] + [hb[c][g] for c in range(8)], W=[pbb[bu]])
                P.op("act", lambda e: e.activation(out=sg[si], in_=pbank[bg][:], func=AF.Silu), R=[pbb[bg]], W=[sgb[si]])
                P.op("dve", lambda e: e.tensor_tensor(out=act[:, fl, g * 512:(g + 1) * 512], in0=pbank[bu][:], in1=sg[si], op=ALU.mult),
                     R=[pbb[bu], sgb[si]], W=[actb[fl][g]])
            for g_ in range(4):
                a_group(g_)

        def phase_b(fh, dc):
            s = cnt["wd"] % 2
            cnt["wd"] += 1
            P.dma("sp", wds[s], wd_d[l][f][dc][:, fh * 11:(fh + 1) * 11, :], W=[wdsb[s]])
            P.op("pool", lambda e: e.tensor_copy(out=wdb[s], in_=wds[s]), R=[wdsb[s]], W=[wdbb[s]])
            def b_group(g):
                bo = 4 + (cnt["p"] % 2)
                cnt["p"] += 1

                def mmd(e):
                    ins = None
                    for fl in range(11):
                        ins = e.matmul(pbank[bo][:], wdb[s][:, fl, :], act[:, fl, g * 512:(g + 1) * 512], start=(fl == 0), stop=(fl == 10))
                    return ins
                P.op("pe", mmd, R=[wdbb[s]] + [actb[fl][g] for fl in range(11)], W=[pbb[bo]])
                P.op("dve", lambda e: e.scalar_tensor_tensor(
                    out=xT[:, dc, g * 512:(g + 1) * 512], in0=pbank[bo][:], scalar=0.5,
                    in1=xT[:, dc, g * 512:(g + 1) * 512], op0=ALU.mult, op1=ALU.add),
                    R=[pbb[bo], xb[dc][g]], W=[xb[dc][g]])
            for g_ in range(4):
                b_group(g_)
        for fh in range(2):
            for fl in range(11):
                phase_a(fh * 11 + fl, fl)
            for dc in range(8):
                phase_b(fh, dc)
        P.fence()

    def final_out(norm):
        o = 0
        sq = arena[:, o:o + 4096].rearrange("p (c t) -> p c t", c=8); o += 4096
        rstd = arena[:, o:o + 512]; o += 512
        ob = [arena[:, o + i * 4096:o + (i + 1) * 4096].rearrange("p (c t) -> p c t", c=8) for i in range(2)]; o += 8192
        sqb, rstdb = Buf(), Buf()
        obb = [Buf(), Buf()]
        outs = []
        for g in range(4):
            s = g % 2
            if norm:
                rstd_group(g, rstd, rstdb, sq, sqb, 7)
                for c in range(8):
                    P.op("dve", lambda e, c=c, g=g, s=s: e.scalar_tensor_tensor(
                        out=ob[s][:, c, :], in0=xT[:, c, g * 512:(g + 1) * 512],
                        scalar=pv[:, PV_FINALG + c:PV_FINALG + c + 1], in1=rstd, op0=ALU.mult, op1=ALU.mult),
                        R=[xb[c][g], rstdb, pvb], W=[obb[s]])
                outs.append(P.dma("sp", out_d[:, :, g * 512:(g + 1) * 512], ob[s], R=[obb[s]]))
            else:
                outs.append(P.dma("sp", out_d[:, :, g * 512:(g + 1) * 512], xT[:, :, g * 512:(g + 1) * 512],
                                  R=[xb[c][g] for c in range(8)]))
        return outs


    def hgrn():
        A = Arena(arena, ARENA)
        hT = A.take(2048).bitcast(BF16).rearrange("p (c t) -> p c t", c=8)
        hb = [Buf() for c in range(8)]
        wts = [A.take(1024).rearrange("p (c m) -> p c m", c=8) for j in range(4)]
        wtsb = [Buf() for j in range(4)]
        wt = [[A.take(512).bitcast(BF16).rearrange("p (c m) -> p c m", c=8) for j in range(4)] for s_ in range(2)]
        wtb = [[Buf() for j in range(4)] for s_ in range(2)]
        names = ["qT", "fT", "lf", "kT", "bT", "eb", "qe", "ke", "e2", "sgT", "oTs", "sqo", "rs2", "tmp"]
        T = {n: A.take(512) for n in names}
        TB = {n: Buf(n) for n in names}
        ke2tm = A.take(512).rearrange("p (c m) -> p c m", c=4); ke2tmb = Buf()
        vtm = A.take(512).rearrange("p (c m) -> p c m", c=4); vtmb = Buf()
        scs = A.take(512).rearrange("p (c m) -> p c m", c=4); scsb = Buf()
        ebl = A.take(8); eblb = Buf()
        state = [A.take(1024).rearrange("p (h v) -> p h v", h=8) for i in range(2)]
        stb = [[Buf() for h in range(8)] for i in range(2)]
        yT = A.take(2048).bitcast(BF16).rearrange("p (c t) -> p c t", c=8)
        yb = [Buf() for h in range(8)]
        wos = [A.take(1024).rearrange("p (c m) -> p c m", c=8) for i in range(2)]
        wosb = [Buf(), Buf()]
        wo = [A.take(512).bitcast(BF16).rearrange("p (c m) -> p c m", c=8) for i in range(2)]
        wob = [Buf(), Buf()]
        sq = A.take(4096).rearrange("p (c t) -> p c t", c=8); sqb = Buf()
        rstd = A.take(512); rstdb = Buf()
        gcol = PV_NORMG + (1 * 3 + 1) * 8
        scur = [0] * 8
        for h in range(8):
            P.op("pool", lambda e, h=h: e.memset(state[0][:, h, :], 0.0), W=[stb[0][h]])
        wi = 0
        woi = 0
        for g in range(4):
            rstd_group(g, rstd, rstdb, sq, sqb, 7)
            for c in range(8):
                P.op("dve", lambda e, c=c, g=g: e.scalar_tensor_tensor(
                    out=hT[:, c, :], in0=xT[:, c, g * 512:(g + 1) * 512],
                    scalar=pv[:, gcol + c:gcol + c + 1], in1=rstd, op0=ALU.mult, op1=ALU.mult),
                    R=[xb[c][g], rstdb, pvb], W=[hb[c]])
            for h in range(8):
                s_ = wi % 2
                wi += 1
                for j in range(4):
                    P.dma("sp", wts[j], odwin_d[j * 8 + h], W=[wtsb[j]])
                    P.op("pool", lambda e, j=j, s_=s_: e.tensor_copy(out=wt[s_][j], in_=wts[j]), R=[wtsb[j]], W=[wtb[s_][j]])

                def proj(j, bank, s_=s_):
                    def mm(e):
                        ins = None
                        for c in range(8):
                            ins = e.matmul(pbank[bank][:], wt[s_][j][:, c, :], hT[:, c, :], start=(c == 0), stop=(c == 7))
                        return ins
                    P.op("pe", mm, R=[wtb[s_][j]] + hb, W=[pbb[bank]])
                proj(0, 0)
                P.op("act", lambda e: e.activation(out=T["qT"], in_=pbank[0][:], func=AF.Copy), R=[pbb[0]], W=[TB["qT"]])
                proj(1, 1)
                P.op("act", lambda e: e.activation(out=T["fT"], in_=pbank[1][:], func=AF.Sigmoid), R=[pbb[1]], W=[TB["fT"]])
                P.op("dve", lambda e, h=h: e.tensor_scalar(out=T["fT"], in0=T["fT"], scalar1=lbt[:, 8 + h:9 + h], scalar2=lbt[:, h:h + 1],
                                                           op0=ALU.mult, op1=ALU.add), R=[TB["fT"], lbb], W=[TB["fT"]])
                P.op("act", lambda e: e.activation(out=T["lf"], in_=T["fT"], func=AF.Ln), R=[TB["fT"]], W=[TB["lf"]])
                P.op("dve", lambda e: e.tensor_scalar(out=T["kT"], in0=T["fT"], scalar1=-1.0, scalar2=1.0, op0=ALU.mult, op1=ALU.add),
                     R=[TB["fT"]], W=[TB["kT"]])
                P.op("dve", lambda e: e.tensor_tensor_scan(out=T["bT"], data0=resetm[:], data1=T["lf"], initial=0.0, op0=ALU.mult, op1=ALU.add),
                     R=[TB["lf"], cb], W=[TB["bT"]])
                P.op("act", lambda e: e.activation(out=T["eb"], in_=T["bT"], func=AF.Exp), R=[TB["bT"]], W=[TB["eb"]])
                P.op("dve", lambda e: e.tensor_tensor(out=T["qe"], in0=T["qT"], in1=T["eb"], op=ALU.mult), R=[TB["qT"], TB["eb"]], W=[TB["qe"]])
                P.op("act", lambda e: e.activation(out=T["eb"], in_=T["bT"], func=AF.Exp, scale=-1.0), R=[TB["bT"], TB["qe"]], W=[TB["eb"]])
                P.op("dve", lambda e: e.tensor_tensor(out=T["ke"], in0=T["kT"], in1=T["eb"], op=ALU.mult), R=[TB["kT"], TB["eb"]], W=[TB["ke"]])

                def e2f(e):
                    ins = None
                    for ci in range(8):
                        ins = e.activation(out=T["e2"][:, ci * 64:(ci + 1) * 64], in_=T["bT"][:, ci * 64:(ci + 1) * 64], func=AF.Exp,
                                           scale=-1.0, bias=T["bT"][:, ci * 64 + 63:ci * 64 + 64])
                    return ins
                P.op("act", e2f, R=[TB["bT"]], W=[TB["e2"]])
                P.op("dve", lambda e: e.tensor_tensor(out=T["e2"], in0=T["e2"], in1=T["kT"], op=ALU.mult), R=[TB["kT"], TB["e2"]], W=[TB["e2"]])
                P.op("act", lambda e: e.activation(out=ebl, in_=T["bT"].rearrange("p (c t) -> p c t", t=64)[:, :, 63], func=AF.Exp),
                     R=[TB["bT"]], W=[eblb])

                def tr(e):
                    ins = None
                    for tt in range(4):
                        ins = e.transpose(pbank[2][:, tt * 128:(tt + 1) * 128], T["e2"][:, tt * 128:(tt + 1) * 128], ident[:])
                    return ins
                P.op("pe", tr, R=[TB["e2"], cb], W=[pbb[2]])
                P.op("act", lambda e: e.activation(out=ke2tm, in_=pbank[2][:].rearrange("p (c m) -> p c m", c=4), func=AF.Copy),
                     R=[pbb[2]], W=[ke2tmb])

                def vproj(e, s_=s_):
                    ins = None
                    for tt in range(4):
                        for c in range(8):
                            ins = e.matmul(pbank[3][:, tt * 128:(tt + 1) * 128], hT[:, c, tt * 128:(tt + 1) * 128], wt[s_][2][:, c, :],
                                           start=(c == 0), stop=(c == 7))
                    return ins
                P.op("pe", vproj, R=[wtb[s_][2]] + hb, W=[pbb[3]])
                P.op("dve", lambda e: e.tensor_copy(out=vtm, in_=pbank[3][:].rearrange("p (c m) -> p c m", c=4)), R=[pbb[3]], W=[vtmb])
                proj(3, 0)
                P.op("act", lambda e: e.activation(out=T["sgT"], in_=pbank[0][:], func=AF.Sigmoid), R=[pbb[0]], W=[TB["sgT"]])

                def scm(e):
                    ins = None
                    for p_ in range(4):
                        ins = e.matmul(pbank[4][:, p_ * 128:(p_ + 1) * 128], T["ke"][:, p_ * 128:(p_ + 1) * 128],
                                       T["qe"][:, p_ * 128:(p_ + 1) * 128], start=True, stop=True)
                    return ins
                P.op("pe", scm, R=[TB["ke"], TB["qe"]], W=[pbb[4]])
                P.op("dve", lambda e: e.tensor_tensor(out=scs, in0=pbank[4][:].rearrange("p (c m) -> p c m", c=4), in1=mask4, op=ALU.mult),
                     R=[pbb[4], cb], W=[scsb])
                for p_ in range(4):
                    for half in range(2):
                        ci = p_ * 2 + half
                        sc = scur[h]
                        cs = p_ * 128 + half * 64

                        def omm(e, p_=p_, half=half, sc=sc, cs=cs, h=h):
                            if half == 0:
                                e.matmul(pbank[5][:, p_ * 128:(p_ + 1) * 128], vtm[:, p_, :], scs[:, p_, :], start=True, stop=False)
                            return e.matmul(pbank[5][:, cs:cs + 64], state[sc][:, h, :], T["qe"][:, cs:cs + 64], start=False, stop=(half == 1))
                        P.op("pe", omm, R=[vtmb, scsb, stb[sc][h], TB["qe"]], W=[pbb[5]])
                        r0 = half * 64
                        P.op("pe", lambda e, p_=p_, r0=r0: e.matmul(pbank[6][:, 0:128], ke2tm[r0:r0 + 64, p_, :], vtm[r0:r0 + 64, p_, :],
                                                                     start=True, stop=True), R=[ke2tmb, vtmb], W=[pbb[6]])
                        P.op("dve", lambda e, sc=sc, h=h, ci=ci: e.scalar_tensor_tensor(
                            out=state[1 - sc][:, h, :], in0=state[sc][:, h, :], scalar=ebl[:, ci:ci + 1], in1=pbank[6][:, 0:128],
                            op0=ALU.mult, op1=ALU.add), R=[stb[sc][h], eblb, pbb[6]], W=[stb[1 - sc][h]])
                        scur[h] = 1 - sc
                P.op("act", lambda e: e.activation(out=T["oTs"], in_=pbank[5][:], func=AF.Copy), R=[pbb[5]], W=[TB["oTs"]])
                P.op("act", lambda e: e.activation(out=T["sqo"], in_=pbank[5][:], func=AF.Square), R=[pbb[5]], W=[TB["sqo"]])
                P.op("pe", lambda e: e.matmul(pbank[7][:], ones[:], T["sqo"], start=True, stop=True), R=[TB["sqo"], onesb], W=[pbb[7]])
                P.op("act", lambda e: e.activation(out=T["rs2"], in_=pbank[7][:], func=AF.Ln, bias=epsb_t[:, 0:1], scale=1.0 / 128.0),
                     R=[pbb[7], epsb], W=[TB["rs2"]])
                P.op("act", lambda e: e.activation(out=T["rs2"], in_=T["rs2"], func=AF.Exp, scale=-0.5), R=[TB["rs2"]], W=[TB["rs2"]])
                P.op("dve", lambda e, h=h: e.scalar_tensor_tensor(out=T["tmp"], in0=T["oTs"], scalar=pv[:, PV_HGNW + h:PV_HGNW + h + 1],
                                                                  in1=T["rs2"], op0=ALU.mult, op1=ALU.mult),
                     R=[TB["oTs"], TB["rs2"], pvb], W=[TB["tmp"]])
                P.op("dve", lambda e, h=h: e.tensor_tensor(out=yT[:, h, :], in0=T["tmp"], in1=T["sgT"], op=ALU.mult),
                     R=[TB["tmp"], TB["sgT"]], W=[yb[h]])
            for dc in range(8):
                s2 = woi % 2
                woi += 1
                P.dma("sp", wos[s2], odwout_d[dc], W=[wosb[s2]])
                P.op("pool", lambda e, s2=s2: e.tensor_copy(out=wo[s2], in_=wos[s2]), R=[wosb[s2]], W=[wob[s2]])

                def mo(e, s2=s2):
                    ins = None
                    for h in range(8):
                        ins = e.matmul(pbank[7][:], wo[s2][:, h, :], yT[:, h, :], start=(h == 0), stop=(h == 7))
                    return ins
                P.op("pe", mo, R=[wob[s2]] + yb, W=[pbb[7]])
                P.op("dve", lambda e, dc=dc, g=g: e.tensor_tensor(out=xT[:, dc, g * 512:(g + 1) * 512], in0=pbank[7][:],
                                                                  in1=xT[:, dc, g * 512:(g + 1) * 512], op=ALU.add),
                     R=[pbb[7], xb[dc][g]], W=[xb[dc][g]])
        P.fence()


    def rwkv():
        G = 128
        NG = S // G
        A = Arena(arena, ARENA)
        hT = A.take(4 * (G + 2)).bitcast(BF16).rearrange("p (c t) -> p c t", c=8); hb = Buf()
        sq = A.take(8 * G).rearrange("p (c t) -> p c t", c=8); sqb = Buf()
        rstd = A.take(G); rstdb = Buf()
        wrkvs = [A.take(512).rearrange("p (c m) -> p c m", c=8) for i in range(3)]
        wrkvsb = [Buf() for i in range(3)]
        wrkv = [[A.take(256).bitcast(BF16).rearrange("p (c m) -> p c m", c=8) for i in range(3)] for s_ in range(2)]
        wrkvb = [[Buf() for i in range(3)] for s_ in range(2)]
        wlos = A.take(1024).rearrange("p (c m) -> p c m", c=8); wlosb = Buf()
        wlo = [A.take(512).bitcast(BF16).rearrange("p (c m) -> p c m", c=8) for i in range(2)]
        wlob = [Buf(), Buf()]
        yfh = A.take(512).bitcast(BF16).rearrange("p (h t) -> p h t", h=8); yfhb = Buf()
        w2a2 = A.take(512); g2 = A.take(512); wconst = Buf()
        praw = [A.take(G + 1) for i in range(2)]; prawb = [Buf(), Buf()]
        dtmp = [A.take(G) for i in range(2)]; dtmpb = [Buf(), Buf()]
        tw = A.take(G); twb = Buf()
        sg = A.take(G); sgb = Buf()
        QN = ["r", "k", "v", "lw", "a", "kk", "Lw", "e", "at", "bt", "kt", "BhT", "KhT"]
        Q = {n: A.take(8 * G).rearrange("p (h t) -> p h t", h=8) for n in QN}
        QB = {n: Buf(n) for n in QN}
        CN = ["P0", "P1", "PT0", "PT1", "TT", "AakT", "ArbT", "ArkT", "Vtm", "Bhtm", "Khtm", "Xs", "Us", "H0", "H1"]
        Cc = {n: A.take(512).rearrange("p (h t) -> p h t", h=8) for n in CN}
        CB = {n: Buf(n) for n in CN}
        gC = A.take(16).rearrange("p (h c) -> p h c", h=8); gCb = Buf()
        wos = A.take(1024).rearrange("p (h m) -> p h m", h=8); wosb = Buf()
        wo = [A.take(512).bitcast(BF16).rearrange("p (h m) -> p h m", h=8) for i in range(2)]; wob = [Buf(), Buf()]
        gcol = PV_NORMG + (0 * 3 + 1) * 8
        MU, W0, A0, KK, KA, OMKA, RK, LNW, LNB = PV_RW, PV_RW + 24, PV_RW + 32, PV_RW + 40, PV_RW + 48, PV_RW + 56, PV_RW + 64, PV_RW + 72, PV_RW + 80
        MUWA, MUG = PV_RW + 88, PV_RW + 89
        o64 = ones[0:64, 0:64]

        P.dma("sp", w2a2[0:64, :], rww2_d[:, :], W=[wconst])
        P.dma("sp", w2a2[64:128, :], rwa2_d[:, :], W=[wconst])
        P.dma("sp", g2, rwg2_d[:, :], W=[wconst])
        P.op("pool", lambda e: e.memset(Cc["H0"][0:64], 0.0), W=[CB["H0"]])
        P.op("pool", lambda e: e.memset(hT[:, :, 0:1], 0.0), W=[hb])
        hcur = 0
        woi = 0
        pi = 0
        def do_group(g):
            nonlocal hcur, woi, pi
            t0 = g * G
            g4 = t0 // 512
            xg = [xb[c][g4] for c in range(8)]
            if g > 0:
                P.op("dve", lambda e: e.tensor_copy(out=hT[:, :, 0:1], in_=hT[:, :, G:G + 1]), R=[hb], W=[hb])
            P.op("act", lambda e, t0=t0: e.activation(out=sq, in_=xT[:, :, t0:t0 + G], func=AF.Square), R=xg, W=[sqb])

            def mmn(e):
                ins = None
                for c in range(8):
                    ins = e.matmul(pbank[7][:, 0:G], ones[:], sq[:, c, :], start=(c == 0), stop=(c == 7))
                return ins
            P.op("pe", mmn, R=[sqb, onesb], W=[pbb[7]])
            P.op("act", lambda e: e.activation(out=rstd, in_=pbank[7][:, 0:G], func=AF.Ln, bias=epsb_t[:, 0:1], scale=1.0 / 1024.0),
                 R=[pbb[7], epsb], W=[rstdb])
            P.op("act", lambda e: e.activation(out=rstd, in_=rstd, func=AF.Exp, scale=-0.5), R=[rstdb], W=[rstdb])

            def hnorm(e, t0=t0):
                ins = None
                for c in range(8):
                    ins = e.scalar_tensor_tensor(out=hT[:, c, 1:G + 1], in0=xT[:, c, t0:t0 + G], scalar=pv[:, gcol + c:gcol + c + 1],
                                                 in1=rstd, op0=ALU.mult, op1=ALU.mult)
                return ins
            P.op("dve", hnorm, R=xg + [rstdb, pvb, hb], W=[hb])
            for h in range(8):
                for qi, qn in enumerate(("r", "k", "v")):
                    P.dma("sp", wrkvs[qi], rwin_d[qi * 8 + h], W=[wrkvsb[qi]])
                    ws_ = h % 2
                    P.op("pool", lambda e, qi=qi, ws_=ws_: e.tensor_copy(out=wrkv[ws_][qi], in_=wrkvs[qi]), R=[wrkvsb[qi]], W=[wrkvb[ws_][qi]])
                    bank = pi % 2
                    sl = pi % 2
                    pi += 1

                    def mm(e, qi=qi, bank=bank, ws_=ws_):
                        ins = None
                        for c in range(8):
                            ins = e.matmul(pbank[bank][0:64, 0:G + 1], wrkv[ws_][qi][:, c, :], hT[:, c, 0:G + 1], start=(c == 0), stop=(c == 7))
                        return ins
                    P.op("pe", mm, R=[wrkvb[ws_][qi], hb], W=[pbb[bank]])
                    P.op("act", lambda e, bank=bank, sl=sl: e.activation(out=praw[sl][0:64, :], in_=pbank[bank][0:64, 0:G + 1], func=AF.Copy),
                         R=[pbb[bank]], W=[prawb[sl]])
                    P.op("dve", lambda e, sl=sl: e.tensor_tensor(out=dtmp[sl][0:64, :], in0=praw[sl][0:64, 0:G], in1=praw[sl][0:64, 1:G + 1],
                                                                 op=ALU.subtract), R=[prawb[sl]], W=[dtmpb[sl]])
                    P.op("dve", lambda e, sl=sl, qn=qn, qi=qi, h=h: e.scalar_tensor_tensor(
                        out=Q[qn][0:64, h, :], in0=dtmp[sl][0:64, :], scalar=pv[0:64, MU + qi * 8 + h:MU + qi * 8 + h + 1],
                        in1=praw[sl][0:64, 1:G + 1], op0=ALU.mult, op1=ALU.add), R=[dtmpb[sl], prawb[sl], pvb], W=[QB[qn]])
            for j in range(2):
                P.dma("sp", wlos, rwlo_d[j], W=[wlosb])
                P.op("pool", lambda e, j=j: e.tensor_copy(out=wlo[j], in_=wlos), R=[wlosb], W=[wlob[j]])
                bank = pi % 2
                sl = pi % 2
                pi += 1

                def mml(e, j=j, bank=bank):
                    ins = None
                    for c in range(8):
                        ins = e.matmul(pbank[bank][:, 0:G + 1], wlo[j][:, c, :], hT[:, c, 0:G + 1], start=(c == 0), stop=(c == 7))
                    return ins
                P.op("pe", mml, R=[wlob[j], hb], W=[pbb[bank]])
                P.op("act", lambda e, bank=bank, sl=sl: e.activation(out=praw[sl], in_=pbank[bank][:, 0:G + 1], func=AF.Copy),
                     R=[pbb[bank]], W=[prawb[sl]])
                P.op("dve", lambda e, sl=sl: e.tensor_tensor(out=dtmp[sl], in0=praw[sl][:, 0:G], in1=praw[sl][:, 1:G + 1], op=ALU.subtract),
                     R=[prawb[sl]], W=[dtmpb[sl]])
                dst, dstb = (tw, twb) if j == 0 else (sg, sgb)
                mcol = MUWA if j == 0 else MUG
                P.op("dve", lambda e, sl=sl, dst=dst, mcol=mcol: e.scalar_tensor_tensor(
                    out=dst, in0=dtmp[sl], scalar=pv[:, mcol:mcol + 1], in1=praw[sl][:, 1:G + 1], op0=ALU.mult, op1=ALU.add),
                    R=[dtmpb[sl], prawb[sl], pvb], W=[dstb])
            P.op("act", lambda e: e.activation(out=tw[0:64, :], in_=tw[0:64, :], func=AF.Tanh), R=[twb], W=[twb])
            P.op("act", lambda e: e.activation(out=sg, in_=sg, func=AF.Sigmoid), R=[sgb], W=[sgb])
            for half in range(2):
                def mmw(e, half=half):
                    ins = None
                    for hh in range(4):
                        h = half * 4 + hh
                        ins = e.matmul(pbank[2][0:64, hh * G:(hh + 1) * G], w2a2[0:64, h * 64:(h + 1) * 64], tw[0:64, :], start=True, stop=True)
                    return ins
                P.op("pe", mmw, R=[wconst, twb], W=[pbb[2]])

                def sw(e, half=half):
                    ins = None
                    for hh in range(4):
                        h = half * 4 + hh
                        ins = e.activation(out=Q["lw"][0:64, h, :], in_=pbank[2][0:64, hh * G:(hh + 1) * G], func=AF.Sigmoid,
                                           bias=pv[0:64, W0 + h:W0 + h + 1])
                    return ins
                P.op("act", sw, R=[pbb[2], pvb], W=[QB["lw"]])

                def mma(e, half=half):
                    ins = None
                    for hh in range(4):
                        h = half * 4 + hh
                        ins = e.matmul(pbank[3][0:64, hh * G:(hh + 1) * G], w2a2[64:128, h * 64:(h + 1) * 64], tw[64:128, :], start=True, stop=True)
                    return ins
                P.op("pe", mma, R=[wconst, twb], W=[pbb[3]])

                def sa(e, half=half):
                    ins = None
                    for hh in range(4):
                        h = half * 4 + hh
                        ins = e.activation(out=Q["a"][0:64, h, :], in_=pbank[3][0:64, hh * G:(hh + 1) * G], func=AF.Sigmoid,
                                           bias=pv[0:64, A0 + h:A0 + h + 1])
                    return ins
                P.op("act", sa, R=[pbb[3], pvb], W=[QB["a"]])
            P.op("dve", lambda e: e.tensor_scalar(out=Q["lw"][0:64], in0=Q["lw"][0:64], scalar1=-0.6065306597126334, scalar2=None, op0=ALU.mult),
                 R=[QB["lw"]], W=[QB["lw"]])
            def kk1(e):
                ins = None
                for h in range(8):
                    ins = e.tensor_scalar(out=Q["kk"][0:64, h, :], in0=Q["k"][0:64, h, :], scalar1=pv[0:64, KK + h:KK + h + 1], scalar2=None, op0=ALU.mult)
                return ins
            P.op("dve", kk1, R=[QB["k"], pvb], W=[QB["kk"]])
            P.op("act", lambda e: e.activation(out=Q["e"][0:64], in_=Q["kk"][0:64], func=AF.Square), R=[QB["kk"]], W=[QB["e"]])
            for half in range(2):
                P.op("pe", lambda e, half=half: e.matmul(pbank[2][0:64, :], o64, Q["e"][0:64, half * 4:(half + 1) * 4, :], start=True, stop=True),
                     R=[QB["e"], onesb], W=[pbb[2]])
                P.op("dve", lambda e, half=half: e.tensor_scalar(out=Q["e"][0:64, half * 4:(half + 1) * 4, :],
                                                                 in0=pbank[2][0:64, :].rearrange("p (h t) -> p h t", h=4),
                                                                 scalar1=1e-24, scalar2=None, op0=ALU.max), R=[pbb[2], QB["e"]], W=[QB["e"]])
            P.op("act", lambda e: e.activation(out=Q["e"][0:64], in_=Q["e"][0:64], func=AF.Ln), R=[QB["e"]], W=[QB["e"]])
            P.op("act", lambda e: e.activation(out=Q["e"][0:64], in_=Q["e"][0:64], func=AF.Exp, scale=-0.5), R=[QB["e"]], W=[QB["e"]])
            P.op("dve", lambda e: e.tensor_tensor(out=Q["kk"][0:64], in0=Q["kk"][0:64], in1=Q["e"][0:64], op=ALU.mult),
                 R=[QB["kk"], QB["e"]], W=[QB["kk"]])
            def km1(e):
                ins = None
                for h in range(8):
                    ins = e.tensor_scalar(out=Q["e"][0:64, h, :], in0=Q["a"][0:64, h, :], scalar1=pv[0:64, KA + h:KA + h + 1],
                                          scalar2=omka[0:64, h:h + 1], op0=ALU.mult, op1=ALU.add)
                return ins
            P.op("dve", km1, R=[QB["a"], pvb, cb2], W=[QB["e"]])
            P.op("dve", lambda e: e.tensor_tensor(out=Q["k"][0:64], in0=Q["k"][0:64], in1=Q["e"][0:64], op=ALU.mult),
                 R=[QB["k"], QB["e"]], W=[QB["k"]])
            P.op("dve", lambda e: e.tensor_tensor(out=Q["a"][0:64], in0=Q["a"][0:64], in1=Q["kk"][0:64], op=ALU.mult),
                 R=[QB["a"], QB["kk"]], W=[QB["a"]])
            def bn1(e):
                ins = None
                for h in range(8):
                    ins = e.scalar_tensor_tensor(out=Q["e"][0:64, h, :], in0=Q["r"][0:64, h, :], scalar=pv[0:64, RK + h:RK + h + 1],
                                                 in1=Q["k"][0:64, h, :], op0=ALU.mult, op1=ALU.mult)
                return ins
            P.op("dve", bn1, R=[QB["r"], QB["k"], pvb], W=[QB["e"]])
            def scn(e):
                ins = None
                for h in range(8):
                    ins = e.tensor_tensor_scan(out=Q["Lw"][0:64, h, :], data0=resetm[0:64, 0:G], data1=Q["lw"][0:64, h, :], initial=0.0,
                                               op0=ALU.mult, op1=ALU.add)
                return ins
            P.op("dve", scn, R=[QB["lw"], cb], W=[QB["Lw"]])
            P.op("dve", lambda e: e.tensor_tensor(out=Q["lw"][0:64], in0=Q["Lw"][0:64], in1=Q["lw"][0:64], op=ALU.subtract),
                 R=[QB["Lw"], QB["lw"]], W=[QB["lw"]])
            for half in range(2):
                P.op("pe", lambda e, half=half: e.matmul(pbank[3][0:64, :], o64, Q["e"][0:64, half * 4:(half + 1) * 4, :], start=True, stop=True),
                     R=[QB["e"], onesb], W=[pbb[3]])
                P.op("dve", lambda e, half=half: e.tensor_tensor(out=Q["BhT"][0:64, half * 4:(half + 1) * 4, :],
                                                                 in0=pbank[3][0:64, :].rearrange("p (h t) -> p h t", h=4),
                                                                 in1=Q["v"][0:64, half * 4:(half + 1) * 4, :], op=ALU.mult),
                     R=[pbb[3], QB["v"]], W=[QB["BhT"]])
            P.op("act", lambda e: e.activation(out=Q["e"][0:64], in_=Q["lw"][0:64], func=AF.Exp), R=[QB["lw"]], W=[QB["e"]])
            P.op("dve", lambda e: e.scalar_tensor_tensor(out=Q["at"][0:64], in0=Q["kk"][0:64], scalar=-1.0, in1=Q["e"][0:64], op0=ALU.mult, op1=ALU.mult),
                 R=[QB["kk"], QB["e"]], W=[QB["at"]])
            P.op("act", lambda e: e.activation(out=Q["lw"][0:64], in_=Q["BhT"][0:64], func=AF.Copy), R=[QB["BhT"], QB["e"]], W=[QB["lw"]])
            P.op("act", lambda e: e.activation(out=Q["e"][0:64], in_=Q["Lw"][0:64], func=AF.Exp), R=[QB["Lw"], QB["at"]], W=[QB["e"]])
            P.op("dve", lambda e: e.tensor_tensor(out=Q["r"][0:64], in0=Q["r"][0:64], in1=Q["e"][0:64], op=ALU.mult), R=[QB["r"], QB["e"]], W=[QB["r"]])
            P.op("act", lambda e: e.activation(out=Q["e"][0:64], in_=Q["Lw"][0:64], func=AF.Exp, scale=-1.0), R=[QB["Lw"], QB["r"]], W=[QB["e"]])
            P.op("dve", lambda e: e.tensor_tensor(out=Q["bt"][0:64], in0=Q["a"][0:64], in1=Q["e"][0:64], op=ALU.mult), R=[QB["a"], QB["e"]], W=[QB["bt"]])
            P.op("dve", lambda e: e.tensor_tensor(out=Q["kt"][0:64], in0=Q["k"][0:64], in1=Q["e"][0:64], op=ALU.mult), R=[QB["k"], QB["e"]], W=[QB["kt"]])
            def eld(e):
                ins = None
                for h in range(8):
                    for ci in range(G // 64):
                        ins = e.activation(out=Q["e"][0:64, h, ci * 64:(ci + 1) * 64], in_=Q["Lw"][0:64, h, ci * 64:(ci + 1) * 64], func=AF.Exp,
                                           scale=-1.0, bias=Q["Lw"][0:64, h, ci * 64 + 63:ci * 64 + 64])
                return ins
            P.op("act", eld, R=[QB["Lw"], QB["bt"], QB["kt"]], W=[QB["e"]])
            P.op("act", lambda e: e.activation(out=gC[0:64], in_=Q["Lw"][0:64].rearrange("p h (c t) -> p h c t", t=64)[:, :, :, 63], func=AF.Exp),
                 R=[QB["Lw"]], W=[gCb])
            P.op("dve", lambda e: e.tensor_tensor(out=Q["BhT"][0:64], in0=Q["a"][0:64], in1=Q["e"][0:64], op=ALU.mult),
                 R=[QB["a"], QB["e"], QB["lw"]], W=[QB["BhT"]])
            P.op("dve", lambda e: e.tensor_tensor(out=Q["KhT"][0:64], in0=Q["k"][0:64], in1=Q["e"][0:64], op=ALU.mult),
                 R=[QB["k"], QB["e"]], W=[QB["KhT"]])
            if dbg_d is not None and g == 0:
                for i_, n_ in enumerate(["r", "k", "v", "lw", "a", "kk", "Lw", "at", "bt", "kt", "BhT", "KhT"]):
                    dbg_ops.append(P.dma("sp", dbg_d[:, i_, :].rearrange("p (h t) -> p h t", h=8), Q[n_][0:64], R=[QB[n_]]))
            def do_chunk(ci):
                nonlocal hcur
                cs = ci * 64

                def amat(bank, ln, rn, dst, mask, eng):
                    def mm(e):
                        ins = None
                        for h in range(8):
                            ins = e.matmul(pbank[bank][0:64, h * 64:(h + 1) * 64], Q[ln][0:64, h, cs:cs + 64], Q[rn][0:64, h, cs:cs + 64],
                                           start=True, stop=True)
                        return ins
                    P.op("pe", mm, R=[QB[ln], QB[rn]], W=[pbb[bank]])
                    P.op(eng, lambda e: e.tensor_tensor(out=Cc[dst][0:64], in0=pbank[bank][0:64, :].rearrange("p (h t) -> p h t", h=8),
                                                        in1=mask, op=ALU.mult), R=[pbb[bank], cb2], W=[CB[dst]])
                amat(2, "at", "bt", "P0", mLs, "dve")
                amat(3, "bt", "at", "PT0", mUs, "dve")
                amat(2, "kt", "at", "AakT", mUs, "dve")
                amat(3, "bt", "r", "ArbT", mUi, "dve")
                amat(2, "kt", "r", "ArkT", mUi, "dve")
                P.op("dve", lambda e: e.tensor_tensor(out=Cc["TT"][0:64], in0=Cc["PT0"][0:64], in1=id8, op=ALU.add), R=[CB["PT0"], cb2], W=[CB["TT"]])
                for src, dst in (("v", "Vtm"), ("BhT", "Bhtm"), ("KhT", "Khtm")):
                    def trp(e, src=src):
                        ins = None
                        for h in range(8):
                            ins = e.transpose(pbank[4][0:64, h * 64:(h + 1) * 64], Q[src][0:64, h, cs:cs + 64], ident[0:64, 0:64])
                        return ins
                    P.op("pe", trp, R=[QB[src], cb], W=[pbb[4]])
                    P.op("act", lambda e, dst=dst: e.activation(out=Cc[dst][0:64], in_=pbank[4][0:64, :].rearrange("p (h t) -> p h t", h=8), func=AF.Copy),
                         R=[pbb[4]], W=[CB[dst]])
                pc = 0
                for lev in range(1, 6):
                    Pn, Pp = "P%d" % (1 - pc), "P%d" % pc
                    PTn, PTp = "PT%d" % (1 - pc), "PT%d" % pc

                    def sqm(e, Pp=Pp, PTp=PTp):
                        ins = None
                        for h in range(8):
                            ins = e.matmul(pbank[2][0:64, h * 64:(h + 1) * 64], Cc[PTp][0:64, h, :], Cc[Pp][0:64, h, :], start=True, stop=True)
                        return ins
                    P.op("pe", sqm, R=[CB[Pp], CB[PTp]], W=[pbb[2]])
                    P.op("act", lambda e, Pn=Pn: e.activation(out=Cc[Pn][0:64], in_=pbank[2][0:64, :].rearrange("p (h t) -> p h t", h=8), func=AF.Copy),
                         R=[pbb[2]], W=[CB[Pn]])
                    if lev < 5:
                        def sqt(e, Pp=Pp, PTp=PTp):
                            ins = None
                            for h in range(8):
                                ins = e.matmul(pbank[3][0:64, h * 64:(h + 1) * 64], Cc[Pp][0:64, h, :], Cc[PTp][0:64, h, :], start=True, stop=True)
                            return ins
                        P.op("pe", sqt, R=[CB[Pp], CB[PTp]], W=[pbb[3]])
                        P.op("dve", lambda e, PTn=PTn: e.tensor_copy(out=Cc[PTn][0:64], in_=pbank[3][0:64, :].rearrange("p (h t) -> p h t", h=8)),
                             R=[pbb[3]], W=[CB[PTn]])

                    def ttm(e, Pn=Pn):
                        ins = None
                        for h in range(8):
                            ins = e.matmul(pbank[5][0:64, h * 64:(h + 1) * 64], Cc[Pn][0:64, h, :], Cc["TT"][0:64, h, :], start=True, stop=True)
                        return ins
                    P.op("pe", ttm, R=[CB[Pn], CB["TT"]], W=[pbb[5]])
                    P.op("dve", lambda e: e.tensor_tensor(out=Cc["TT"][0:64], in0=pbank[5][0:64, :].rearrange("p (h t) -> p h t", h=8),
                                                          in1=Cc["TT"][0:64], op=ALU.add), R=[pbb[5], CB["TT"]], W=[CB["TT"]])
                    pc = 1 - pc
                Hc, Hn = "H%d" % hcur, "H%d" % (1 - hcur)

                def xmm(e, Hc=Hc):
                    ins = None
                    for h in range(8):
                        e.matmul(pbank[6][0:64, h * 64:(h + 1) * 64], Q["at"][0:64, h, cs:cs + 64], Cc[Hc][0:64, h, :], start=True, stop=False)
                        ins = e.matmul(pbank[6][0:64, h * 64:(h + 1) * 64], Cc["AakT"][0:64, h, :], Cc["Vtm"][0:64, h, :], start=False, stop=True)
                    return ins
                P.op("pe", xmm, R=[QB["at"], CB[Hc], CB["AakT"], CB["Vtm"]], W=[pbb[6]])
                P.op("act", lambda e: e.activation(out=Cc["Xs"][0:64], in_=pbank[6][0:64, :].rearrange("p (h t) -> p h t", h=8), func=AF.Copy),
                     R=[pbb[6]], W=[CB["Xs"]])

                def umm(e):
                    ins = None
                    for h in range(8):
                        ins = e.matmul(pbank[6][0:64, h * 64:(h + 1) * 64], Cc["TT"][0:64, h, :], Cc["Xs"][0:64, h, :], start=True, stop=True)
                    return ins
                P.op("pe", umm, R=[CB["TT"], CB["Xs"]], W=[pbb[6]])
                P.op("act", lambda e: e.activation(out=Cc["Us"][0:64], in_=pbank[6][0:64, :].rearrange("p (h t) -> p h t", h=8), func=AF.Copy),
                     R=[pbb[6]], W=[CB["Us"]])

                def ymm(e, Hc=Hc):
                    ins = None
                    for h in range(8):
                        e.matmul(pbank[5][0:64, h * 64:(h + 1) * 64], Cc[Hc][0:64, h, :], Q["r"][0:64, h, cs:cs + 64], start=True, stop=False)
                        e.matmul(pbank[5][0:64, h * 64:(h + 1) * 64], Cc["Us"][0:64, h, :], Cc["ArbT"][0:64, h, :], start=False, stop=False)
                        ins = e.matmul(pbank[5][0:64, h * 64:(h + 1) * 64], Cc["Vtm"][0:64, h, :], Cc["ArkT"][0:64, h, :], start=False, stop=True)
                    return ins
                P.op("pe", ymm, R=[CB[Hc], QB["r"], CB["Us"], CB["ArbT"], CB["Vtm"], CB["ArkT"]], W=[pbb[5]])
                P.op("act", lambda e: e.activation(out=Q["Lw"][0:64, :, cs:cs + 64], in_=pbank[5][0:64, :].rearrange("p (h t) -> p h t", h=8), func=AF.Copy),
                     R=[pbb[5], gCb], W=[QB["Lw"]])

                def hmm(e):
                    ins = None
                    for h in range(8):
                        e.matmul(pbank[6][0:64, h * 64:(h + 1) * 64], Cc["Bhtm"][0:64, h, :], Cc["Us"][0:64, h, :], start=True, stop=False)
                        ins = e.matmul(pbank[6][0:64, h * 64:(h + 1) * 64], Cc["Khtm"][0:64, h, :], Cc["Vtm"][0:64, h, :], start=False, stop=True)
                    return ins
                P.op("pe", hmm, R=[CB["Bhtm"], CB["Us"], CB["Khtm"], CB["Vtm"]], W=[pbb[6]])

                def hup(e, Hc=Hc, Hn=Hn, ci=ci):
                    ins = None
                    for h in range(8):
                        ins = e.scalar_tensor_tensor(out=Cc[Hn][0:64, h, :], in0=Cc[Hc][0:64, h, :], scalar=gC[0:64, h, ci:ci + 1],
                                                     in1=pbank[6][0:64, h * 64:(h + 1) * 64], op0=ALU.mult, op1=ALU.add)
                    return ins
                P.op("dve", hup, R=[CB[Hc], gCb, pbb[6]], W=[CB[Hn]])
                hcur = 1 - hcur
                if dbg_d is not None and g == 0 and ci == 0:
                    for i_, n_ in enumerate(["P0", "PT0", "TT", "AakT", "ArbT", "ArkT", "Vtm", "Bhtm", "Khtm", "Xs", "Us", Hn]):
                        dbg_ops.append(P.dma("sp", dbg_d[:, 12 + i_, 0:512].rearrange("p (h t) -> p h t", h=8), Cc[n_][0:64], R=[CB[n_]]))
            for ci_ in range(G // 64):
                do_chunk(ci_)
            if dbg_d is not None and g == 0:
                dbg_ops.append(P.dma("sp", dbg_d[:, 24, :].rearrange("p (h t) -> p h t", h=8), Q["Lw"][0:64], R=[QB["Lw"]]))
            yr = Q["Lw"]; yrb = QB["Lw"]
            cen = Q["at"]; cenb = QB["at"]
            for half in range(2):
                P.op("pe", lambda e, half=half: e.matmul(pbank[2][0:64, :], o64, yr[0:64, half * 4:(half + 1) * 4, :], start=True, stop=True),
                     R=[yrb, onesb], W=[pbb[2]])
                P.op("dve", lambda e, half=half: e.scalar_tensor_tensor(out=cen[0:64, half * 4:(half + 1) * 4, :],
                                                                        in0=pbank[2][0:64, :].rearrange("p (h t) -> p h t", h=4), scalar=-1.0 / 64.0,
                                                                        in1=yr[0:64, half * 4:(half + 1) * 4, :], op0=ALU.mult, op1=ALU.add),
                     R=[pbb[2], yrb, CB["H0"], CB["H1"]], W=[cenb])
            P.op("act", lambda e: e.activation(out=Q["e"][0:64], in_=cen[0:64], func=AF.Square), R=[cenb, QB["BhT"], QB["KhT"]], W=[QB["e"]])
            for half in range(2):
                P.op("pe", lambda e, half=half: e.matmul(pbank[3][0:64, :], o64, Q["e"][0:64, half * 4:(half + 1) * 4, :], start=True, stop=True),
                     R=[QB["e"], onesb], W=[pbb[3]])
                P.op("act", lambda e, half=half: e.activation(out=Q["bt"][0:64, half * 4:(half + 1) * 4, :],
                                                               in_=pbank[3][0:64, :].rearrange("p (h t) -> p h t", h=4), func=AF.Ln,
                                                               bias=epsb_t[0:64, 1:2], scale=1.0 / 64.0), R=[pbb[3], epsb], W=[QB["bt"]])
            P.op("act", lambda e: e.activation(out=Q["bt"][0:64], in_=Q["bt"][0:64], func=AF.Exp, scale=-0.5), R=[QB["bt"]], W=[QB["bt"]])
            P.op("dve", lambda e: e.tensor_tensor(out=cen[0:64], in0=cen[0:64], in1=Q["bt"][0:64], op=ALU.mult), R=[cenb, QB["bt"]], W=[cenb])

            def lnx(e):
                ins = None
                for h in range(8):
                    ins = e.tensor_scalar(out=cen[0:64, h, :], in0=cen[0:64, h, :], scalar1=pv[0:64, LNW + h:LNW + h + 1],
                                          scalar2=pv[0:64, LNB + h:LNB + h + 1], op0=ALU.mult, op1=ALU.add)
                return ins
            P.op("dve", lnx, R=[cenb, pvb], W=[cenb])
            P.op("dve", lambda e: e.tensor_tensor(out=cen[0:64], in0=cen[0:64], in1=Q["lw"][0:64], op=ALU.add), R=[cenb, QB["lw"]], W=[cenb])
            for half in range(2):
                def gmm(e, half=half):
                    ins = None
                    for hh in range(4):
                        h = half * 4 + hh
                        ins = e.matmul(pbank[2][0:64, hh * G:(hh + 1) * G], g2[:, h * 64:(h + 1) * 64], sg, start=True, stop=True)
                    return ins
                P.op("pe", gmm, R=[wconst, sgb], W=[pbb[2]])
                P.op("dve", lambda e, half=half: e.tensor_tensor(out=yfh[0:64, half * 4:(half + 1) * 4, :], in0=pbank[2][0:64, :].rearrange("p (h t) -> p h t", h=4),
                                                                 in1=cen[0:64, half * 4:(half + 1) * 4, :], op=ALU.mult),
                     R=[pbb[2], cenb], W=[yfhb])
            yf = yfh; yfb = yfhb
            for dh in range(2):
                for dd in range(4):
                    dc = dh * 4 + dd
                    s2 = woi % 2
                    woi += 1
                    P.dma("sp", wos[0:64], rwout_d[dc], W=[wosb])
                    P.op("pool", lambda e, s2=s2: e.tensor_copy(out=wo[s2][0:64], in_=wos[0:64]), R=[wosb], W=[wob[s2]])

                    def mo(e, s2=s2, dd=dd, dh=dh):
                        ins = None
                        for h in range(8):
                            ins = e.matmul(pbank[7][:, dd * G:(dd + 1) * G], wo[s2][0:64, h, :], yf[0:64, h, :], start=(h == 0), stop=(h == 7))
                        return ins
                    P.op("pe", mo, R=[wob[s2], yfb], W=[pbb[7]])
                P.op("dve", lambda e, dh=dh, t0=t0: e.tensor_tensor(out=xT[:, dh * 4:(dh + 1) * 4, t0:t0 + G],
                                                                    in0=pbank[7][:, 0:4 * G].rearrange("p (c t) -> p c t", c=4),
                                                                    in1=xT[:, dh * 4:(dh + 1) * 4, t0:t0 + G], op=ALU.add),
                     R=[pbb[7]] + [xb[c][g4] for c in range(dh * 4, dh * 4 + 4)], W=[xb[c][g4] for c in range(dh * 4, dh * 4 + 4)])
        for g_ in range(NG):
            do_group(g_)
        P.fence()


    def moba():
        G = 256
        NEG = 240000.0
        A = Arena(arena, ARENA)
        kT = A.take(8192).rearrange("p (c t) -> p c t", c=4)
        kTb = [[Buf() for g in range(8)] for c in range(4)]
        Va = A.take(16 * 8 * 65).rearrange("p (k h d) -> p k h d", k=16, h=8)
        Vab = [Buf() for kt in range(16)]
        hT = A.take(2048).rearrange("p (c t) -> p c t", c=8); hb = Buf()
        sq = A.take(2048).rearrange("p (c t) -> p c t", c=8); sqb = Buf()
        rstd = A.take(256); rstdb = Buf()
        wt = [A.take(1024).rearrange("p (c m) -> p c m", c=8) for i in range(2)]; wtb = [Buf(), Buf()]
        qT = A.take(1024).rearrange("p (c t) -> p c t", c=4); qTb = [Buf() for c in range(4)]
        ksum = A.take(32).rearrange("p (c j) -> p c j", c=4); ksb = Buf()
        gsel = A.take(16).rearrange("p (a j) -> p a j", a=2); gselb = Buf()
        m8 = A.take(16).rearrange("p (a j) -> p a j", a=2); m8b = Buf()
        negm = A.take(16).rearrange("p (a j) -> p a j", a=2); negmb = Buf()
        qaug = [A.take(256) for i in range(2)]; qaugb = [Buf(), Buf()]
        kaug = A.take(2048); kaugb = Buf()
        PT = [A.take(256) for i in range(2)]; PTb = [Buf() for i in range(2)]
        stmp = [A.take(256) for i in range(1)]; stmpb = [Buf()]
        oa = A.take(256); oab = Buf()
        rden = A.take(256); rdenb = Buf()
        oh = A.take(2048).rearrange("p (h t) -> p h t", h=8); ohb = [Buf() for h in range(8)]
        wo = [A.take(1024).rearrange("p (h m) -> p h m", h=8) for i in range(1)]; wob = [Buf()]
        stg = [A.take(512).rearrange("p (c t) -> p c t", c=2) for i in range(2)]; stgb = [Buf(), Buf()]
        ncA = A.take(256); ncB = A.take(256); sel65 = A.take(64); mcb = Buf()
        gcol = PV_NORMG + (0 * 3 + 1) * 8
        cnt = {"w": 0, "p": 0, "pt": 0, "st": 0, "wo": 0, "qa": 0}

        def setup(e):
            e.memset(ncA[:], 0.0)
            e.memset(ncB[:], 0.0)
            e.affine_select(out=ncA[:], in_=ncA[:], pattern=[[1, 256]], compare_op=ALU.is_ge, fill=-NEG, base=0, channel_multiplier=-1)
            e.affine_select(out=ncB[:], in_=ncB[:], pattern=[[1, 256]], compare_op=ALU.is_ge, fill=-NEG, base=-128, channel_multiplier=-1)
            e.memset(sel65[0:64, :], 0.0)
            e.memset(sel65[64:65, :], 1.0)
            return e.memset(Va[:, :, :, 64:65], 1.0)
        P.op("pool", setup, W=[mcb] + Vab)
        P.dma("sp", kaug[0:10, :], mbkaug_d[:, :], W=[kaugb])

        def proj_fm(widx, dst_ap, dst_bufs):
            s_ = cnt["w"] % 2
            cnt["w"] += 1
            bank = cnt["p"] % 2
            cnt["p"] += 1
            P.dma("sp", wt[s_], mbin_d[widx], W=[wtb[s_]])

            def mm(e):
                ins = None
                for c in range(8):
                    ins = e.matmul(pbank[bank][:, 0:G], wt[s_][:, c, :], hT[:, c, :], start=(c == 0), stop=(c == 7))
                return ins
            P.op("pe", mm, R=[wtb[s_], hb], W=[pbb[bank]])
            P.op("act", lambda e: e.activation(out=dst_ap, in_=pbank[bank][:, 0:G], func=AF.Copy), R=[pbb[bank]], W=dst_bufs)

        def proj_v(g, hp):
            s_ = cnt["w"] % 2
            cnt["w"] += 1
            bank = cnt["p"] % 2
            cnt["p"] += 1
            P.dma("sp", wt[s_], mbin_d[8 + hp], W=[wtb[s_]])

            def mm(e):
                ins = None
                for tt in range(2):
                    for c in range(8):
                        ins = e.matmul(pbank[bank][:, tt * 128:(tt + 1) * 128], hT[:, c, tt * 128:(tt + 1) * 128], wt[s_][:, c, :],
                                       start=(c == 0), stop=(c == 7))
                return ins
            P.op("pe", mm, R=[wtb[s_], hb], W=[pbb[bank]])
            P.op("dve", lambda e: e.tensor_copy(out=Va[:, 2 * g:2 * g + 2, 2 * hp:2 * hp + 2, 0:64],
                                                in_=pbank[bank][:, 0:256].rearrange("p (k h d) -> p k h d", k=2, h=2)),
                 R=[pbb[bank]], W=[Vab[2 * g], Vab[2 * g + 1]])

        def do_head(g, h):
            hp, ph = h // 2, h % 2
            r0 = ph * 64
            ob = g
            qs = cnt["qa"] % 2
            cnt["qa"] += 1
            qa = qaug[qs]
            qab = qaugb[qs]
            P.dma("sp", qa[8:10, :], mbqc_d[h, :, g * G:(g + 1) * G], W=[qab])
            if ob >= 4:
                def gmm(e):
                    ins = None
                    for tt in range(2):
                        ins = e.matmul(pbank[2][:, tt * 8:(tt + 1) * 8], qT[r0:r0 + 64, hp, tt * 128:(tt + 1) * 128], ksum[r0:r0 + 64, hp, :],
                                       start=True, stop=True)
                    return ins
                P.op("pe", gmm, R=[qTb[hp], ksb], W=[pbb[2]])
                P.op("pool", lambda e: e.memset(gsel, -1e30), W=[gselb])
                P.op("dve", lambda e: e.tensor_copy(out=gsel[:, :, 0:ob], in_=pbank[2][:, 0:16].rearrange("p (a j) -> p a j", a=2)[:, :, 0:ob]),
                     R=[pbb[2]], W=[gselb])

                def mx(e):
                    e.max(out=m8[:, 0, :], in_=gsel[:, 0, :])
                    return e.max(out=m8[:, 1, :], in_=gsel[:, 1, :])
                P.op("dve", mx, R=[gselb], W=[m8b])

                def ng(e):
                    ins = None
                    for tt in range(2):
                        ins = e.tensor_scalar(out=negm[:, tt, :], in0=gsel[:, tt, :], scalar1=m8[:, tt, 2:3], scalar2=1.0, op0=ALU.is_ge, op1=ALU.subtract)
                    return ins
                P.op("dve", ng, R=[gselb, m8b], W=[negmb])
                P.op("dve", lambda e: e.memset(negm[:, :, ob:8], 0.0), R=[], W=[negmb])

                def trn(e):
                    ins = None
                    for tt in range(2):
                        ins = e.transpose(pbank[2][0:8, 128 + tt * 128:128 + (tt + 1) * 128], negm[:, tt, :], ident[:])
                    return ins
                P.op("pe", trn, R=[negmb, cb], W=[pbb[2]])
                P.op("act", lambda e: e.activation(out=qa[0:8, :], in_=pbank[2][0:8, 128:384], func=AF.Copy), R=[pbb[2]], W=[qab])
            else:
                P.op("pool", lambda e: e.memset(qa[0:8, :], 0.0), W=[qab])
            nkt = 2 * ob + 2
            pend = []

            def emit_pv(kt, c0, pti):
                P.op("pe", lambda e: e.matmul(pbank[5][0:65, c0:G], Va[:, kt, h, :], PT[pti][:, c0:G], start=(kt == 0), stop=(kt == nkt - 1)),
                     R=[Vab[kt], PTb[pti]], W=[pbb[5]])
            for kt in range(nkt):
                diag = kt - 2 * ob
                c0 = 128 if diag == 1 else 0
                n = G - c0
                sb_ = 3 + (cnt["st"] % 2)
                cnt["st"] += 1
                pti = cnt["pt"] % 2
                cnt["pt"] += 1

                def smm(e, kt=kt, c0=c0, sb_=sb_):
                    e.matmul(pbank[sb_][:, c0:G], kT[r0:r0 + 64, hp, kt * 128:(kt + 1) * 128], qT[r0:r0 + 64, hp, c0:G], start=True, stop=False)
                    return e.matmul(pbank[sb_][:, c0:G], kaug[0:10, kt * 128:(kt + 1) * 128], qa[0:10, c0:G], start=False, stop=True)
                P.op("pe", smm, R=[kTb[hp][kt // 2], qTb[hp], kaugb, qab], W=[pbb[sb_]])
                if diag >= 0:
                    nc_ = ncA if diag == 0 else ncB
                    si = 0
                    P.op("dve", lambda e, c0=c0, sb_=sb_, nc_=nc_, si=si: e.tensor_tensor(out=stmp[si][:, c0:G], in0=pbank[sb_][:, c0:G], in1=nc_[:, c0:G], op=ALU.add),
                         R=[pbb[sb_], mcb], W=[stmpb[si]])
                    P.op("act", lambda e, c0=c0, pti=pti, si=si: e.activation(out=PT[pti][:, c0:G], in_=stmp[si][:, c0:G], func=AF.Exp, scale=0.125),
                         R=[stmpb[si]], W=[PTb[pti]])
                else:
                    P.op("act", lambda e, c0=c0, pti=pti, sb_=sb_: e.activation(out=PT[pti][:, c0:G], in_=pbank[sb_][:, c0:G], func=AF.Exp, scale=0.125),
                         R=[pbb[sb_]], W=[PTb[pti]])
                pend.append((kt, c0, pti))
                if len(pend) > 1:
                    emit_pv(*pend.pop(0))
            while pend:
                emit_pv(*pend.pop(0))
            P.op("act", lambda e: e.activation(out=oa[0:65, :], in_=pbank[5][0:65, 0:G], func=AF.Copy), R=[pbb[5]], W=[oab])
            P.op("pe", lambda e: e.matmul(pbank[6][0:64, 0:G], sel65[0:65, :], oa[0:65, :], start=True, stop=True), R=[oab, mcb], W=[pbb[6]])
            P.op("dve", lambda e: e.reciprocal(out=rden[0:64, :], in_=pbank[6][0:64, 0:G]), R=[pbb[6]], W=[rdenb])
            P.op("dve", lambda e: e.tensor_tensor(out=oh[0:64, h, :], in0=oa[0:64, :], in1=rden[0:64, :], op=ALU.mult), R=[oab, rdenb], W=[ohb[h]])

        def do_group(g):
            t0 = g * G
            g4 = t0 // 512
            xg = [xb[c][g4] for c in range(8)]
            P.op("act", lambda e: e.activation(out=sq, in_=xT[:, :, t0:t0 + G], func=AF.Square), R=xg, W=[sqb])

            def mmn(e):
                ins = None
                for c in range(8):
                    ins = e.matmul(pbank[7][:, 0:G], ones[:], sq[:, c, :], start=(c == 0), stop=(c == 7))
                return ins
            P.op("pe", mmn, R=[sqb, onesb], W=[pbb[7]])
            P.op("act", lambda e: e.activation(out=rstd, in_=pbank[7][:, 0:G], func=AF.Ln, bias=epsb_t[:, 0:1], scale=1.0 / 1024.0),
                 R=[pbb[7], epsb], W=[rstdb])
            P.op("act", lambda e: e.activation(out=rstd, in_=rstd, func=AF.Exp, scale=-0.5), R=[rstdb], W=[rstdb])

            def hnorm(e):
                ins = None
                for c in range(8):
                    ins = e.scalar_tensor_tensor(out=hT[:, c, :], in0=xT[:, c, t0:t0 + G], scalar=pv[:, gcol + c:gcol + c + 1],
                                                 in1=rstd, op0=ALU.mult, op1=ALU.mult)
                return ins
            P.op("dve", hnorm, R=xg + [rstdb, pvb], W=[hb])
            for hp in range(4):
                proj_fm(hp, qT[:, hp, :], [qTb[hp]])
                proj_fm(4 + hp, kT[:, hp, t0:t0 + G], [kTb[hp][g]])
                proj_v(g, hp)
            P.op("dve", lambda e: e.tensor_reduce(out=ksum[:, :, g], in_=kT[:, :, t0:t0 + G], axis=AX.X, op=ALU.add),
                 R=[kTb[hp][g] for hp in range(4)], W=[ksb])
            for h in range(8):
                do_head(g, h)
            if dbg_d is not None and g == 0:
                dbg_ops.append(P.dma("sp", dbg_d[:, 0:2, :].rearrange("p a (h t) -> p (a h) t", h=4), oh[0:64], R=ohb))
                dbg_ops.append(P.dma("sp", dbg_d[:, 2, 0:256], qT[0:64, 0, :], R=qTb))
                dbg_ops.append(P.dma("sp", dbg_d[:, 3, 0:256], kT[0:64, 0, 0:256], R=[kTb[0][0]]))
                dbg_ops.append(P.dma("sp", dbg_d[:, 4:6, :].rearrange("p a (h d) -> p (a h) d", h=4)[:, :, 0:65], Va[0:64, 0, :, :], R=[Vab[0]]))
                dbg_ops.append(P.dma("sp", dbg_d[:, 6, 0:256], oa[0:64, :], R=[oab]))
                dbg_ops.append(P.dma("sp", dbg_d[:, 7, 0:256], rden[0:64, :], R=[rdenb]))
                dbg_ops.append(P.dma("sp", dbg_d[:, 8, 0:256], PT[0][0:64, :], R=[PTb[0]]))
                dbg_ops.append(P.dma("sp", dbg_d[:, 9, 0:256], PT[1][0:64, :], R=[PTb[1]]))
                dbg_ops.append(P.dma("sp", dbg_d[:, 10, 0:256], ncA[0:64, :], R=[mcb]))
                dbg_ops.append(P.dma("sp", dbg_d[0:10, 11, 0:256], qaug[0][0:10, :], R=[qaugb[0]]))
                dbg_ops.append(P.dma("sp", dbg_d[0:10, 12, 0:256], kaug[0:10, 0:256], R=[kaugb]))
            for dh in range(4):
                for dd in range(2):
                    dc = dh * 2 + dd
                    s2 = 0
                    P.dma("sp", wo[s2][0:64], mbout_d[dc], W=[wob[s2]])

                    def mo(e, s2=s2, dd=dd):
                        ins = None
                        for h in range(8):
                            ins = e.matmul(pbank[7][:, dd * G:(dd + 1) * G], wo[s2][0:64, h, :], oh[0:64, h, :], start=(h == 0), stop=(h == 7))
                        return ins
                    P.op("pe", mo, R=[wob[s2]] + ohb, W=[pbb[7]])
                si = cnt["wo"] % 2
                cnt["wo"] += 1
                P.op("act", lambda e, si=si: e.activation(out=stg[si], in_=pbank[7][:, 0:2 * G].rearrange("p (c t) -> p c t", c=2), func=AF.Copy),
                     R=[pbb[7]], W=[stgb[si]])
                P.dma("sp", mscr_d[:, dh * 2:(dh + 1) * 2, t0:t0 + G], stg[si], R=[stgb[si]], W=[mscr_b[g]])
        for g_ in range(8):
            do_group(g_)
        P.fence()

    def moba_add():
        A = Arena(arena, ARENA)
        tb = [A.take(4096).rearrange("p (c t) -> p c t", c=8) for i in range(2)]
        tbb = [Buf(), Buf()]
        for g in range(4):
            si = g % 2
            P.dma("sp", tb[si], mscr_d[:, :, g * 512:(g + 1) * 512], R=[mscr_b[2 * g], mscr_b[2 * g + 1]], W=[tbb[si]])
            P.op("dve", lambda e, g=g, si=si: e.tensor_tensor(out=xT[:, :, g * 512:(g + 1) * 512], in0=xT[:, :, g * 512:(g + 1) * 512],
                                                              in1=tb[si], op=ALU.add),
                 R=[tbb[si]] + [xb[c][g] for c in range(8)], W=[xb[c][g] for c in range(8)])
        P.fence()

    P.fence()
    st = stage
    if "f00" in st:
        ffn(0, 0)
    if "moba" in st:
        moba()
    if "rwkv" in st:
        rwkv()
    if "moba" in st:
        moba_add()
    if "f01" in st:
        ffn(0, 2)
    if "f10" in st:
        ffn(1, 0)
    if "hgrn" in st:
        hgrn()
    if "f11" in st:
        ffn(1, 2)
    outs = final_out("final" in st)
    P.emit(nc, k.es, outs + dbg_ops)


PV_NORMG = 0
PV_FINALG = 48
PV_HGNW = 56
PV_LBZ = 64
PV_RW = 80
NPV = 176


class Arena:
    def __init__(self, ap, size):
        self.ap, self.o, self.size = ap, 0, size

    def take(self, n):
        a = self.ap[:, self.o:self.o + n]
        self.o += n
        assert self.o <= self.size, self.o
        return a


def _tile_w_in(w, ncols):
    n = ncols // 128
    return np.ascontiguousarray(w.reshape(8, 128, n, 128).transpose(2, 1, 0, 3))


def _prep_shared(inp):
    sh = {}
    for l in range(2):
        for f, nm in enumerate(("ffn1", "ffn2")):
            sh["wg%d%d" % (l, f)] = _tile_w_in(inp[nm + "_wg"][l], FF)
            sh["wu%d%d" % (l, f)] = _tile_w_in(inp[nm + "_wu"][l], FF)
            wd = inp[nm + "_wd"][l]
            sh["wd%d%d" % (l, f)] = np.ascontiguousarray(wd.reshape(NFC, 128, 8, 128).transpose(2, 1, 0, 3))
    pv = np.zeros((128, NPV), np.float32)
    ng = inp["norm_g"].reshape(6, 8, 128)
    pv[:, PV_NORMG:PV_NORMG + 48] = ng.transpose(2, 0, 1).reshape(128, 48)
    pv[:, PV_FINALG:PV_FINALG + 8] = inp["final_g"].reshape(8, 128).T
    pv[:, PV_HGNW:PV_HGNW + 8] = inp["hg_norm_w"][0].reshape(8, 128).T
    pv[:, PV_LBZ:PV_LBZ + 16] = inp["hg_lb_logits"].reshape(2, 8, 128).transpose(2, 0, 1).reshape(128, 16)
    h64 = lambda v: np.asarray(v).reshape(8, 64).T
    mu = inp["rw_mu"][0]
    pv[0:64, PV_RW:PV_RW + 24] = mu[0:1536].reshape(24, 64).T
    pv[0:64, PV_RW + 24:PV_RW + 32] = h64(inp["rw_w0"][0])
    pv[0:64, PV_RW + 32:PV_RW + 40] = h64(inp["rw_a0"][0])
    pv[0:64, PV_RW + 40:PV_RW + 48] = h64(inp["rw_k_k"][0])
    pv[0:64, PV_RW + 48:PV_RW + 56] = h64(inp["rw_k_a"][0])
    pv[0:64, PV_RW + 64:PV_RW + 72] = h64(inp["rw_r_k"][0])
    pv[0:64, PV_RW + 72:PV_RW + 80] = h64(inp["rw_lnx_w"][0])
    pv[0:64, PV_RW + 80:PV_RW + 88] = h64(inp["rw_lnx_b"][0])
    pv[:, PV_RW + 88] = mu[1536:1664]
    pv[:, PV_RW + 89] = mu[1664:1792]
    wi = inp["ev_w_in"][0]
    sh["rwin"] = np.ascontiguousarray(wi[:, 0:1536].reshape(8, 128, 24, 64).transpose(2, 1, 0, 3))
    sh["rwlo"] = _tile_w_in(wi[:, 1536:1792], 256)
    sh["rww2"] = np.ascontiguousarray(inp["rw_w2"][0])
    sh["rwa2"] = np.ascontiguousarray(inp["rw_a2"][0])
    sh["rwg2"] = np.ascontiguousarray(inp["rw_g2"][0])
    wo_ = inp["ev_w_out"][0]
    sh["rwout"] = np.ascontiguousarray(wo_[0:512].reshape(8, 64, 8, 128).transpose(2, 1, 0, 3))
    sh["pvec"] = pv
    sh["odwin"] = _tile_w_in(inp["od_w_in"][0], 4096)
    sh["odwout"] = _tile_w_in(inp["od_w_out"][0], 1024)
    sh["mbin"] = _tile_w_in(wi[:, 1792:3328], 1536)
    sh["mbout"] = np.ascontiguousarray(wo_[512:1024].reshape(8, 64, 8, 128).transpose(2, 1, 0, 3))
    pos = np.arange(S, dtype=np.float32)
    kaug = np.zeros((10, S), np.float32)
    for j in range(8):
        kaug[j, j * 256:(j + 1) * 256] = 240000.0
    kaug[8] = pos
    kaug[9] = 1.0
    sh["mbkaug"] = kaug
    slopes = np.exp2(-np.arange(1, 9, dtype=np.float32))
    qc = np.zeros((8, 2, S), np.float32)
    qc[:, 0, :] = 8.0 * slopes[:, None]
    qc[:, 1, :] = -8.0 * slopes[:, None] * pos[None, :]
    sh["mbqc"] = qc
    return sh


_NC_CACHE = {}


def run(inputs, stage=ALL_STAGES, ncores=8, trace=False):
    stage = tuple(stage)
    import time
    t0 = time.time()
    inp = {k_: np.asarray(v, dtype=np.float32) for k_, v in inputs.items()}
    sh = _prep_shared(inp)
    t1 = time.time()
    if stage not in _NC_CACHE:
        _NC_CACHE[stage] = build(stage)
    t2 = time.time()
    print("[kernel] prep %.1fs build %.1fs" % (t1 - t0, t2 - t1), flush=True)
    nc = _NC_CACHE[stage]
    in_maps = []
    for b in range(ncores):
        m = dict(sh)
        m["xT"] = np.ascontiguousarray(inp["x"][b].T.reshape(8, 128, S).transpose(1, 0, 2))
        in_maps.append(m)
    t3 = time.time()
    res = run_bass_kernel_spmd(nc, in_maps, core_ids=list(range(ncores)), trace=trace)
    print("[kernel] run %.1fs" % (time.time() - t3), flush=True)
    outs = []
    for b in range(ncores):
        o = np.asarray(res.results[b]["outT"])
        outs.append(o.transpose(1, 0, 2).reshape(D, S).T)
    if "dbg" in stage:
        np.save("dbg_out.npy", np.asarray(res.results[0]["dbg"]))
    return np.stack(outs).astype(np.float32), res


def kernel(**inputs):
    out, _ = run(inputs)
    return out
```

```python
import numpy as np
from contextlib import ExitStack
import concourse.bass as bass
import concourse.mybir as mybir
from concourse.bass_utils import run_bass_kernel_spmd

F32 = mybir.dt.float32
F32R = mybir.dt.float32r
BF16 = mybir.dt.bfloat16
AF = mybir.ActivationFunctionType
ALU = mybir.AluOpType
AX = mybir.AxisListType

D = 1024
S = 2048
FF = 2816
NFC = 22
EPS = 1e-6


class Buf:
    __slots__ = ("lw", "rd", "name")

    def __init__(self, name=""):
        self.lw = None
        self.rd = []
        self.name = name


class Op:
    __slots__ = ("eng", "fn", "deps", "needed", "sem", "val", "is_dma", "prev_dma")

    def __init__(self, eng, fn):
        self.eng = eng
        self.fn = fn
        self.deps = []
        self.needed = False
        self.sem = None
        self.val = 0
        self.is_dma = False
        self.prev_dma = None


ENGS = ("pe", "dve", "act", "pool", "sp")


class Prog:
    NSLOT = 6

    def __init__(self):
        self.ops = {e: [] for e in ENGS}
        self.fence_deps = []
        self.last = {e: None for e in ENGS}
        self.dma_slots = {e: [] for e in ENGS}
        self.dma_count = {e: 0 for e in ENGS}
        self.all_dma_last = {}

    def _collect(self, op, R, W):
        deps = []
        for b in R:
            if b.lw is not None:
                deps.append(b.lw)
        for b in W:
            if b.lw is not None:
                deps.append(b.lw)
            deps.extend(b.rd)
        deps.extend(self.fence_deps)
        seen = set()
        for d in deps:
            if id(d) in seen or d is op:
                continue
            seen.add(id(d))
            if d.eng == "pe" and op.eng == "pe" and not d.is_dma and not op.is_dma:
                continue
            op.deps.append(d)
            d.needed = True
        for b in W:
            b.lw = op
            b.rd = []
        for b in R:
            b.rd.append(op)

    def op(self, eng, fn, R=(), W=()):
        o = Op(eng, fn)
        self._collect(o, R, W)
        self.ops[eng].append(o)
        self.last[eng] = o
        return o

    def dma(self, q, out, in_, R=(), W=()):
        o = Op(q, lambda e: e.dma_start(out=out, in_=in_))
        o.is_dma = True
        o.needed = True
        k = self.dma_count[q]
        self.dma_count[q] += 1
        slot = k % self.NSLOT
        o.sem = ("dma", q, slot)
        o.val = 16 * (k // self.NSLOT + 1)
        slots = self.dma_slots[q]
        if len(slots) <= slot:
            slots.append(None)
        o.prev_dma = slots[slot]
        slots[slot] = o
        self._collect(o, R, W)
        self.ops[q].append(o)
        self.all_dma_last[(q, slot)] = o
        return o

    def fence(self):
        deps = [o for o in self.last.values() if o is not None]
        deps += list(self.all_dma_last.values())
        for d in deps:
            d.needed = True
        self.fence_deps = deps

    def emit(self, nc, es, final_waits):
        sems = {}
        for e in ENGS:
            sems[("eng", e)] = es.enter_context(nc.semaphore("s_" + e))
            for sl in range(len(self.dma_slots[e])):
                sems[("dma", e, sl)] = es.enter_context(nc.semaphore("d_%s_%d" % (e, sl)))
        for e in ENGS:
            cnt = 0
            for o in self.ops[e]:
                if o.is_dma:
                    continue
                o.sem = ("eng", e)
                if o.needed:
                    cnt += 1
                    o.val = cnt
        handles = {"pe": "tensor", "dve": "vector", "act": "scalar", "pool": "gpsimd", "sp": "sync"}
        block = es.enter_context(nc.Block())

        def make(e):
            def body(eng):
                seen = {}
                for o in self.ops[e]:
                    waits = list(o.deps)
                    if o.is_dma and o.prev_dma is not None:
                        waits.append(o.prev_dma)
                    for d in waits:
                        if seen.get(d.sem, 0) < d.val:
                            eng.wait_ge(sems[d.sem], d.val)
                            seen[d.sem] = d.val
                    ins = o.fn(eng)
                    if o.is_dma:
                        ins.then_inc(sems[o.sem], 16)
                    elif o.needed:
                        ins.then_inc(sems[o.sem], 1)
                if e == "sp":
                    for d in final_waits:
                        if seen.get(d.sem, 0) < d.val:
                            eng.wait_ge(sems[d.sem], d.val)
                            seen[d.sem] = d.val
            return body

        for e in ENGS:
            getattr(block, handles[e])(make(e))


def r32(ap):
    return ap


class K:
    def __init__(self, stage):
        self.stage = stage
        self.nc = bass.Bass("TRN2", target_bir_lowering=False)
        self.P = Prog()
        self.es = ExitStack()
        self.wq = 0

    def dram_in(self, name, shape, dt=F32):
        return self.nc.dram_tensor(name, list(shape), dt, kind="ExternalInput").ap()

    def sb(self, name, shape, dt=F32):
        return self.es.enter_context(self.nc.sbuf_tensor(name, list(shape), dt))

    def ps(self, name, shape, dt=F32):
        return self.es.enter_context(self.nc.psum_tensor(name, list(shape), dt))


ALL_STAGES = ("f00", "rwkv", "moba", "f01", "f10", "hgrn", "f11", "final")


def build(stage=ALL_STAGES):
    k = K(stage)
    nc, P, es = k.nc, k.P, k.es
    with es:
        _build(k)
    return nc


def _build(k):
    nc, P = k.nc, k.P
    stage = k.stage
    xT_d = k.dram_in("xT", [128, 8, S])
    pv_d = k.dram_in("pvec", [128, NPV])
    wg_d = [[k.dram_in("wg%d%d" % (l, f), [NFC, 128, 8, 128]) for f in range(2)] for l in range(2)]
    wu_d = [[k.dram_in("wu%d%d" % (l, f), [NFC, 128, 8, 128]) for f in range(2)] for l in range(2)]
    wd_d = [[k.dram_in("wd%d%d" % (l, f), [8, 128, NFC, 128]) for f in range(2)] for l in range(2)]
    odwin_d = k.dram_in("odwin", [32, 128, 8, 128])
    odwout_d = k.dram_in("odwout", [8, 128, 8, 128])
    rwin_d = k.dram_in("rwin", [24, 128, 8, 64])
    rwlo_d = k.dram_in("rwlo", [2, 128, 8, 128])
    rww2_d = k.dram_in("rww2", [64, 512])
    rwa2_d = k.dram_in("rwa2", [64, 512])
    rwg2_d = k.dram_in("rwg2", [128, 512])
    rwout_d = k.dram_in("rwout", [8, 64, 8, 128])
    mbin_d = k.dram_in("mbin", [12, 128, 8, 128])
    mbout_d = k.dram_in("mbout", [8, 64, 8, 128])
    mbkaug_d = k.dram_in("mbkaug", [10, S])
    mbqc_d = k.dram_in("mbqc", [8, 2, S])
    mscr_d = nc.dram_tensor("mscr", [128, 8, S], F32).ap()
    mscr_b = [Buf() for g in range(8)]
    dbg_d = nc.dram_tensor("dbg", [64, 32, 1024], F32, kind="ExternalOutput").ap() if "dbg" in stage else None
    dbg_ops = []
    out_d = nc.dram_tensor("outT", [128, 8, S], F32, kind="ExternalOutput").ap()

    xT = k.sb("xT_sb", [128, 8, S])
    xb = [[Buf("x%d_%d" % (c, g)) for g in range(4)] for c in range(8)]
    pv = k.sb("pv_sb", [128, NPV])
    pvb = Buf("pv")
    ones = k.sb("ones", [128, 128])
    onesb = Buf("ones")
    epsb_t = k.sb("epsc", [128, 4])
    epsb = Buf("eps")
    ARENA = 32800
    arena = k.sb("arena", [128, ARENA])

    pbank = [k.ps("pb%d" % i, [128, 512]) for i in range(8)]
    pbb = [Buf("pb%d" % i) for i in range(8)]

    P.op("pool", lambda e: e.memset(ones[:], 1.0), W=[onesb])
    P.op("pool", lambda e: e.memset(epsb_t[:, 0:1], EPS), W=[epsb])
    P.dma("sp", pv[:], pv_d[:], W=[pvb])
    for c in range(8):
        for g in range(4):
            P.dma("sp", xT[:, c, g * 512:(g + 1) * 512], xT_d[:, c, g * 512:(g + 1) * 512], W=[xb[c][g]])

    ident = k.sb("ident", [128, 128])
    mask4t = k.sb("mask4", [128, 512])
    mask4 = mask4t[:].rearrange("p (c m) -> p c m", c=4)
    resetm = k.sb("resetm", [128, 512])
    lbt = k.sb("lbt", [128, 16])
    cb = Buf("consts")
    lbb = Buf("lb")

    def setup_consts(e):
        e.memset(ident[:], 0.0)
        e.affine_select(out=ident[:], in_=ones[:], pattern=[[1, 128]], compare_op=ALU.is_equal, fill=0.0, base=0, channel_multiplier=-1)
        for i in range(4):
            e.affine_select(out=mask4t[:, i * 128:(i + 1) * 128], in_=ones[:], pattern=[[1, 128]], compare_op=ALU.is_ge, fill=0.0,
                            base=0, channel_multiplier=-1)
            e.memset(mask4t[0:64, i * 128 + 64:(i + 1) * 128], 0.0)
        e.memset(resetm[:], 1.0)
        ins = None
        for i in range(8):
            ins = e.memset(resetm[:, i * 64:i * 64 + 1], 0.0)
        return ins
    P.op("pool", setup_consts, R=[onesb], W=[cb])
    P.op("dve", lambda e: e.tensor_tensor(out=lbt[:, 0:8], in0=pv[:, PV_LBZ + 8:PV_LBZ + 16], in1=pv[:, PV_LBZ:PV_LBZ + 8], op=ALU.subtract),
         R=[pvb], W=[lbb])
    P.op("act", lambda e: e.activation(out=lbt[:, 0:8], in_=lbt[:, 0:8], func=AF.Sigmoid), R=[lbb], W=[lbb])
    P.op("dve", lambda e: e.tensor_scalar(out=lbt[:, 8:16], in0=lbt[:, 0:8], scalar1=-1.0, scalar2=1.0, op0=ALU.mult, op1=ALU.add),
         R=[lbb], W=[lbb])

    mk = k.sb("rwmask", [64, 4 * 512])
    mLs = mk[:, 0:512].rearrange("p (h t) -> p h t", h=8)
    mUs = mk[:, 512:1024].rearrange("p (h t) -> p h t", h=8)
    mUi = mk[:, 1024:1536].rearrange("p (h t) -> p h t", h=8)
    id8 = mk[:, 1536:2048].rearrange("p (h t) -> p h t", h=8)
    omka = k.sb("omka", [64, 8])
    cb2 = Buf("consts2")

    def setup2(e):
        o3 = ones[0:64, :].rearrange("p (a b) -> p a b", a=2)
        e.memset(mk[:], 1.0)
        e.affine_select(out=mLs, in_=mLs, pattern=[[0, 8], [-1, 64]], compare_op=ALU.is_gt, fill=0.0, base=0, channel_multiplier=1)
        e.affine_select(out=mUs, in_=mUs, pattern=[[0, 8], [1, 64]], compare_op=ALU.is_gt, fill=0.0, base=0, channel_multiplier=-1)
        e.affine_select(out=mUi, in_=mUi, pattern=[[0, 8], [1, 64]], compare_op=ALU.is_ge, fill=0.0, base=0, channel_multiplier=-1)
        return e.affine_select(out=id8, in_=id8, pattern=[[0, 8], [1, 64]], compare_op=ALU.is_equal, fill=0.0, base=0, channel_multiplier=-1)
    P.op("pool", setup2, W=[cb2])
    P.op("dve", lambda e: e.tensor_scalar(out=omka[:], in0=pv[0:64, PV_RW + 48:PV_RW + 56], scalar1=-1.0, scalar2=1.0, op0=ALU.mult, op1=ALU.add),
         R=[pvb], W=[cb2])
    P.op("pool", lambda e: e.memset(epsb_t[:, 1:2], 64e-5), W=[epsb])

    def rstd_group(g, rstd_ap, rstd_buf, sq_ap, sq_buf, bank, ndiv=1024.0):
        P.op("act", lambda e: e.activation(out=sq_ap, in_=xT[:, :, g * 512:(g + 1) * 512], func=AF.Square),
             R=[xb[c][g] for c in range(8)], W=[sq_buf])

        def mm(e):
            ins = None
            for c in range(8):
                ins = e.matmul(pbank[bank][:], ones[:], sq_ap[:, c, :], start=(c == 0), stop=(c == 7))
            return ins
        P.op("pe", mm, R=[sq_buf, onesb], W=[pbb[bank]])
        P.op("act", lambda e: e.activation(out=rstd_ap, in_=pbank[bank][:], func=AF.Ln, bias=epsb_t[:, 0:1], scale=1.0 / ndiv),
             R=[pbb[bank], epsb], W=[rstd_buf])
        P.op("act", lambda e: e.activation(out=rstd_ap, in_=rstd_ap, func=AF.Exp, scale=-0.5),
             R=[rstd_buf], W=[rstd_buf])

    def ffn(l, which):
        f = 0 if which == 0 else 1
        gcol = PV_NORMG + (l * 3 + which) * 8
        A = Arena(arena, ARENA)
        hT = A.take(8192).bitcast(BF16).rearrange("p (c t) -> p c t", c=8)
        act = A.take(11264).bitcast(BF16).rearrange("p (c t) -> p c t", c=11)
        sq = arena[:, 8192:8192 + 4096].rearrange("p (c t) -> p c t", c=8)
        rstd = A.take(512)
        sg = [A.take(512) for i in range(2)]
        NW = 3
        wgb = [A.take(512).bitcast(BF16).rearrange("p (c m) -> p c m", c=8) for i in range(NW)]
        wub = [A.take(512).bitcast(BF16).rearrange("p (c m) -> p c m", c=8) for i in range(NW)]
        wdb = [A.take(704).bitcast(BF16).rearrange("p (c m) -> p c m", c=11) for i in range(2)]
        wgs = [A.take(1024).rearrange("p (c m) -> p c m", c=8) for i in range(2)]
        wus = [A.take(1024).rearrange("p (c m) -> p c m", c=8) for i in range(2)]
        wds = [A.take(1408).rearrange("p (c m) -> p c m", c=11) for i in range(2)]
        wgsb = [Buf(), Buf()]; wusb = [Buf(), Buf()]; wdsb = [Buf(), Buf()]
        hb = [[Buf() for g in range(4)] for c in range(8)]
        actb = [[Buf() for g in range(4)] for c in range(11)]
        sqb, rstdb = Buf(), Buf()
        sgb = [Buf(), Buf()]
        wgbb = [Buf() for i in range(NW)]
        wubb = [Buf() for i in range(NW)]
        wdbb = [Buf(), Buf()]
        cnt = {"w": 0, "wd": 0, "p": 0, "s": 0, "ws": 0}
        for g in range(4):
            rstd_group(g, rstd, rstdb, sq, sqb, 7)
            for c in range(8):
                P.op("dve", lambda e, c=c, g=g: e.scalar_tensor_tensor(
                    out=hT[:, c, g * 512:(g + 1) * 512], in0=xT[:, c, g * 512:(g + 1) * 512],
                    scalar=pv[:, gcol + c:gcol + c + 1], in1=rstd, op0=ALU.mult, op1=ALU.mult),
                    R=[xb[c][g], rstdb, pvb], W=[hb[c][g]])
        P.fence()

        def phase_a(fc, fl):
            s = cnt["w"] % NW
            cnt["w"] += 1
            ss_ = cnt["ws"] % 2
            cnt["ws"] += 1
            P.dma("sp", wgs[ss_], wg_d[l][f][fc], W=[wgsb[ss_]])
            P.dma("sp", wus[ss_], wu_d[l][f][fc], W=[wusb[ss_]])
            P.op("pool", lambda e: e.tensor_copy(out=wgb[s], in_=wgs[ss_]), R=[wgsb[ss_]], W=[wgbb[s]])
            P.op("pool", lambda e: e.tensor_copy(out=wub[s], in_=wus[ss_]), R=[wusb[ss_]], W=[wubb[s]])
            def a_group(g):
                bg, bu = (cnt["p"] % 2) * 2, (cnt["p"] % 2) * 2 + 1
                cnt["p"] += 1
                si = cnt["s"] % 2
                cnt["s"] += 1

                def mmg(e):
                    ins = None
                    for c in range(8):
                        ins = e.matmul(pbank[bg][:], wgb[s][:, c, :], hT[:, c, g * 512:(g + 1) * 512], start=(c == 0), stop=(c == 7))
                    return ins

                def mmu(e):
                    ins = None
                    for c in range(8):
                        ins = e.matmul(pbank[bu][:], wub[s][:, c, :], hT[:, c, g * 512:(g + 1) * 512], start=(c == 0), stop=(c == 7))
                    return ins
                P.op("pe", mmg, R=[wgbb[s]] + [hb[c][g] for c in range(8)], W=[pbb[bg]])
                P.op("pe", mmu, R=[wubb[s]] + [hb[c][g] for c in range(8)], W=[pbb[bu]])
                P.op("act", lambda e: e.activation(out=sg[si], in_=pbank[bg][:], func=AF.Silu), R=[pbb[bg]], W=[sgb[si]])
                P.op("dve", lambda e: e.tensor_tensor(out=act[:, fl, g * 512:(g + 1) * 512], in0=pbank[bu][:], in1=sg[si], op=ALU.mult),
                     R=[pbb[bu], sgb[si]], W=[actb[fl][g]])
            for g_ in range(4):
                a_group(g_)

        def phase_b(fh, dc):
            s = cnt["wd"] % 2
            cnt["wd"] += 1
            P.dma("sp", wds[s], wd_d[l][f][dc][:, fh * 11:(fh + 1) * 11, :], W=[wdsb[s]])
            P.op("pool", lambda e: e.tensor_copy(out=wdb[s], in_=wds[s]), R=[wdsb[s]], W=[wdbb[s]])
            def b_group(g):
                bo = 4 + (cnt["p"] % 2)
                cnt["p"] += 1

                def mmd(e):
                    ins = None
                    for fl in range(11):
                        ins = e.matmul(pbank[bo][:], wdb[s][:, fl, :], act[:, fl, g * 512:(g + 1) * 512], start=(fl == 0), stop=(fl == 10))
                    return ins
                P.op("pe", mmd, R=[wdbb[s]] + [actb[fl][g] for fl in range(11)], W=[pbb[bo]])
                P.op("dve", lambda e: e.scalar_tensor_tensor(
                    out=xT[:, dc, g * 512:(g + 1) * 512], in0=pbank[bo][:], scalar=0.5,
                    in1=xT[:, dc, g * 512:(g + 1) * 512], op0=ALU.mult, op1=ALU.add),
                    R=[pbb[bo], xb[dc][g]], W=[xb[dc][g]])
            for g_ in range(4):
                b_group(g_)
        for fh in range(2):
            for fl in range(11):
                phase_a(fh * 11 + fl, fl)
            for dc in range(8):
                phase_b(fh, dc)
        P.fence()

    def final_out(norm):
        o = 0
        sq = arena[:, o:o + 4096].rearrange("p (c t) -> p c t", c=8); o += 4096
        rstd = arena[:, o:o + 512]; o += 512
        ob = [arena[:, o + i * 4096:o + (i + 1) * 4096].rearrange("p (c t) -> p c t", c=8) for i in range(2)]; o += 8192
        sqb, rstdb = Buf(), Buf()
        obb = [Buf(), Buf()]
        outs = []
        for g in range(4):
            s = g % 2
            if norm:
                rstd_group(g, rstd, rstdb, sq, sqb, 7)
                for c in range(8):
                    P.op("dve", lambda e, c=c, g=g, s=s: e.scalar_tensor_tensor(
                        out=ob[s][:, c, :], in0=xT[:, c, g * 512:(g + 1) * 512],
                        scalar=pv[:, PV_FINALG + c:PV_FINALG + c + 1], in1=rstd, op0=ALU.mult, op1=ALU.mult),
                        R=[xb[c][g], rstdb, pvb], W=[obb[s]])
                outs.append(P.dma("sp", out_d[:, :, g * 512:(g + 1) * 512], ob[s], R=[obb[s]]))
            else:
                outs.append(P.dma("sp", out_d[:, :, g * 512:(g + 1) * 512], xT[:, :, g * 512:(g + 1) * 512],
                                  R=[xb[c][g] for c in range(8)]))
        return outs


    def hgrn():
        A = Arena(arena, ARENA)
        hT = A.take(2048).bitcast(BF16).rearrange("p (c t) -> p c t", c=8)
        hb = [Buf() for c in range(8)]
        wts = [A.take(1024).rearrange("p (c m) -> p c m", c=8) for j in range(4)]
        wtsb = [Buf() for j in range(4)]
        wt = [[A.take(512).bitcast(BF16).rearrange("p (c m) -> p c m", c=8) for j in range(4)] for s_ in range(2)]
        wtb = [[Buf() for j in range(4)] for s_ in range(2)]
        names = ["qT", "fT", "lf", "kT", "bT", "eb", "e2", "oTs", "sqo", "rs2", "tmp"]
        T = {n: A.take(512) for n in names}
        TB = {n: Buf(n) for n in names}
        DB = []
        for i in range(2):
            d = {}
            for n in ("qe", "ke", "sgT"):
                d[n] = A.take(512); d[n + "b"] = Buf()
            for n in ("ke2tm", "vtm", "scs"):
                d[n] = A.take(512).rearrange("p (c m) -> p c m", c=4); d[n + "b"] = Buf()
            d["ebl"] = A.take(8); d["eblb"] = Buf()
            DB.append(d)
        state = [A.take(1024).rearrange("p (h v) -> p h v", h=8) for i in range(2)]
        stb = [[Buf() for h in range(8)] for i in range(2)]
        yT = A.take(2048).bitcast(BF16).rearrange("p (c t) -> p c t", c=8)
        yb = [Buf() for h in range(8)]
        wos = [A.take(1024).rearrange("p (c m) -> p c m", c=8) for i in range(1)]
        wosb = [Buf()]
        wo = [A.take(512).bitcast(BF16).rearrange("p (c m) -> p c m", c=8) for i in range(2)]
        wob = [Buf(), Buf()]
        sq = A.take(4096).rearrange("p (c t) -> p c t", c=8); sqb = Buf()
        rstd = A.take(512); rstdb = Buf()
        gcol = PV_NORMG + (1 * 3 + 1) * 8
        scur = [0] * 8
        cnt = {"wo": 0}
        for h in range(8):
            P.op("pool", lambda e, h=h: e.memset(state[0][:, h, :], 0.0), W=[stb[0][h]])

        def norm(g):
            rstd_group(g, rstd, rstdb, sq, sqb, 7)

            def hn(e):
                ins = None
                for c in range(8):
                    ins = e.scalar_tensor_tensor(out=hT[:, c, :], in0=xT[:, c, g * 512:(g + 1) * 512],
                                                 scalar=pv[:, gcol + c:gcol + c + 1], in1=rstd, op0=ALU.mult, op1=ALU.mult)
                return ins
            P.op("dve", hn, R=[xb[c][g] for c in range(8)] + [rstdb, pvb], W=hb)

        def prep(g, h, s_):
            d = DB[s_]
            for j in range(4):
                P.dma("sp", wts[j], odwin_d[j * 8 + h], W=[wtsb[j]])
                P.op("pool", lambda e, j=j: e.tensor_copy(out=wt[s_][j], in_=wts[j]), R=[wtsb[j]], W=[wtb[s_][j]])
            yield

            def proj(j, bank):
                def mm(e):
                    ins = None
                    for c in range(8):
                        ins = e.matmul(pbank[bank][:], wt[s_][j][:, c, :], hT[:, c, :], start=(c == 0), stop=(c == 7))
                    return ins
                P.op("pe", mm, R=[wtb[s_][j]] + hb, W=[pbb[bank]])
            proj(0, 0)
            P.op("act", lambda e: e.activation(out=T["qT"], in_=pbank[0][:], func=AF.Copy), R=[pbb[0]], W=[TB["qT"]])
            yield
            proj(1, 1)
            P.op("act", lambda e: e.activation(out=T["fT"], in_=pbank[1][:], func=AF.Sigmoid), R=[pbb[1]], W=[TB["fT"]])
            yield
            P.op("dve", lambda e: e.tensor_scalar(out=T["fT"], in0=T["fT"], scalar1=lbt[:, 8 + h:9 + h], scalar2=lbt[:, h:h + 1],
                                                  op0=ALU.mult, op1=ALU.add), R=[TB["fT"], lbb], W=[TB["fT"]])
            yield
            P.op("act", lambda e: e.activation(out=T["lf"], in_=T["fT"], func=AF.Ln), R=[TB["fT"]], W=[TB["lf"]])
            P.op("dve", lambda e: e.tensor_scalar(out=T["kT"], in0=T["fT"], scalar1=-1.0, scalar2=1.0, op0=ALU.mult, op1=ALU.add),
                 R=[TB["fT"]], W=[TB["kT"]])
            yield
            P.op("dve", lambda e: e.tensor_tensor_scan(out=T["bT"], data0=resetm[:], data1=T["lf"], initial=0.0, op0=ALU.mult, op1=ALU.add),
                 R=[TB["lf"], cb], W=[TB["bT"]])
            yield
            P.op("act", lambda e: e.activation(out=T["eb"], in_=T["bT"], func=AF.Exp), R=[TB["bT"]], W=[TB["eb"]])
            yield
            P.op("dve", lambda e: e.tensor_tensor(out=d["qe"], in0=T["qT"], in1=T["eb"], op=ALU.mult), R=[TB["qT"], TB["eb"]], W=[d["qeb"]])
            yield
            P.op("act", lambda e: e.activation(out=T["eb"], in_=T["bT"], func=AF.Exp, scale=-1.0), R=[TB["bT"], d["qeb"]], W=[TB["eb"]])
            yield
            P.op("dve", lambda e: e.tensor_tensor(out=d["ke"], in0=T["kT"], in1=T["eb"], op=ALU.mult), R=[TB["kT"], TB["eb"]], W=[d["keb"]])
            yield

            def e2f(e):
                ins = None
                for ci in range(8):
                    ins = e.activation(out=T["e2"][:, ci * 64:(ci + 1) * 64], in_=T["bT"][:, ci * 64:(ci + 1) * 64], func=AF.Exp,
                                       scale=-1.0, bias=T["bT"][:, ci * 64 + 63:ci * 64 + 64])
                return ins
            P.op("act", e2f, R=[TB["bT"]], W=[TB["e2"]])
            yield
            P.op("dve", lambda e: e.tensor_tensor(out=T["e2"], in0=T["e2"], in1=T["kT"], op=ALU.mult), R=[TB["kT"], TB["e2"]], W=[TB["e2"]])
            P.op("act", lambda e: e.activation(out=d["ebl"], in_=T["bT"].rearrange("p (c t) -> p c t", t=64)[:, :, 63], func=AF.Exp),
                 R=[TB["bT"]], W=[d["eblb"]])
            yield

            def tr(e):
                ins = None
                for tt in range(4):
                    ins = e.transpose(pbank[2][:, tt * 128:(tt + 1) * 128], T["e2"][:, tt * 128:(tt + 1) * 128], ident[:])
                return ins
            P.op("pe", tr, R=[TB["e2"], cb], W=[pbb[2]])
            P.op("act", lambda e: e.activation(out=d["ke2tm"], in_=pbank[2][:].rearrange("p (c m) -> p c m", c=4), func=AF.Copy),
                 R=[pbb[2]], W=[d["ke2tmb"]])
            yield

            def vproj(e):
                ins = None
                for tt in range(4):
                    for c in range(8):
                        ins = e.matmul(pbank[3][:, tt * 128:(tt + 1) * 128], hT[:, c, tt * 128:(tt + 1) * 128], wt[s_][2][:, c, :],
                                       start=(c == 0), stop=(c == 7))
                return ins
            P.op("pe", vproj, R=[wtb[s_][2]] + hb, W=[pbb[3]])
            P.op("dve", lambda e: e.tensor_copy(out=d["vtm"], in_=pbank[3][:].rearrange("p (c m) -> p c m", c=4)), R=[pbb[3]], W=[d["vtmb"]])
            yield
            proj(3, 0)
            P.op("act", lambda e: e.activation(out=d["sgT"], in_=pbank[0][:], func=AF.Sigmoid), R=[pbb[0]], W=[d["sgTb"]])
            yield

            def scm(e):
                ins = None
                for p_ in range(4):
                    ins = e.matmul(pbank[4][:, p_ * 128:(p_ + 1) * 128], d["ke"][:, p_ * 128:(p_ + 1) * 128],
                                   d["qe"][:, p_ * 128:(p_ + 1) * 128], start=True, stop=True)
                return ins
            P.op("pe", scm, R=[d["keb"], d["qeb"]], W=[pbb[4]])
            P.op("dve", lambda e: e.tensor_tensor(out=d["scs"], in0=pbank[4][:].rearrange("p (c m) -> p c m", c=4), in1=mask4, op=ALU.mult),
                 R=[pbb[4], cb], W=[d["scsb"]])
            yield

        def chunks(g, h, s_):
            d = DB[s_]

            def one(p_, half):
                ci = p_ * 2 + half
                sc = scur[h]
                cs = p_ * 128 + half * 64
                r0 = half * 64

                def omm(e):
                    if half == 0:
                        e.matmul(pbank[5][:, p_ * 128:(p_ + 1) * 128], d["vtm"][:, p_, :], d["scs"][:, p_, :], start=True, stop=False)
                    return e.matmul(pbank[5][:, cs:cs + 64], state[sc][:, h, :], d["qe"][:, cs:cs + 64], start=False, stop=(half == 1))
                P.op("pe", omm, R=[d["vtmb"], d["scsb"], stb[sc][h], d["qeb"]], W=[pbb[5]])
                P.op("pe", lambda e: e.matmul(pbank[6][:, 0:128], d["ke2tm"][r0:r0 + 64, p_, :], d["vtm"][r0:r0 + 64, p_, :],
                                              start=True, stop=True), R=[d["ke2tmb"], d["vtmb"]], W=[pbb[6]])
                P.op("dve", lambda e: e.scalar_tensor_tensor(
                    out=state[1 - sc][:, h, :], in0=state[sc][:, h, :], scalar=d["ebl"][:, ci:ci + 1], in1=pbank[6][:, 0:128],
                    op0=ALU.mult, op1=ALU.add), R=[stb[sc][h], d["eblb"], pbb[6]], W=[stb[1 - sc][h]])
                scur[h] = 1 - sc
            for p_ in range(4):
                for half in range(2):
                    one(p_, half)
                    yield
            P.op("act", lambda e: e.activation(out=T["oTs"], in_=pbank[5][:], func=AF.Copy), R=[pbb[5]], W=[TB["oTs"]])
            P.op("act", lambda e: e.activation(out=T["sqo"], in_=pbank[5][:], func=AF.Square), R=[pbb[5]], W=[TB["sqo"]])
            yield
            P.op("pe", lambda e: e.matmul(pbank[7][:], ones[:], T["sqo"], start=True, stop=True), R=[TB["sqo"], onesb], W=[pbb[7]])
            P.op("act", lambda e: e.activation(out=T["rs2"], in_=pbank[7][:], func=AF.Ln, bias=epsb_t[:, 0:1], scale=1.0 / 128.0),
                 R=[pbb[7], epsb], W=[TB["rs2"]])
            yield
            P.op("act", lambda e: e.activation(out=T["rs2"], in_=T["rs2"], func=AF.Exp, scale=-0.5), R=[TB["rs2"]], W=[TB["rs2"]])
            yield
            P.op("dve", lambda e: e.scalar_tensor_tensor(out=T["tmp"], in0=T["oTs"], scalar=pv[:, PV_HGNW + h:PV_HGNW + h + 1],
                                                         in1=T["rs2"], op0=ALU.mult, op1=ALU.mult),
                 R=[TB["oTs"], TB["rs2"], pvb], W=[TB["tmp"]])
            yield
            P.op("dve", lambda e: e.tensor_tensor(out=yT[:, h, :], in0=T["tmp"], in1=d["sgT"], op=ALU.mult),
                 R=[TB["tmp"], d["sgTb"]], W=[yb[h]])
            yield

        def outproj(g):
            for dc in range(8):
                s2 = cnt["wo"] % 2
                cnt["wo"] += 1
                P.dma("sp", wos[0], odwout_d[dc], W=[wosb[0]])
                P.op("pool", lambda e, s2=s2: e.tensor_copy(out=wo[s2], in_=wos[0]), R=[wosb[0]], W=[wob[s2]])

                def mo(e, s2=s2):
                    ins = None
                    for h in range(8):
                        ins = e.matmul(pbank[7][:], wo[s2][:, h, :], yT[:, h, :], start=(h == 0), stop=(h == 7))
                    return ins
                P.op("pe", mo, R=[wob[s2]] + yb, W=[pbb[7]])
                P.op("dve", lambda e, dc=dc: e.tensor_tensor(out=xT[:, dc, g * 512:(g + 1) * 512], in0=pbank[7][:],
                                                             in1=xT[:, dc, g * 512:(g + 1) * 512], op=ALU.add),
                     R=[pbb[7], xb[dc][g]], W=[xb[dc][g]])

        def interleave(a_, b_):
            gens = [x for x in (a_, b_) if x is not None]
            while gens:
                for x in list(gens):
                    try:
                        next(x)
                    except StopIteration:
                        gens.remove(x)
        prev = None
        prev_g = None
        idx = 0
        for g in range(4):
            for h in range(8):
                if h == 0:
                    norm(g)
                interleave(prev, prep(g, h, idx % 2))
                if prev is not None and h == 0:
                    outproj(prev_g)
                prev = chunks(g, h, idx % 2)
                prev_g = g
                idx += 1
        interleave(prev, None)
        outproj(3)
        P.fence()

    def rwkv():
        G = 128
        NG = S // G
        A = Arena(arena, ARENA)
        hT = A.take(4 * (G + 2)).bitcast(BF16).rearrange("p (c t) -> p c t", c=8); hb = Buf()
        sq = A.take(8 * G).rearrange("p (c t) -> p c t", c=8); sqb = Buf()
        rstd = A.take(G); rstdb = Buf()
        wrkvs = [A.take(512).rearrange("p (c m) -> p c m", c=8) for i in range(3)]
        wrkvsb = [Buf() for i in range(3)]
        wrkv = [[A.take(256).bitcast(BF16).rearrange("p (c m) -> p c m", c=8) for i in range(3)] for s_ in range(2)]
        wrkvb = [[Buf() for i in range(3)] for s_ in range(2)]
        wlos = A.take(1024).rearrange("p (c m) -> p c m", c=8); wlosb = Buf()
        wlo = [A.take(512).bitcast(BF16).rearrange("p (c m) -> p c m", c=8) for i in range(2)]
        wlob = [Buf(), Buf()]
        yfh = A.take(512).bitcast(BF16).rearrange("p (h t) -> p h t", h=8); yfhb = Buf()
        w2a2 = A.take(512); g2 = A.take(512); wconst = Buf()
        praw = [A.take(G + 1) for i in range(2)]; prawb = [Buf(), Buf()]
        dtmp = [A.take(G) for i in range(2)]; dtmpb = [Buf(), Buf()]
        tw = A.take(G); twb = Buf()
        sg = A.take(G); sgb = Buf()
        QN = ["r", "k", "v", "lw", "a", "kk", "Lw", "e", "at", "bt", "kt", "BhT", "KhT"]
        Q = {n: A.take(8 * G).rearrange("p (h t) -> p h t", h=8) for n in QN}
        QB = {n: Buf(n) for n in QN}
        HO_ = ("TT", "AakT", "Vtm")
        CN = ["P0", "P1", "PT0", "PT1", "Xs", "Us", "H0", "H1", "ArbT", "ArkT", "Bhtm", "Khtm"] + [n + "0" for n in HO_] + [n + "1" for n in HO_]
        Cc = {n: A.take(512).rearrange("p (h t) -> p h t", h=8) for n in CN}
        CB = {n: Buf(n) for n in CN}
        gC = A.take(16).rearrange("p (h c) -> p h c", h=8); gCb = Buf()
        wos = wlos.rearrange("p c m -> p (c m)").rearrange("p (h m) -> p h m", h=8); wosb = wlosb
        wo = [A.take(512).bitcast(BF16).rearrange("p (h m) -> p h m", h=8) for i in range(2)]; wob = [Buf(), Buf()]
        gcol = PV_NORMG + (0 * 3 + 1) * 8
        MU, W0, A0, KK, KA, OMKA, RK, LNW, LNB = PV_RW, PV_RW + 24, PV_RW + 32, PV_RW + 40, PV_RW + 48, PV_RW + 56, PV_RW + 64, PV_RW + 72, PV_RW + 80
        MUWA, MUG = PV_RW + 88, PV_RW + 89
        o64 = ones[0:64, 0:64]

        P.dma("sp", w2a2[0:64, :], rww2_d[:, :], W=[wconst])
        P.dma("sp", w2a2[64:128, :], rwa2_d[:, :], W=[wconst])
        P.dma("sp", g2, rwg2_d[:, :], W=[wconst])
        P.op("pool", lambda e: e.memset(Cc["H0"][0:64], 0.0), W=[CB["H0"]])
        P.op("pool", lambda e: e.memset(hT[:, :, 0:1], 0.0), W=[hb])
        hcur = 0
        woi = 0
        pi = 0
        def do_group(g):
            nonlocal hcur, woi, pi
            t0 = g * G
            g4 = t0 // 512
            xg = [xb[c][g4] for c in range(8)]
            if g > 0:
                P.op("dve", lambda e: e.tensor_copy(out=hT[:, :, 0:1], in_=hT[:, :, G:G + 1]), R=[hb], W=[hb])
            P.op("act", lambda e, t0=t0: e.activation(out=sq, in_=xT[:, :, t0:t0 + G], func=AF.Square), R=xg, W=[sqb])

            def mmn(e):
                ins = None
                for c in range(8):
                    ins = e.matmul(pbank[7][:, 0:G], ones[:], sq[:, c, :], start=(c == 0), stop=(c == 7))
                return ins
            P.op("pe", mmn, R=[sqb, onesb], W=[pbb[7]])
            P.op("act", lambda e: e.activation(out=rstd, in_=pbank[7][:, 0:G], func=AF.Ln, bias=epsb_t[:, 0:1], scale=1.0 / 1024.0),
                 R=[pbb[7], epsb], W=[rstdb])
            P.op("act", lambda e: e.activation(out=rstd, in_=rstd, func=AF.Exp, scale=-0.5), R=[rstdb], W=[rstdb])

            def hnorm(e, t0=t0):
                ins = None
                for c in range(8):
                    ins = e.scalar_tensor_tensor(out=hT[:, c, 1:G + 1], in0=xT[:, c, t0:t0 + G], scalar=pv[:, gcol + c:gcol + c + 1],
                                                 in1=rstd, op0=ALU.mult, op1=ALU.mult)
                return ins
            P.op("dve", hnorm, R=xg + [rstdb, pvb, hb], W=[hb])
            for h in range(8):
                for qi, qn in enumerate(("r", "k", "v")):
                    P.dma("sp", wrkvs[qi], rwin_d[qi * 8 + h], W=[wrkvsb[qi]])
                    ws_ = h % 2
                    P.op("pool", lambda e, qi=qi, ws_=ws_: e.tensor_copy(out=wrkv[ws_][qi], in_=wrkvs[qi]), R=[wrkvsb[qi]], W=[wrkvb[ws_][qi]])
                    bank = pi % 2
                    sl = pi % 2
                    pi += 1

                    def mm(e, qi=qi, bank=bank, ws_=ws_):
                        ins = None
                        for c in range(8):
                            ins = e.matmul(pbank[bank][0:64, 0:G + 1], wrkv[ws_][qi][:, c, :], hT[:, c, 0:G + 1], start=(c == 0), stop=(c == 7))
                        return ins
                    P.op("pe", mm, R=[wrkvb[ws_][qi], hb], W=[pbb[bank]])
                    P.op("act", lambda e, bank=bank, sl=sl: e.activation(out=praw[sl][0:64, :], in_=pbank[bank][0:64, 0:G + 1], func=AF.Copy),
                         R=[pbb[bank]], W=[prawb[sl]])
                    P.op("dve", lambda e, sl=sl: e.tensor_tensor(out=dtmp[sl][0:64, :], in0=praw[sl][0:64, 0:G], in1=praw[sl][0:64, 1:G + 1],
                                                                 op=ALU.subtract), R=[prawb[sl]], W=[dtmpb[sl]])
                    P.op("dve", lambda e, sl=sl, qn=qn, qi=qi, h=h: e.scalar_tensor_tensor(
                        out=Q[qn][0:64, h, :], in0=dtmp[sl][0:64, :], scalar=pv[0:64, MU + qi * 8 + h:MU + qi * 8 + h + 1],
                        in1=praw[sl][0:64, 1:G + 1], op0=ALU.mult, op1=ALU.add), R=[dtmpb[sl], prawb[sl], pvb], W=[QB[qn]])
            for j in range(2):
                P.dma("sp", wlos, rwlo_d[j], W=[wlosb])
                P.op("pool", lambda e, j=j: e.tensor_copy(out=wlo[j], in_=wlos), R=[wlosb], W=[wlob[j]])
                bank = pi % 2
                sl = pi % 2
                pi += 1

                def mml(e, j=j, bank=bank):
                    ins = None
                    for c in range(8):
                        ins = e.matmul(pbank[bank][:, 0:G + 1], wlo[j][:, c, :], hT[:, c, 0:G + 1], start=(c == 0), stop=(c == 7))
                    return ins
                P.op("pe", mml, R=[wlob[j], hb], W=[pbb[bank]])
                P.op("act", lambda e, bank=bank, sl=sl: e.activation(out=praw[sl], in_=pbank[bank][:, 0:G + 1], func=AF.Copy),
                     R=[pbb[bank]], W=[prawb[sl]])
                P.op("dve", lambda e, sl=sl: e.tensor_tensor(out=dtmp[sl], in0=praw[sl][:, 0:G], in1=praw[sl][:, 1:G + 1], op=ALU.subtract),
                     R=[prawb[sl]], W=[dtmpb[sl]])
                dst, dstb = (tw, twb) if j == 0 else (sg, sgb)
                mcol = MUWA if j == 0 else MUG
                P.op("dve", lambda e, sl=sl, dst=dst, mcol=mcol: e.scalar_tensor_tensor(
                    out=dst, in0=dtmp[sl], scalar=pv[:, mcol:mcol + 1], in1=praw[sl][:, 1:G + 1], op0=ALU.mult, op1=ALU.add),
                    R=[dtmpb[sl], prawb[sl], pvb], W=[dstb])
            P.op("act", lambda e: e.activation(out=tw[0:64, :], in_=tw[0:64, :], func=AF.Tanh), R=[twb], W=[twb])
            P.op("act", lambda e: e.activation(out=sg, in_=sg, func=AF.Sigmoid), R=[sgb], W=[sgb])
            for half in range(2):
                def mmw(e, half=half):
                    ins = None
                    for hh in range(4):
                        h = half * 4 + hh
                        ins = e.matmul(pbank[2][0:64, hh * G:(hh + 1) * G], w2a2[0:64, h * 64:(h + 1) * 64], tw[0:64, :], start=True, stop=True)
                    return ins
                P.op("pe", mmw, R=[wconst, twb], W=[pbb[2]])

                def sw(e, half=half):
                    ins = None
                    for hh in range(4):
                        h = half * 4 + hh
                        ins = e.activation(out=Q["lw"][0:64, h, :], in_=pbank[2][0:64, hh * G:(hh + 1) * G], func=AF.Sigmoid,
                                           bias=pv[0:64, W0 + h:W0 + h + 1])
                    return ins
                P.op("act", sw, R=[pbb[2], pvb], W=[QB["lw"]])

                def mma(e, half=half):
                    ins = None
                    for hh in range(4):
                        h = half * 4 + hh
                        ins = e.matmul(pbank[3][0:64, hh * G:(hh + 1) * G], w2a2[64:128, h * 64:(h + 1) * 64], tw[64:128, :], start=True, stop=True)
                    return ins
                P.op("pe", mma, R=[wconst, twb], W=[pbb[3]])

                def sa(e, half=half):
                    ins = None
                    for hh in range(4):
                        h = half * 4 + hh
                        ins = e.activation(out=Q["a"][0:64, h, :], in_=pbank[3][0:64, hh * G:(hh + 1) * G], func=AF.Sigmoid,
                                           bias=pv[0:64, A0 + h:A0 + h + 1])
                    return ins
                P.op("act", sa, R=[pbb[3], pvb], W=[QB["a"]])
            P.op("dve", lambda e: e.tensor_scalar(out=Q["lw"][0:64], in0=Q["lw"][0:64], scalar1=-0.6065306597126334, scalar2=None, op0=ALU.mult),
                 R=[QB["lw"]], W=[QB["lw"]])
            def kk1(e):
                ins = None
                for h in range(8):
                    ins = e.tensor_scalar(out=Q["kk"][0:64, h, :], in0=Q["k"][0:64, h, :], scalar1=pv[0:64, KK + h:KK + h + 1], scalar2=None, op0=ALU.mult)
                return ins
            P.op("dve", kk1, R=[QB["k"], pvb], W=[QB["kk"]])
            P.op("act", lambda e: e.activation(out=Q["e"][0:64], in_=Q["kk"][0:64], func=AF.Square), R=[QB["kk"]], W=[QB["e"]])
            for half in range(2):
                P.op("pe", lambda e, half=half: e.matmul(pbank[2][0:64, :], o64, Q["e"][0:64, half * 4:(half + 1) * 4, :], start=True, stop=True),
                     R=[QB["e"], onesb], W=[pbb[2]])
                P.op("dve", lambda e, half=half: e.tensor_scalar(out=Q["e"][0:64, half * 4:(half + 1) * 4, :],
                                                                 in0=pbank[2][0:64, :].rearrange("p (h t) -> p h t", h=4),
                                                                 scalar1=1e-24, scalar2=None, op0=ALU.max), R=[pbb[2], QB["e"]], W=[QB["e"]])
            P.op("act", lambda e: e.activation(out=Q["e"][0:64], in_=Q["e"][0:64], func=AF.Ln), R=[QB["e"]], W=[QB["e"]])
            P.op("act", lambda e: e.activation(out=Q["e"][0:64], in_=Q["e"][0:64], func=AF.Exp, scale=-0.5), R=[QB["e"]], W=[QB["e"]])
            P.op("dve", lambda e: e.tensor_tensor(out=Q["kk"][0:64], in0=Q["kk"][0:64], in1=Q["e"][0:64], op=ALU.mult),
                 R=[QB["kk"], QB["e"]], W=[QB["kk"]])
            def km1(e):
                ins = None
                for h in range(8):
                    ins = e.tensor_scalar(out=Q["e"][0:64, h, :], in0=Q["a"][0:64, h, :], scalar1=pv[0:64, KA + h:KA + h + 1],
                                          scalar2=omka[0:64, h:h + 1], op0=ALU.mult, op1=ALU.add)
                return ins
            P.op("dve", km1, R=[QB["a"], pvb, cb2], W=[QB["e"]])
            P.op("dve", lambda e: e.tensor_tensor(out=Q["k"][0:64], in0=Q["k"][0:64], in1=Q["e"][0:64], op=ALU.mult),
                 R=[QB["k"], QB["e"]], W=[QB["k"]])
            P.op("dve", lambda e: e.tensor_tensor(out=Q["a"][0:64], in0=Q["a"][0:64], in1=Q["kk"][0:64], op=ALU.mult),
                 R=[QB["a"], QB["kk"]], W=[QB["a"]])
            def bn1(e):
                ins = None
                for h in range(8):
                    ins = e.scalar_tensor_tensor(out=Q["e"][0:64, h, :], in0=Q["r"][0:64, h, :], scalar=pv[0:64, RK + h:RK + h + 1],
                                                 in1=Q["k"][0:64, h, :], op0=ALU.mult, op1=ALU.mult)
                return ins
            P.op("dve", bn1, R=[QB["r"], QB["k"], pvb], W=[QB["e"]])
            def scn(e):
                ins = None
                for h in range(8):
                    ins = e.tensor_tensor_scan(out=Q["Lw"][0:64, h, :], data0=resetm[0:64, 0:G], data1=Q["lw"][0:64, h, :], initial=0.0,
                                               op0=ALU.mult, op1=ALU.add)
                return ins
            P.op("dve", scn, R=[QB["lw"], cb], W=[QB["Lw"]])
            P.op("dve", lambda e: e.tensor_tensor(out=Q["lw"][0:64], in0=Q["Lw"][0:64], in1=Q["lw"][0:64], op=ALU.subtract),
                 R=[QB["Lw"], QB["lw"]], W=[QB["lw"]])
            for half in range(2):
                P.op("pe", lambda e, half=half: e.matmul(pbank[3][0:64, :], o64, Q["e"][0:64, half * 4:(half + 1) * 4, :], start=True, stop=True),
                     R=[QB["e"], onesb], W=[pbb[3]])
                P.op("dve", lambda e, half=half: e.tensor_tensor(out=Q["BhT"][0:64, half * 4:(half + 1) * 4, :],
                                                                 in0=pbank[3][0:64, :].rearrange("p (h t) -> p h t", h=4),
                                                                 in1=Q["v"][0:64, half * 4:(half + 1) * 4, :], op=ALU.mult),
                     R=[pbb[3], QB["v"]], W=[QB["BhT"]])
            P.op("act", lambda e: e.activation(out=Q["e"][0:64], in_=Q["lw"][0:64], func=AF.Exp), R=[QB["lw"]], W=[QB["e"]])
            P.op("dve", lambda e: e.scalar_tensor_tensor(out=Q["at"][0:64], in0=Q["kk"][0:64], scalar=-1.0, in1=Q["e"][0:64], op0=ALU.mult, op1=ALU.mult),
                 R=[QB["kk"], QB["e"]], W=[QB["at"]])
            P.op("act", lambda e: e.activation(out=Q["lw"][0:64], in_=Q["BhT"][0:64], func=AF.Copy), R=[QB["BhT"], QB["e"]], W=[QB["lw"]])
            P.op("act", lambda e: e.activation(out=Q["e"][0:64], in_=Q["Lw"][0:64], func=AF.Exp), R=[QB["Lw"], QB["at"]], W=[QB["e"]])
            P.op("dve", lambda e: e.tensor_tensor(out=Q["r"][0:64], in0=Q["r"][0:64], in1=Q["e"][0:64], op=ALU.mult), R=[QB["r"], QB["e"]], W=[QB["r"]])
            P.op("act", lambda e: e.activation(out=Q["e"][0:64], in_=Q["Lw"][0:64], func=AF.Exp, scale=-1.0), R=[QB["Lw"], QB["r"]], W=[QB["e"]])
            P.op("dve", lambda e: e.tensor_tensor(out=Q["bt"][0:64], in0=Q["a"][0:64], in1=Q["e"][0:64], op=ALU.mult), R=[QB["a"], QB["e"]], W=[QB["bt"]])
            P.op("dve", lambda e: e.tensor_tensor(out=Q["kt"][0:64], in0=Q["k"][0:64], in1=Q["e"][0:64], op=ALU.mult), R=[QB["k"], QB["e"]], W=[QB["kt"]])
            def eld(e):
                ins = None
                for h in range(8):
                    for ci in range(G // 64):
                        ins = e.activation(out=Q["e"][0:64, h, ci * 64:(ci + 1) * 64], in_=Q["Lw"][0:64, h, ci * 64:(ci + 1) * 64], func=AF.Exp,
                                           scale=-1.0, bias=Q["Lw"][0:64, h, ci * 64 + 63:ci * 64 + 64])
                return ins
            P.op("act", eld, R=[QB["Lw"], QB["bt"], QB["kt"]], W=[QB["e"]])
            P.op("act", lambda e: e.activation(out=gC[0:64], in_=Q["Lw"][0:64].rearrange("p h (c t) -> p h c t", t=64)[:, :, :, 63], func=AF.Exp),
                 R=[QB["Lw"]], W=[gCb])
            P.op("dve", lambda e: e.tensor_tensor(out=Q["BhT"][0:64], in0=Q["a"][0:64], in1=Q["e"][0:64], op=ALU.mult),
                 R=[QB["a"], QB["e"], QB["lw"]], W=[QB["BhT"]])
            P.op("dve", lambda e: e.tensor_tensor(out=Q["KhT"][0:64], in0=Q["k"][0:64], in1=Q["e"][0:64], op=ALU.mult),
                 R=[QB["k"], QB["e"]], W=[QB["KhT"]])
            if dbg_d is not None and g == 0:
                for i_, n_ in enumerate(["r", "k", "v", "lw", "a", "kk", "Lw", "at", "bt", "kt", "BhT", "KhT"]):
                    dbg_ops.append(P.dma("sp", dbg_d[:, i_, :].rearrange("p (h t) -> p h t", h=8), Q[n_][0:64], R=[QB[n_]]))
            def chunk_indep(ci):
                cs = ci * 64
                sfx = str(ci % 2)
                N_ = lambda n: n + sfx if n in HO_ else n

                def amat(bank, ln, rn, dst, mask, eng):
                    def mm(e):
                        ins = None
                        for h in range(8):
                            ins = e.matmul(pbank[bank][0:64, h * 64:(h + 1) * 64], Q[ln][0:64, h, cs:cs + 64], Q[rn][0:64, h, cs:cs + 64],
                                           start=True, stop=True)
                        return ins
                    P.op("pe", mm, R=[QB[ln], QB[rn]], W=[pbb[bank]])
                    P.op(eng, lambda e: e.tensor_tensor(out=Cc[N_(dst)][0:64], in0=pbank[bank][0:64, :].rearrange("p (h t) -> p h t", h=8),
                                                        in1=mask, op=ALU.mult), R=[pbb[bank], cb2], W=[CB[N_(dst)]])
                amat(2, "at", "bt", "P0", mLs, "dve")
                yield
                amat(3, "bt", "at", "PT0", mUs, "dve")
                yield
                amat(2, "kt", "at", "AakT", mUs, "dve")
                yield
                P.op("dve", lambda e: e.tensor_tensor(out=Cc[N_("TT")][0:64], in0=Cc["PT0"][0:64], in1=id8, op=ALU.add), R=[CB["PT0"], cb2], W=[CB[N_("TT")]])
                def do_tr(pairs):
                    for src, dst in pairs:
                        def trp(e, src=src):
                            ins = None
                            for h in range(8):
                                ins = e.transpose(pbank[4][0:64, h * 64:(h + 1) * 64], Q[src][0:64, h, cs:cs + 64], ident[0:64, 0:64])
                            return ins
                        P.op("pe", trp, R=[QB[src], cb], W=[pbb[4]])
                        P.op("act", lambda e, dst=dst: e.activation(out=Cc[N_(dst)][0:64], in_=pbank[4][0:64, :].rearrange("p (h t) -> p h t", h=8), func=AF.Copy),
                             R=[pbb[4]], W=[CB[N_(dst)]])
                        yield

                yield from do_tr((("v", "Vtm"),))
                pc = 0
                for lev in range(1, 6):
                    Pn, Pp = "P%d" % (1 - pc), "P%d" % pc
                    PTn, PTp = "PT%d" % (1 - pc), "PT%d" % pc

                    def sqm(e, Pp=Pp, PTp=PTp):
                        ins = None
                        for h in range(8):
                            ins = e.matmul(pbank[2][0:64, h * 64:(h + 1) * 64], Cc[PTp][0:64, h, :], Cc[Pp][0:64, h, :], start=True, stop=True)
                        return ins
                    P.op("pe", sqm, R=[CB[Pp], CB[PTp]], W=[pbb[2]])
                    P.op("act", lambda e, Pn=Pn: e.activation(out=Cc[Pn][0:64], in_=pbank[2][0:64, :].rearrange("p (h t) -> p h t", h=8), func=AF.Copy),
                         R=[pbb[2]], W=[CB[Pn]])
                    yield
                    if lev < 5:
                        def sqt(e, Pp=Pp, PTp=PTp):
                            ins = None
                            for h in range(8):
                                ins = e.matmul(pbank[3][0:64, h * 64:(h + 1) * 64], Cc[Pp][0:64, h, :], Cc[PTp][0:64, h, :], start=True, stop=True)
                            return ins
                        P.op("pe", sqt, R=[CB[Pp], CB[PTp]], W=[pbb[3]])
                        P.op("dve", lambda e, PTn=PTn: e.tensor_copy(out=Cc[PTn][0:64], in_=pbank[3][0:64, :].rearrange("p (h t) -> p h t", h=8)),
                             R=[pbb[3]], W=[CB[PTn]])
                        yield

                    def ttm(e, Pn=Pn):
                        ins = None
                        for h in range(8):
                            ins = e.matmul(pbank[7][0:64, h * 64:(h + 1) * 64], Cc[Pn][0:64, h, :], Cc[N_("TT")][0:64, h, :], start=True, stop=True)
                        return ins
                    P.op("pe", ttm, R=[CB[Pn], CB[N_("TT")]], W=[pbb[7]])
                    P.op("dve", lambda e: e.tensor_tensor(out=Cc[N_("TT")][0:64], in0=pbank[7][0:64, :].rearrange("p (h t) -> p h t", h=8),
                                                          in1=Cc[N_("TT")][0:64], op=ALU.add), R=[pbb[7], CB[N_("TT")]], W=[CB[N_("TT")]])
                    yield
                    pc = 1 - pc
                amat(3, "bt", "r", "ArbT", mUi, "dve")
                yield
                amat(2, "kt", "r", "ArkT", mUi, "dve")
                yield
                yield from do_tr((("BhT", "Bhtm"), ("KhT", "Khtm")))
            def chunk_dep(ci):
                nonlocal hcur
                cs = ci * 64
                sfx = str(ci % 2)
                N_ = lambda n: n + sfx if n in HO_ else n
                Hc, Hn = "H%d" % hcur, "H%d" % (1 - hcur)

                def xmm(e, Hc=Hc):
                    ins = None
                    for h in range(8):
                        e.matmul(pbank[6][0:64, h * 64:(h + 1) * 64], Q["at"][0:64, h, cs:cs + 64], Cc[Hc][0:64, h, :], start=True, stop=False)
                        ins = e.matmul(pbank[6][0:64, h * 64:(h + 1) * 64], Cc[N_("AakT")][0:64, h, :], Cc[N_("Vtm")][0:64, h, :], start=False, stop=True)
                    return ins
                P.op("pe", xmm, R=[QB["at"], CB[Hc], CB[N_("AakT")], CB[N_("Vtm")]], W=[pbb[6]])
                P.op("act", lambda e: e.activation(out=Cc["Xs"][0:64], in_=pbank[6][0:64, :].rearrange("p (h t) -> p h t", h=8), func=AF.Copy),
                     R=[pbb[6]], W=[CB["Xs"]])
                yield
                yield
                yield
                yield

                def umm(e):
                    ins = None
                    for h in range(8):
                        ins = e.matmul(pbank[6][0:64, h * 64:(h + 1) * 64], Cc[N_("TT")][0:64, h, :], Cc["Xs"][0:64, h, :], start=True, stop=True)
                    return ins
                P.op("pe", umm, R=[CB[N_("TT")], CB["Xs"]], W=[pbb[6]])
                P.op("act", lambda e: e.activation(out=Cc["Us"][0:64], in_=pbank[6][0:64, :].rearrange("p (h t) -> p h t", h=8), func=AF.Copy),
                     R=[pbb[6]], W=[CB["Us"]])
                yield
                yield
                yield
                yield

                def ymm(e, Hc=Hc):
                    ins = None
                    for h in range(8):
                        e.matmul(pbank[0][0:64, h * 64:(h + 1) * 64], Cc[Hc][0:64, h, :], Q["r"][0:64, h, cs:cs + 64], start=True, stop=False)
                        e.matmul(pbank[0][0:64, h * 64:(h + 1) * 64], Cc["Us"][0:64, h, :], Cc[N_("ArbT")][0:64, h, :], start=False, stop=False)
                        ins = e.matmul(pbank[0][0:64, h * 64:(h + 1) * 64], Cc[N_("Vtm")][0:64, h, :], Cc[N_("ArkT")][0:64, h, :], start=False, stop=True)
                    return ins
                P.op("pe", ymm, R=[CB[Hc], QB["r"], CB["Us"], CB[N_("ArbT")], CB[N_("Vtm")], CB[N_("ArkT")]], W=[pbb[0]])
                P.op("act", lambda e: e.activation(out=Q["Lw"][0:64, :, cs:cs + 64], in_=pbank[0][0:64, :].rearrange("p (h t) -> p h t", h=8), func=AF.Copy),
                     R=[pbb[0], gCb], W=[QB["Lw"]])

                def hmm(e):
                    ins = None
                    for h in range(8):
                        e.matmul(pbank[1][0:64, h * 64:(h + 1) * 64], Cc[N_("Bhtm")][0:64, h, :], Cc["Us"][0:64, h, :], start=True, stop=False)
                        ins = e.matmul(pbank[1][0:64, h * 64:(h + 1) * 64], Cc[N_("Khtm")][0:64, h, :], Cc[N_("Vtm")][0:64, h, :], start=False, stop=True)
                    return ins
                P.op("pe", hmm, R=[CB[N_("Bhtm")], CB["Us"], CB[N_("Khtm")], CB[N_("Vtm")]], W=[pbb[1]])

                def hup(e, Hc=Hc, Hn=Hn, ci=ci):
                    ins = None
                    for h in range(8):
                        ins = e.scalar_tensor_tensor(out=Cc[Hn][0:64, h, :], in0=Cc[Hc][0:64, h, :], scalar=gC[0:64, h, ci:ci + 1],
                                                     in1=pbank[1][0:64, h * 64:(h + 1) * 64], op0=ALU.mult, op1=ALU.add)
                    return ins
                P.op("dve", hup, R=[CB[Hc], gCb, pbb[1]], W=[CB[Hn]])
                hcur = 1 - hcur
                yield
                if dbg_d is not None and g == 0 and ci == 0:
                    for i_, n_ in enumerate(["P0", "PT0", "TT0", "AakT0", "ArbT", "ArkT", "Vtm0", "Bhtm", "Khtm", "Xs", "Us", Hn]):
                        dbg_ops.append(P.dma("sp", dbg_d[:, 12 + i_, 0:512].rearrange("p (h t) -> p h t", h=8), Cc[n_][0:64], R=[CB[n_]]))
            def _il(a_, b_):
                gens = [x for x in (a_, b_) if x is not None]
                while gens:
                    for x in list(gens):
                        try:
                            next(x)
                        except StopIteration:
                            gens.remove(x)
            nch = G // 64
            _il(chunk_indep(0), None)
            for ci_ in range(nch):
                _il(chunk_dep(ci_), chunk_indep(ci_ + 1) if ci_ + 1 < nch else None)
            if dbg_d is not None and g == 0:
                dbg_ops.append(P.dma("sp", dbg_d[:, 24, :].rearrange("p (h t) -> p h t", h=8), Q["Lw"][0:64], R=[QB["Lw"]]))
            yr = Q["Lw"]; yrb = QB["Lw"]
            cen = Q["at"]; cenb = QB["at"]
            for half in range(2):
                P.op("pe", lambda e, half=half: e.matmul(pbank[2][0:64, :], o64, yr[0:64, half * 4:(half + 1) * 4, :], start=True, stop=True),
                     R=[yrb, onesb], W=[pbb[2]])
                P.op("dve", lambda e, half=half: e.scalar_tensor_tensor(out=cen[0:64, half * 4:(half + 1) * 4, :],
                                                                        in0=pbank[2][0:64, :].rearrange("p (h t) -> p h t", h=4), scalar=-1.0 / 64.0,
                                                                        in1=yr[0:64, half * 4:(half + 1) * 4, :], op0=ALU.mult, op1=ALU.add),
                     R=[pbb[2], yrb, CB["H0"], CB["H1"]], W=[cenb])
            P.op("act", lambda e: e.activation(out=Q["e"][0:64], in_=cen[0:64], func=AF.Square), R=[cenb, QB["BhT"], QB["KhT"]], W=[QB["e"]])
            for half in range(2):
                P.op("pe", lambda e, half=half: e.matmul(pbank[3][0:64, :], o64, Q["e"][0:64, half * 4:(half + 1) * 4, :], start=True, stop=True),
                     R=[QB["e"], onesb], W=[pbb[3]])
                P.op("act", lambda e, half=half: e.activation(out=Q["bt"][0:64, half * 4:(half + 1) * 4, :],
                                                               in_=pbank[3][0:64, :].rearrange("p (h t) -> p h t", h=4), func=AF.Ln,
                                                               bias=epsb_t[0:64, 1:2], scale=1.0 / 64.0), R=[pbb[3], epsb], W=[QB["bt"]])
            P.op("act", lambda e: e.activation(out=Q["bt"][0:64], in_=Q["bt"][0:64], func=AF.Exp, scale=-0.5), R=[QB["bt"]], W=[QB["bt"]])
            P.op("dve", lambda e: e.tensor_tensor(out=cen[0:64], in0=cen[0:64], in1=Q["bt"][0:64], op=ALU.mult), R=[cenb, QB["bt"]], W=[cenb])

            def lnx(e):
                ins = None
                for h in range(8):
                    ins = e.tensor_scalar(out=cen[0:64, h, :], in0=cen[0:64, h, :], scalar1=pv[0:64, LNW + h:LNW + h + 1],
                                          scalar2=pv[0:64, LNB + h:LNB + h + 1], op0=ALU.mult, op1=ALU.add)
                return ins
            P.op("dve", lnx, R=[cenb, pvb], W=[cenb])
            P.op("dve", lambda e: e.tensor_tensor(out=cen[0:64], in0=cen[0:64], in1=Q["lw"][0:64], op=ALU.add), R=[cenb, QB["lw"]], W=[cenb])
            for half in range(2):
                def gmm(e, half=half):
                    ins = None
                    for hh in range(4):
                        h = half * 4 + hh
                        ins = e.matmul(pbank[2][0:64, hh * G:(hh + 1) * G], g2[:, h * 64:(h + 1) * 64], sg, start=True, stop=True)
                    return ins
                P.op("pe", gmm, R=[wconst, sgb], W=[pbb[2]])
                P.op("dve", lambda e, half=half: e.tensor_tensor(out=yfh[0:64, half * 4:(half + 1) * 4, :], in0=pbank[2][0:64, :].rearrange("p (h t) -> p h t", h=4),
                                                                 in1=cen[0:64, half * 4:(half + 1) * 4, :], op=ALU.mult),
                     R=[pbb[2], cenb], W=[yfhb])
            yf = yfh; yfb = yfhb
            for dh in range(2):
                for dd in range(4):
                    dc = dh * 4 + dd
                    s2 = woi % 2
                    woi += 1
                    P.dma("sp", wos[0:64], rwout_d[dc], W=[wosb])
                    P.op("pool", lambda e, s2=s2: e.tensor_copy(out=wo[s2][0:64], in_=wos[0:64]), R=[wosb], W=[wob[s2]])

                    def mo(e, s2=s2, dd=dd, dh=dh):
                        ins = None
                        for h in range(8):
                            ins = e.matmul(pbank[7][:, dd * G:(dd + 1) * G], wo[s2][0:64, h, :], yf[0:64, h, :], start=(h == 0), stop=(h == 7))
                        return ins
                    P.op("pe", mo, R=[wob[s2], yfb], W=[pbb[7]])
                P.op("dve", lambda e, dh=dh, t0=t0: e.tensor_tensor(out=xT[:, dh * 4:(dh + 1) * 4, t0:t0 + G],
                                                                    in0=pbank[7][:, 0:4 * G].rearrange("p (c t) -> p c t", c=4),
                                                                    in1=xT[:, dh * 4:(dh + 1) * 4, t0:t0 + G], op=ALU.add),
                     R=[pbb[7]] + [xb[c][g4] for c in range(dh * 4, dh * 4 + 4)], W=[xb[c][g4] for c in range(dh * 4, dh * 4 + 4)])
        for g_ in range(NG):
            do_group(g_)
        P.fence()


    def moba():
        G = 256
        NEG = 240000.0
        A = Arena(arena, ARENA)
        kT = A.take(8192).rearrange("p (c t) -> p c t", c=4)
        kTb = [[Buf() for g in range(8)] for c in range(4)]
        Va = A.take(16 * 8 * 65).rearrange("p (k h d) -> p k h d", k=16, h=8)
        Vab = [Buf() for kt in range(16)]
        hT = A.take(2048).rearrange("p (c t) -> p c t", c=8); hb = Buf()
        sq = A.take(2048).rearrange("p (c t) -> p c t", c=8); sqb = Buf()
        rstd = A.take(256); rstdb = Buf()
        wt = [A.take(1024).rearrange("p (c m) -> p c m", c=8) for i in range(2)]; wtb = [Buf(), Buf()]
        qT = A.take(1024).rearrange("p (c t) -> p c t", c=4); qTb = [Buf() for c in range(4)]
        ksum = A.take(32).rearrange("p (c j) -> p c j", c=4); ksb = Buf()
        gsel = A.take(16).rearrange("p (a j) -> p a j", a=2); gselb = Buf()
        m8 = A.take(16).rearrange("p (a j) -> p a j", a=2); m8b = Buf()
        negm = A.take(16).rearrange("p (a j) -> p a j", a=2); negmb = Buf()
        qaug = [A.take(256) for i in range(2)]; qaugb = [Buf(), Buf()]
        kaug = A.take(2048); kaugb = Buf()
        PT = [A.take(256) for i in range(2)]; PTb = [Buf() for i in range(2)]
        stmp = [A.take(256) for i in range(1)]; stmpb = [Buf()]
        oa = A.take(256); oab = Buf()
        rden = A.take(256); rdenb = Buf()
        oh = A.take(2048).rearrange("p (h t) -> p h t", h=8); ohb = [Buf() for h in range(8)]
        wo = [A.take(1024).rearrange("p (h m) -> p h m", h=8) for i in range(1)]; wob = [Buf()]
        stg = [A.take(512).rearrange("p (c t) -> p c t", c=2) for i in range(2)]; stgb = [Buf(), Buf()]
        ncA = A.take(256); ncB = A.take(256); sel65 = A.take(64); mcb = Buf()
        gcol = PV_NORMG + (0 * 3 + 1) * 8
        cnt = {"w": 0, "p": 0, "pt": 0, "st": 0, "wo": 0, "qa": 0}

        def setup(e):
            e.memset(ncA[:], 0.0)
            e.memset(ncB[:], 0.0)
            e.affine_select(out=ncA[:], in_=ncA[:], pattern=[[1, 256]], compare_op=ALU.is_ge, fill=-NEG, base=0, channel_multiplier=-1)
            e.affine_select(out=ncB[:], in_=ncB[:], pattern=[[1, 256]], compare_op=ALU.is_ge, fill=-NEG, base=-128, channel_multiplier=-1)
            e.memset(sel65[0:64, :], 0.0)
            e.memset(sel65[64:65, :], 1.0)
            return e.memset(Va[:, :, :, 64:65], 1.0)
        P.op("pool", setup, W=[mcb] + Vab)
        P.dma("sp", kaug[0:10, :], mbkaug_d[:, :], W=[kaugb])

        def proj_fm(widx, dst_ap, dst_bufs):
            s_ = cnt["w"] % 2
            cnt["w"] += 1
            bank = cnt["p"] % 2
            cnt["p"] += 1
            P.dma("sp", wt[s_], mbin_d[widx], W=[wtb[s_]])

            def mm(e):
                ins = None
                for c in range(8):
                    ins = e.matmul(pbank[bank][:, 0:G], wt[s_][:, c, :], hT[:, c, :], start=(c == 0), stop=(c == 7))
                return ins
            P.op("pe", mm, R=[wtb[s_], hb], W=[pbb[bank]])
            P.op("act", lambda e: e.activation(out=dst_ap, in_=pbank[bank][:, 0:G], func=AF.Copy), R=[pbb[bank]], W=dst_bufs)

        def proj_v(g, hp):
            s_ = cnt["w"] % 2
            cnt["w"] += 1
            bank = cnt["p"] % 2
            cnt["p"] += 1
            P.dma("sp", wt[s_], mbin_d[8 + hp], W=[wtb[s_]])

            def mm(e):
                ins = None
                for tt in range(2):
                    for c in range(8):
                        ins = e.matmul(pbank[bank][:, tt * 128:(tt + 1) * 128], hT[:, c, tt * 128:(tt + 1) * 128], wt[s_][:, c, :],
                                       start=(c == 0), stop=(c == 7))
                return ins
            P.op("pe", mm, R=[wtb[s_], hb], W=[pbb[bank]])
            P.op("dve", lambda e: e.tensor_copy(out=Va[:, 2 * g:2 * g + 2, 2 * hp:2 * hp + 2, 0:64],
                                                in_=pbank[bank][:, 0:256].rearrange("p (k h d) -> p k h d", k=2, h=2)),
                 R=[pbb[bank]], W=[Vab[2 * g], Vab[2 * g + 1]])

        def prep_head(g, h):
            hp, ph = h // 2, h % 2
            r0 = ph * 64
            ob = g
            qa = qaug[h % 2]
            qab = qaugb[h % 2]
            P.dma("sp", qa[8:10, :], mbqc_d[h, :, g * G:(g + 1) * G], W=[qab])
            if ob >= 4:
                def gmm(e):
                    ins = None
                    for tt in range(2):
                        ins = e.matmul(pbank[2][:, tt * 8:(tt + 1) * 8], qT[r0:r0 + 64, hp, tt * 128:(tt + 1) * 128], ksum[r0:r0 + 64, hp, :],
                                       start=True, stop=True)
                    return ins
                P.op("pe", gmm, R=[qTb[hp], ksb], W=[pbb[2]])
                P.op("pool", lambda e: e.memset(gsel, -1e30), W=[gselb])
                P.op("dve", lambda e: e.tensor_copy(out=gsel[:, :, 0:ob], in_=pbank[2][:, 0:16].rearrange("p (a j) -> p a j", a=2)[:, :, 0:ob]),
                     R=[pbb[2]], W=[gselb])

                def mx(e):
                    e.max(out=m8[:, 0, :], in_=gsel[:, 0, :])
                    return e.max(out=m8[:, 1, :], in_=gsel[:, 1, :])
                P.op("dve", mx, R=[gselb], W=[m8b])

                def ng(e):
                    ins = None
                    for tt in range(2):
                        ins = e.tensor_scalar(out=negm[:, tt, :], in0=gsel[:, tt, :], scalar1=m8[:, tt, 2:3], scalar2=1.0, op0=ALU.is_ge, op1=ALU.subtract)
                    return ins
                P.op("dve", ng, R=[gselb, m8b], W=[negmb])
                P.op("dve", lambda e: e.memset(negm[:, :, ob:8], 0.0), R=[], W=[negmb])

                def trn(e):
                    ins = None
                    for tt in range(2):
                        ins = e.transpose(pbank[2][0:8, 128 + tt * 128:128 + (tt + 1) * 128], negm[:, tt, :], ident[:])
                    return ins
                P.op("pe", trn, R=[negmb, cb], W=[pbb[2]])
                P.op("act", lambda e: e.activation(out=qa[0:8, :], in_=pbank[2][0:8, 128:384], func=AF.Copy), R=[pbb[2]], W=[qab])
            else:
                P.op("pool", lambda e: e.memset(qa[0:8, :], 0.0), W=[qab])

        def attn_head(g, h):
            hp, ph = h // 2, h % 2
            r0 = ph * 64
            ob = g
            qa = qaug[h % 2]
            qab = qaugb[h % 2]
            ob5 = 5 + (h % 2)
            nkt = 2 * ob + 2
            pend = []

            def emit_pv(kt, c0, pti):
                P.op("pe", lambda e: e.matmul(pbank[ob5][0:65, c0:G], Va[:, kt, h, :], PT[pti][:, c0:G], start=(kt == 0), stop=(kt == nkt - 1)),
                     R=[Vab[kt], PTb[pti]], W=[pbb[ob5]])
            for kt in range(nkt):
                diag = kt - 2 * ob
                c0 = 128 if diag == 1 else 0
                n = G - c0
                sb_ = 3 + (cnt["st"] % 2)
                cnt["st"] += 1
                pti = cnt["pt"] % 2
                cnt["pt"] += 1

                def smm(e, kt=kt, c0=c0, sb_=sb_):
                    e.matmul(pbank[sb_][:, c0:G], kT[r0:r0 + 64, hp, kt * 128:(kt + 1) * 128], qT[r0:r0 + 64, hp, c0:G], start=True, stop=False)
                    return e.matmul(pbank[sb_][:, c0:G], kaug[0:10, kt * 128:(kt + 1) * 128], qa[0:10, c0:G], start=False, stop=True)
                P.op("pe", smm, R=[kTb[hp][kt // 2], qTb[hp], kaugb, qab], W=[pbb[sb_]])
                if diag >= 0:
                    nc_ = ncA if diag == 0 else ncB
                    si = 0
                    P.op("dve", lambda e, c0=c0, sb_=sb_, nc_=nc_, si=si: e.tensor_tensor(out=stmp[si][:, c0:G], in0=pbank[sb_][:, c0:G], in1=nc_[:, c0:G], op=ALU.add),
                         R=[pbb[sb_], mcb], W=[stmpb[si]])
                    P.op("act", lambda e, c0=c0, pti=pti, si=si: e.activation(out=PT[pti][:, c0:G], in_=stmp[si][:, c0:G], func=AF.Exp, scale=0.125),
                         R=[stmpb[si]], W=[PTb[pti]])
                else:
                    P.op("act", lambda e, c0=c0, pti=pti, sb_=sb_: e.activation(out=PT[pti][:, c0:G], in_=pbank[sb_][:, c0:G], func=AF.Exp, scale=0.125),
                         R=[pbb[sb_]], W=[PTb[pti]])
                pend.append((kt, c0, pti))
                if len(pend) > 1:
                    emit_pv(*pend.pop(0))
            while pend:
                emit_pv(*pend.pop(0))
            P.op("act", lambda e: e.activation(out=oa[0:65, :], in_=pbank[ob5][0:65, 0:G], func=AF.Copy), R=[pbb[ob5]], W=[oab])
            P.op("pe", lambda e: e.matmul(pbank[7][0:64, 0:G], sel65[0:65, :], oa[0:65, :], start=True, stop=True), R=[oab, mcb], W=[pbb[7]])
            P.op("dve", lambda e: e.reciprocal(out=rden[0:64, :], in_=pbank[7][0:64, 0:G]), R=[pbb[7]], W=[rdenb])
            P.op("dve", lambda e: e.tensor_tensor(out=oh[0:64, h, :], in0=oa[0:64, :], in1=rden[0:64, :], op=ALU.mult), R=[oab, rdenb], W=[ohb[h]])

        def do_group(g):
            t0 = g * G
            g4 = t0 // 512
            xg = [xb[c][g4] for c in range(8)]
            P.op("act", lambda e: e.activation(out=sq, in_=xT[:, :, t0:t0 + G], func=AF.Square), R=xg, W=[sqb])

            def mmn(e):
                ins = None
                for c in range(8):
                    ins = e.matmul(pbank[7][:, 0:G], ones[:], sq[:, c, :], start=(c == 0), stop=(c == 7))
                return ins
            P.op("pe", mmn, R=[sqb, onesb], W=[pbb[7]])
            P.op("act", lambda e: e.activation(out=rstd, in_=pbank[7][:, 0:G], func=AF.Ln, bias=epsb_t[:, 0:1], scale=1.0 / 1024.0),
                 R=[pbb[7], epsb], W=[rstdb])
            P.op("act", lambda e: e.activation(out=rstd, in_=rstd, func=AF.Exp, scale=-0.5), R=[rstdb], W=[rstdb])

            def hnorm(e):
                ins = None
                for c in range(8):
                    ins = e.scalar_tensor_tensor(out=hT[:, c, :], in0=xT[:, c, t0:t0 + G], scalar=pv[:, gcol + c:gcol + c + 1],
                                                 in1=rstd, op0=ALU.mult, op1=ALU.mult)
                return ins
            P.op("dve", hnorm, R=xg + [rstdb, pvb], W=[hb])
            for hp in range(4):
                proj_fm(hp, qT[:, hp, :], [qTb[hp]])
                proj_fm(4 + hp, kT[:, hp, t0:t0 + G], [kTb[hp][g]])
                proj_v(g, hp)
            P.op("dve", lambda e: e.tensor_reduce(out=ksum[:, :, g], in_=kT[:, :, t0:t0 + G], axis=AX.X, op=ALU.add),
                 R=[kTb[hp][g] for hp in range(4)], W=[ksb])
            prep_head(g, 0)
            for h in range(8):
                if h + 1 < 8:
                    prep_head(g, h + 1)
                attn_head(g, h)
            if dbg_d is not None and g == 0:
                dbg_ops.append(P.dma("sp", dbg_d[:, 0:2, :].rearrange("p a (h t) -> p (a h) t", h=4), oh[0:64], R=ohb))
                dbg_ops.append(P.dma("sp", dbg_d[:, 2, 0:256], qT[0:64, 0, :], R=qTb))
                dbg_ops.append(P.dma("sp", dbg_d[:, 3, 0:256], kT[0:64, 0, 0:256], R=[kTb[0][0]]))
                dbg_ops.append(P.dma("sp", dbg_d[:, 4:6, :].rearrange("p a (h d) -> p (a h) d", h=4)[:, :, 0:65], Va[0:64, 0, :, :], R=[Vab[0]]))
                dbg_ops.append(P.dma("sp", dbg_d[:, 6, 0:256], oa[0:64, :], R=[oab]))
                dbg_ops.append(P.dma("sp", dbg_d[:, 7, 0:256], rden[0:64, :], R=[rdenb]))
                dbg_ops.append(P.dma("sp", dbg_d[:, 8, 0:256], PT[0][0:64, :], R=[PTb[0]]))
                dbg_ops.append(P.dma("sp", dbg_d[:, 9, 0:256], PT[1][0:64, :], R=[PTb[1]]))
                dbg_ops.append(P.dma("sp", dbg_d[:, 10, 0:256], ncA[0:64, :], R=[mcb]))
                dbg_ops.append(P.dma("sp", dbg_d[0:10, 11, 0:256], qaug[0][0:10, :], R=[qaugb[0]]))
                dbg_ops.append(P.dma("sp", dbg_d[0:10, 12, 0:256], kaug[0:10, 0:256], R=[kaugb]))
            for dh in range(4):
                for dd in range(2):
                    dc = dh * 2 + dd
                    s2 = 0
                    P.dma("sp", wo[s2][0:64], mbout_d[dc], W=[wob[s2]])

                    def mo(e, s2=s2, dd=dd):
                        ins = None
                        for h in range(8):
                            ins = e.matmul(pbank[7][:, dd * G:(dd + 1) * G], wo[s2][0:64, h, :], oh[0:64, h, :], start=(h == 0), stop=(h == 7))
                        return ins
                    P.op("pe", mo, R=[wob[s2]] + ohb, W=[pbb[7]])
                si = cnt["wo"] % 2
                cnt["wo"] += 1
                P.op("act", lambda e, si=si: e.activation(out=stg[si], in_=pbank[7][:, 0:2 * G].rearrange("p (c t) -> p c t", c=2), func=AF.Copy),
                     R=[pbb[7]], W=[stgb[si]])
                P.dma("sp", mscr_d[:, dh * 2:(dh + 1) * 2, t0:t0 + G], stg[si], R=[stgb[si]], W=[mscr_b[g]])
        for g_ in range(8):
            do_group(g_)
        P.fence()

    def moba_add():
        A = Arena(arena, ARENA)
        tb = [A.take(4096).rearrange("p (c t) -> p c t", c=8) for i in range(2)]
        tbb = [Buf(), Buf()]
        for g in range(4):
            si = g % 2
            P.dma("sp", tb[si], mscr_d[:, :, g * 512:(g + 1) * 512], R=[mscr_b[2 * g], mscr_b[2 * g + 1]], W=[tbb[si]])
            P.op("dve", lambda e, g=g, si=si: e.tensor_tensor(out=xT[:, :, g * 512:(g + 1) * 512], in0=xT[:, :, g * 512:(g + 1) * 512],
                                                              in1=tb[si], op=ALU.add),
                 R=[tbb[si]] + [xb[c][g] for c in range(8)], W=[xb[c][g] for c in range(8)])
        P.fence()

    P.fence()
    st = stage
    if "f00" in st:
        ffn(0, 0)
    if "moba" in st:
        moba()
    if "rwkv" in st:
        rwkv()
    if "moba" in st:
        moba_add()
    if "f01" in st:
        ffn(0, 2)
    if "f10" in st:
        ffn(1, 0)
    if "hgrn" in st:
        hgrn()
    if "f11" in st:
        ffn(1, 2)
    outs = final_out("final" in st)
    P.emit(nc, k.es, outs + dbg_ops)


PV_NORMG = 0
PV_FINALG = 48
PV_HGNW = 56
PV_LBZ = 64
PV_RW = 80
NPV = 176


class Arena:
    def __init__(self, ap, size):
        self.ap, self.o, self.size = ap, 0, size

    def take(self, n):
        a = self.ap[:, self.o:self.o + n]
        self.o += n
        assert self.o <= self.size, self.o
        return a


def _tile_w_in(w, ncols):
    n = ncols // 128
    return np.ascontiguousarray(w.reshape(8, 128, n, 128).transpose(2, 1, 0, 3))


def _prep_shared(inp):
    sh = {}
    for l in range(2):
        for f, nm in enumerate(("ffn1", "ffn2")):
            sh["wg%d%d" % (l, f)] = _tile_w_in(inp[nm + "_wg"][l], FF)
            sh["wu%d%d" % (l, f)] = _tile_w_in(inp[nm + "_wu"][l], FF)
            wd = inp[nm + "_wd"][l]
            sh["wd%d%d" % (l, f)] = np.ascontiguousarray(wd.reshape(NFC, 128, 8, 128).transpose(2, 1, 0, 3))
    pv = np.zeros((128, NPV), np.float32)
    ng = inp["norm_g"].reshape(6, 8, 128)
    pv[:, PV_NORMG:PV_NORMG + 48] = ng.transpose(2, 0, 1).reshape(128, 48)
    pv[:, PV_FINALG:PV_FINALG + 8] = inp["final_g"].reshape(8, 128).T
    pv[:, PV_HGNW:PV_HGNW + 8] = inp["hg_norm_w"][0].reshape(8, 128).T
    pv[:, PV_LBZ:PV_LBZ + 16] = inp["hg_lb_logits"].reshape(2, 8, 128).transpose(2, 0, 1).reshape(128, 16)
    h64 = lambda v: np.asarray(v).reshape(8, 64).T
    mu = inp["rw_mu"][0]
    pv[0:64, PV_RW:PV_RW + 24] = mu[0:1536].reshape(24, 64).T
    pv[0:64, PV_RW + 24:PV_RW + 32] = h64(inp["rw_w0"][0])
    pv[0:64, PV_RW + 32:PV_RW + 40] = h64(inp["rw_a0"][0])
    pv[0:64, PV_RW + 40:PV_RW + 48] = h64(inp["rw_k_k"][0])
    pv[0:64, PV_RW + 48:PV_RW + 56] = h64(inp["rw_k_a"][0])
    pv[0:64, PV_RW + 64:PV_RW + 72] = h64(inp["rw_r_k"][0])
    pv[0:64, PV_RW + 72:PV_RW + 80] = h64(inp["rw_lnx_w"][0])
    pv[0:64, PV_RW + 80:PV_RW + 88] = h64(inp["rw_lnx_b"][0])
    pv[:, PV_RW + 88] = mu[1536:1664]
    pv[:, PV_RW + 89] = mu[1664:1792]
    wi = inp["ev_w_in"][0]
    sh["rwin"] = np.ascontiguousarray(wi[:, 0:1536].reshape(8, 128, 24, 64).transpose(2, 1, 0, 3))
    sh["rwlo"] = _tile_w_in(wi[:, 1536:1792], 256)
    sh["rww2"] = np.ascontiguousarray(inp["rw_w2"][0])
    sh["rwa2"] = np.ascontiguousarray(inp["rw_a2"][0])
    sh["rwg2"] = np.ascontiguousarray(inp["rw_g2"][0])
    wo_ = inp["ev_w_out"][0]
    sh["rwout"] = np.ascontiguousarray(wo_[0:512].reshape(8, 64, 8, 128).transpose(2, 1, 0, 3))
    sh["pvec"] = pv
    sh["odwin"] = _tile_w_in(inp["od_w_in"][0], 4096)
    sh["odwout"] = _tile_w_in(inp["od_w_out"][0], 1024)
    sh["mbin"] = _tile_w_in(wi[:, 1792:3328], 1536)
    sh["mbout"] = np.ascontiguousarray(wo_[512:1024].reshape(8, 64, 8, 128).transpose(2, 1, 0, 3))
    pos = np.arange(S, dtype=np.float32)
    kaug = np.zeros((10, S), np.float32)
    for j in range(8):
        kaug[j, j * 256:(j + 1) * 256] = 240000.0
    kaug[8] = pos
    kaug[9] = 1.0
    sh["mbkaug"] = kaug
    slopes = np.exp2(-np.arange(1, 9, dtype=np.float32))
    qc = np.zeros((8, 2, S), np.float32)
    qc[:, 0, :] = 8.0 * slopes[:, None]
    qc[:, 1, :] = -8.0 * slopes[:, None] * pos[None, :]
    sh["mbqc"] = qc
    return sh


_NC_CACHE = {}


def run(inputs, stage=ALL_STAGES, ncores=8, trace=False):
    stage = tuple(stage)
    import time
    t0 = time.time()
    inp = {k_: np.asarray(v, dtype=np.float32) for k_, v in inputs.items()}
    sh = _prep_shared(inp)
    t1 = time.time()
    if stage not in _NC_CACHE:
        _NC_CACHE[stage] = build(stage)
    t2 = time.time()
    print("[kernel] prep %.1fs build %.1fs" % (t1 - t0, t2 - t1), flush=True)
    nc = _NC_CACHE[stage]
    in_maps = []
    for b in range(ncores):
        m = dict(sh)
        m["xT"] = np.ascontiguousarray(inp["x"][b].T.reshape(8, 128, S).transpose(1, 0, 2))
        in_maps.append(m)
    t3 = time.time()
    res = run_bass_kernel_spmd(nc, in_maps, core_ids=list(range(ncores)), trace=trace)
    print("[kernel] run %.1fs" % (time.time() - t3), flush=True)
    outs = []
    for b in range(ncores):
        o = np.asarray(res.results[b]["outT"])
        outs.append(o.transpose(1, 0, 2).reshape(D, S).T)
    if "dbg" in stage:
        np.save("dbg_out.npy", np.asarray(res.results[0]["dbg"]))
    return np.stack(outs).astype(np.float32), res


def kernel(**inputs):
    out, _ = run(inputs)
    return out
```

```python
import numpy as np
from contextlib import ExitStack
import concourse.bass as bass
import concourse.mybir as mybir
from concourse.bass_utils import run_bass_kernel_spmd

F32 = mybir.dt.float32
F32R = mybir.dt.float32r
BF16 = mybir.dt.bfloat16
AF = mybir.ActivationFunctionType
ALU = mybir.AluOpType
AX = mybir.AxisListType

D = 1024
S = 2048
FF = 2816
NFC = 22
EPS = 1e-6


class Buf:
    __slots__ = ("lw", "rd", "name")

    def __init__(self, name=""):
        self.lw = None
        self.rd = []
        self.name = name


class Op:
    __slots__ = ("eng", "fn", "deps", "needed", "sem", "val", "is_dma", "prev_dma")

    def __init__(self, eng, fn):
        self.eng = eng
        self.fn = fn
        self.deps = []
        self.needed = False
        self.sem = None
        self.val = 0
        self.is_dma = False
        self.prev_dma = None


ENGS = ("pe", "dve", "act", "pool", "sp")


class Prog:
    NSLOT = 6

    def __init__(self):
        self.ops = {e: [] for e in ENGS}
        self.fence_deps = []
        self.last = {e: None for e in ENGS}
        self.dma_slots = {e: [] for e in ENGS}
        self.dma_count = {e: 0 for e in ENGS}
        self.all_dma_last = {}

    def _collect(self, op, R, W):
        deps = []
        for b in R:
            if b.lw is not None:
                deps.append(b.lw)
        for b in W:
            if b.lw is not None:
                deps.append(b.lw)
            deps.extend(b.rd)
        deps.extend(self.fence_deps)
        seen = set()
        for d in deps:
            if id(d) in seen or d is op:
                continue
            seen.add(id(d))
            if d.eng == "pe" and op.eng == "pe" and not d.is_dma and not op.is_dma:
                continue
            op.deps.append(d)
            d.needed = True
        for b in W:
            b.lw = op
            b.rd = []
        for b in R:
            b.rd.append(op)

    def op(self, eng, fn, R=(), W=()):
        o = Op(eng, fn)
        self._collect(o, R, W)
        self.ops[eng].append(o)
        self.last[eng] = o
        return o

    def dma(self, q, out, in_, R=(), W=()):
        o = Op(q, lambda e: e.dma_start(out=out, in_=in_))
        o.is_dma = True
        o.needed = True
        k = self.dma_count[q]
        self.dma_count[q] += 1
        slot = k % self.NSLOT
        o.sem = ("dma", q, slot)
        o.val = 16 * (k // self.NSLOT + 1)
        slots = self.dma_slots[q]
        if len(slots) <= slot:
            slots.append(None)
        o.prev_dma = slots[slot]
        slots[slot] = o
        self._collect(o, R, W)
        self.ops[q].append(o)
        self.all_dma_last[(q, slot)] = o
        return o

    def fence(self):
        deps = [o for o in self.last.values() if o is not None]
        deps += list(self.all_dma_last.values())
        for d in deps:
            d.needed = True
        self.fence_deps = deps

    def emit(self, nc, es, final_waits):
        sems = {}
        for e in ENGS:
            sems[("eng", e)] = es.enter_context(nc.semaphore("s_" + e))
            for sl in range(len(self.dma_slots[e])):
                sems[("dma", e, sl)] = es.enter_context(nc.semaphore("d_%s_%d" % (e, sl)))
        for e in ENGS:
            cnt = 0
            for o in self.ops[e]:
                if o.is_dma:
                    continue
                o.sem = ("eng", e)
                if o.needed:
                    cnt += 1
                    o.val = cnt
        handles = {"pe": "tensor", "dve": "vector", "act": "scalar", "pool": "gpsimd", "sp": "sync"}
        block = es.enter_context(nc.Block())

        def make(e):
            def body(eng):
                seen = {}
                for o in self.ops[e]:
                    waits = list(o.deps)
                    if o.is_dma and o.prev_dma is not None:
                        waits.append(o.prev_dma)
                    for d in waits:
                        if seen.get(d.sem, 0) < d.val:
                            eng.wait_ge(sems[d.sem], d.val)
                            seen[d.sem] = d.val
                    ins = o.fn(eng)
                    if o.is_dma:
                        ins.then_inc(sems[o.sem], 16)
                    elif o.needed:
                        ins.then_inc(sems[o.sem], 1)
                if e == "sp":
                    for d in final_waits:
                        if seen.get(d.sem, 0) < d.val:
                            eng.wait_ge(sems[d.sem], d.val)
                            seen[d.sem] = d.val
            return body

        for e in ENGS:
            getattr(block, handles[e])(make(e))


def r32(ap):
    return ap


class K:
    def __init__(self, stage):
        self.stage = stage
        self.nc = bass.Bass("TRN2", target_bir_lowering=False)
        self.P = Prog()
        self.es = ExitStack()
        self.wq = 0

    def dram_in(self, name, shape, dt=F32):
        return self.nc.dram_tensor(name, list(shape), dt, kind="ExternalInput").ap()

    def sb(self, name, shape, dt=F32):
        return self.es.enter_context(self.nc.sbuf_tensor(name, list(shape), dt))

    def ps(self, name, shape, dt=F32):
        return self.es.enter_context(self.nc.psum_tensor(name, list(shape), dt))


ALL_STAGES = ("f00", "rwkv", "moba", "f01", "f10", "hgrn", "f11", "final")


def build(stage=ALL_STAGES):
    k = K(stage)
    nc, P, es = k.nc, k.P, k.es
    with es:
        _build(k)
    return nc


def _build(k):
    nc, P = k.nc, k.P
    stage = k.stage
    xT_d = k.dram_in("xT", [128, 8, S])
    pv_d = k.dram_in("pvec", [128, NPV])
    wg_d = [[k.dram_in("wg%d%d" % (l, f), [NFC, 128, 8, 128]) for f in range(2)] for l in range(2)]
    wu_d = [[k.dram_in("wu%d%d" % (l, f), [NFC, 128, 8, 128]) for f in range(2)] for l in range(2)]
    wd_d = [[k.dram_in("wd%d%d" % (l, f), [8, 128, NFC, 128]) for f in range(2)] for l in range(2)]
    odwin_d = k.dram_in("odwin", [32, 128, 8, 128])
    odwout_d = k.dram_in("odwout", [8, 128, 8, 128])
    rwin_d = k.dram_in("rwin", [24, 128, 8, 64])
    rwlo_d = k.dram_in("rwlo", [2, 128, 8, 128])
    rww2_d = k.dram_in("rww2", [64, 512])
    rwa2_d = k.dram_in("rwa2", [64, 512])
    rwg2_d = k.dram_in("rwg2", [128, 512])
    rwout_d = k.dram_in("rwout", [8, 64, 8, 128])
    mbin_d = k.dram_in("mbin", [12, 128, 8, 128])
    mbout_d = k.dram_in("mbout", [8, 64, 8, 128])
    mbkaug_d = k.dram_in("mbkaug", [10, S])
    mbqc_d = k.dram_in("mbqc", [8, 2, S])
    mscr_d = nc.dram_tensor("mscr", [128, 8, S], F32).ap()
    mscr_b = [Buf() for g in range(8)]
    dbg_d = nc.dram_tensor("dbg", [64, 32, 1024], F32, kind="ExternalOutput").ap() if "dbg" in stage else None
    dbg_ops = []
    out_d = nc.dram_tensor("outT", [128, 8, S], F32, kind="ExternalOutput").ap()

    xT = k.sb("xT_sb", [128, 8, S])
    xb = [[Buf("x%d_%d" % (c, g)) for g in range(4)] for c in range(8)]
    pv = k.sb("pv_sb", [128, NPV])
    pvb = Buf("pv")
    ones = k.sb("ones", [128, 128])
    onesb = Buf("ones")
    epsb_t = k.sb("epsc", [128, 4])
    epsb = Buf("eps")
    ARENA = 32800
    arena = k.sb("arena", [128, ARENA])

    pbank = [k.ps("pb%d" % i, [128, 512]) for i in range(8)]
    pbb = [Buf("pb%d" % i) for i in range(8)]

    P.op("pool", lambda e: e.memset(ones[:], 1.0), W=[onesb])
    P.op("pool", lambda e: e.memset(epsb_t[:, 0:1], EPS), W=[epsb])
    P.dma("sp", pv[:], pv_d[:], W=[pvb])
    for c in range(8):
        for g in range(4):
            P.dma("sp", xT[:, c, g * 512:(g + 1) * 512], xT_d[:, c, g * 512:(g + 1) * 512], W=[xb[c][g]])

    ident = k.sb("ident", [128, 128])
    mask4t = k.sb("mask4", [128, 512])
    mask4 = mask4t[:].rearrange("p (c m) -> p c m", c=4)
    resetm = k.sb("resetm", [128, 512])
    lbt = k.sb("lbt", [128, 16])
    cb = Buf("consts")
    lbb = Buf("lb")

    def setup_consts(e):
        e.memset(ident[:], 0.0)
        e.affine_select(out=ident[:], in_=ones[:], pattern=[[1, 128]], compare_op=ALU.is_equal, fill=0.0, base=0, channel_multiplier=-1)
        for i in range(4):
            e.affine_select(out=mask4t[:, i * 128:(i + 1) * 128], in_=ones[:], pattern=[[1, 128]], compare_op=ALU.is_ge, fill=0.0,
                            base=0, channel_multiplier=-1)
            e.memset(mask4t[0:64, i * 128 + 64:(i + 1) * 128], 0.0)
        e.memset(resetm[:], 1.0)
        ins = None
        for i in range(8):
            ins = e.memset(resetm[:, i * 64:i * 64 + 1], 0.0)
        return ins
    P.op("pool", setup_consts, R=[onesb], W=[cb])
    P.op("dve", lambda e: e.tensor_tensor(out=lbt[:, 0:8], in0=pv[:, PV_LBZ + 8:PV_LBZ + 16], in1=pv[:, PV_LBZ:PV_LBZ + 8], op=ALU.subtract),
         R=[pvb], W=[lbb])
    P.op("act", lambda e: e.activation(out=lbt[:, 0:8], in_=lbt[:, 0:8], func=AF.Sigmoid), R=[lbb], W=[lbb])
    P.op("dve", lambda e: e.tensor_scalar(out=lbt[:, 8:16], in0=lbt[:, 0:8], scalar1=-1.0, scalar2=1.0, op0=ALU.mult, op1=ALU.add),
         R=[lbb], W=[lbb])

    mk = k.sb("rwmask", [64, 4 * 512])
    mLs = mk[:, 0:512].rearrange("p (h t) -> p h t", h=8)
    mUs = mk[:, 512:1024].rearrange("p (h t) -> p h t", h=8)
    mUi = mk[:, 1024:1536].rearrange("p (h t) -> p h t", h=8)
    id8 = mk[:, 1536:2048].rearrange("p (h t) -> p h t", h=8)
    omka = k.sb("omka", [64, 8])
    cb2 = Buf("consts2")

    def setup2(e):
        o3 = ones[0:64, :].rearrange("p (a b) -> p a b", a=2)
        e.memset(mk[:], 1.0)
        e.affine_select(out=mLs, in_=mLs, pattern=[[0, 8], [-1, 64]], compare_op=ALU.is_gt, fill=0.0, base=0, channel_multiplier=1)
        e.affine_select(out=mUs, in_=mUs, pattern=[[0, 8], [1, 64]], compare_op=ALU.is_gt, fill=0.0, base=0, channel_multiplier=-1)
        e.affine_select(out=mUi, in_=mUi, pattern=[[0, 8], [1, 64]], compare_op=ALU.is_ge, fill=0.0, base=0, channel_multiplier=-1)
        return e.affine_select(out=id8, in_=id8, pattern=[[0, 8], [1, 64]], compare_op=ALU.is_equal, fill=0.0, base=0, channel_multiplier=-1)
    P.op("pool", setup2, W=[cb2])
    P.op("dve", lambda e: e.tensor_scalar(out=omka[:], in0=pv[0:64, PV_RW + 48:PV_RW + 56], scalar1=-1.0, scalar2=1.0, op0=ALU.mult, op1=ALU.add),
         R=[pvb], W=[cb2])
    P.op("pool", lambda e: e.memset(epsb_t[:, 1:2], 64e-5), W=[epsb])

    def rstd_group(g, rstd_ap, rstd_buf, sq_ap, sq_buf, bank, ndiv=1024.0):
        P.op("act", lambda e: e.activation(out=sq_ap, in_=xT[:, :, g * 512:(g + 1) * 512], func=AF.Square),
             R=[xb[c][g] for c in range(8)], W=[sq_buf])

        def mm(e):
            ins = None
            for c in range(8):
                ins = e.matmul(pbank[bank][:], ones[:], sq_ap[:, c, :], start=(c == 0), stop=(c == 7))
            return ins
        P.op("pe", mm, R=[sq_buf, onesb], W=[pbb[bank]])
        P.op("act", lambda e: e.activation(out=rstd_ap, in_=pbank[bank][:], func=AF.Ln, bias=epsb_t[:, 0:1], scale=1.0 / ndiv),
             R=[pbb[bank], epsb], W=[rstd_buf])
        P.op("act", lambda e: e.activation(out=rstd_ap, in_=rstd_ap, func=AF.Exp, scale=-0.5),
             R=[rstd_buf], W=[rstd_buf])

    def ffn(l, which):
        f = 0 if which == 0 else 1
        gcol = PV_NORMG + (l * 3 + which) * 8
        A = Arena(arena, ARENA)
        hT = A.take(8192).bitcast(BF16).rearrange("p (c t) -> p c t", c=8)
        act = A.take(11264).bitcast(BF16).rearrange("p (c t) -> p c t", c=11)
        sq = arena[:, 8192:8192 + 4096].rearrange("p (c t) -> p c t", c=8)
        rstd = A.take(512)
        sg = [A.take(512) for i in range(2)]
        NW = 3
        wgb = [A.take(512).bitcast(BF16).rearrange("p (c m) -> p c m", c=8) for i in range(NW)]
        wub = [A.take(512).bitcast(BF16).rearrange("p (c m) -> p c m", c=8) for i in range(NW)]
        wdb = [A.take(704).bitcast(BF16).rearrange("p (c m) -> p c m", c=11) for i in range(2)]
        wgs = [A.take(1024).rearrange("p (c m) -> p c m", c=8) for i in range(2)]
        wus = [A.take(1024).rearrange("p (c m) -> p c m", c=8) for i in range(2)]
        wds = [A.take(1408).rearrange("p (c m) -> p c m", c=11) for i in range(2)]
        wgsb = [Buf(), Buf()]; wusb = [Buf(), Buf()]; wdsb = [Buf(), Buf()]
        hb = [[Buf() for g in range(4)] for c in range(8)]
        actb = [[Buf() for g in range(4)] for c in range(11)]
        sqb, rstdb = Buf(), Buf()
        sgb = [Buf(), Buf()]
        wgbb = [Buf() for i in range(NW)]
        wubb = [Buf() for i in range(NW)]
        wdbb = [Buf(), Buf()]
        cnt = {"w": 0, "wd": 0, "p": 0, "s": 0, "ws": 0}
        for g in range(4):
            rstd_group(g, rstd, rstdb, sq, sqb, 7)
            for c in range(8):
                P.op("dve", lambda e, c=c, g=g: e.scalar_tensor_tensor(
                    out=hT[:, c, g * 512:(g + 1) * 512], in0=xT[:, c, g * 512:(g + 1) * 512],
                    scalar=pv[:, gcol + c:gcol + c + 1], in1=rstd, op0=ALU.mult, op1=ALU.mult),
                    R=[xb[c][g], rstdb, pvb], W=[hb[c][g]])
        P.fence()

        def phase_a(fc, fl):
            s = cnt["w"] % NW
            cnt["w"] += 1
            ss_ = cnt["ws"] % 2
            cnt["ws"] += 1
            P.dma("sp", wgs[ss_], wg_d[l][f][fc], W=[wgsb[ss_]])
            P.dma("sp", wus[ss_], wu_d[l][f][fc], W=[wusb[ss_]])
            P.op("pool", lambda e: e.tensor_copy(out=wgb[s], in_=wgs[ss_]), R=[wgsb[ss_]], W=[wgbb[s]])
            P.op("pool", lambda e: e.tensor_copy(out=wub[s], in_=wus[ss_]), R=[wusb[ss_]], W=[wubb[s]])
            def a_group(g):
                bg, bu = (cnt["p"] % 2) * 2, (cnt["p"] % 2) * 2 + 1
                cnt["p"] += 1
                si = cnt["s"] % 2
                cnt["s"] += 1

                def mmg(e):
                    ins = None
                    for c in range(8):
                        ins = e.matmul(pbank[bg][:], wgb[s][:, c, :], hT[:, c, g * 512:(g + 1) * 512], start=(c == 0), stop=(c == 7))
                    return ins

                def mmu(e):
                    ins = None
                    for c in range(8):
                        ins = e.matmul(pbank[bu][:], wub[s][:, c, :], hT[:, c, g * 512:(g + 1) * 512], start=(c == 0), stop=(c == 7))
                    return ins
                P.op("pe", mmg, R=[wgbb[s]] + [hb[c][g] for c in range(8)], W=[pbb[bg]])
                P.op("pe", mmu, R=[wubb[s]] + [hb[c][g] for c in range(8)], W=[pbb[bu]])
                P.op("act", lambda e: e.activation(out=sg[si], in_=pbank[bg][:], func=AF.Silu), R=[pbb[bg]], W=[sgb[si]])
                P.op("dve", lambda e: e.tensor_tensor(out=act[:, fl, g * 512:(g + 1) * 512], in0=pbank[bu][:], in1=sg[si], op=ALU.mult),
                     R=[pbb[bu], sgb[si]], W=[actb[fl][g]])
            for g_ in range(4):
                a_group(g_)

        def phase_b(fh, dc):
            s = cnt["wd"] % 2
            cnt["wd"] += 1
            P.dma("sp", wds[s], wd_d[l][f][dc][:, fh * 11:(fh + 1) * 11, :], W=[wdsb[s]])
            P.op("pool", lambda e: e.tensor_copy(out=wdb[s], in_=wds[s]), R=[wdsb[s]], W=[wdbb[s]])
            def b_group(g):
                bo = 4 + (cnt["p"] % 2)
                cnt["p"] += 1

                def mmd(e):
                    ins = None
                    for fl in range(11):
                        ins = e.matmul(pbank[bo][:], wdb[s][:, fl, :], act[:, fl, g * 512:(g + 1) * 512], start=(fl == 0), stop=(fl == 10))
                    return ins
                P.op("pe", mmd, R=[wdbb[s]] + [actb[fl][g] for fl in range(11)], W=[pbb[bo]])
                P.op("dve", lambda e: e.scalar_tensor_tensor(
                    out=xT[:, dc, g * 512:(g + 1) * 512], in0=pbank[bo][:], scalar=0.5,
                    in1=xT[:, dc, g * 512:(g + 1) * 512], op0=ALU.mult, op1=ALU.add),
                    R=[pbb[bo], xb[dc][g]], W=[xb[dc][g]])
            for g_ in range(4):
                b_group(g_)
        for fh in range(2):
            for fl in range(11):
                phase_a(fh * 11 + fl, fl)
            for dc in range(8):
                phase_b(fh, dc)
        P.fence()

    def final_out(norm):
        o = 0
        sq = arena[:, o:o + 4096].rearrange("p (c t) -> p c t", c=8); o += 4096
        rstd = arena[:, o:o + 512]; o += 512
        ob = [arena[:, o + i * 4096:o + (i + 1) * 4096].rearrange("p (c t) -> p c t", c=8) for i in range(2)]; o += 8192
        sqb, rstdb = Buf(), Buf()
        obb = [Buf(), Buf()]
        outs = []
        for g in range(4):
            s = g % 2
            if norm:
                rstd_group(g, rstd, rstdb, sq, sqb, 7)
                for c in range(8):
                    P.op("dve", lambda e, c=c, g=g, s=s: e.scalar_tensor_tensor(
                        out=ob[s][:, c, :], in0=xT[:, c, g * 512:(g + 1) * 512],
                        scalar=pv[:, PV_FINALG + c:PV_FINALG + c + 1], in1=rstd, op0=ALU.mult, op1=ALU.mult),
                        R=[xb[c][g], rstdb, pvb], W=[obb[s]])
                outs.append(P.dma("sp", out_d[:, :, g * 512:(g + 1) * 512], ob[s], R=[obb[s]]))
            else:
                outs.append(P.dma("sp", out_d[:, :, g * 512:(g + 1) * 512], xT[:, :, g * 512:(g + 1) * 512],
                                  R=[xb[c][g] for c in range(8)]))
        return outs


    def hgrn():
        A = Arena(arena, ARENA)
        hT = A.take(2048).bitcast(BF16).rearrange("p (c t) -> p c t", c=8)
        hb = [Buf() for c in range(8)]
        wts = [A.take(1024).rearrange("p (c m) -> p c m", c=8) for j in range(4)]
        wtsb = [Buf() for j in range(4)]
        wt = [[A.take(512).bitcast(BF16).rearrange("p (c m) -> p c m", c=8) for j in range(4)] for s_ in range(2)]
        wtb = [[Buf() for j in range(4)] for s_ in range(2)]
        names = ["qT", "fT", "lf", "kT", "bT", "eb", "e2", "oTs", "sqo", "rs2", "tmp"]
        T = {n: A.take(512) for n in names}
        TB = {n: Buf(n) for n in names}
        DB = []
        for i in range(2):
            d = {}
            for n in ("qe", "ke", "sgT"):
                d[n] = A.take(512); d[n + "b"] = Buf()
            for n in ("ke2tm", "vtm", "scs"):
                d[n] = A.take(512).rearrange("p (c m) -> p c m", c=4); d[n + "b"] = Buf()
            d["ebl"] = A.take(8); d["eblb"] = Buf()
            DB.append(d)
        state = [A.take(1024).rearrange("p (h v) -> p h v", h=8) for i in range(2)]
        stb = [[Buf() for h in range(8)] for i in range(2)]
        yT = A.take(2048).bitcast(BF16).rearrange("p (c t) -> p c t", c=8)
        yb = [Buf() for h in range(8)]
        wos = [A.take(1024).rearrange("p (c m) -> p c m", c=8) for i in range(1)]
        wosb = [Buf()]
        wo = [A.take(512).bitcast(BF16).rearrange("p (c m) -> p c m", c=8) for i in range(2)]
        wob = [Buf(), Buf()]
        sq = A.take(4096).rearrange("p (c t) -> p c t", c=8); sqb = Buf()
        rstd = A.take(512); rstdb = Buf()
        gcol = PV_NORMG + (1 * 3 + 1) * 8
        scur = [0] * 8
        cnt = {"wo": 0}
        for h in range(8):
            P.op("pool", lambda e, h=h: e.memset(state[0][:, h, :], 0.0), W=[stb[0][h]])

        def norm(g):
            rstd_group(g, rstd, rstdb, sq, sqb, 7)

            def hn(e):
                ins = None
                for c in range(8):
                    ins = e.scalar_tensor_tensor(out=hT[:, c, :], in0=xT[:, c, g * 512:(g + 1) * 512],
                                                 scalar=pv[:, gcol + c:gcol + c + 1], in1=rstd, op0=ALU.mult, op1=ALU.mult)
                return ins
            P.op("dve", hn, R=[xb[c][g] for c in range(8)] + [rstdb, pvb], W=hb)

        def prep(g, h, s_):
            d = DB[s_]
            for j in range(4):
                P.dma("sp", wts[j], odwin_d[j * 8 + h], W=[wtsb[j]])
                P.op("pool", lambda e, j=j: e.tensor_copy(out=wt[s_][j], in_=wts[j]), R=[wtsb[j]], W=[wtb[s_][j]])
            yield

            def proj(j, bank):
                def mm(e):
                    ins = None
                    for c in range(8):
                        ins = e.matmul(pbank[bank][:], wt[s_][j][:, c, :], hT[:, c, :], start=(c == 0), stop=(c == 7))
                    return ins
                P.op("pe", mm, R=[wtb[s_][j]] + hb, W=[pbb[bank]])
            proj(0, 0)
            P.op("act", lambda e: e.activation(out=T["qT"], in_=pbank[0][:], func=AF.Copy), R=[pbb[0]], W=[TB["qT"]])
            yield
            proj(1, 1)
            P.op("act", lambda e: e.activation(out=T["fT"], in_=pbank[1][:], func=AF.Sigmoid), R=[pbb[1]], W=[TB["fT"]])
            yield
            P.op("dve", lambda e: e.tensor_scalar(out=T["fT"], in0=T["fT"], scalar1=lbt[:, 8 + h:9 + h], scalar2=lbt[:, h:h + 1],
                                                  op0=ALU.mult, op1=ALU.add), R=[TB["fT"], lbb], W=[TB["fT"]])
            yield
            P.op("act", lambda e: e.activation(out=T["lf"], in_=T["fT"], func=AF.Ln), R=[TB["fT"]], W=[TB["lf"]])
            P.op("dve", lambda e: e.tensor_scalar(out=T["kT"], in0=T["fT"], scalar1=-1.0, scalar2=1.0, op0=ALU.mult, op1=ALU.add),
                 R=[TB["fT"]], W=[TB["kT"]])
            yield
            P.op("dve", lambda e: e.tensor_tensor_scan(out=T["bT"], data0=resetm[:], data1=T["lf"], initial=0.0, op0=ALU.mult, op1=ALU.add),
                 R=[TB["lf"], cb], W=[TB["bT"]])
            yield
            P.op("act", lambda e: e.activation(out=T["eb"], in_=T["bT"], func=AF.Exp), R=[TB["bT"]], W=[TB["eb"]])
            yield
            P.op("dve", lambda e: e.tensor_tensor(out=d["qe"], in0=T["qT"], in1=T["eb"], op=ALU.mult), R=[TB["qT"], TB["eb"]], W=[d["qeb"]])
            yield
            P.op("act", lambda e: e.activation(out=T["eb"], in_=T["bT"], func=AF.Exp, scale=-1.0), R=[TB["bT"], d["qeb"]], W=[TB["eb"]])
            yield
            P.op("dve", lambda e: e.tensor_tensor(out=d["ke"], in0=T["kT"], in1=T["eb"], op=ALU.mult), R=[TB["kT"], TB["eb"]], W=[d["keb"]])
            yield

            def e2f(e):
                ins = None
                for ci in range(8):
                    ins = e.activation(out=T["e2"][:, ci * 64:(ci + 1) * 64], in_=T["bT"][:, ci * 64:(ci + 1) * 64], func=AF.Exp,
                                       scale=-1.0, bias=T["bT"][:, ci * 64 + 63:ci * 64 + 64])
                return ins
            P.op("act", e2f, R=[TB["bT"]], W=[TB["e2"]])
            yield
            P.op("dve", lambda e: e.tensor_tensor(out=T["e2"], in0=T["e2"], in1=T["kT"], op=ALU.mult), R=[TB["kT"], TB["e2"]], W=[TB["e2"]])
            P.op("act", lambda e: e.activation(out=d["ebl"], in_=T["bT"].rearrange("p (c t) -> p c t", t=64)[:, :, 63], func=AF.Exp),
                 R=[TB["bT"]], W=[d["eblb"]])
            yield

            def tr(e):
                ins = None
                for tt in range(4):
                    ins = e.transpose(pbank[2][:, tt * 128:(tt + 1) * 128], T["e2"][:, tt * 128:(tt + 1) * 128], ident[:])
                return ins
            P.op("pe", tr, R=[TB["e2"], cb], W=[pbb[2]])
            P.op("act", lambda e: e.activation(out=d["ke2tm"], in_=pbank[2][:].rearrange("p (c m) -> p c m", c=4), func=AF.Copy),
                 R=[pbb[2]], W=[d["ke2tmb"]])
            yield

            def vproj(e):
                ins = None
                for tt in range(4):
                    for c in range(8):
                        ins = e.matmul(pbank[3][:, tt * 128:(tt + 1) * 128], hT[:, c, tt * 128:(tt + 1) * 128], wt[s_][2][:, c, :],
                                       start=(c == 0), stop=(c == 7))
                return ins
            P.op("pe", vproj, R=[wtb[s_][2]] + hb, W=[pbb[3]])
            P.op("dve", lambda e: e.tensor_copy(out=d["vtm"], in_=pbank[3][:].rearrange("p (c m) -> p c m", c=4)), R=[pbb[3]], W=[d["vtmb"]])
            yield
            proj(3, 0)
            P.op("act", lambda e: e.activation(out=d["sgT"], in_=pbank[0][:], func=AF.Sigmoid), R=[pbb[0]], W=[d["sgTb"]])
            yield

            def scm(e):
                ins = None
                for p_ in range(4):
                    ins = e.matmul(pbank[4][:, p_ * 128:(p_ + 1) * 128], d["ke"][:, p_ * 128:(p_ + 1) * 128],
                                   d["qe"][:, p_ * 128:(p_ + 1) * 128], start=True, stop=True)
                return ins
            P.op("pe", scm, R=[d["keb"], d["qeb"]], W=[pbb[4]])
            P.op("dve", lambda e: e.tensor_tensor(out=d["scs"], in0=pbank[4][:].rearrange("p (c m) -> p c m", c=4), in1=mask4, op=ALU.mult),
                 R=[pbb[4], cb], W=[d["scsb"]])
            yield

        def chunks(g, h, s_):
            d = DB[s_]

            def one(p_, half):
                ci = p_ * 2 + half
                sc = scur[h]
                cs = p_ * 128 + half * 64
                r0 = half * 64

                def omm(e):
                    if half == 0:
                        e.matmul(pbank[5][:, p_ * 128:(p_ + 1) * 128], d["vtm"][:, p_, :], d["scs"][:, p_, :], start=True, stop=False)
                    return e.matmul(pbank[5][:, cs:cs + 64], state[sc][:, h, :], d["qe"][:, cs:cs + 64], start=False, stop=(half == 1))
                P.op("pe", omm, R=[d["vtmb"], d["scsb"], stb[sc][h], d["qeb"]], W=[pbb[5]])
                P.op("pe", lambda e: e.matmul(pbank[6][:, 0:128], d["ke2tm"][r0:r0 + 64, p_, :], d["vtm"][r0:r0 + 64, p_, :],
                                              start=True, stop=True), R=[d["ke2tmb"], d["vtmb"]], W=[pbb[6]])
                P.op("dve", lambda e: e.scalar_tensor_tensor(
                    out=state[1 - sc][:, h, :], in0=state[sc][:, h, :], scalar=d["ebl"][:, ci:ci + 1], in1=pbank[6][:, 0:128],
                    op0=ALU.mult, op1=ALU.add), R=[stb[sc][h], d["eblb"], pbb[6]], W=[stb[1 - sc][h]])
                scur[h] = 1 - sc
            for p_ in range(4):
                for half in range(2):
                    one(p_, half)
                    yield
            P.op("act", lambda e: e.activation(out=T["oTs"], in_=pbank[5][:], func=AF.Copy), R=[pbb[5]], W=[TB["oTs"]])
            P.op("act", lambda e: e.activation(out=T["sqo"], in_=pbank[5][:], func=AF.Square), R=[pbb[5]], W=[TB["sqo"]])
            yield
            P.op("pe", lambda e: e.matmul(pbank[7][:], ones[:], T["sqo"], start=True, stop=True), R=[TB["sqo"], onesb], W=[pbb[7]])
            P.op("act", lambda e: e.activation(out=T["rs2"], in_=pbank[7][:], func=AF.Ln, bias=epsb_t[:, 0:1], scale=1.0 / 128.0),
                 R=[pbb[7], epsb], W=[TB["rs2"]])
            yield
            P.op("act", lambda e: e.activation(out=T["rs2"], in_=T["rs2"], func=AF.Exp, scale=-0.5), R=[TB["rs2"]], W=[TB["rs2"]])
            yield
            P.op("dve", lambda e: e.scalar_tensor_tensor(out=T["tmp"], in0=T["oTs"], scalar=pv[:, PV_HGNW + h:PV_HGNW + h + 1],
                                                         in1=T["rs2"], op0=ALU.mult, op1=ALU.mult),
                 R=[TB["oTs"], TB["rs2"], pvb], W=[TB["tmp"]])
            yield
            P.op("dve", lambda e: e.tensor_tensor(out=yT[:, h, :], in0=T["tmp"], in1=d["sgT"], op=ALU.mult),
                 R=[TB["tmp"], d["sgTb"]], W=[yb[h]])
            yield

        def outproj(g):
            for dc in range(8):
                s2 = cnt["wo"] % 2
                cnt["wo"] += 1
                P.dma("sp", wos[0], odwout_d[dc], W=[wosb[0]])
                P.op("pool", lambda e, s2=s2: e.tensor_copy(out=wo[s2], in_=wos[0]), R=[wosb[0]], W=[wob[s2]])

                def mo(e, s2=s2):
                    ins = None
                    for h in range(8):
                        ins = e.matmul(pbank[7][:], wo[s2][:, h, :], yT[:, h, :], start=(h == 0), stop=(h == 7))
                    return ins
                P.op("pe", mo, R=[wob[s2]] + yb, W=[pbb[7]])
                P.op("dve", lambda e, dc=dc: e.tensor_tensor(out=xT[:, dc, g * 512:(g + 1) * 512], in0=pbank[7][:],
                                                             in1=xT[:, dc, g * 512:(g + 1) * 512], op=ALU.add),
                     R=[pbb[7], xb[dc][g]], W=[xb[dc][g]])

        def interleave(a_, b_):
            gens = [x for x in (a_, b_) if x is not None]
            while gens:
                for x in list(gens):
                    try:
                        next(x)
                    except StopIteration:
                        gens.remove(x)
        prev = None
        prev_g = None
        idx = 0
        for g in range(4):
            for h in range(8):
                if h == 0:
                    norm(g)
                interleave(prev, prep(g, h, idx % 2))
                if prev is not None and h == 0:
                    outproj(prev_g)
                prev = chunks(g, h, idx % 2)
                prev_g = g
                idx += 1
        interleave(prev, None)
        outproj(3)
        P.fence()

    def rwkv():
        G = 128
        NG = S // G
        A = Arena(arena, ARENA)
        hT = A.take(4 * (G + 2)).bitcast(BF16).rearrange("p (c t) -> p c t", c=8); hb = Buf()
        sq = A.take(8 * G).rearrange("p (c t) -> p c t", c=8); sqb = Buf()
        rstd = A.take(G); rstdb = Buf()
        wrkvs = [A.take(512).rearrange("p (c m) -> p c m", c=8) for i in range(3)]
        wrkvsb = [Buf() for i in range(3)]
        wrkv = [[A.take(256).bitcast(BF16).rearrange("p (c m) -> p c m", c=8) for i in range(3)] for s_ in range(2)]
        wrkvb = [[Buf() for i in range(3)] for s_ in range(2)]
        wlos = A.take(1024).rearrange("p (c m) -> p c m", c=8); wlosb = Buf()
        wlo = [A.take(512).bitcast(BF16).rearrange("p (c m) -> p c m", c=8) for i in range(2)]
        wlob = [Buf(), Buf()]
        yfh = A.take(512).bitcast(BF16).rearrange("p (h t) -> p h t", h=8); yfhb = Buf()
        w2a2 = A.take(512); g2 = A.take(512); wconst = Buf()
        praw = [A.take(G + 1) for i in range(2)]; prawb = [Buf(), Buf()]
        dtmp = [A.take(G) for i in range(2)]; dtmpb = [Buf(), Buf()]
        tw = A.take(G); twb = Buf()
        sg = A.take(G); sgb = Buf()
        QN = ["r", "k", "v", "lw", "a", "kk", "Lw", "e", "at", "bt", "kt", "BhT", "KhT"]
        Q = {n: A.take(8 * G).rearrange("p (h t) -> p h t", h=8) for n in QN}
        QB = {n: Buf(n) for n in QN}
        HO_ = ("TT", "AakT", "Vtm")
        CN = ["P0", "P1", "PT0", "PT1", "Xs", "Us", "H0", "H1", "ArbT", "ArkT", "Bhtm", "Khtm"] + [n + "0" for n in HO_] + [n + "1" for n in HO_]
        Cc = {n: A.take(512).rearrange("p (h t) -> p h t", h=8) for n in CN}
        CB = {n: Buf(n) for n in CN}
        gC = A.take(16).rearrange("p (h c) -> p h c", h=8); gCb = Buf()
        wos = wlos.rearrange("p c m -> p (c m)").rearrange("p (h m) -> p h m", h=8); wosb = wlosb
        wo = [A.take(512).bitcast(BF16).rearrange("p (h m) -> p h m", h=8) for i in range(2)]; wob = [Buf(), Buf()]
        gcol = PV_NORMG + (0 * 3 + 1) * 8
        MU, W0, A0, KK, KA, OMKA, RK, LNW, LNB = PV_RW, PV_RW + 24, PV_RW + 32, PV_RW + 40, PV_RW + 48, PV_RW + 56, PV_RW + 64, PV_RW + 72, PV_RW + 80
        MUWA, MUG = PV_RW + 88, PV_RW + 89
        o64 = ones[0:64, 0:64]

        P.dma("sp", w2a2[0:64, :], rww2_d[:, :], W=[wconst])
        P.dma("sp", w2a2[64:128, :], rwa2_d[:, :], W=[wconst])
        P.dma("sp", g2, rwg2_d[:, :], W=[wconst])
        P.op("pool", lambda e: e.memset(Cc["H0"][0:64], 0.0), W=[CB["H0"]])
        P.op("pool", lambda e: e.memset(hT[:, :, 0:1], 0.0), W=[hb])
        hcur = 0
        woi = 0
        pi = 0
        def do_group(g):
            nonlocal hcur, woi, pi
            t0 = g * G
            g4 = t0 // 512
            xg = [xb[c][g4] for c in range(8)]
            if g > 0:
                P.op("dve", lambda e: e.tensor_copy(out=hT[:, :, 0:1], in_=hT[:, :, G:G + 1]), R=[hb], W=[hb])
            P.op("act", lambda e, t0=t0: e.activation(out=sq, in_=xT[:, :, t0:t0 + G], func=AF.Square), R=xg, W=[sqb])

            def mmn(e):
                ins = None
                for c in range(8):
                    ins = e.matmul(pbank[7][:, 0:G], ones[:], sq[:, c, :], start=(c == 0), stop=(c == 7))
                return ins
            P.op("pe", mmn, R=[sqb, onesb], W=[pbb[7]])
            P.op("act", lambda e: e.activation(out=rstd, in_=pbank[7][:, 0:G], func=AF.Ln, bias=epsb_t[:, 0:1], scale=1.0 / 1024.0),
                 R=[pbb[7], epsb], W=[rstdb])
            P.op("act", lambda e: e.activation(out=rstd, in_=rstd, func=AF.Exp, scale=-0.5), R=[rstdb], W=[rstdb])

            def hnorm(e, t0=t0):
                ins = None
                for c in range(8):
                    ins = e.scalar_tensor_tensor(out=hT[:, c, 1:G + 1], in0=xT[:, c, t0:t0 + G], scalar=pv[:, gcol + c:gcol + c + 1],
                                                 in1=rstd, op0=ALU.mult, op1=ALU.mult)
                return ins
            P.op("dve", hnorm, R=xg + [rstdb, pvb, hb], W=[hb])
            for h in range(8):
                for qi, qn in enumerate(("r", "k", "v")):
                    P.dma("sp", wrkvs[qi], rwin_d[qi * 8 + h], W=[wrkvsb[qi]])
                    ws_ = h % 2
                    P.op("pool", lambda e, qi=qi, ws_=ws_: e.tensor_copy(out=wrkv[ws_][qi], in_=wrkvs[qi]), R=[wrkvsb[qi]], W=[wrkvb[ws_][qi]])
                    bank = pi % 2
                    sl = pi % 2
                    pi += 1

                    def mm(e, qi=qi, bank=bank, ws_=ws_):
                        ins = None
                        for c in range(8):
                            ins = e.matmul(pbank[bank][0:64, 0:G + 1], wrkv[ws_][qi][:, c, :], hT[:, c, 0:G + 1], start=(c == 0), stop=(c == 7))
                        return ins
                    P.op("pe", mm, R=[wrkvb[ws_][qi], hb], W=[pbb[bank]])
                    P.op("act", lambda e, bank=bank, sl=sl: e.activation(out=praw[sl][0:64, :], in_=pbank[bank][0:64, 0:G + 1], func=AF.Copy),
                         R=[pbb[bank]], W=[prawb[sl]])
                    P.op("dve", lambda e, sl=sl: e.tensor_tensor(out=dtmp[sl][0:64, :], in0=praw[sl][0:64, 0:G], in1=praw[sl][0:64, 1:G + 1],
                                                                 op=ALU.subtract), R=[prawb[sl]], W=[dtmpb[sl]])
                    P.op("dve", lambda e, sl=sl, qn=qn, qi=qi, h=h: e.scalar_tensor_tensor(
                        out=Q[qn][0:64, h, :], in0=dtmp[sl][0:64, :], scalar=pv[0:64, MU + qi * 8 + h:MU + qi * 8 + h + 1],
                        in1=praw[sl][0:64, 1:G + 1], op0=ALU.mult, op1=ALU.add), R=[dtmpb[sl], prawb[sl], pvb], W=[QB[qn]])
            for j in range(2):
                P.dma("sp", wlos, rwlo_d[j], W=[wlosb])
                P.op("pool", lambda e, j=j: e.tensor_copy(out=wlo[j], in_=wlos), R=[wlosb], W=[wlob[j]])
                bank = pi % 2
                sl = pi % 2
                pi += 1

                def mml(e, j=j, bank=bank):
                    ins = None
                    for c in range(8):
                        ins = e.matmul(pbank[bank][:, 0:G + 1], wlo[j][:, c, :], hT[:, c, 0:G + 1], start=(c == 0), stop=(c == 7))
                    return ins
                P.op("pe", mml, R=[wlob[j], hb], W=[pbb[bank]])
                P.op("act", lambda e, bank=bank, sl=sl: e.activation(out=praw[sl], in_=pbank[bank][:, 0:G + 1], func=AF.Copy),
                     R=[pbb[bank]], W=[prawb[sl]])
                P.op("dve", lambda e, sl=sl: e.tensor_tensor(out=dtmp[sl], in0=praw[sl][:, 0:G], in1=praw[sl][:, 1:G + 1], op=ALU.subtract),
                     R=[prawb[sl]], W=[dtmpb[sl]])
                dst, dstb = (tw, twb) if j == 0 else (sg, sgb)
                mcol = MUWA if j == 0 else MUG
                P.op("dve", lambda e, sl=sl, dst=dst, mcol=mcol: e.scalar_tensor_tensor(
                    out=dst, in0=dtmp[sl], scalar=pv[:, mcol:mcol + 1], in1=praw[sl][:, 1:G + 1], op0=ALU.mult, op1=ALU.add),
                    R=[dtmpb[sl], prawb[sl], pvb], W=[dstb])
            P.op("act", lambda e: e.activation(out=tw[0:64, :], in_=tw[0:64, :], func=AF.Tanh), R=[twb], W=[twb])
            P.op("act", lambda e: e.activation(out=sg, in_=sg, func=AF.Sigmoid), R=[sgb], W=[sgb])
            for half in range(2):
                def mmw(e, half=half):
                    ins = None
                    for hh in range(4):
                        h = half * 4 + hh
                        ins = e.matmul(pbank[2][0:64, hh * G:(hh + 1) * G], w2a2[0:64, h * 64:(h + 1) * 64], tw[0:64, :], start=True, stop=True)
                    return ins
                P.op("pe", mmw, R=[wconst, twb], W=[pbb[2]])

                def sw(e, half=half):
                    ins = None
                    for hh in range(4):
                        h = half * 4 + hh
                        ins = e.activation(out=Q["lw"][0:64, h, :], in_=pbank[2][0:64, hh * G:(hh + 1) * G], func=AF.Sigmoid,
                                           bias=pv[0:64, W0 + h:W0 + h + 1])
                    return ins
                P.op("act", sw, R=[pbb[2], pvb], W=[QB["lw"]])

                def mma(e, half=half):
                    ins = None
                    for hh in range(4):
                        h = half * 4 + hh
                        ins = e.matmul(pbank[3][0:64, hh * G:(hh + 1) * G], w2a2[64:128, h * 64:(h + 1) * 64], tw[64:128, :], start=True, stop=True)
                    return ins
                P.op("pe", mma, R=[wconst, twb], W=[pbb[3]])

                def sa(e, half=half):
                    ins = None
                    for hh in range(4):
                        h = half * 4 + hh
                        ins = e.activation(out=Q["a"][0:64, h, :], in_=pbank[3][0:64, hh * G:(hh + 1) * G], func=AF.Sigmoid,
                                           bias=pv[0:64, A0 + h:A0 + h + 1])
                    return ins
                P.op("act", sa, R=[pbb[3], pvb], W=[QB["a"]])
            P.op("dve", lambda e: e.tensor_scalar(out=Q["lw"][0:64], in0=Q["lw"][0:64], scalar1=-0.6065306597126334, scalar2=None, op0=ALU.mult),
                 R=[QB["lw"]], W=[QB["lw"]])
            def kk1(e):
                ins = None
                for h in range(8):
                    ins = e.tensor_scalar(out=Q["kk"][0:64, h, :], in0=Q["k"][0:64, h, :], scalar1=pv[0:64, KK + h:KK + h + 1], scalar2=None, op0=ALU.mult)
                return ins
            P.op("dve", kk1, R=[QB["k"], pvb], W=[QB["kk"]])
            P.op("act", lambda e: e.activation(out=Q["e"][0:64], in_=Q["kk"][0:64], func=AF.Square), R=[QB["kk"]], W=[QB["e"]])
            for half in range(2):
                P.op("pe", lambda e, half=half: e.matmul(pbank[2][0:64, :], o64, Q["e"][0:64, half * 4:(half + 1) * 4, :], start=True, stop=True),
                     R=[QB["e"], onesb], W=[pbb[2]])
                P.op("dve", lambda e, half=half: e.tensor_scalar(out=Q["e"][0:64, half * 4:(half + 1) * 4, :],
                                                                 in0=pbank[2][0:64, :].rearrange("p (h t) -> p h t", h=4),
                                                                 scalar1=1e-24, scalar2=None, op0=ALU.max), R=[pbb[2], QB["e"]], W=[QB["e"]])
            P.op("act", lambda e: e.activation(out=Q["e"][0:64], in_=Q["e"][0:64], func=AF.Ln), R=[QB["e"]], W=[QB["e"]])
            P.op("act", lambda e: e.activation(out=Q["e"][0:64], in_=Q["e"][0:64], func=AF.Exp, scale=-0.5), R=[QB["e"]], W=[QB["e"]])
            P.op("dve", lambda e: e.tensor_tensor(out=Q["kk"][0:64], in0=Q["kk"][0:64], in1=Q["e"][0:64], op=ALU.mult),
                 R=[QB["kk"], QB["e"]], W=[QB["kk"]])
            def km1(e):
                ins = None
                for h in range(8):
                    ins = e.tensor_scalar(out=Q["e"][0:64, h, :], in0=Q["a"][0:64, h, :], scalar1=pv[0:64, KA + h:KA + h + 1],
                                          scalar2=omka[0:64, h:h + 1], op0=ALU.mult, op1=ALU.add)
                return ins
            P.op("dve", km1, R=[QB["a"], pvb, cb2], W=[QB["e"]])
            P.op("dve", lambda e: e.tensor_tensor(out=Q["k"][0:64], in0=Q["k"][0:64], in1=Q["e"][0:64], op=ALU.mult),
                 R=[QB["k"], QB["e"]], W=[QB["k"]])
            P.op("dve", lambda e: e.tensor_tensor(out=Q["a"][0:64], in0=Q["a"][0:64], in1=Q["kk"][0:64], op=ALU.mult),
                 R=[QB["a"], QB["kk"]], W=[QB["a"]])
            def bn1(e):
                ins = None
                for h in range(8):
                    ins = e.scalar_tensor_tensor(out=Q["e"][0:64, h, :], in0=Q["r"][0:64, h, :], scalar=pv[0:64, RK + h:RK + h + 1],
                                                 in1=Q["k"][0:64, h, :], op0=ALU.mult, op1=ALU.mult)
                return ins
            P.op("dve", bn1, R=[QB["r"], QB["k"], pvb], W=[QB["e"]])
            def scn(e):
                ins = None
                for h in range(8):
                    ins = e.tensor_tensor_scan(out=Q["Lw"][0:64, h, :], data0=resetm[0:64, 0:G], data1=Q["lw"][0:64, h, :], initial=0.0,
                                               op0=ALU.mult, op1=ALU.add)
                return ins
            P.op("dve", scn, R=[QB["lw"], cb], W=[QB["Lw"]])
            P.op("dve", lambda e: e.tensor_tensor(out=Q["lw"][0:64], in0=Q["Lw"][0:64], in1=Q["lw"][0:64], op=ALU.subtract),
                 R=[QB["Lw"], QB["lw"]], W=[QB["lw"]])
            for half in range(2):
                P.op("pe", lambda e, half=half: e.matmul(pbank[3][0:64, :], o64, Q["e"][0:64, half * 4:(half + 1) * 4, :], start=True, stop=True),
                     R=[QB["e"], onesb], W=[pbb[3]])
                P.op("dve", lambda e, half=half: e.tensor_tensor(out=Q["BhT"][0:64, half * 4:(half + 1) * 4, :],
                                                                 in0=pbank[3][0:64, :].rearrange("p (h t) -> p h t", h=4),
                                                                 in1=Q["v"][0:64, half * 4:(half + 1) * 4, :], op=ALU.mult),
                     R=[pbb[3], QB["v"]], W=[QB["BhT"]])
            P.op("act", lambda e: e.activation(out=Q["e"][0:64], in_=Q["lw"][0:64], func=AF.Exp), R=[QB["lw"]], W=[QB["e"]])
            P.op("dve", lambda e: e.scalar_tensor_tensor(out=Q["at"][0:64], in0=Q["kk"][0:64], scalar=-1.0, in1=Q["e"][0:64], op0=ALU.mult, op1=ALU.mult),
                 R=[QB["kk"], QB["e"]], W=[QB["at"]])
            P.op("act", lambda e: e.activation(out=Q["lw"][0:64], in_=Q["BhT"][0:64], func=AF.Copy), R=[QB["BhT"], QB["e"]], W=[QB["lw"]])
            P.op("act", lambda e: e.activation(out=Q["e"][0:64], in_=Q["Lw"][0:64], func=AF.Exp), R=[QB["Lw"], QB["at"]], W=[QB["e"]])
            P.op("dve", lambda e: e.tensor_tensor(out=Q["r"][0:64], in0=Q["r"][0:64], in1=Q["e"][0:64], op=ALU.mult), R=[QB["r"], QB["e"]], W=[QB["r"]])
            P.op("act", lambda e: e.activation(out=Q["e"][0:64], in_=Q["Lw"][0:64], func=AF.Exp, scale=-1.0), R=[QB["Lw"], QB["r"]], W=[QB["e"]])
            P.op("dve", lambda e: e.tensor_tensor(out=Q["bt"][0:64], in0=Q["a"][0:64], in1=Q["e"][0:64], op=ALU.mult), R=[QB["a"], QB["e"]], W=[QB["bt"]])
            P.op("dve", lambda e: e.tensor_tensor(out=Q["kt"][0:64], in0=Q["k"][0:64], in1=Q["e"][0:64], op=ALU.mult), R=[QB["k"], QB["e"]], W=[QB["kt"]])
            def eld(e):
                ins = None
                for h in range(8):
                    for ci in range(G // 64):
                        ins = e.activation(out=Q["e"][0:64, h, ci * 64:(ci + 1) * 64], in_=Q["Lw"][0:64, h, ci * 64:(ci + 1) * 64], func=AF.Exp,
                                           scale=-1.0, bias=Q["Lw"][0:64, h, ci * 64 + 63:ci * 64 + 64])
                return ins
            P.op("act", eld, R=[QB["Lw"], QB["bt"], QB["kt"]], W=[QB["e"]])
            P.op("act", lambda e: e.activation(out=gC[0:64], in_=Q["Lw"][0:64].rearrange("p h (c t) -> p h c t", t=64)[:, :, :, 63], func=AF.Exp),
                 R=[QB["Lw"]], W=[gCb])
            P.op("dve", lambda e: e.tensor_tensor(out=Q["BhT"][0:64], in0=Q["a"][0:64], in1=Q["e"][0:64], op=ALU.mult),
                 R=[QB["a"], QB["e"], QB["lw"]], W=[QB["BhT"]])
            P.op("dve", lambda e: e.tensor_tensor(out=Q["KhT"][0:64], in0=Q["k"][0:64], in1=Q["e"][0:64], op=ALU.mult),
                 R=[QB["k"], QB["e"]], W=[QB["KhT"]])
            if dbg_d is not None and g == 0:
                for i_, n_ in enumerate(["r", "k", "v", "lw", "a", "kk", "Lw", "at", "bt", "kt", "BhT", "KhT"]):
                    dbg_ops.append(P.dma("sp", dbg_d[:, i_, :].rearrange("p (h t) -> p h t", h=8), Q[n_][0:64], R=[QB[n_]]))
            def chunk_indep(ci):
                cs = ci * 64
                sfx = str(ci % 2)
                N_ = lambda n: n + sfx if n in HO_ else n

                def amat(bank, ln, rn, dst, mask, eng):
                    def mm(e):
                        ins = None
                        for h in range(8):
                            ins = e.matmul(pbank[bank][0:64, h * 64:(h + 1) * 64], Q[ln][0:64, h, cs:cs + 64], Q[rn][0:64, h, cs:cs + 64],
                                           start=True, stop=True)
                        return ins
                    P.op("pe", mm, R=[QB[ln], QB[rn]], W=[pbb[bank]])
                    P.op(eng, lambda e: e.tensor_tensor(out=Cc[N_(dst)][0:64], in0=pbank[bank][0:64, :].rearrange("p (h t) -> p h t", h=8),
                                                        in1=mask, op=ALU.mult), R=[pbb[bank], cb2], W=[CB[N_(dst)]])
                amat(2, "at", "bt", "P0", mLs, "dve")
                yield
                amat(3, "bt", "at", "PT0", mUs, "dve")
                yield
                amat(2, "kt", "at", "AakT", mUs, "dve")
                yield
                P.op("dve", lambda e: e.tensor_tensor(out=Cc[N_("TT")][0:64], in0=Cc["PT0"][0:64], in1=id8, op=ALU.add), R=[CB["PT0"], cb2], W=[CB[N_("TT")]])
                def do_tr(pairs):
                    for src, dst in pairs:
                        def trp(e, src=src):
                            ins = None
                            for h in range(8):
                                ins = e.transpose(pbank[4][0:64, h * 64:(h + 1) * 64], Q[src][0:64, h, cs:cs + 64], ident[0:64, 0:64])
                            return ins
                        P.op("pe", trp, R=[QB[src], cb], W=[pbb[4]])
                        P.op("act", lambda e, dst=dst: e.activation(out=Cc[N_(dst)][0:64], in_=pbank[4][0:64, :].rearrange("p (h t) -> p h t", h=8), func=AF.Copy),
                             R=[pbb[4]], W=[CB[N_(dst)]])
                        yield

                yield from do_tr((("v", "Vtm"),))
                pc = 0
                for lev in range(1, 6):
                    Pn, Pp = "P%d" % (1 - pc), "P%d" % pc
                    PTn, PTp = "PT%d" % (1 - pc), "PT%d" % pc

                    def sqm(e, Pp=Pp, PTp=PTp):
                        ins = None
                        for h in range(8):
                            ins = e.matmul(pbank[2][0:64, h * 64:(h + 1) * 64], Cc[PTp][0:64, h, :], Cc[Pp][0:64, h, :], start=True, stop=True)
                        return ins
                    P.op("pe", sqm, R=[CB[Pp], CB[PTp]], W=[pbb[2]])
                    P.op("act", lambda e, Pn=Pn: e.activation(out=Cc[Pn][0:64], in_=pbank[2][0:64, :].rearrange("p (h t) -> p h t", h=8), func=AF.Copy),
                         R=[pbb[2]], W=[CB[Pn]])
                    yield
                    if lev < 5:
                        def sqt(e, Pp=Pp, PTp=PTp):
                            ins = None
                            for h in range(8):
                                ins = e.matmul(pbank[3][0:64, h * 64:(h + 1) * 64], Cc[Pp][0:64, h, :], Cc[PTp][0:64, h, :], start=True, stop=True)
                            return ins
                        P.op("pe", sqt, R=[CB[Pp], CB[PTp]], W=[pbb[3]])
                        P.op("dve", lambda e, PTn=PTn: e.tensor_copy(out=Cc[PTn][0:64], in_=pbank[3][0:64, :].rearrange("p (h t) -> p h t", h=8)),
                             R=[pbb[3]], W=[CB[PTn]])
                        yield

                    def ttm(e, Pn=Pn):
                        ins = None
                        for h in range(8):
                            ins = e.matmul(pbank[7][0:64, h * 64:(h + 1) * 64], Cc[Pn][0:64, h, :], Cc[N_("TT")][0:64, h, :], start=True, stop=True)
                        return ins
                    P.op("pe", ttm, R=[CB[Pn], CB[N_("TT")]], W=[pbb[7]])
                    P.op("dve", lambda e: e.tensor_tensor(out=Cc[N_("TT")][0:64], in0=pbank[7][0:64, :].rearrange("p (h t) -> p h t", h=8),
                                                          in1=Cc[N_("TT")][0:64], op=ALU.add), R=[pbb[7], CB[N_("TT")]], W=[CB[N_("TT")]])
                    yield
                    pc = 1 - pc
                amat(3, "bt", "r", "ArbT", mUi, "dve")
                yield
                amat(2, "kt", "r", "ArkT", mUi, "dve")
                yield
                yield from do_tr((("BhT", "Bhtm"), ("KhT", "Khtm")))
            def chunk_dep(ci):
                nonlocal hcur
                cs = ci * 64
                sfx = str(ci % 2)
                N_ = lambda n: n + sfx if n in HO_ else n
                Hc, Hn = "H%d" % hcur, "H%d" % (1 - hcur)

                def xmm(e, Hc=Hc):
                    ins = None
                    for h in range(8):
                        e.matmul(pbank[6][0:64, h * 64:(h + 1) * 64], Q["at"][0:64, h, cs:cs + 64], Cc[Hc][0:64, h, :], start=True, stop=False)
                        ins = e.matmul(pbank[6][0:64, h * 64:(h + 1) * 64], Cc[N_("AakT")][0:64, h, :], Cc[N_("Vtm")][0:64, h, :], start=False, stop=True)
                    return ins
                P.op("pe", xmm, R=[QB["at"], CB[Hc], CB[N_("AakT")], CB[N_("Vtm")]], W=[pbb[6]])
                P.op("act", lambda e: e.activation(out=Cc["Xs"][0:64], in_=pbank[6][0:64, :].rearrange("p (h t) -> p h t", h=8), func=AF.Copy),
                     R=[pbb[6]], W=[CB["Xs"]])
                yield
                yield
                yield
                yield

                def umm(e):
                    ins = None
                    for h in range(8):
                        ins = e.matmul(pbank[6][0:64, h * 64:(h + 1) * 64], Cc[N_("TT")][0:64, h, :], Cc["Xs"][0:64, h, :], start=True, stop=True)
                    return ins
                P.op("pe", umm, R=[CB[N_("TT")], CB["Xs"]], W=[pbb[6]])
                P.op("act", lambda e: e.activation(out=Cc["Us"][0:64], in_=pbank[6][0:64, :].rearrange("p (h t) -> p h t", h=8), func=AF.Copy),
                     R=[pbb[6]], W=[CB["Us"]])
                yield
                yield
                yield
                yield

                def ymm(e, Hc=Hc):
                    ins = None
                    for h in range(8):
                        e.matmul(pbank[0][0:64, h * 64:(h + 1) * 64], Cc[Hc][0:64, h, :], Q["r"][0:64, h, cs:cs + 64], start=True, stop=False)
                        e.matmul(pbank[0][0:64, h * 64:(h + 1) * 64], Cc["Us"][0:64, h, :], Cc[N_("ArbT")][0:64, h, :], start=False, stop=False)
                        ins = e.matmul(pbank[0][0:64, h * 64:(h + 1) * 64], Cc[N_("Vtm")][0:64, h, :], Cc[N_("ArkT")][0:64, h, :], start=False, stop=True)
                    return ins
                P.op("pe", ymm, R=[CB[Hc], QB["r"], CB["Us"], CB[N_("ArbT")], CB[N_("Vtm")], CB[N_("ArkT")]], W=[pbb[0]])
                P.op("act", lambda e: e.activation(out=Q["Lw"][0:64, :, cs:cs + 64], in_=pbank[0][0:64, :].rearrange("p (h t) -> p h t", h=8), func=AF.Copy),
                     R=[pbb[0], gCb], W=[QB["Lw"]])

                def hmm(e):
                    ins = None
                    for h in range(8):
                        e.matmul(pbank[1][0:64, h * 64:(h + 1) * 64], Cc[N_("Bhtm")][0:64, h, :], Cc["Us"][0:64, h, :], start=True, stop=False)
                        ins = e.matmul(pbank[1][0:64, h * 64:(h + 1) * 64], Cc[N_("Khtm")][0:64, h, :], Cc[N_("Vtm")][0:64, h, :], start=False, stop=True)
                    return ins
                P.op("pe", hmm, R=[CB[N_("Bhtm")], CB["Us"], CB[N_("Khtm")], CB[N_("Vtm")]], W=[pbb[1]])

                def hup(e, Hc=Hc, Hn=Hn, ci=ci):
                    ins = None
                    for h in range(8):
                        ins = e.scalar_tensor_tensor(out=Cc[Hn][0:64, h, :], in0=Cc[Hc][0:64, h, :], scalar=gC[0:64, h, ci:ci + 1],
                                                     in1=pbank[1][0:64, h * 64:(h + 1) * 64], op0=ALU.mult, op1=ALU.add)
                    return ins
                P.op("dve", hup, R=[CB[Hc], gCb, pbb[1]], W=[CB[Hn]])
                hcur = 1 - hcur
                yield
                if dbg_d is not None and g == 0 and ci == 0:
                    for i_, n_ in enumerate(["P0", "PT0", "TT0", "AakT0", "ArbT", "ArkT", "Vtm0", "Bhtm", "Khtm", "Xs", "Us", Hn]):
                        dbg_ops.append(P.dma("sp", dbg_d[:, 12 + i_, 0:512].rearrange("p (h t) -> p h t", h=8), Cc[n_][0:64], R=[CB[n_]]))
            def _il(a_, b_):
                gens = [x for x in (a_, b_) if x is not None]
                while gens:
                    for x in list(gens):
                        try:
                            next(x)
                        except StopIteration:
                            gens.remove(x)
            nch = G // 64
            _il(chunk_indep(0), None)
            for ci_ in range(nch):
                _il(chunk_dep(ci_), chunk_indep(ci_ + 1) if ci_ + 1 < nch else None)
            if dbg_d is not None and g == 0:
                dbg_ops.append(P.dma("sp", dbg_d[:, 24, :].rearrange("p (h t) -> p h t", h=8), Q["Lw"][0:64], R=[QB["Lw"]]))
            yr = Q["Lw"]; yrb = QB["Lw"]
            cen = Q["at"]; cenb = QB["at"]
            for half in range(2):
                P.op("pe", lambda e, half=half: e.matmul(pbank[2][0:64, :], o64, yr[0:64, half * 4:(half + 1) * 4, :], start=True, stop=True),
                     R=[yrb, onesb], W=[pbb[2]])
                P.op("dve", lambda e, half=half: e.scalar_tensor_tensor(out=cen[0:64, half * 4:(half + 1) * 4, :],
                                                                        in0=pbank[2][0:64, :].rearrange("p (h t) -> p h t", h=4), scalar=-1.0 / 64.0,
                                                                        in1=yr[0:64, half * 4:(half + 1) * 4, :], op0=ALU.mult, op1=ALU.add),
                     R=[pbb[2], yrb, CB["H0"], CB["H1"]], W=[cenb])
            P.op("act", lambda e: e.activation(out=Q["e"][0:64], in_=cen[0:64], func=AF.Square), R=[cenb, QB["BhT"], QB["KhT"]], W=[QB["e"]])
            for half in range(2):
                P.op("pe", lambda e, half=half: e.matmul(pbank[3][0:64, :], o64, Q["e"][0:64, half * 4:(half + 1) * 4, :], start=True, stop=True),
                     R=[QB["e"], onesb], W=[pbb[3]])
                P.op("act", lambda e, half=half: e.activation(out=Q["bt"][0:64, half * 4:(half + 1) * 4, :],
                                                               in_=pbank[3][0:64, :].rearrange("p (h t) -> p h t", h=4), func=AF.Ln,
                                                               bias=epsb_t[0:64, 1:2], scale=1.0 / 64.0), R=[pbb[3], epsb], W=[QB["bt"]])
            P.op("act", lambda e: e.activation(out=Q["bt"][0:64], in_=Q["bt"][0:64], func=AF.Exp, scale=-0.5), R=[QB["bt"]], W=[QB["bt"]])
            P.op("dve", lambda e: e.tensor_tensor(out=cen[0:64], in0=cen[0:64], in1=Q["bt"][0:64], op=ALU.mult), R=[cenb, QB["bt"]], W=[cenb])

            def lnx(e):
                ins = None
                for h in range(8):
                    ins = e.tensor_scalar(out=cen[0:64, h, :], in0=cen[0:64, h, :], scalar1=pv[0:64, LNW + h:LNW + h + 1],
                                          scalar2=pv[0:64, LNB + h:LNB + h + 1], op0=ALU.mult, op1=ALU.add)
                return ins
            P.op("dve", lnx, R=[cenb, pvb], W=[cenb])
            P.op("dve", lambda e: e.tensor_tensor(out=cen[0:64], in0=cen[0:64], in1=Q["lw"][0:64], op=ALU.add), R=[cenb, QB["lw"]], W=[cenb])
            for half in range(2):
                def gmm(e, half=half):
                    ins = None
                    for hh in range(4):
                        h = half * 4 + hh
                        ins = e.matmul(pbank[2][0:64, hh * G:(hh + 1) * G], g2[:, h * 64:(h + 1) * 64], sg, start=True, stop=True)
                    return ins
                P.op("pe", gmm, R=[wconst, sgb], W=[pbb[2]])
                P.op("dve", lambda e, half=half: e.tensor_tensor(out=yfh[0:64, half * 4:(half + 1) * 4, :], in0=pbank[2][0:64, :].rearrange("p (h t) -> p h t", h=4),
                                                                 in1=cen[0:64, half * 4:(half + 1) * 4, :], op=ALU.mult),
                     R=[pbb[2], cenb], W=[yfhb])
            yf = yfh; yfb = yfhb
            for dh in range(2):
                for dd in range(4):
                    dc = dh * 4 + dd
                    s2 = woi % 2
                    woi += 1
                    P.dma("sp", wos[0:64], rwout_d[dc], W=[wosb])
                    P.op("pool", lambda e, s2=s2: e.tensor_copy(out=wo[s2][0:64], in_=wos[0:64]), R=[wosb], W=[wob[s2]])

                    def mo(e, s2=s2, dd=dd, dh=dh):
                        ins = None
                        for h in range(8):
                            ins = e.matmul(pbank[7][:, dd * G:(dd + 1) * G], wo[s2][0:64, h, :], yf[0:64, h, :], start=(h == 0), stop=(h == 7))
                        return ins
                    P.op("pe", mo, R=[wob[s2], yfb], W=[pbb[7]])
                P.op("dve", lambda e, dh=dh, t0=t0: e.tensor_tensor(out=xT[:, dh * 4:(dh + 1) * 4, t0:t0 + G],
                                                                    in0=pbank[7][:, 0:4 * G].rearrange("p (c t) -> p c t", c=4),
                                                                    in1=xT[:, dh * 4:(dh + 1) * 4, t0:t0 + G], op=ALU.add),
                     R=[pbb[7]] + [xb[c][g4] for c in range(dh * 4, dh * 4 + 4)], W=[xb[c][g4] for c in range(dh * 4, dh * 4 + 4)])
        for g_ in range(NG):
            do_group(g_)
        P.fence()


    def moba():
        G = 256
        NEG = 240000.0
        A = Arena(arena, ARENA)
        kT = A.take(8192).rearrange("p (c t) -> p c t", c=4)
        kTb = [[Buf() for g in range(8)] for c in range(4)]
        Va = A.take(16 * 8 * 65).rearrange("p (k h d) -> p k h d", k=16, h=8)
        Vab = [Buf() for kt in range(16)]
        hT = A.take(2048).rearrange("p (c t) -> p c t", c=8); hb = Buf()
        sq = A.take(2048).rearrange("p (c t) -> p c t", c=8); sqb = Buf()
        rstd = A.take(256); rstdb = Buf()
        wt = [A.take(1024).rearrange("p (c m) -> p c m", c=8) for i in range(2)]; wtb = [Buf(), Buf()]
        qT = A.take(1024).rearrange("p (c t) -> p c t", c=4); qTb = [Buf() for c in range(4)]
        ksum = A.take(32).rearrange("p (c j) -> p c j", c=4); ksb = Buf()
        gsel = A.take(16).rearrange("p (a j) -> p a j", a=2); gselb = Buf()
        m8 = A.take(16).rearrange("p (a j) -> p a j", a=2); m8b = Buf()
        negm = A.take(16).rearrange("p (a j) -> p a j", a=2); negmb = Buf()
        qaug = [A.take(256) for i in range(2)]; qaugb = [Buf(), Buf()]
        kaug = A.take(2048); kaugb = Buf()
        PT = [A.take(256) for i in range(2)]; PTb = [Buf() for i in range(2)]
        stmp = [A.take(256) for i in range(1)]; stmpb = [Buf()]
        oa = A.take(256); oab = Buf()
        rden = A.take(256); rdenb = Buf()
        oh = A.take(2048).rearrange("p (h t) -> p h t", h=8); ohb = [Buf() for h in range(8)]
        wo = [A.take(1024).rearrange("p (h m) -> p h m", h=8) for i in range(1)]; wob = [Buf()]
        stg = [A.take(512).rearrange("p (c t) -> p c t", c=2) for i in range(2)]; stgb = [Buf(), Buf()]
        ncA = A.take(256); ncB = A.take(256); sel65 = A.take(64); mcb = Buf()
        gcol = PV_NORMG + (0 * 3 + 1) * 8
        cnt = {"w": 0, "p": 0, "pt": 0, "st": 0, "wo": 0, "qa": 0}

        def setup(e):
            e.memset(ncA[:], 0.0)
            e.memset(ncB[:], 0.0)
            e.affine_select(out=ncA[:], in_=ncA[:], pattern=[[1, 256]], compare_op=ALU.is_ge, fill=-NEG, base=0, channel_multiplier=-1)
            e.affine_select(out=ncB[:], in_=ncB[:], pattern=[[1, 256]], compare_op=ALU.is_ge, fill=-NEG, base=-128, channel_multiplier=-1)
            e.memset(sel65[0:64, :], 0.0)
            e.memset(sel65[64:65, :], 1.0)
            return e.memset(Va[:, :, :, 64:65], 1.0)
        P.op("pool", setup, W=[mcb] + Vab)
        P.dma("sp", kaug[0:10, :], mbkaug_d[:, :], W=[kaugb])

        def proj_fm(widx, dst_ap, dst_bufs):
            s_ = cnt["w"] % 2
            cnt["w"] += 1
            bank = cnt["p"] % 2
            cnt["p"] += 1
            P.dma("sp", wt[s_], mbin_d[widx], W=[wtb[s_]])

            def mm(e):
                ins = None
                for c in range(8):
                    ins = e.matmul(pbank[bank][:, 0:G], wt[s_][:, c, :], hT[:, c, :], start=(c == 0), stop=(c == 7))
                return ins
            P.op("pe", mm, R=[wtb[s_], hb], W=[pbb[bank]])
            P.op("act", lambda e: e.activation(out=dst_ap, in_=pbank[bank][:, 0:G], func=AF.Copy), R=[pbb[bank]], W=dst_bufs)

        def proj_v(g, hp):
            s_ = cnt["w"] % 2
            cnt["w"] += 1
            bank = cnt["p"] % 2
            cnt["p"] += 1
            P.dma("sp", wt[s_], mbin_d[8 + hp], W=[wtb[s_]])

            def mm(e):
                ins = None
                for tt in range(2):
                    for c in range(8):
                        ins = e.matmul(pbank[bank][:, tt * 128:(tt + 1) * 128], hT[:, c, tt * 128:(tt + 1) * 128], wt[s_][:, c, :],
                                       start=(c == 0), stop=(c == 7))
                return ins
            P.op("pe", mm, R=[wtb[s_], hb], W=[pbb[bank]])
            P.op("dve", lambda e: e.tensor_copy(out=Va[:, 2 * g:2 * g + 2, 2 * hp:2 * hp + 2, 0:64],
                                                in_=pbank[bank][:, 0:256].rearrange("p (k h d) -> p k h d", k=2, h=2)),
                 R=[pbb[bank]], W=[Vab[2 * g], Vab[2 * g + 1]])

        def prep_head(g, h):
            hp, ph = h // 2, h % 2
            r0 = ph * 64
            ob = g
            qa = qaug[h % 2]
            qab = qaugb[h % 2]
            P.dma("sp", qa[8:10, :], mbqc_d[h, :, g * G:(g + 1) * G], W=[qab])
            if ob >= 4:
                def gmm(e):
                    ins = None
                    for tt in range(2):
                        ins = e.matmul(pbank[2][:, tt * 8:(tt + 1) * 8], qT[r0:r0 + 64, hp, tt * 128:(tt + 1) * 128], ksum[r0:r0 + 64, hp, :],
                                       start=True, stop=True)
                    return ins
                P.op("pe", gmm, R=[qTb[hp], ksb], W=[pbb[2]])
                P.op("pool", lambda e: e.memset(gsel, -1e30), W=[gselb])
                P.op("dve", lambda e: e.tensor_copy(out=gsel[:, :, 0:ob], in_=pbank[2][:, 0:16].rearrange("p (a j) -> p a j", a=2)[:, :, 0:ob]),
                     R=[pbb[2]], W=[gselb])

                def mx(e):
                    e.max(out=m8[:, 0, :], in_=gsel[:, 0, :])
                    return e.max(out=m8[:, 1, :], in_=gsel[:, 1, :])
                P.op("dve", mx, R=[gselb], W=[m8b])

                def ng(e):
                    ins = None
                    for tt in range(2):
                        ins = e.tensor_scalar(out=negm[:, tt, :], in0=gsel[:, tt, :], scalar1=m8[:, tt, 2:3], scalar2=1.0, op0=ALU.is_ge, op1=ALU.subtract)
                    return ins
                P.op("dve", ng, R=[gselb, m8b], W=[negmb])
                P.op("dve", lambda e: e.memset(negm[:, :, ob:8], 0.0), R=[], W=[negmb])

                def trn(e):
                    ins = None
                    for tt in range(2):
                        ins = e.transpose(pbank[2][0:8, 128 + tt * 128:128 + (tt + 1) * 128], negm[:, tt, :], ident[:])
                    return ins
                P.op("pe", trn, R=[negmb, cb], W=[pbb[2]])
                P.op("act", lambda e: e.activation(out=qa[0:8, :], in_=pbank[2][0:8, 128:384], func=AF.Copy), R=[pbb[2]], W=[qab])
            else:
                P.op("pool", lambda e: e.memset(qa[0:8, :], 0.0), W=[qab])

        def attn_head(g, h, pending_tail=None):
            hp, ph = h // 2, h % 2
            r0 = ph * 64
            ob = g
            qa = qaug[h % 2]
            qab = qaugb[h % 2]
            ob5 = 5 + (h % 2)
            nkt = 2 * ob + 2
            pend = []

            def emit_pv(kt, c0, pti):
                P.op("pe", lambda e: e.matmul(pbank[ob5][0:65, c0:G], Va[:, kt, h, :], PT[pti][:, c0:G], start=(kt == 0), stop=(kt == nkt - 1)),
                     R=[Vab[kt], PTb[pti]], W=[pbb[ob5]])
            for kt in range(nkt):
                diag = kt - 2 * ob
                c0 = 128 if diag == 1 else 0
                n = G - c0
                sb_ = 3 + (cnt["st"] % 2)
                cnt["st"] += 1
                pti = cnt["pt"] % 2
                cnt["pt"] += 1

                def smm(e, kt=kt, c0=c0, sb_=sb_):
                    e.matmul(pbank[sb_][:, c0:G], kT[r0:r0 + 64, hp, kt * 128:(kt + 1) * 128], qT[r0:r0 + 64, hp, c0:G], start=True, stop=False)
                    return e.matmul(pbank[sb_][:, c0:G], kaug[0:10, kt * 128:(kt + 1) * 128], qa[0:10, c0:G], start=False, stop=True)
                P.op("pe", smm, R=[kTb[hp][kt // 2], qTb[hp], kaugb, qab], W=[pbb[sb_]])
                if diag >= 0:
                    nc_ = ncA if diag == 0 else ncB
                    si = 0
                    P.op("dve", lambda e, c0=c0, sb_=sb_, nc_=nc_, si=si: e.tensor_tensor(out=stmp[si][:, c0:G], in0=pbank[sb_][:, c0:G], in1=nc_[:, c0:G], op=ALU.add),
                         R=[pbb[sb_], mcb], W=[stmpb[si]])
                    P.op("act", lambda e, c0=c0, pti=pti, si=si: e.activation(out=PT[pti][:, c0:G], in_=stmp[si][:, c0:G], func=AF.Exp, scale=0.125),
                         R=[stmpb[si]], W=[PTb[pti]])
                else:
                    P.op("act", lambda e, c0=c0, pti=pti, sb_=sb_: e.activation(out=PT[pti][:, c0:G], in_=pbank[sb_][:, c0:G], func=AF.Exp, scale=0.125),
                         R=[pbb[sb_]], W=[PTb[pti]])
                pend.append((kt, c0, pti))
                if len(pend) > 1:
                    emit_pv(*pend.pop(0))
                if pending_tail is not None and kt == min(1, nkt - 1):
                    pending_tail()
                    pending_tail = None
            while pend:
                emit_pv(*pend.pop(0))
            def tail():
                P.op("act", lambda e: e.activation(out=oa[0:65, :], in_=pbank[ob5][0:65, 0:G], func=AF.Copy), R=[pbb[ob5]], W=[oab])
                P.op("pe", lambda e: e.matmul(pbank[7][0:64, 0:G], sel65[0:65, :], oa[0:65, :], start=True, stop=True), R=[oab, mcb], W=[pbb[7]])
                P.op("dve", lambda e: e.reciprocal(out=rden[0:64, :], in_=pbank[7][0:64, 0:G]), R=[pbb[7]], W=[rdenb])
                P.op("dve", lambda e: e.tensor_tensor(out=oh[0:64, h, :], in0=oa[0:64, :], in1=rden[0:64, :], op=ALU.mult), R=[oab, rdenb], W=[ohb[h]])
            return tail

        def do_group(g):
            t0 = g * G
            g4 = t0 // 512
            xg = [xb[c][g4] for c in range(8)]
            P.op("act", lambda e: e.activation(out=sq, in_=xT[:, :, t0:t0 + G], func=AF.Square), R=xg, W=[sqb])

            def mmn(e):
                ins = None
                for c in range(8):
                    ins = e.matmul(pbank[7][:, 0:G], ones[:], sq[:, c, :], start=(c == 0), stop=(c == 7))
                return ins
            P.op("pe", mmn, R=[sqb, onesb], W=[pbb[7]])
            P.op("act", lambda e: e.activation(out=rstd, in_=pbank[7][:, 0:G], func=AF.Ln, bias=epsb_t[:, 0:1], scale=1.0 / 1024.0),
                 R=[pbb[7], epsb], W=[rstdb])
            P.op("act", lambda e: e.activation(out=rstd, in_=rstd, func=AF.Exp, scale=-0.5), R=[rstdb], W=[rstdb])

            def hnorm(e):
                ins = None
                for c in range(8):
                    ins = e.scalar_tensor_tensor(out=hT[:, c, :], in0=xT[:, c, t0:t0 + G], scalar=pv[:, gcol + c:gcol + c + 1],
                                                 in1=rstd, op0=ALU.mult, op1=ALU.mult)
                return ins
            P.op("dve", hnorm, R=xg + [rstdb, pvb], W=[hb])
            for hp in range(4):
                proj_fm(hp, qT[:, hp, :], [qTb[hp]])
                proj_fm(4 + hp, kT[:, hp, t0:t0 + G], [kTb[hp][g]])
                proj_v(g, hp)
            P.op("dve", lambda e: e.tensor_reduce(out=ksum[:, :, g], in_=kT[:, :, t0:t0 + G], axis=AX.X, op=ALU.add),
                 R=[kTb[hp][g] for hp in range(4)], W=[ksb])
            prep_head(g, 0)
            tl = None
            for h in range(8):
                if h + 1 < 8:
                    prep_head(g, h + 1)
                tl = attn_head(g, h, tl)
            tl()
            if dbg_d is not None and g == 0:
                dbg_ops.append(P.dma("sp", dbg_d[:, 0:2, :].rearrange("p a (h t) -> p (a h) t", h=4), oh[0:64], R=ohb))
                dbg_ops.append(P.dma("sp", dbg_d[:, 2, 0:256], qT[0:64, 0, :], R=qTb))
                dbg_ops.append(P.dma("sp", dbg_d[:, 3, 0:256], kT[0:64, 0, 0:256], R=[kTb[0][0]]))
                dbg_ops.append(P.dma("sp", dbg_d[:, 4:6, :].rearrange("p a (h d) -> p (a h) d", h=4)[:, :, 0:65], Va[0:64, 0, :, :], R=[Vab[0]]))
                dbg_ops.append(P.dma("sp", dbg_d[:, 6, 0:256], oa[0:64, :], R=[oab]))
                dbg_ops.append(P.dma("sp", dbg_d[:, 7, 0:256], rden[0:64, :], R=[rdenb]))
                dbg_ops.append(P.dma("sp", dbg_d[:, 8, 0:256], PT[0][0:64, :], R=[PTb[0]]))
                dbg_ops.append(P.dma("sp", dbg_d[:, 9, 0:256], PT[1][0:64, :], R=[PTb[1]]))
                dbg_ops.append(P.dma("sp", dbg_d[:, 10, 0:256], ncA[0:64, :], R=[mcb]))
                dbg_ops.append(P.dma("sp", dbg_d[0:10, 11, 0:256], qaug[0][0:10, :], R=[qaugb[0]]))
                dbg_ops.append(P.dma("sp", dbg_d[0:10, 12, 0:256], kaug[0:10, 0:256], R=[kaugb]))
            for dh in range(4):
                for dd in range(2):
                    dc = dh * 2 + dd
                    s2 = 0
                    P.dma("sp", wo[s2][0:64], mbout_d[dc], W=[wob[s2]])

                    def mo(e, s2=s2, dd=dd):
                        ins = None
                        for h in range(8):
                            ins = e.matmul(pbank[7][:, dd * G:(dd + 1) * G], wo[s2][0:64, h, :], oh[0:64, h, :], start=(h == 0), stop=(h == 7))
                        return ins
                    P.op("pe", mo, R=[wob[s2]] + ohb, W=[pbb[7]])
                si = cnt["wo"] % 2
                cnt["wo"] += 1
                P.op("act", lambda e, si=si: e.activation(out=stg[si], in_=pbank[7][:, 0:2 * G].rearrange("p (c t) -> p c t", c=2), func=AF.Copy),
                     R=[pbb[7]], W=[stgb[si]])
                P.dma("sp", mscr_d[:, dh * 2:(dh + 1) * 2, t0:t0 + G], stg[si], R=[stgb[si]], W=[mscr_b[g]])
        for g_ in range(8):
            do_group(g_)
        P.fence()

    def moba_add():
        A = Arena(arena, ARENA)
        tb = [A.take(4096).rearrange("p (c t) -> p c t", c=8) for i in range(2)]
        tbb = [Buf(), Buf()]
        for g in range(4):
            si = g % 2
            P.dma("sp", tb[si], mscr_d[:, :, g * 512:(g + 1) * 512], R=[mscr_b[2 * g], mscr_b[2 * g + 1]], W=[tbb[si]])
            P.op("dve", lambda e, g=g, si=si: e.tensor_tensor(out=xT[:, :, g * 512:(g + 1) * 512], in0=xT[:, :, g * 512:(g + 1) * 512],
                                                              in1=tb[si], op=ALU.add),
                 R=[tbb[si]] + [xb[c][g] for c in range(8)], W=[xb[c][g] for c in range(8)])
        P.fence()

    P.fence()
    st = stage
    if "f00" in st:
        ffn(0, 0)
    if "moba" in st:
        moba()
    if "rwkv" in st:
        rwkv()
    if "moba" in st:
        moba_add()
    if "f01" in st:
        ffn(0, 2)
    if "f10" in st:
        ffn(1, 0)
    if "hgrn" in st:
        hgrn()
    if "f11" in st:
        ffn(1, 2)
    outs = final_out("final" in st)
    P.emit(nc, k.es, outs + dbg_ops)


PV_NORMG = 0
PV_FINALG = 48
PV_HGNW = 56
PV_LBZ = 64
PV_RW = 80
NPV = 176


class Arena:
    def __init__(self, ap, size):
        self.ap, self.o, self.size = ap, 0, size

    def take(self, n):
        a = self.ap[:, self.o:self.o + n]
        self.o += n
        assert self.o <= self.size, self.o
        return a


def _tile_w_in(w, ncols):
    n = ncols // 128
    return np.ascontiguousarray(w.reshape(8, 128, n, 128).transpose(2, 1, 0, 3))


def _prep_shared(inp):
    sh = {}
    for l in range(2):
        for f, nm in enumerate(("ffn1", "ffn2")):
            sh["wg%d%d" % (l, f)] = _tile_w_in(inp[nm + "_wg"][l], FF)
            sh["wu%d%d" % (l, f)] = _tile_w_in(inp[nm + "_wu"][l], FF)
            wd = inp[nm + "_wd"][l]
            sh["wd%d%d" % (l, f)] = np.ascontiguousarray(wd.reshape(NFC, 128, 8, 128).transpose(2, 1, 0, 3))
    pv = np.zeros((128, NPV), np.float32)
    ng = inp["norm_g"].reshape(6, 8, 128)
    pv[:, PV_NORMG:PV_NORMG + 48] = ng.transpose(2, 0, 1).reshape(128, 48)
    pv[:, PV_FINALG:PV_FINALG + 8] = inp["final_g"].reshape(8, 128).T
    pv[:, PV_HGNW:PV_HGNW + 8] = inp["hg_norm_w"][0].reshape(8, 128).T
    pv[:, PV_LBZ:PV_LBZ + 16] = inp["hg_lb_logits"].reshape(2, 8, 128).transpose(2, 0, 1).reshape(128, 16)
    h64 = lambda v: np.asarray(v).reshape(8, 64).T
    mu = inp["rw_mu"][0]
    pv[0:64, PV_RW:PV_RW + 24] = mu[0:1536].reshape(24, 64).T
    pv[0:64, PV_RW + 24:PV_RW + 32] = h64(inp["rw_w0"][0])
    pv[0:64, PV_RW + 32:PV_RW + 40] = h64(inp["rw_a0"][0])
    pv[0:64, PV_RW + 40:PV_RW + 48] = h64(inp["rw_k_k"][0])
    pv[0:64, PV_RW + 48:PV_RW + 56] = h64(inp["rw_k_a"][0])
    pv[0:64, PV_RW + 64:PV_RW + 72] = h64(inp["rw_r_k"][0])
    pv[0:64, PV_RW + 72:PV_RW + 80] = h64(inp["rw_lnx_w"][0])
    pv[0:64, PV_RW + 80:PV_RW + 88] = h64(inp["rw_lnx_b"][0])
    pv[:, PV_RW + 88] = mu[1536:1664]
    pv[:, PV_RW + 89] = mu[1664:1792]
    wi = inp["ev_w_in"][0]
    sh["rwin"] = np.ascontiguousarray(wi[:, 0:1536].reshape(8, 128, 24, 64).transpose(2, 1, 0, 3))
    sh["rwlo"] = _tile_w_in(wi[:, 1536:1792], 256)
    sh["rww2"] = np.ascontiguousarray(inp["rw_w2"][0])
    sh["rwa2"] = np.ascontiguousarray(inp["rw_a2"][0])
    sh["rwg2"] = np.ascontiguousarray(inp["rw_g2"][0])
    wo_ = inp["ev_w_out"][0]
    sh["rwout"] = np.ascontiguousarray(wo_[0:512].reshape(8, 64, 8, 128).transpose(2, 1, 0, 3))
    sh["pvec"] = pv
    sh["odwin"] = _tile_w_in(inp["od_w_in"][0], 4096)
    sh["odwout"] = _tile_w_in(inp["od_w_out"][0], 1024)
    sh["mbin"] = _tile_w_in(wi[:, 1792:3328], 1536)
    sh["mbout"] = np.ascontiguousarray(wo_[512:1024].reshape(8, 64, 8, 128).transpose(2, 1, 0, 3))
    pos = np.arange(S, dtype=np.float32)
    kaug = np.zeros((10, S), np.float32)
    for j in range(8):
        kaug[j, j * 256:(j + 1) * 256] = 240000.0
    kaug[8] = pos
    kaug[9] = 1.0
    sh["mbkaug"] = kaug
    slopes = np.exp2(-np.arange(1, 9, dtype=np.float32))
    qc = np.zeros((8, 2, S), np.float32)
    qc[:, 0, :] = 8.0 * slopes[:, None]
    qc[:, 1, :] = -8.0 * slopes[:, None] * pos[None, :]
    sh["mbqc"] = qc
    return sh


_NC_CACHE = {}


def run(inputs, stage=ALL_STAGES, ncores=8, trace=False):
    stage = tuple(stage)
    import time
    t0 = time.time()
    inp = {k_: np.asarray(v, dtype=np.float32) for k_, v in inputs.items()}
    sh = _prep_shared(inp)
    t1 = time.time()
    if stage not in _NC_CACHE:
        _NC_CACHE[stage] = build(stage)
    t2 = time.time()
    print("[kernel] prep %.1fs build %.1fs" % (t1 - t0, t2 - t1), flush=True)
    nc = _NC_CACHE[stage]
    in_maps = []
    for b in range(ncores):
        m = dict(sh)
        m["xT"] = np.ascontiguousarray(inp["x"][b].T.reshape(8, 128, S).transpose(1, 0, 2))
        in_maps.append(m)
    t3 = time.time()
    res = run_bass_kernel_spmd(nc, in_maps, core_ids=list(range(ncores)), trace=trace)
    print("[kernel] run %.1fs" % (time.time() - t3), flush=True)
    outs = []
    for b in range(ncores):
        o = np.asarray(res.results[b]["outT"])
        outs.append(o.transpose(1, 0, 2).reshape(D, S).T)
    if "dbg" in stage:
        np.save("dbg_out.npy", np.asarray(res.results[0]["dbg"]))
    return np.stack(outs).astype(np.float32), res


def kernel(**inputs):
    out, _ = run(inputs)
    return out
```

```python
import numpy as np
from contextlib import ExitStack
import concourse.bass as bass
import concourse.mybir as mybir
from concourse.bass_utils import run_bass_kernel_spmd

F32 = mybir.dt.float32
F32R = mybir.dt.float32r
BF16 = mybir.dt.bfloat16
AF = mybir.ActivationFunctionType
ALU = mybir.AluOpType
AX = mybir.AxisListType

D = 1024
S = 2048
FF = 2816
NFC = 22
EPS = 1e-6


class Buf:
    __slots__ = ("lw", "rd", "name")

    def __init__(self, name=""):
        self.lw = None
        self.rd = []
        self.name = name


class Op:
    __slots__ = ("eng", "fn", "deps", "needed", "sem", "val", "is_dma", "prev_dma")

    def __init__(self, eng, fn):
        self.eng = eng
        self.fn = fn
        self.deps = []
        self.needed = False
        self.sem = None
        self.val = 0
        self.is_dma = False
        self.prev_dma = None


ENGS = ("pe", "dve", "act", "pool", "sp")


class Prog:
    NSLOT = 6

    def __init__(self):
        self.ops = {e: [] for e in ENGS}
        self.fence_deps = []
        self.last = {e: None for e in ENGS}
        self.dma_slots = {e: [] for e in ENGS}
        self.dma_count = {e: 0 for e in ENGS}
        self.all_dma_last = {}

    def _collect(self, op, R, W):
        deps = []
        for b in R:
            if b.lw is not None:
                deps.append(b.lw)
        for b in W:
            if b.lw is not None:
                deps.append(b.lw)
            deps.extend(b.rd)
        deps.extend(self.fence_deps)
        seen = set()
        for d in deps:
            if id(d) in seen or d is op:
                continue
            seen.add(id(d))
            if d.eng == "pe" and op.eng == "pe" and not d.is_dma and not op.is_dma:
                continue
            op.deps.append(d)
            d.needed = True
        for b in W:
            b.lw = op
            b.rd = []
        for b in R:
            b.rd.append(op)

    def op(self, eng, fn, R=(), W=()):
        o = Op(eng, fn)
        self._collect(o, R, W)
        self.ops[eng].append(o)
        self.last[eng] = o
        return o

    def dma(self, q, out, in_, R=(), W=()):
        o = Op(q, lambda e: e.dma_start(out=out, in_=in_))
        o.is_dma = True
        o.needed = True
        k = self.dma_count[q]
        self.dma_count[q] += 1
        slot = k % self.NSLOT
        o.sem = ("dma", q, slot)
        o.val = 16 * (k // self.NSLOT + 1)
        slots = self.dma_slots[q]
        if len(slots) <= slot:
            slots.append(None)
        o.prev_dma = slots[slot]
        slots[slot] = o
        self._collect(o, R, W)
        self.ops[q].append(o)
        self.all_dma_last[(q, slot)] = o
        return o

    def fence(self):
        deps = [o for o in self.last.values() if o is not None]
        deps += list(self.all_dma_last.values())
        for d in deps:
            d.needed = True
        self.fence_deps = deps

    def emit(self, nc, es, final_waits):
        sems = {}
        for e in ENGS:
            sems[("eng", e)] = es.enter_context(nc.semaphore("s_" + e))
            for sl in range(len(self.dma_slots[e])):
                sems[("dma", e, sl)] = es.enter_context(nc.semaphore("d_%s_%d" % (e, sl)))
        for e in ENGS:
            cnt = 0
            for o in self.ops[e]:
                if o.is_dma:
                    continue
                o.sem = ("eng", e)
                if o.needed:
                    cnt += 1
                    o.val = cnt
        handles = {"pe": "tensor", "dve": "vector", "act": "scalar", "pool": "gpsimd", "sp": "sync"}
        block = es.enter_context(nc.Block())

        def make(e):
            def body(eng):
                seen = {}
                for o in self.ops[e]:
                    waits = list(o.deps)
                    if o.is_dma and o.prev_dma is not None:
                        waits.append(o.prev_dma)
                    for d in waits:
                        if seen.get(d.sem, 0) < d.val:
                            eng.wait_ge(sems[d.sem], d.val)
                            seen[d.sem] = d.val
                    ins = o.fn(eng)
                    if o.is_dma:
                        ins.then_inc(sems[o.sem], 16)
                    elif o.needed:
                        ins.then_inc(sems[o.sem], 1)
                if e == "sp":
                    for d in final_waits:
                        if seen.get(d.sem, 0) < d.val:
                            eng.wait_ge(sems[d.sem], d.val)
                            seen[d.sem] = d.val
            return body

        for e in ENGS:
            getattr(block, handles[e])(make(e))


def r32(ap):
    return ap


class K:
    def __init__(self, stage):
        self.stage = stage
        self.nc = bass.Bass("TRN2", target_bir_lowering=False)
        self.P = Prog()
        self.es = ExitStack()
        self.wq = 0

    def dram_in(self, name, shape, dt=F32):
        return self.nc.dram_tensor(name, list(shape), dt, kind="ExternalInput").ap()

    def sb(self, name, shape, dt=F32):
        return self.es.enter_context(self.nc.sbuf_tensor(name, list(shape), dt))

    def ps(self, name, shape, dt=F32):
        return self.es.enter_context(self.nc.psum_tensor(name, list(shape), dt))


ALL_STAGES = ("f00", "rwkv", "moba", "f01", "f10", "hgrn", "f11", "final")


def build(stage=ALL_STAGES):
    k = K(stage)
    nc, P, es = k.nc, k.P, k.es
    with es:
        _build(k)
    return nc


def _build(k):
    nc, P = k.nc, k.P
    stage = k.stage
    xT_d = k.dram_in("xT", [128, 8, S])
    pv_d = k.dram_in("pvec", [128, NPV])
    wg_d = [[k.dram_in("wg%d%d" % (l, f), [NFC, 128, 8, 128]) for f in range(2)] for l in range(2)]
    wu_d = [[k.dram_in("wu%d%d" % (l, f), [NFC, 128, 8, 128]) for f in range(2)] for l in range(2)]
    wd_d = [[k.dram_in("wd%d%d" % (l, f), [8, 128, NFC, 128]) for f in range(2)] for l in range(2)]
    odwin_d = k.dram_in("odwin", [32, 128, 8, 128])
    odwout_d = k.dram_in("odwout", [8, 128, 8, 128])
    rwin_d = k.dram_in("rwin", [24, 128, 8, 64])
    rwlo_d = k.dram_in("rwlo", [2, 128, 8, 128])
    rww2_d = k.dram_in("rww2", [64, 512])
    rwa2_d = k.dram_in("rwa2", [64, 512])
    rwg2_d = k.dram_in("rwg2", [128, 512])
    rwout_d = k.dram_in("rwout", [8, 64, 8, 128])
    mbin_d = k.dram_in("mbin", [12, 128, 8, 128])
    mbout_d = k.dram_in("mbout", [8, 64, 8, 128])
    mbkaug_d = k.dram_in("mbkaug", [10, S])
    mbqc_d = k.dram_in("mbqc", [8, 2, S])
    mscr_d = nc.dram_tensor("mscr", [128, 8, S], F32).ap()
    mscr_b = [Buf() for g in range(8)]
    dbg_d = nc.dram_tensor("dbg", [64, 32, 1024], F32, kind="ExternalOutput").ap() if "dbg" in stage else None
    dbg_ops = []
    out_d = nc.dram_tensor("outT", [128, 8, S], F32, kind="ExternalOutput").ap()

    xT = k.sb("xT_sb", [128, 8, S])
    xb = [[Buf("x%d_%d" % (c, g)) for g in range(4)] for c in range(8)]
    pv = k.sb("pv_sb", [128, NPV])
    pvb = Buf("pv")
    ones = k.sb("ones", [128, 128])
    onesb = Buf("ones")
    epsb_t = k.sb("epsc", [128, 4])
    epsb = Buf("eps")
    ARENA = 32800
    arena = k.sb("arena", [128, ARENA])

    pbank = [k.ps("pb%d" % i, [128, 512]) for i in range(8)]
    pbb = [Buf("pb%d" % i) for i in range(8)]

    P.op("pool", lambda e: e.memset(ones[:], 1.0), W=[onesb])
    P.op("pool", lambda e: e.memset(epsb_t[:, 0:1], EPS), W=[epsb])
    P.dma("sp", pv[:], pv_d[:], W=[pvb])
    for c in range(8):
        for g in range(4):
            P.dma("sp", xT[:, c, g * 512:(g + 1) * 512], xT_d[:, c, g * 512:(g + 1) * 512], W=[xb[c][g]])

    ident = k.sb("ident", [128, 128])
    mask4t = k.sb("mask4", [128, 512])
    mask4 = mask4t[:].rearrange("p (c m) -> p c m", c=4)
    resetm = k.sb("resetm", [128, 512])
    lbt = k.sb("lbt", [128, 16])
    cb = Buf("consts")
    lbb = Buf("lb")

    def setup_consts(e):
        e.memset(ident[:], 0.0)
        e.affine_select(out=ident[:], in_=ones[:], pattern=[[1, 128]], compare_op=ALU.is_equal, fill=0.0, base=0, channel_multiplier=-1)
        for i in range(4):
            e.affine_select(out=mask4t[:, i * 128:(i + 1) * 128], in_=ones[:], pattern=[[1, 128]], compare_op=ALU.is_ge, fill=0.0,
                            base=0, channel_multiplier=-1)
            e.memset(mask4t[0:64, i * 128 + 64:(i + 1) * 128], 0.0)
        e.memset(resetm[:], 1.0)
        ins = None
        for i in range(8):
            ins = e.memset(resetm[:, i * 64:i * 64 + 1], 0.0)
        return ins
    P.op("pool", setup_consts, R=[onesb], W=[cb])
    P.op("dve", lambda e: e.tensor_tensor(out=lbt[:, 0:8], in0=pv[:, PV_LBZ + 8:PV_LBZ + 16], in1=pv[:, PV_LBZ:PV_LBZ + 8], op=ALU.subtract),
         R=[pvb], W=[lbb])
    P.op("act", lambda e: e.activation(out=lbt[:, 0:8], in_=lbt[:, 0:8], func=AF.Sigmoid), R=[lbb], W=[lbb])
    P.op("dve", lambda e: e.tensor_scalar(out=lbt[:, 8:16], in0=lbt[:, 0:8], scalar1=-1.0, scalar2=1.0, op0=ALU.mult, op1=ALU.add),
         R=[lbb], W=[lbb])

    mk = k.sb("rwmask", [64, 4 * 512])
    mLs = mk[:, 0:512].rearrange("p (h t) -> p h t", h=8)
    mUs = mk[:, 512:1024].rearrange("p (h t) -> p h t", h=8)
    mUi = mk[:, 1024:1536].rearrange("p (h t) -> p h t", h=8)
    id8 = mk[:, 1536:2048].rearrange("p (h t) -> p h t", h=8)
    omka = k.sb("omka", [64, 8])
    cb2 = Buf("consts2")

    def setup2(e):
        o3 = ones[0:64, :].rearrange("p (a b) -> p a b", a=2)
        e.memset(mk[:], 1.0)
        e.affine_select(out=mLs, in_=mLs, pattern=[[0, 8], [-1, 64]], compare_op=ALU.is_gt, fill=0.0, base=0, channel_multiplier=1)
        e.affine_select(out=mUs, in_=mUs, pattern=[[0, 8], [1, 64]], compare_op=ALU.is_gt, fill=0.0, base=0, channel_multiplier=-1)
        e.affine_select(out=mUi, in_=mUi, pattern=[[0, 8], [1, 64]], compare_op=ALU.is_ge, fill=0.0, base=0, channel_multiplier=-1)
        return e.affine_select(out=id8, in_=id8, pattern=[[0, 8], [1, 64]], compare_op=ALU.is_equal, fill=0.0, base=0, channel_multiplier=-1)
    P.op("pool", setup2, W=[cb2])
    P.op("dve", lambda e: e.tensor_scalar(out=omka[:], in0=pv[0:64, PV_RW + 48:PV_RW + 56], scalar1=-1.0, scalar2=1.0, op0=ALU.mult, op1=ALU.add),
         R=[pvb], W=[cb2])
    P.op("pool", lambda e: e.memset(epsb_t[:, 1:2], 64e-5), W=[epsb])

    def rstd_group(g, rstd_ap, rstd_buf, sq_ap, sq_buf, bank, ndiv=1024.0):
        P.op("act", lambda e: e.activation(out=sq_ap, in_=xT[:, :, g * 512:(g + 1) * 512], func=AF.Square),
             R=[xb[c][g] for c in range(8)], W=[sq_buf])

        def mm(e):
            ins = None
            for c in range(8):
                ins = e.matmul(pbank[bank][:], ones[:], sq_ap[:, c, :], start=(c == 0), stop=(c == 7))
            return ins
        P.op("pe", mm, R=[sq_buf, onesb], W=[pbb[bank]])
        P.op("act", lambda e: e.activation(out=rstd_ap, in_=pbank[bank][:], func=AF.Ln, bias=epsb_t[:, 0:1], scale=1.0 / ndiv),
             R=[pbb[bank], epsb], W=[rstd_buf])
        P.op("act", lambda e: e.activation(out=rstd_ap, in_=rstd_ap, func=AF.Exp, scale=-0.5),
             R=[rstd_buf], W=[rstd_buf])

    def ffn(l, which):
        f = 0 if which == 0 else 1
        gcol = PV_NORMG + (l * 3 + which) * 8
        A = Arena(arena, ARENA)
        hT = A.take(8192).bitcast(BF16).rearrange("p (c t) -> p c t", c=8)
        act = A.take(11264).bitcast(BF16).rearrange("p (c t) -> p c t", c=11)
        sq = arena[:, 8192:8192 + 4096].rearrange("p (c t) -> p c t", c=8)
        rstd = A.take(512)
        sg = [A.take(512) for i in range(2)]
        NW = 3
        wgb = [A.take(512).bitcast(BF16).rearrange("p (c m) -> p c m", c=8) for i in range(NW)]
        wub = [A.take(512).bitcast(BF16).rearrange("p (c m) -> p c m", c=8) for i in range(NW)]
        wdb = [A.take(704).bitcast(BF16).rearrange("p (c m) -> p c m", c=11) for i in range(2)]
        wgs = [A.take(1024).rearrange("p (c m) -> p c m", c=8) for i in range(2)]
        wus = [A.take(1024).rearrange("p (c m) -> p c m", c=8) for i in range(2)]
        wds = [A.take(1408).rearrange("p (c m) -> p c m", c=11) for i in range(2)]
        wgsb = [Buf(), Buf()]; wusb = [Buf(), Buf()]; wdsb = [Buf(), Buf()]
        hb = [[Buf() for g in range(4)] for c in range(8)]
        actb = [[Buf() for g in range(4)] for c in range(11)]
        sqb, rstdb = Buf(), Buf()
        sgb = [Buf(), Buf()]
        wgbb = [Buf() for i in range(NW)]
        wubb = [Buf() for i in range(NW)]
        wdbb = [Buf(), Buf()]
        cnt = {"w": 0, "wd": 0, "p": 0, "s": 0, "ws": 0}
        for g in range(4):
            rstd_group(g, rstd, rstdb, sq, sqb, 7)
            for c in range(8):
                P.op("dve", lambda e, c=c, g=g: e.scalar_tensor_tensor(
                    out=hT[:, c, g * 512:(g + 1) * 512], in0=xT[:, c, g * 512:(g + 1) * 512],
                    scalar=pv[:, gcol + c:gcol + c + 1], in1=rstd, op0=ALU.mult, op1=ALU.mult),
                    R=[xb[c][g], rstdb, pvb], W=[hb[c][g]])
        P.fence()

        def phase_a(fc, fl):
            s = cnt["w"] % NW
            cnt["w"] += 1
            ss_ = cnt["ws"] % 2
            cnt["ws"] += 1
            P.dma("sp", wgs[ss_], wg_d[l][f][fc], W=[wgsb[ss_]])
            P.dma("sp", wus[ss_], wu_d[l][f][fc], W=[wusb[ss_]])
            P.op("pool", lambda e: e.tensor_copy(out=wgb[s], in_=wgs[ss_]), R=[wgsb[ss_]], W=[wgbb[s]])
            P.op("pool", lambda e: e.tensor_copy(out=wub[s], in_=wus[ss_]), R=[wusb[ss_]], W=[wubb[s]])
            def a_group(g):
                bg, bu = (cnt["p"] % 2) * 2, (cnt["p"] % 2) * 2 + 1
                cnt["p"] += 1
                si = cnt["s"] % 2
                cnt["s"] += 1

                def mmg(e):
                    ins = None
                    for c in range(8):
                        ins = e.matmul(pbank[bg][:], wgb[s][:, c, :], hT[:, c, g * 512:(g + 1) * 512], start=(c == 0), stop=(c == 7))
                    return ins

                def mmu(e):
                    ins = None
                    for c in range(8):
                        ins = e.matmul(pbank[bu][:], wub[s][:, c, :], hT[:, c, g * 512:(g + 1) * 512], start=(c == 0), stop=(c == 7))
                    return ins
                P.op("pe", mmg, R=[wgbb[s]] + [hb[c][g] for c in range(8)], W=[pbb[bg]])
                P.op("pe", mmu, R=[wubb[s]] + [hb[c][g] for c in range(8)], W=[pbb[bu]])
                P.op("act", lambda e: e.activation(out=sg[si], in_=pbank[bg][:], func=AF.Silu), R=[pbb[bg]], W=[sgb[si]])
                P.op("dve", lambda e: e.tensor_tensor(out=act[:, fl, g * 512:(g + 1) * 512], in0=pbank[bu][:], in1=sg[si], op=ALU.mult),
                     R=[pbb[bu], sgb[si]], W=[actb[fl][g]])
            for g_ in range(4):
                a_group(g_)

        def phase_b(fh, dc):
            s = cnt["wd"] % 2
            cnt["wd"] += 1
            P.dma("sp", wds[s], wd_d[l][f][dc][:, fh * 11:(fh + 1) * 11, :], W=[wdsb[s]])
            P.op("pool", lambda e: e.tensor_copy(out=wdb[s], in_=wds[s]), R=[wdsb[s]], W=[wdbb[s]])
            def b_group(g):
                bo = 4 + (cnt["p"] % 2)
                cnt["p"] += 1

                def mmd(e):
                    ins = None
                    for fl in range(11):
                        ins = e.matmul(pbank[bo][:], wdb[s][:, fl, :], act[:, fl, g * 512:(g + 1) * 512], start=(fl == 0), stop=(fl == 10))
                    return ins
                P.op("pe", mmd, R=[wdbb[s]] + [actb[fl][g] for fl in range(11)], W=[pbb[bo]])
                P.op("dve", lambda e: e.scalar_tensor_tensor(
                    out=xT[:, dc, g * 512:(g + 1) * 512], in0=pbank[bo][:], scalar=0.5,
                    in1=xT[:, dc, g * 512:(g + 1) * 512], op0=ALU.mult, op1=ALU.add),
                    R=[pbb[bo], xb[dc][g]], W=[xb[dc][g]])
            for g_ in range(4):
                b_group(g_)
        for fh in range(2):
            for fl in range(11):
                phase_a(fh * 11 + fl, fl)
            for dc in range(8):
                phase_b(fh, dc)
        P.fence()

    def final_out(norm):
        o = 0
        sq = arena[:, o:o + 4096].rearrange("p (c t) -> p c t", c=8); o += 4096
        rstd = arena[:, o:o + 512]; o += 512
        ob = [arena[:, o + i * 4096:o + (i + 1) * 4096].rearrange("p (c t) -> p c t", c=8) for i in range(2)]; o += 8192
        sqb, rstdb = Buf(), Buf()
        obb = [Buf(), Buf()]
        outs = []
        for g in range(4):
            s = g % 2
            if norm:
                rstd_group(g, rstd, rstdb, sq, sqb, 7)
                for c in range(8):
                    P.op("dve", lambda e, c=c, g=g, s=s: e.scalar_tensor_tensor(
                        out=ob[s][:, c, :], in0=xT[:, c, g * 512:(g + 1) * 512],
                        scalar=pv[:, PV_FINALG + c:PV_FINALG + c + 1], in1=rstd, op0=ALU.mult, op1=ALU.mult),
                        R=[xb[c][g], rstdb, pvb], W=[obb[s]])
                outs.append(P.dma("sp", out_d[:, :, g * 512:(g + 1) * 512], ob[s], R=[obb[s]]))
            else:
                outs.append(P.dma("sp", out_d[:, :, g * 512:(g + 1) * 512], xT[:, :, g * 512:(g + 1) * 512],
                                  R=[xb[c][g] for c in range(8)]))
        return outs


    def hgrn():
        A = Arena(arena, ARENA)
        hT = A.take(2048).bitcast(BF16).rearrange("p (c t) -> p c t", c=8)
        hb = [Buf() for c in range(8)]
        wts = [A.take(1024).rearrange("p (c m) -> p c m", c=8) for j in range(4)]
        wtsb = [Buf() for j in range(4)]
        wt = [[A.take(512).bitcast(BF16).rearrange("p (c m) -> p c m", c=8) for j in range(4)] for s_ in range(2)]
        wtb = [[Buf() for j in range(4)] for s_ in range(2)]
        names = ["qT", "fT", "lf", "kT", "bT", "eb", "e2", "oTs", "sqo", "rs2", "tmp"]
        T = {n: A.take(512) for n in names}
        TB = {n: Buf(n) for n in names}
        DB = []
        for i in range(2):
            d = {}
            for n in ("qe", "ke", "sgT"):
                d[n] = A.take(512); d[n + "b"] = Buf()
            for n in ("ke2tm", "vtm", "scs"):
                d[n] = A.take(512).rearrange("p (c m) -> p c m", c=4); d[n + "b"] = Buf()
            d["ebl"] = A.take(8); d["eblb"] = Buf()
            DB.append(d)
        state = [A.take(1024).rearrange("p (h v) -> p h v", h=8) for i in range(2)]
        stb = [[Buf() for h in range(8)] for i in range(2)]
        yT = A.take(2048).bitcast(BF16).rearrange("p (c t) -> p c t", c=8)
        yb = [Buf() for h in range(8)]
        wos = [A.take(1024).rearrange("p (c m) -> p c m", c=8) for i in range(1)]
        wosb = [Buf()]
        wo = [A.take(512).bitcast(BF16).rearrange("p (c m) -> p c m", c=8) for i in range(2)]
        wob = [Buf(), Buf()]
        sq = A.take(4096).rearrange("p (c t) -> p c t", c=8); sqb = Buf()
        rstd = A.take(512); rstdb = Buf()
        gcol = PV_NORMG + (1 * 3 + 1) * 8
        scur = [0] * 8
        cnt = {"wo": 0}
        for h in range(8):
            P.op("pool", lambda e, h=h: e.memset(state[0][:, h, :], 0.0), W=[stb[0][h]])

        def norm(g):
            rstd_group(g, rstd, rstdb, sq, sqb, 7)

            def hn(e):
                ins = None
                for c in range(8):
                    ins = e.scalar_tensor_tensor(out=hT[:, c, :], in0=xT[:, c, g * 512:(g + 1) * 512],
                                                 scalar=pv[:, gcol + c:gcol + c + 1], in1=rstd, op0=ALU.mult, op1=ALU.mult)
                return ins
            P.op("dve", hn, R=[xb[c][g] for c in range(8)] + [rstdb, pvb], W=hb)

        def prep(g, h, s_):
            d = DB[s_]
            for j in range(4):
                P.dma("sp", wts[j], odwin_d[j * 8 + h], W=[wtsb[j]])
                P.op("pool", lambda e, j=j: e.tensor_copy(out=wt[s_][j], in_=wts[j]), R=[wtsb[j]], W=[wtb[s_][j]])
            yield

            def proj(j, bank):
                def mm(e):
                    ins = None
                    for c in range(8):
                        ins = e.matmul(pbank[bank][:], wt[s_][j][:, c, :], hT[:, c, :], start=(c == 0), stop=(c == 7))
                    return ins
                P.op("pe", mm, R=[wtb[s_][j]] + hb, W=[pbb[bank]])
            proj(0, 0)
            P.op("act", lambda e: e.activation(out=T["qT"], in_=pbank[0][:], func=AF.Copy), R=[pbb[0]], W=[TB["qT"]])
            yield
            proj(1, 1)
            P.op("act", lambda e: e.activation(out=T["fT"], in_=pbank[1][:], func=AF.Sigmoid), R=[pbb[1]], W=[TB["fT"]])
            yield
            P.op("dve", lambda e: e.tensor_scalar(out=T["fT"], in0=T["fT"], scalar1=lbt[:, 8 + h:9 + h], scalar2=lbt[:, h:h + 1],
                                                  op0=ALU.mult, op1=ALU.add), R=[TB["fT"], lbb], W=[TB["fT"]])
            yield
            P.op("act", lambda e: e.activation(out=T["lf"], in_=T["fT"], func=AF.Ln), R=[TB["fT"]], W=[TB["lf"]])
            P.op("dve", lambda e: e.tensor_scalar(out=T["kT"], in0=T["fT"], scalar1=-1.0, scalar2=1.0, op0=ALU.mult, op1=ALU.add),
                 R=[TB["fT"]], W=[TB["kT"]])
            yield
            P.op("dve", lambda e: e.tensor_tensor_scan(out=T["bT"], data0=resetm[:], data1=T["lf"], initial=0.0, op0=ALU.mult, op1=ALU.add),
                 R=[TB["lf"], cb], W=[TB["bT"]])
            yield
            P.op("act", lambda e: e.activation(out=T["eb"], in_=T["bT"], func=AF.Exp), R=[TB["bT"]], W=[TB["eb"]])
            yield
            P.op("dve", lambda e: e.tensor_tensor(out=d["qe"], in0=T["qT"], in1=T["eb"], op=ALU.mult), R=[TB["qT"], TB["eb"]], W=[d["qeb"]])
            yield
            P.op("act", lambda e: e.activation(out=T["eb"], in_=T["bT"], func=AF.Exp, scale=-1.0), R=[TB["bT"], d["qeb"]], W=[TB["eb"]])
            yield
            P.op("dve", lambda e: e.tensor_tensor(out=d["ke"], in0=T["kT"], in1=T["eb"], op=ALU.mult), R=[TB["kT"], TB["eb"]], W=[d["keb"]])
            yield

            def e2f(e):
                ins = None
                for ci in range(8):
                    ins = e.activation(out=T["e2"][:, ci * 64:(ci + 1) * 64], in_=T["bT"][:, ci * 64:(ci + 1) * 64], func=AF.Exp,
                                       scale=-1.0, bias=T["bT"][:, ci * 64 + 63:ci * 64 + 64])
                return ins
            P.op("act", e2f, R=[TB["bT"]], W=[TB["e2"]])
            yield
            P.op("dve", lambda e: e.tensor_tensor(out=T["e2"], in0=T["e2"], in1=T["kT"], op=ALU.mult), R=[TB["kT"], TB["e2"]], W=[TB["e2"]])
            P.op("act", lambda e: e.activation(out=d["ebl"], in_=T["bT"].rearrange("p (c t) -> p c t", t=64)[:, :, 63], func=AF.Exp),
                 R=[TB["bT"]], W=[d["eblb"]])
            yield

            def tr(e):
                ins = None
                for tt in range(4):
                    ins = e.transpose(pbank[2][:, tt * 128:(tt + 1) * 128], T["e2"][:, tt * 128:(tt + 1) * 128], ident[:])
                return ins
            P.op("pe", tr, R=[TB["e2"], cb], W=[pbb[2]])
            P.op("act", lambda e: e.activation(out=d["ke2tm"], in_=pbank[2][:].rearrange("p (c m) -> p c m", c=4), func=AF.Copy),
                 R=[pbb[2]], W=[d["ke2tmb"]])
            yield

            def vproj(e):
                ins = None
                for tt in range(4):
                    for c in range(8):
                        ins = e.matmul(pbank[3][:, tt * 128:(tt + 1) * 128], hT[:, c, tt * 128:(tt + 1) * 128], wt[s_][2][:, c, :],
                                       start=(c == 0), stop=(c == 7))
                return ins
            P.op("pe", vproj, R=[wtb[s_][2]] + hb, W=[pbb[3]])
            P.op("dve", lambda e: e.tensor_copy(out=d["vtm"], in_=pbank[3][:].rearrange("p (c m) -> p c m", c=4)), R=[pbb[3]], W=[d["vtmb"]])
            yield
            proj(3, 0)
            P.op("act", lambda e: e.activation(out=d["sgT"], in_=pbank[0][:], func=AF.Sigmoid), R=[pbb[0]], W=[d["sgTb"]])
            yield

            def scm(e):
                ins = None
                for p_ in range(4):
                    ins = e.matmul(pbank[4][:, p_ * 128:(p_ + 1) * 128], d["ke"][:, p_ * 128:(p_ + 1) * 128],
                                   d["qe"][:, p_ * 128:(p_ + 1) * 128], start=True, stop=True)
                return ins
            P.op("pe", scm, R=[d["keb"], d["qeb"]], W=[pbb[4]])
            P.op("dve", lambda e: e.tensor_tensor(out=d["scs"], in0=pbank[4][:].rearrange("p (c m) -> p c m", c=4), in1=mask4, op=ALU.mult),
                 R=[pbb[4], cb], W=[d["scsb"]])
            yield

        def chunks(g, h, s_):
            d = DB[s_]

            def one(p_, half):
                ci = p_ * 2 + half
                sc = scur[h]
                cs = p_ * 128 + half * 64
                r0 = half * 64

                def omm(e):
                    if half == 0:
                        e.matmul(pbank[5][:, p_ * 128:(p_ + 1) * 128], d["vtm"][:, p_, :], d["scs"][:, p_, :], start=True, stop=False)
                    return e.matmul(pbank[5][:, cs:cs + 64], state[sc][:, h, :], d["qe"][:, cs:cs + 64], start=False, stop=(half == 1))
                P.op("pe", omm, R=[d["vtmb"], d["scsb"], stb[sc][h], d["qeb"]], W=[pbb[5]])
                P.op("pe", lambda e: e.matmul(pbank[6][:, 0:128], d["ke2tm"][r0:r0 + 64, p_, :], d["vtm"][r0:r0 + 64, p_, :],
                                              start=True, stop=True), R=[d["ke2tmb"], d["vtmb"]], W=[pbb[6]])
                P.op("dve", lambda e: e.scalar_tensor_tensor(
                    out=state[1 - sc][:, h, :], in0=state[sc][:, h, :], scalar=d["ebl"][:, ci:ci + 1], in1=pbank[6][:, 0:128],
                    op0=ALU.mult, op1=ALU.add), R=[stb[sc][h], d["eblb"], pbb[6]], W=[stb[1 - sc][h]])
                scur[h] = 1 - sc
            for p_ in range(4):
                for half in range(2):
                    one(p_, half)
                    yield
            P.op("act", lambda e: e.activation(out=T["oTs"], in_=pbank[5][:], func=AF.Copy), R=[pbb[5]], W=[TB["oTs"]])
            P.op("act", lambda e: e.activation(out=T["sqo"], in_=pbank[5][:], func=AF.Square), R=[pbb[5]], W=[TB["sqo"]])
            yield
            P.op("pe", lambda e: e.matmul(pbank[7][:], ones[:], T["sqo"], start=True, stop=True), R=[TB["sqo"], onesb], W=[pbb[7]])
            P.op("act", lambda e: e.activation(out=T["rs2"], in_=pbank[7][:], func=AF.Ln, bias=epsb_t[:, 0:1], scale=1.0 / 128.0),
                 R=[pbb[7], epsb], W=[TB["rs2"]])
            yield
            P.op("act", lambda e: e.activation(out=T["rs2"], in_=T["rs2"], func=AF.Exp, scale=-0.5), R=[TB["rs2"]], W=[TB["rs2"]])
            yield
            P.op("dve", lambda e: e.scalar_tensor_tensor(out=T["tmp"], in0=T["oTs"], scalar=pv[:, PV_HGNW + h:PV_HGNW + h + 1],
                                                         in1=T["rs2"], op0=ALU.mult, op1=ALU.mult),
                 R=[TB["oTs"], TB["rs2"], pvb], W=[TB["tmp"]])
            yield
            P.op("dve", lambda e: e.tensor_tensor(out=yT[:, h, :], in0=T["tmp"], in1=d["sgT"], op=ALU.mult),
                 R=[TB["tmp"], d["sgTb"]], W=[yb[h]])
            yield

        def outproj(g):
            for dc in range(8):
                s2 = cnt["wo"] % 2
                cnt["wo"] += 1
                P.dma("sp", wos[0], odwout_d[dc], W=[wosb[0]])
                P.op("pool", lambda e, s2=s2: e.tensor_copy(out=wo[s2], in_=wos[0]), R=[wosb[0]], W=[wob[s2]])

                def mo(e, s2=s2):
                    ins = None
                    for h in range(8):
                        ins = e.matmul(pbank[7][:], wo[s2][:, h, :], yT[:, h, :], start=(h == 0), stop=(h == 7))
                    return ins
                P.op("pe", mo, R=[wob[s2]] + yb, W=[pbb[7]])
                P.op("dve", lambda e, dc=dc: e.tensor_tensor(out=xT[:, dc, g * 512:(g + 1) * 512], in0=pbank[7][:],
                                                             in1=xT[:, dc, g * 512:(g + 1) * 512], op=ALU.add),
                     R=[pbb[7], xb[dc][g]], W=[xb[dc][g]])

        def interleave(a_, b_):
            gens = [x for x in (a_, b_) if x is not None]
            while gens:
                for x in list(gens):
                    try:
                        next(x)
                    except StopIteration:
                        gens.remove(x)
        prev = None
        prev_g = None
        idx = 0
        for g in range(4):
            for h in range(8):
                if h == 0:
                    norm(g)
                interleave(prev, prep(g, h, idx % 2))
                if prev is not None and h == 0:
                    outproj(prev_g)
                prev = chunks(g, h, idx % 2)
                prev_g = g
                idx += 1
        interleave(prev, None)
        outproj(3)
        P.fence()

    def rwkv():
        G = 128
        NG = S // G
        A = Arena(arena, ARENA)
        hT = A.take(4 * (G + 2)).bitcast(BF16).rearrange("p (c t) -> p c t", c=8); hb = Buf()
        sq = A.take(8 * G).rearrange("p (c t) -> p c t", c=8); sqb = Buf()
        rstd = A.take(G); rstdb = Buf()
        wrkvs = [A.take(512).rearrange("p (c m) -> p c m", c=8) for i in range(3)]
        wrkvsb = [Buf() for i in range(3)]
        wrkv = [[A.take(256).bitcast(BF16).rearrange("p (c m) -> p c m", c=8) for i in range(3)] for s_ in range(2)]
        wrkvb = [[Buf() for i in range(3)] for s_ in range(2)]
        wlos = A.take(1024).rearrange("p (c m) -> p c m", c=8); wlosb = Buf()
        wlo = [A.take(512).bitcast(BF16).rearrange("p (c m) -> p c m", c=8) for i in range(2)]
        wlob = [Buf(), Buf()]
        yfh = A.take(512).bitcast(BF16).rearrange("p (h t) -> p h t", h=8); yfhb = Buf()
        w2a2 = A.take(512); g2 = A.take(512); wconst = Buf()
        praw = [A.take(G + 1) for i in range(2)]; prawb = [Buf(), Buf()]
        dtmp = [A.take(G) for i in range(2)]; dtmpb = [Buf(), Buf()]
        tw = A.take(G); twb = Buf()
        sg = A.take(G); sgb = Buf()
        QN = ["r", "k", "v", "lw", "a", "kk", "Lw", "e", "at", "bt", "kt", "BhT", "KhT"]
        Q = {n: A.take(8 * G).rearrange("p (h t) -> p h t", h=8) for n in QN}
        QB = {n: Buf(n) for n in QN}
        HO_ = ("TT", "AakT", "Vtm")
        CN = ["P0", "P1", "PT0", "PT1", "Xs", "Us", "H0", "H1", "ArbT", "ArkT", "Bhtm", "Khtm"] + [n + "0" for n in HO_] + [n + "1" for n in HO_]
        Cc = {n: A.take(512).rearrange("p (h t) -> p h t", h=8) for n in CN}
        CB = {n: Buf(n) for n in CN}
        gC = A.take(16).rearrange("p (h c) -> p h c", h=8); gCb = Buf()
        wos = wlos.rearrange("p c m -> p (c m)").rearrange("p (h m) -> p h m", h=8); wosb = wlosb
        wo = [A.take(512).bitcast(BF16).rearrange("p (h m) -> p h m", h=8) for i in range(2)]; wob = [Buf(), Buf()]
        gcol = PV_NORMG + (0 * 3 + 1) * 8
        MU, W0, A0, KK, KA, OMKA, RK, LNW, LNB = PV_RW, PV_RW + 24, PV_RW + 32, PV_RW + 40, PV_RW + 48, PV_RW + 56, PV_RW + 64, PV_RW + 72, PV_RW + 80
        MUWA, MUG = PV_RW + 88, PV_RW + 89
        o64 = ones[0:64, 0:64]

        P.dma("sp", w2a2[0:64, :], rww2_d[:, :], W=[wconst])
        P.dma("sp", w2a2[64:128, :], rwa2_d[:, :], W=[wconst])
        P.dma("sp", g2, rwg2_d[:, :], W=[wconst])
        P.op("pool", lambda e: e.memset(Cc["H0"][0:64], 0.0), W=[CB["H0"]])
        P.op("pool", lambda e: e.memset(hT[:, :, 0:1], 0.0), W=[hb])
        hcur = 0
        woi = 0
        pi = 0
        def do_group(g):
            nonlocal hcur, woi, pi
            t0 = g * G
            g4 = t0 // 512
            xg = [xb[c][g4] for c in range(8)]
            if g > 0:
                P.op("dve", lambda e: e.tensor_copy(out=hT[:, :, 0:1], in_=hT[:, :, G:G + 1]), R=[hb], W=[hb])
            P.op("act", lambda e, t0=t0: e.activation(out=sq, in_=xT[:, :, t0:t0 + G], func=AF.Square), R=xg, W=[sqb])

            def mmn(e):
                ins = None
                for c in range(8):
                    ins = e.matmul(pbank[7][:, 0:G], ones[:], sq[:, c, :], start=(c == 0), stop=(c == 7))
                return ins
            P.op("pe", mmn, R=[sqb, onesb], W=[pbb[7]])
            P.op("act", lambda e: e.activation(out=rstd, in_=pbank[7][:, 0:G], func=AF.Ln, bias=epsb_t[:, 0:1], scale=1.0 / 1024.0),
                 R=[pbb[7], epsb], W=[rstdb])
            P.op("act", lambda e: e.activation(out=rstd, in_=rstd, func=AF.Exp, scale=-0.5), R=[rstdb], W=[rstdb])

            def hnorm(e, t0=t0):
                ins = None
                for c in range(8):
                    ins = e.scalar_tensor_tensor(out=hT[:, c, 1:G + 1], in0=xT[:, c, t0:t0 + G], scalar=pv[:, gcol + c:gcol + c + 1],
                                                 in1=rstd, op0=ALU.mult, op1=ALU.mult)
                return ins
            P.op("dve", hnorm, R=xg + [rstdb, pvb, hb], W=[hb])
            for h in range(8):
                for qi, qn in enumerate(("r", "k", "v")):
                    P.dma("sp", wrkvs[qi], rwin_d[qi * 8 + h], W=[wrkvsb[qi]])
                    ws_ = h % 2
                    P.op("pool", lambda e, qi=qi, ws_=ws_: e.tensor_copy(out=wrkv[ws_][qi], in_=wrkvs[qi]), R=[wrkvsb[qi]], W=[wrkvb[ws_][qi]])
                    bank = pi % 2
                    sl = pi % 2
                    pi += 1

                    def mm(e, qi=qi, bank=bank, ws_=ws_):
                        ins = None
                        for c in range(8):
                            ins = e.matmul(pbank[bank][0:64, 0:G + 1], wrkv[ws_][qi][:, c, :], hT[:, c, 0:G + 1], start=(c == 0), stop=(c == 7))
                        return ins
                    P.op("pe", mm, R=[wrkvb[ws_][qi], hb], W=[pbb[bank]])
                    P.op("act", lambda e, bank=bank, sl=sl: e.activation(out=praw[sl][0:64, :], in_=pbank[bank][0:64, 0:G + 1], func=AF.Copy),
                         R=[pbb[bank]], W=[prawb[sl]])
                    P.op("dve", lambda e, sl=sl: e.tensor_tensor(out=dtmp[sl][0:64, :], in0=praw[sl][0:64, 0:G], in1=praw[sl][0:64, 1:G + 1],
                                                                 op=ALU.subtract), R=[prawb[sl]], W=[dtmpb[sl]])
                    P.op("dve", lambda e, sl=sl, qn=qn, qi=qi, h=h: e.scalar_tensor_tensor(
                        out=Q[qn][0:64, h, :], in0=dtmp[sl][0:64, :], scalar=pv[0:64, MU + qi * 8 + h:MU + qi * 8 + h + 1],
                        in1=praw[sl][0:64, 1:G + 1], op0=ALU.mult, op1=ALU.add), R=[dtmpb[sl], prawb[sl], pvb], W=[QB[qn]])
            for j in range(2):
                P.dma("sp", wlos, rwlo_d[j], W=[wlosb])
                P.op("pool", lambda e, j=j: e.tensor_copy(out=wlo[j], in_=wlos), R=[wlosb], W=[wlob[j]])
                bank = pi % 2
                sl = pi % 2
                pi += 1

                def mml(e, j=j, bank=bank):
                    ins = None
                    for c in range(8):
                        ins = e.matmul(pbank[bank][:, 0:G + 1], wlo[j][:, c, :], hT[:, c, 0:G + 1], start=(c == 0), stop=(c == 7))
                    return ins
                P.op("pe", mml, R=[wlob[j], hb], W=[pbb[bank]])
                P.op("act", lambda e, bank=bank, sl=sl: e.activation(out=praw[sl], in_=pbank[bank][:, 0:G + 1], func=AF.Copy),
                     R=[pbb[bank]], W=[prawb[sl]])
                P.op("dve", lambda e, sl=sl: e.tensor_tensor(out=dtmp[sl], in0=praw[sl][:, 0:G], in1=praw[sl][:, 1:G + 1], op=ALU.subtract),
                     R=[prawb[sl]], W=[dtmpb[sl]])
                dst, dstb = (tw, twb) if j == 0 else (sg, sgb)
                mcol = MUWA if j == 0 else MUG
                P.op("dve", lambda e, sl=sl, dst=dst, mcol=mcol: e.scalar_tensor_tensor(
                    out=dst, in0=dtmp[sl], scalar=pv[:, mcol:mcol + 1], in1=praw[sl][:, 1:G + 1], op0=ALU.mult, op1=ALU.add),
                    R=[dtmpb[sl], prawb[sl], pvb], W=[dstb])
            P.op("act", lambda e: e.activation(out=tw[0:64, :], in_=tw[0:64, :], func=AF.Tanh), R=[twb], W=[twb])
            P.op("act", lambda e: e.activation(out=sg, in_=sg, func=AF.Sigmoid), R=[sgb], W=[sgb])
            for half in range(2):
                def mmw(e, half=half):
                    ins = None
                    for hh in range(4):
                        h = half * 4 + hh
                        ins = e.matmul(pbank[2][0:64, hh * G:(hh + 1) * G], w2a2[0:64, h * 64:(h + 1) * 64], tw[0:64, :], start=True, stop=True)
                    return ins
                P.op("pe", mmw, R=[wconst, twb], W=[pbb[2]])

                def sw(e, half=half):
                    ins = None
                    for hh in range(4):
                        h = half * 4 + hh
                        ins = e.activation(out=Q["lw"][0:64, h, :], in_=pbank[2][0:64, hh * G:(hh + 1) * G], func=AF.Sigmoid,
                                           bias=pv[0:64, W0 + h:W0 + h + 1])
                    return ins
                P.op("act", sw, R=[pbb[2], pvb], W=[QB["lw"]])

                def mma(e, half=half):
                    ins = None
                    for hh in range(4):
                        h = half * 4 + hh
                        ins = e.matmul(pbank[3][0:64, hh * G:(hh + 1) * G], w2a2[64:128, h * 64:(h + 1) * 64], tw[64:128, :], start=True, stop=True)
                    return ins
                P.op("pe", mma, R=[wconst, twb], W=[pbb[3]])

                def sa(e, half=half):
                    ins = None
                    for hh in range(4):
                        h = half * 4 + hh
                        ins = e.activation(out=Q["a"][0:64, h, :], in_=pbank[3][0:64, hh * G:(hh + 1) * G], func=AF.Sigmoid,
                                           bias=pv[0:64, A0 + h:A0 + h + 1])
                    return ins
                P.op("act", sa, R=[pbb[3], pvb], W=[QB["a"]])
            P.op("dve", lambda e: e.tensor_scalar(out=Q["lw"][0:64], in0=Q["lw"][0:64], scalar1=-0.6065306597126334, scalar2=None, op0=ALU.mult),
                 R=[QB["lw"]], W=[QB["lw"]])
            def kk1(e):
                ins = None
                for h in range(8):
                    ins = e.tensor_scalar(out=Q["kk"][0:64, h, :], in0=Q["k"][0:64, h, :], scalar1=pv[0:64, KK + h:KK + h + 1], scalar2=None, op0=ALU.mult)
                return ins
            P.op("dve", kk1, R=[QB["k"], pvb], W=[QB["kk"]])
            P.op("act", lambda e: e.activation(out=Q["e"][0:64], in_=Q["kk"][0:64], func=AF.Square), R=[QB["kk"]], W=[QB["e"]])
            for half in range(2):
                P.op("pe", lambda e, half=half: e.matmul(pbank[2][0:64, :], o64, Q["e"][0:64, half * 4:(half + 1) * 4, :], start=True, stop=True),
                     R=[QB["e"], onesb], W=[pbb[2]])
                P.op("dve", lambda e, half=half: e.tensor_scalar(out=Q["e"][0:64, half * 4:(half + 1) * 4, :],
                                                                 in0=pbank[2][0:64, :].rearrange("p (h t) -> p h t", h=4),
                                                                 scalar1=1e-24, scalar2=None, op0=ALU.max), R=[pbb[2], QB["e"]], W=[QB["e"]])
            P.op("act", lambda e: e.activation(out=Q["e"][0:64], in_=Q["e"][0:64], func=AF.Ln), R=[QB["e"]], W=[QB["e"]])
            P.op("act", lambda e: e.activation(out=Q["e"][0:64], in_=Q["e"][0:64], func=AF.Exp, scale=-0.5), R=[QB["e"]], W=[QB["e"]])
            P.op("dve", lambda e: e.tensor_tensor(out=Q["kk"][0:64], in0=Q["kk"][0:64], in1=Q["e"][0:64], op=ALU.mult),
                 R=[QB["kk"], QB["e"]], W=[QB["kk"]])
            def km1(e):
                ins = None
                for h in range(8):
                    ins = e.tensor_scalar(out=Q["e"][0:64, h, :], in0=Q["a"][0:64, h, :], scalar1=pv[0:64, KA + h:KA + h + 1],
                                          scalar2=omka[0:64, h:h + 1], op0=ALU.mult, op1=ALU.add)
                return ins
            P.op("dve", km1, R=[QB["a"], pvb, cb2], W=[QB["e"]])
            P.op("dve", lambda e: e.tensor_tensor(out=Q["k"][0:64], in0=Q["k"][0:64], in1=Q["e"][0:64], op=ALU.mult),
                 R=[QB["k"], QB["e"]], W=[QB["k"]])
            P.op("dve", lambda e: e.tensor_tensor(out=Q["a"][0:64], in0=Q["a"][0:64], in1=Q["kk"][0:64], op=ALU.mult),
                 R=[QB["a"], QB["kk"]], W=[QB["a"]])
            def bn1(e):
                ins = None
                for h in range(8):
                    ins = e.scalar_tensor_tensor(out=Q["e"][0:64, h, :], in0=Q["r"][0:64, h, :], scalar=pv[0:64, RK + h:RK + h + 1],
                                                 in1=Q["k"][0:64, h, :], op0=ALU.mult, op1=ALU.mult)
                return ins
            P.op("dve", bn1, R=[QB["r"], QB["k"], pvb], W=[QB["e"]])
            def scn(e):
                ins = None
                for h in range(8):
                    ins = e.tensor_tensor_scan(out=Q["Lw"][0:64, h, :], data0=resetm[0:64, 0:G], data1=Q["lw"][0:64, h, :], initial=0.0,
                                               op0=ALU.mult, op1=ALU.add)
                return ins
            P.op("dve", scn, R=[QB["lw"], cb], W=[QB["Lw"]])
            P.op("dve", lambda e: e.tensor_tensor(out=Q["lw"][0:64], in0=Q["Lw"][0:64], in1=Q["lw"][0:64], op=ALU.subtract),
                 R=[QB["Lw"], QB["lw"]], W=[QB["lw"]])
            for half in range(2):
                P.op("pe", lambda e, half=half: e.matmul(pbank[3][0:64, :], o64, Q["e"][0:64, half * 4:(half + 1) * 4, :], start=True, stop=True),
                     R=[QB["e"], onesb], W=[pbb[3]])
                P.op("dve", lambda e, half=half: e.tensor_tensor(out=Q["BhT"][0:64, half * 4:(half + 1) * 4, :],
                                                                 in0=pbank[3][0:64, :].rearrange("p (h t) -> p h t", h=4),
                                                                 in1=Q["v"][0:64, half * 4:(half + 1) * 4, :], op=ALU.mult),
                     R=[pbb[3], QB["v"]], W=[QB["BhT"]])
            P.op("act", lambda e: e.activation(out=Q["e"][0:64], in_=Q["lw"][0:64], func=AF.Exp), R=[QB["lw"]], W=[QB["e"]])
            P.op("dve", lambda e: e.scalar_tensor_tensor(out=Q["at"][0:64], in0=Q["kk"][0:64], scalar=-1.0, in1=Q["e"][0:64], op0=ALU.mult, op1=ALU.mult),
                 R=[QB["kk"], QB["e"]], W=[QB["at"]])
            P.op("act", lambda e: e.activation(out=Q["lw"][0:64], in_=Q["BhT"][0:64], func=AF.Copy), R=[QB["BhT"], QB["e"]], W=[QB["lw"]])
            P.op("act", lambda e: e.activation(out=Q["e"][0:64], in_=Q["Lw"][0:64], func=AF.Exp), R=[QB["Lw"], QB["at"]], W=[QB["e"]])
            P.op("dve", lambda e: e.tensor_tensor(out=Q["r"][0:64], in0=Q["r"][0:64], in1=Q["e"][0:64], op=ALU.mult), R=[QB["r"], QB["e"]], W=[QB["r"]])
            P.op("act", lambda e: e.activation(out=Q["e"][0:64], in_=Q["Lw"][0:64], func=AF.Exp, scale=-1.0), R=[QB["Lw"], QB["r"]], W=[QB["e"]])
            P.op("dve", lambda e: e.tensor_tensor(out=Q["bt"][0:64], in0=Q["a"][0:64], in1=Q["e"][0:64], op=ALU.mult), R=[QB["a"], QB["e"]], W=[QB["bt"]])
            P.op("dve", lambda e: e.tensor_tensor(out=Q["kt"][0:64], in0=Q["k"][0:64], in1=Q["e"][0:64], op=ALU.mult), R=[QB["k"], QB["e"]], W=[QB["kt"]])
            def eld(e):
                ins = None
                for h in range(8):
                    for ci in range(G // 64):
                        ins = e.activation(out=Q["e"][0:64, h, ci * 64:(ci + 1) * 64], in_=Q["Lw"][0:64, h, ci * 64:(ci + 1) * 64], func=AF.Exp,
                                           scale=-1.0, bias=Q["Lw"][0:64, h, ci * 64 + 63:ci * 64 + 64])
                return ins
            P.op("act", eld, R=[QB["Lw"], QB["bt"], QB["kt"]], W=[QB["e"]])
            P.op("act", lambda e: e.activation(out=gC[0:64], in_=Q["Lw"][0:64].rearrange("p h (c t) -> p h c t", t=64)[:, :, :, 63], func=AF.Exp),
                 R=[QB["Lw"]], W=[gCb])
            P.op("dve", lambda e: e.tensor_tensor(out=Q["BhT"][0:64], in0=Q["a"][0:64], in1=Q["e"][0:64], op=ALU.mult),
                 R=[QB["a"], QB["e"], QB["lw"]], W=[QB["BhT"]])
            P.op("dve", lambda e: e.tensor_tensor(out=Q["KhT"][0:64], in0=Q["k"][0:64], in1=Q["e"][0:64], op=ALU.mult),
                 R=[QB["k"], QB["e"]], W=[QB["KhT"]])
            if dbg_d is not None and g == 0:
                for i_, n_ in enumerate(["r", "k", "v", "lw", "a", "kk", "Lw", "at", "bt", "kt", "BhT", "KhT"]):
                    dbg_ops.append(P.dma("sp", dbg_d[:, i_, :].rearrange("p (h t) -> p h t", h=8), Q[n_][0:64], R=[QB[n_]]))
            def chunk_indep(ci):
                cs = ci * 64
                sfx = str(ci % 2)
                N_ = lambda n: n + sfx if n in HO_ else n

                def amat(bank, ln, rn, dst, mask, eng):
                    def mm(e):
                        ins = None
                        for h in range(8):
                            ins = e.matmul(pbank[bank][0:64, h * 64:(h + 1) * 64], Q[ln][0:64, h, cs:cs + 64], Q[rn][0:64, h, cs:cs + 64],
                                           start=True, stop=True)
                        return ins
                    P.op("pe", mm, R=[QB[ln], QB[rn]], W=[pbb[bank]])
                    P.op(eng, lambda e: e.tensor_tensor(out=Cc[N_(dst)][0:64], in0=pbank[bank][0:64, :].rearrange("p (h t) -> p h t", h=8),
                                                        in1=mask, op=ALU.mult), R=[pbb[bank], cb2], W=[CB[N_(dst)]])
                amat(2, "at", "bt", "P0", mLs, "dve")
                yield
                amat(3, "bt", "at", "PT0", mUs, "dve")
                yield
                amat(2, "kt", "at", "AakT", mUs, "dve")
                yield
                P.op("dve", lambda e: e.tensor_tensor(out=Cc[N_("TT")][0:64], in0=Cc["PT0"][0:64], in1=id8, op=ALU.add), R=[CB["PT0"], cb2], W=[CB[N_("TT")]])
                def do_tr(pairs):
                    for src, dst in pairs:
                        def trp(e, src=src):
                            ins = None
                            for h in range(8):
                                ins = e.transpose(pbank[4][0:64, h * 64:(h + 1) * 64], Q[src][0:64, h, cs:cs + 64], ident[0:64, 0:64])
                            return ins
                        P.op("pe", trp, R=[QB[src], cb], W=[pbb[4]])
                        P.op("act", lambda e, dst=dst: e.activation(out=Cc[N_(dst)][0:64], in_=pbank[4][0:64, :].rearrange("p (h t) -> p h t", h=8), func=AF.Copy),
                             R=[pbb[4]], W=[CB[N_(dst)]])
                        yield

                yield from do_tr((("v", "Vtm"),))
                pc = 0
                for lev in range(1, 6):
                    Pn, Pp = "P%d" % (1 - pc), "P%d" % pc
                    PTn, PTp = "PT%d" % (1 - pc), "PT%d" % pc

                    def sqm(e, Pp=Pp, PTp=PTp):
                        ins = None
                        for h in range(8):
                            ins = e.matmul(pbank[2][0:64, h * 64:(h + 1) * 64], Cc[PTp][0:64, h, :], Cc[Pp][0:64, h, :], start=True, stop=True)
                        return ins
                    P.op("pe", sqm, R=[CB[Pp], CB[PTp]], W=[pbb[2]])
                    P.op("act", lambda e, Pn=Pn: e.activation(out=Cc[Pn][0:64], in_=pbank[2][0:64, :].rearrange("p (h t) -> p h t", h=8), func=AF.Copy),
                         R=[pbb[2]], W=[CB[Pn]])
                    yield
                    if lev < 5:
                        def sqt(e, Pp=Pp, PTp=PTp):
                            ins = None
                            for h in range(8):
                                ins = e.matmul(pbank[3][0:64, h * 64:(h + 1) * 64], Cc[Pp][0:64, h, :], Cc[PTp][0:64, h, :], start=True, stop=True)
                            return ins
                        P.op("pe", sqt, R=[CB[Pp], CB[PTp]], W=[pbb[3]])
                        P.op("dve", lambda e, PTn=PTn: e.tensor_copy(out=Cc[PTn][0:64], in_=pbank[3][0:64, :].rearrange("p (h t) -> p h t", h=8)),
                             R=[pbb[3]], W=[CB[PTn]])
                        yield

                    def ttm(e, Pn=Pn):
                        ins = None
                        for h in range(8):
                            ins = e.matmul(pbank[7][0:64, h * 64:(h + 1) * 64], Cc[Pn][0:64, h, :], Cc[N_("TT")][0:64, h, :], start=True, stop=True)
                        return ins
                    P.op("pe", ttm, R=[CB[Pn], CB[N_("TT")]], W=[pbb[7]])
                    P.op("dve", lambda e: e.tensor_tensor(out=Cc[N_("TT")][0:64], in0=pbank[7][0:64, :].rearrange("p (h t) -> p h t", h=8),
                                                          in1=Cc[N_("TT")][0:64], op=ALU.add), R=[pbb[7], CB[N_("TT")]], W=[CB[N_("TT")]])
                    yield
                    pc = 1 - pc
                amat(3, "bt", "r", "ArbT", mUi, "dve")
                yield
                amat(2, "kt", "r", "ArkT", mUi, "dve")
                yield
                yield from do_tr((("BhT", "Bhtm"), ("KhT", "Khtm")))
            def chunk_dep(ci):
                nonlocal hcur
                cs = ci * 64
                sfx = str(ci % 2)
                N_ = lambda n: n + sfx if n in HO_ else n
                Hc, Hn = "H%d" % hcur, "H%d" % (1 - hcur)

                def xmm(e, Hc=Hc):
                    ins = None
                    for h in range(8):
                        e.matmul(pbank[6][0:64, h * 64:(h + 1) * 64], Q["at"][0:64, h, cs:cs + 64], Cc[Hc][0:64, h, :], start=True, stop=False)
                        ins = e.matmul(pbank[6][0:64, h * 64:(h + 1) * 64], Cc[N_("AakT")][0:64, h, :], Cc[N_("Vtm")][0:64, h, :], start=False, stop=True)
                    return ins
                P.op("pe", xmm, R=[QB["at"], CB[Hc], CB[N_("AakT")], CB[N_("Vtm")]], W=[pbb[6]])
                P.op("act", lambda e: e.activation(out=Cc["Xs"][0:64], in_=pbank[6][0:64, :].rearrange("p (h t) -> p h t", h=8), func=AF.Copy),
                     R=[pbb[6]], W=[CB["Xs"]])
                yield
                yield
                yield
                yield

                def umm(e):
                    ins = None
                    for h in range(8):
                        ins = e.matmul(pbank[6][0:64, h * 64:(h + 1) * 64], Cc[N_("TT")][0:64, h, :], Cc["Xs"][0:64, h, :], start=True, stop=True)
                    return ins
                P.op("pe", umm, R=[CB[N_("TT")], CB["Xs"]], W=[pbb[6]])
                P.op("act", lambda e: e.activation(out=Cc["Us"][0:64], in_=pbank[6][0:64, :].rearrange("p (h t) -> p h t", h=8), func=AF.Copy),
                     R=[pbb[6]], W=[CB["Us"]])
                yield
                yield
                yield
                yield

                def ymm(e, Hc=Hc):
                    ins = None
                    for h in range(8):
                        e.matmul(pbank[0][0:64, h * 64:(h + 1) * 64], Cc[Hc][0:64, h, :], Q["r"][0:64, h, cs:cs + 64], start=True, stop=False)
                        e.matmul(pbank[0][0:64, h * 64:(h + 1) * 64], Cc["Us"][0:64, h, :], Cc[N_("ArbT")][0:64, h, :], start=False, stop=False)
                        ins = e.matmul(pbank[0][0:64, h * 64:(h + 1) * 64], Cc[N_("Vtm")][0:64, h, :], Cc[N_("ArkT")][0:64, h, :], start=False, stop=True)
                    return ins
                P.op("pe", ymm, R=[CB[Hc], QB["r"], CB["Us"], CB[N_("ArbT")], CB[N_("Vtm")], CB[N_("ArkT")]], W=[pbb[0]])
                P.op("act", lambda e: e.activation(out=Q["Lw"][0:64, :, cs:cs + 64], in_=pbank[0][0:64, :].rearrange("p (h t) -> p h t", h=8), func=AF.Copy),
                     R=[pbb[0], gCb], W=[QB["Lw"]])

                def hmm(e):
                    ins = None
                    for h in range(8):
                        e.matmul(pbank[1][0:64, h * 64:(h + 1) * 64], Cc[N_("Bhtm")][0:64, h, :], Cc["Us"][0:64, h, :], start=True, stop=False)
                        ins = e.matmul(pbank[1][0:64, h * 64:(h + 1) * 64], Cc[N_("Khtm")][0:64, h, :], Cc[N_("Vtm")][0:64, h, :], start=False, stop=True)
                    return ins
                P.op("pe", hmm, R=[CB[N_("Bhtm")], CB["Us"], CB[N_("Khtm")], CB[N_("Vtm")]], W=[pbb[1]])

                def hup(e, Hc=Hc, Hn=Hn, ci=ci):
                    ins = None
                    for h in range(8):
                        ins = e.scalar_tensor_tensor(out=Cc[Hn][0:64, h, :], in0=Cc[Hc][0:64, h, :], scalar=gC[0:64, h, ci:ci + 1],
                                                     in1=pbank[1][0:64, h * 64:(h + 1) * 64], op0=ALU.mult, op1=ALU.add)
                    return ins
                P.op("dve", hup, R=[CB[Hc], gCb, pbb[1]], W=[CB[Hn]])
                hcur = 1 - hcur
                yield
                if dbg_d is not None and g == 0 and ci == 0:
                    for i_, n_ in enumerate(["P0", "PT0", "TT0", "AakT0", "ArbT", "ArkT", "Vtm0", "Bhtm", "Khtm", "Xs", "Us", Hn]):
                        dbg_ops.append(P.dma("sp", dbg_d[:, 12 + i_, 0:512].rearrange("p (h t) -> p h t", h=8), Cc[n_][0:64], R=[CB[n_]]))
            def _il(a_, b_):
                gens = [x for x in (a_, b_) if x is not None]
                while gens:
                    for x in list(gens):
                        try:
                            next(x)
                        except StopIteration:
                            gens.remove(x)
            nch = G // 64
            _il(chunk_indep(0), None)
            for ci_ in range(nch):
                _il(chunk_dep(ci_), chunk_indep(ci_ + 1) if ci_ + 1 < nch else None)
            if dbg_d is not None and g == 0:
                dbg_ops.append(P.dma("sp", dbg_d[:, 24, :].rearrange("p (h t) -> p h t", h=8), Q["Lw"][0:64], R=[QB["Lw"]]))
            yr = Q["Lw"]; yrb = QB["Lw"]
            cen = Q["at"]; cenb = QB["at"]
            for half in range(2):
                P.op("pe", lambda e, half=half: e.matmul(pbank[2][0:64, :], o64, yr[0:64, half * 4:(half + 1) * 4, :], start=True, stop=True),
                     R=[yrb, onesb], W=[pbb[2]])
                P.op("dve", lambda e, half=half: e.scalar_tensor_tensor(out=cen[0:64, half * 4:(half + 1) * 4, :],
                                                                        in0=pbank[2][0:64, :].rearrange("p (h t) -> p h t", h=4), scalar=-1.0 / 64.0,
                                                                        in1=yr[0:64, half * 4:(half + 1) * 4, :], op0=ALU.mult, op1=ALU.add),
                     R=[pbb[2], yrb, CB["H0"], CB["H1"]], W=[cenb])
            P.op("act", lambda e: e.activation(out=Q["e"][0:64], in_=cen[0:64], func=AF.Square), R=[cenb, QB["BhT"], QB["KhT"]], W=[QB["e"]])
            for half in range(2):
                P.op("pe", lambda e, half=half: e.matmul(pbank[3][0:64, :], o64, Q["e"][0:64, half * 4:(half + 1) * 4, :], start=True, stop=True),
                     R=[QB["e"], onesb], W=[pbb[3]])
                P.op("act", lambda e, half=half: e.activation(out=Q["bt"][0:64, half * 4:(half + 1) * 4, :],
                                                               in_=pbank[3][0:64, :].rearrange("p (h t) -> p h t", h=4), func=AF.Ln,
                                                               bias=epsb_t[0:64, 1:2], scale=1.0 / 64.0), R=[pbb[3], epsb], W=[QB["bt"]])
            P.op("act", lambda e: e.activation(out=Q["bt"][0:64], in_=Q["bt"][0:64], func=AF.Exp, scale=-0.5), R=[QB["bt"]], W=[QB["bt"]])
            P.op("dve", lambda e: e.tensor_tensor(out=cen[0:64], in0=cen[0:64], in1=Q["bt"][0:64], op=ALU.mult), R=[cenb, QB["bt"]], W=[cenb])

            def lnx(e):
                ins = None
                for h in range(8):
                    ins = e.tensor_scalar(out=cen[0:64, h, :], in0=cen[0:64, h, :], scalar1=pv[0:64, LNW + h:LNW + h + 1],
                                          scalar2=pv[0:64, LNB + h:LNB + h + 1], op0=ALU.mult, op1=ALU.add)
                return ins
            P.op("dve", lnx, R=[cenb, pvb], W=[cenb])
            P.op("dve", lambda e: e.tensor_tensor(out=cen[0:64], in0=cen[0:64], in1=Q["lw"][0:64], op=ALU.add), R=[cenb, QB["lw"]], W=[cenb])
            for half in range(2):
                def gmm(e, half=half):
                    ins = None
                    for hh in range(4):
                        h = half * 4 + hh
                        ins = e.matmul(pbank[2][0:64, hh * G:(hh + 1) * G], g2[:, h * 64:(h + 1) * 64], sg, start=True, stop=True)
                    return ins
                P.op("pe", gmm, R=[wconst, sgb], W=[pbb[2]])
                P.op("dve", lambda e, half=half: e.tensor_tensor(out=yfh[0:64, half * 4:(half + 1) * 4, :], in0=pbank[2][0:64, :].rearrange("p (h t) -> p h t", h=4),
                                                                 in1=cen[0:64, half * 4:(half + 1) * 4, :], op=ALU.mult),
                     R=[pbb[2], cenb], W=[yfhb])
            yf = yfh; yfb = yfhb
            for dh in range(2):
                for dd in range(4):
                    dc = dh * 4 + dd
                    s2 = woi % 2
                    woi += 1
                    stg_ap, stg_b = (wos, wosb) if dc % 2 == 0 else (sq, sqb)
                    ob_ = 7 if dh == 0 else 4
                    P.dma("sp", stg_ap[0:64], rwout_d[dc], W=[stg_b])
                    P.op("pool", lambda e, s2=s2, stg_ap=stg_ap: e.tensor_copy(out=wo[s2][0:64], in_=stg_ap[0:64]), R=[stg_b], W=[wob[s2]])

                    def mo(e, s2=s2, dd=dd, dh=dh, ob_=ob_):
                        ins = None
                        for h in range(8):
                            ins = e.matmul(pbank[ob_][:, dd * G:(dd + 1) * G], wo[s2][0:64, h, :], yf[0:64, h, :], start=(h == 0), stop=(h == 7))
                        return ins
                    P.op("pe", mo, R=[wob[s2], yfb], W=[pbb[ob_]])
                P.op("dve", lambda e, dh=dh, t0=t0, ob_=ob_: e.tensor_tensor(out=xT[:, dh * 4:(dh + 1) * 4, t0:t0 + G],
                                                                    in0=pbank[ob_][:, 0:4 * G].rearrange("p (c t) -> p c t", c=4),
                                                                    in1=xT[:, dh * 4:(dh + 1) * 4, t0:t0 + G], op=ALU.add),
                     R=[pbb[ob_]] + [xb[c][g4] for c in range(dh * 4, dh * 4 + 4)], W=[xb[c][g4] for c in range(dh * 4, dh * 4 + 4)])
        for g_ in range(NG):
            do_group(g_)
        P.fence()


    def moba():
        G = 256
        NEG = 240000.0
        A = Arena(arena, ARENA)
        kT = A.take(8192).rearrange("p (c t) -> p c t", c=4)
        kTb = [[Buf() for g in range(8)] for c in range(4)]
        Va = A.take(16 * 8 * 65).rearrange("p (k h d) -> p k h d", k=16, h=8)
        Vab = [Buf() for kt in range(16)]
        hT = A.take(2048).rearrange("p (c t) -> p c t", c=8); hb = Buf()
        sq = A.take(2048).rearrange("p (c t) -> p c t", c=8); sqb = Buf()
        rstd = A.take(256); rstdb = Buf()
        wt = [A.take(1024).rearrange("p (c m) -> p c m", c=8) for i in range(2)]; wtb = [Buf(), Buf()]
        qT = A.take(1024).rearrange("p (c t) -> p c t", c=4); qTb = [Buf() for c in range(4)]
        ksum = A.take(32).rearrange("p (c j) -> p c j", c=4); ksb = Buf()
        gsel = A.take(16).rearrange("p (a j) -> p a j", a=2); gselb = Buf()
        m8 = A.take(16).rearrange("p (a j) -> p a j", a=2); m8b = Buf()
        negm = A.take(16).rearrange("p (a j) -> p a j", a=2); negmb = Buf()
        qaug = [A.take(256) for i in range(2)]; qaugb = [Buf(), Buf()]
        kaug = A.take(2048); kaugb = Buf()
        PT = [A.take(256) for i in range(2)]; PTb = [Buf() for i in range(2)]
        stmp = [A.take(256) for i in range(1)]; stmpb = [Buf()]
        oa = A.take(256); oab = Buf()
        rden = A.take(256); rdenb = Buf()
        oh = A.take(2048).rearrange("p (h t) -> p h t", h=8); ohb = [Buf() for h in range(8)]
        wo = [A.take(1024).rearrange("p (h m) -> p h m", h=8) for i in range(1)]; wob = [Buf()]
        stg = [A.take(512).rearrange("p (c t) -> p c t", c=2) for i in range(2)]; stgb = [Buf(), Buf()]
        ncA = A.take(256); ncB = A.take(256); sel65 = A.take(64); mcb = Buf()
        gcol = PV_NORMG + (0 * 3 + 1) * 8
        cnt = {"w": 0, "p": 0, "pt": 0, "st": 0, "wo": 0, "qa": 0}

        def setup(e):
            e.memset(ncA[:], 0.0)
            e.memset(ncB[:], 0.0)
            e.affine_select(out=ncA[:], in_=ncA[:], pattern=[[1, 256]], compare_op=ALU.is_ge, fill=-NEG, base=0, channel_multiplier=-1)
            e.affine_select(out=ncB[:], in_=ncB[:], pattern=[[1, 256]], compare_op=ALU.is_ge, fill=-NEG, base=-128, channel_multiplier=-1)
            e.memset(sel65[0:64, :], 0.0)
            e.memset(sel65[64:65, :], 1.0)
            return e.memset(Va[:, :, :, 64:65], 1.0)
        P.op("pool", setup, W=[mcb] + Vab)
        P.dma("sp", kaug[0:10, :], mbkaug_d[:, :], W=[kaugb])

        def proj_fm(widx, dst_ap, dst_bufs):
            s_ = cnt["w"] % 2
            cnt["w"] += 1
            bank = cnt["p"] % 2
            cnt["p"] += 1
            P.dma("sp", wt[s_], mbin_d[widx], W=[wtb[s_]])

            def mm(e):
                ins = None
                for c in range(8):
                    ins = e.matmul(pbank[bank][:, 0:G], wt[s_][:, c, :], hT[:, c, :], start=(c == 0), stop=(c == 7))
                return ins
            P.op("pe", mm, R=[wtb[s_], hb], W=[pbb[bank]])
            P.op("act", lambda e: e.activation(out=dst_ap, in_=pbank[bank][:, 0:G], func=AF.Copy), R=[pbb[bank]], W=dst_bufs)

        def proj_v(g, hp):
            s_ = cnt["w"] % 2
            cnt["w"] += 1
            bank = cnt["p"] % 2
            cnt["p"] += 1
            P.dma("sp", wt[s_], mbin_d[8 + hp], W=[wtb[s_]])

            def mm(e):
                ins = None
                for tt in range(2):
                    for c in range(8):
                        ins = e.matmul(pbank[bank][:, tt * 128:(tt + 1) * 128], hT[:, c, tt * 128:(tt + 1) * 128], wt[s_][:, c, :],
                                       start=(c == 0), stop=(c == 7))
                return ins
            P.op("pe", mm, R=[wtb[s_], hb], W=[pbb[bank]])
            P.op("dve", lambda e: e.tensor_copy(out=Va[:, 2 * g:2 * g + 2, 2 * hp:2 * hp + 2, 0:64],
                                                in_=pbank[bank][:, 0:256].rearrange("p (k h d) -> p k h d", k=2, h=2)),
                 R=[pbb[bank]], W=[Vab[2 * g], Vab[2 * g + 1]])

        def prep_head(g, h):
            hp, ph = h // 2, h % 2
            r0 = ph * 64
            ob = g
            qa = qaug[h % 2]
            qab = qaugb[h % 2]
            P.dma("sp", qa[8:10, :], mbqc_d[h, :, g * G:(g + 1) * G], W=[qab])
            if ob >= 4:
                def gmm(e):
                    ins = None
                    for tt in range(2):
                        ins = e.matmul(pbank[2][:, tt * 8:(tt + 1) * 8], qT[r0:r0 + 64, hp, tt * 128:(tt + 1) * 128], ksum[r0:r0 + 64, hp, :],
                                       start=True, stop=True)
                    return ins
                P.op("pe", gmm, R=[qTb[hp], ksb], W=[pbb[2]])
                P.op("pool", lambda e: e.memset(gsel, -1e30), W=[gselb])
                P.op("dve", lambda e: e.tensor_copy(out=gsel[:, :, 0:ob], in_=pbank[2][:, 0:16].rearrange("p (a j) -> p a j", a=2)[:, :, 0:ob]),
                     R=[pbb[2]], W=[gselb])

                def mx(e):
                    e.max(out=m8[:, 0, :], in_=gsel[:, 0, :])
                    return e.max(out=m8[:, 1, :], in_=gsel[:, 1, :])
                P.op("dve", mx, R=[gselb], W=[m8b])

                def ng(e):
                    ins = None
                    for tt in range(2):
                        ins = e.tensor_scalar(out=negm[:, tt, :], in0=gsel[:, tt, :], scalar1=m8[:, tt, 2:3], scalar2=1.0, op0=ALU.is_ge, op1=ALU.subtract)
                    return ins
                P.op("dve", ng, R=[gselb, m8b], W=[negmb])
                P.op("dve", lambda e: e.memset(negm[:, :, ob:8], 0.0), R=[], W=[negmb])

                def trn(e):
                    ins = None
                    for tt in range(2):
                        ins = e.transpose(pbank[2][0:8, 128 + tt * 128:128 + (tt + 1) * 128], negm[:, tt, :], ident[:])
                    return ins
                P.op("pe", trn, R=[negmb, cb], W=[pbb[2]])
                P.op("act", lambda e: e.activation(out=qa[0:8, :], in_=pbank[2][0:8, 128:384], func=AF.Copy), R=[pbb[2]], W=[qab])
            else:
                P.op("pool", lambda e: e.memset(qa[0:8, :], 0.0), W=[qab])

        def attn_head(g, h, pending_tail=None):
            hp, ph = h // 2, h % 2
            r0 = ph * 64
            ob = g
            qa = qaug[h % 2]
            qab = qaugb[h % 2]
            ob5 = 5 + (h % 2)
            nkt = 2 * ob + 2
            pend = []

            def emit_pv(kt, c0, pti):
                P.op("pe", lambda e: e.matmul(pbank[ob5][0:65, c0:G], Va[:, kt, h, :], PT[pti][:, c0:G], start=(kt == 0), stop=(kt == nkt - 1)),
                     R=[Vab[kt], PTb[pti]], W=[pbb[ob5]])
            for kt in range(nkt):
                diag = kt - 2 * ob
                c0 = 128 if diag == 1 else 0
                n = G - c0
                sb_ = 3 + (cnt["st"] % 2)
                cnt["st"] += 1
                pti = cnt["pt"] % 2
                cnt["pt"] += 1

                def smm(e, kt=kt, c0=c0, sb_=sb_):
                    e.matmul(pbank[sb_][:, c0:G], kT[r0:r0 + 64, hp, kt * 128:(kt + 1) * 128], qT[r0:r0 + 64, hp, c0:G], start=True, stop=False)
                    return e.matmul(pbank[sb_][:, c0:G], kaug[0:10, kt * 128:(kt + 1) * 128], qa[0:10, c0:G], start=False, stop=True)
                P.op("pe", smm, R=[kTb[hp][kt // 2], qTb[hp], kaugb, qab], W=[pbb[sb_]])
                if diag >= 0:
                    nc_ = ncA if diag == 0 else ncB
                    si = 0
                    P.op("dve", lambda e, c0=c0, sb_=sb_, nc_=nc_, si=si: e.tensor_tensor(out=stmp[si][:, c0:G], in0=pbank[sb_][:, c0:G], in1=nc_[:, c0:G], op=ALU.add),
                         R=[pbb[sb_], mcb], W=[stmpb[si]])
                    P.op("act", lambda e, c0=c0, pti=pti, si=si: e.activation(out=PT[pti][:, c0:G], in_=stmp[si][:, c0:G], func=AF.Exp, scale=0.125),
                         R=[stmpb[si]], W=[PTb[pti]])
                else:
                    P.op("act", lambda e, c0=c0, pti=pti, sb_=sb_: e.activation(out=PT[pti][:, c0:G], in_=pbank[sb_][:, c0:G], func=AF.Exp, scale=0.125),
                         R=[pbb[sb_]], W=[PTb[pti]])
                pend.append((kt, c0, pti))
                if len(pend) > 1:
                    emit_pv(*pend.pop(0))
                if pending_tail is not None and kt == min(1, nkt - 1):
                    pending_tail()
                    pending_tail = None
            while pend:
                emit_pv(*pend.pop(0))
            def tail():
                P.op("act", lambda e: e.activation(out=oa[0:65, :], in_=pbank[ob5][0:65, 0:G], func=AF.Copy), R=[pbb[ob5]], W=[oab])
                P.op("pe", lambda e: e.matmul(pbank[7][0:64, 0:G], sel65[0:65, :], oa[0:65, :], start=True, stop=True), R=[oab, mcb], W=[pbb[7]])
                P.op("dve", lambda e: e.reciprocal(out=rden[0:64, :], in_=pbank[7][0:64, 0:G]), R=[pbb[7]], W=[rdenb])
                P.op("dve", lambda e: e.tensor_tensor(out=oh[0:64, h, :], in0=oa[0:64, :], in1=rden[0:64, :], op=ALU.mult), R=[oab, rdenb], W=[ohb[h]])
            return tail

        def do_group(g):
            t0 = g * G
            g4 = t0 // 512
            xg = [xb[c][g4] for c in range(8)]
            P.op("act", lambda e: e.activation(out=sq, in_=xT[:, :, t0:t0 + G], func=AF.Square), R=xg, W=[sqb])

            def mmn(e):
                ins = None
                for c in range(8):
                    ins = e.matmul(pbank[7][:, 0:G], ones[:], sq[:, c, :], start=(c == 0), stop=(c == 7))
                return ins
            P.op("pe", mmn, R=[sqb, onesb], W=[pbb[7]])
            P.op("act", lambda e: e.activation(out=rstd, in_=pbank[7][:, 0:G], func=AF.Ln, bias=epsb_t[:, 0:1], scale=1.0 / 1024.0),
                 R=[pbb[7], epsb], W=[rstdb])
            P.op("act", lambda e: e.activation(out=rstd, in_=rstd, func=AF.Exp, scale=-0.5), R=[rstdb], W=[rstdb])

            def hnorm(e):
                ins = None
                for c in range(8):
                    ins = e.scalar_tensor_tensor(out=hT[:, c, :], in0=xT[:, c, t0:t0 + G], scalar=pv[:, gcol + c:gcol + c + 1],
                                                 in1=rstd, op0=ALU.mult, op1=ALU.mult)
                return ins
            P.op("dve", hnorm, R=xg + [rstdb, pvb], W=[hb])
            for hp in range(4):
                proj_fm(hp, qT[:, hp, :], [qTb[hp]])
                proj_fm(4 + hp, kT[:, hp, t0:t0 + G], [kTb[hp][g]])
                proj_v(g, hp)
            P.op("dve", lambda e: e.tensor_reduce(out=ksum[:, :, g], in_=kT[:, :, t0:t0 + G], axis=AX.X, op=ALU.add),
                 R=[kTb[hp][g] for hp in range(4)], W=[ksb])
            prep_head(g, 0)
            tl = None
            for h in range(8):
                if h + 1 < 8:
                    prep_head(g, h + 1)
                tl = attn_head(g, h, tl)
            tl()
            if dbg_d is not None and g == 0:
                dbg_ops.append(P.dma("sp", dbg_d[:, 0:2, :].rearrange("p a (h t) -> p (a h) t", h=4), oh[0:64], R=ohb))
                dbg_ops.append(P.dma("sp", dbg_d[:, 2, 0:256], qT[0:64, 0, :], R=qTb))
                dbg_ops.append(P.dma("sp", dbg_d[:, 3, 0:256], kT[0:64, 0, 0:256], R=[kTb[0][0]]))
                dbg_ops.append(P.dma("sp", dbg_d[:, 4:6, :].rearrange("p a (h d) -> p (a h) d", h=4)[:, :, 0:65], Va[0:64, 0, :, :], R=[Vab[0]]))
                dbg_ops.append(P.dma("sp", dbg_d[:, 6, 0:256], oa[0:64, :], R=[oab]))
                dbg_ops.append(P.dma("sp", dbg_d[:, 7, 0:256], rden[0:64, :], R=[rdenb]))
                dbg_ops.append(P.dma("sp", dbg_d[:, 8, 0:256], PT[0][0:64, :], R=[PTb[0]]))
                dbg_ops.append(P.dma("sp", dbg_d[:, 9, 0:256], PT[1][0:64, :], R=[PTb[1]]))
                dbg_ops.append(P.dma("sp", dbg_d[:, 10, 0:256], ncA[0:64, :], R=[mcb]))
                dbg_ops.append(P.dma("sp", dbg_d[0:10, 11, 0:256], qaug[0][0:10, :], R=[qaugb[0]]))
                dbg_ops.append(P.dma("sp", dbg_d[0:10, 12, 0:256], kaug[0:10, 0:256], R=[kaugb]))
            for dh in range(4):
                for dd in range(2):
                    dc = dh * 2 + dd
                    s2 = 0
                    P.dma("sp", wo[s2][0:64], mbout_d[dc], W=[wob[s2]])

                    def mo(e, s2=s2, dd=dd):
                        ins = None
                        for h in range(8):
                            ins = e.matmul(pbank[7][:, dd * G:(dd + 1) * G], wo[s2][0:64, h, :], oh[0:64, h, :], start=(h == 0), stop=(h == 7))
                        return ins
                    P.op("pe", mo, R=[wob[s2]] + ohb, W=[pbb[7]])
                si = cnt["wo"] % 2
                cnt["wo"] += 1
                P.op("act", lambda e, si=si: e.activation(out=stg[si], in_=pbank[7][:, 0:2 * G].rearrange("p (c t) -> p c t", c=2), func=AF.Copy),
                     R=[pbb[7]], W=[stgb[si]])
                P.dma("sp", mscr_d[:, dh * 2:(dh + 1) * 2, t0:t0 + G], stg[si], R=[stgb[si]], W=[mscr_b[g]])
        for g_ in range(8):
            do_group(g_)
        P.fence()

    def moba_add():
        A = Arena(arena, ARENA)
        tb = [A.take(4096).rearrange("p (c t) -> p c t", c=8) for i in range(2)]
        tbb = [Buf(), Buf()]
        for g in range(4):
            si = g % 2
            P.dma("sp", tb[si], mscr_d[:, :, g * 512:(g + 1) * 512], R=[mscr_b[2 * g], mscr_b[2 * g + 1]], W=[tbb[si]])
            P.op("dve", lambda e, g=g, si=si: e.tensor_tensor(out=xT[:, :, g * 512:(g + 1) * 512], in0=xT[:, :, g * 512:(g + 1) * 512],
                                                              in1=tb[si], op=ALU.add),
                 R=[tbb[si]] + [xb[c][g] for c in range(8)], W=[xb[c][g] for c in range(8)])
        P.fence()

    P.fence()
    st = stage
    if "f00" in st:
        ffn(0, 0)
    if "moba" in st:
        moba()
    if "rwkv" in st:
        rwkv()
    if "moba" in st:
        moba_add()
    if "f01" in st:
        ffn(0, 2)
    if "f10" in st:
        ffn(1, 0)
    if "hgrn" in st:
        hgrn()
    if "f11" in st:
        ffn(1, 2)
    outs = final_out("final" in st)
    P.emit(nc, k.es, outs + dbg_ops)


PV_NORMG = 0
PV_FINALG = 48
PV_HGNW = 56
PV_LBZ = 64
PV_RW = 80
NPV = 176


class Arena:
    def __init__(self, ap, size):
        self.ap, self.o, self.size = ap, 0, size

    def take(self, n):
        a = self.ap[:, self.o:self.o + n]
        self.o += n
        assert self.o <= self.size, self.o
        return a


def _tile_w_in(w, ncols):
    n = ncols // 128
    return np.ascontiguousarray(w.reshape(8, 128, n, 128).transpose(2, 1, 0, 3))


def _prep_shared(inp):
    sh = {}
    for l in range(2):
        for f, nm in enumerate(("ffn1", "ffn2")):
            sh["wg%d%d" % (l, f)] = _tile_w_in(inp[nm + "_wg"][l], FF)
            sh["wu%d%d" % (l, f)] = _tile_w_in(inp[nm + "_wu"][l], FF)
            wd = inp[nm + "_wd"][l]
            sh["wd%d%d" % (l, f)] = np.ascontiguousarray(wd.reshape(NFC, 128, 8, 128).transpose(2, 1, 0, 3))
    pv = np.zeros((128, NPV), np.float32)
    ng = inp["norm_g"].reshape(6, 8, 128)
    pv[:, PV_NORMG:PV_NORMG + 48] = ng.transpose(2, 0, 1).reshape(128, 48)
    pv[:, PV_FINALG:PV_FINALG + 8] = inp["final_g"].reshape(8, 128).T
    pv[:, PV_HGNW:PV_HGNW + 8] = inp["hg_norm_w"][0].reshape(8, 128).T
    pv[:, PV_LBZ:PV_LBZ + 16] = inp["hg_lb_logits"].reshape(2, 8, 128).transpose(2, 0, 1).reshape(128, 16)
    h64 = lambda v: np.asarray(v).reshape(8, 64).T
    mu = inp["rw_mu"][0]
    pv[0:64, PV_RW:PV_RW + 24] = mu[0:1536].reshape(24, 64).T
    pv[0:64, PV_RW + 24:PV_RW + 32] = h64(inp["rw_w0"][0])
    pv[0:64, PV_RW + 32:PV_RW + 40] = h64(inp["rw_a0"][0])
    pv[0:64, PV_RW + 40:PV_RW + 48] = h64(inp["rw_k_k"][0])
    pv[0:64, PV_RW + 48:PV_RW + 56] = h64(inp["rw_k_a"][0])
    pv[0:64, PV_RW + 64:PV_RW + 72] = h64(inp["rw_r_k"][0])
    pv[0:64, PV_RW + 72:PV_RW + 80] = h64(inp["rw_lnx_w"][0])
    pv[0:64, PV_RW + 80:PV_RW + 88] = h64(inp["rw_lnx_b"][0])
    pv[:, PV_RW + 88] = mu[1536:1664]
    pv[:, PV_RW + 89] = mu[1664:1792]
    wi = inp["ev_w_in"][0]
    sh["rwin"] = np.ascontiguousarray(wi[:, 0:1536].reshape(8, 128, 24, 64).transpose(2, 1, 0, 3))
    sh["rwlo"] = _tile_w_in(wi[:, 1536:1792], 256)
    sh["rww2"] = np.ascontiguousarray(inp["rw_w2"][0])
    sh["rwa2"] = np.ascontiguousarray(inp["rw_a2"][0])
    sh["rwg2"] = np.ascontiguousarray(inp["rw_g2"][0])
    wo_ = inp["ev_w_out"][0]
    sh["rwout"] = np.ascontiguousarray(wo_[0:512].reshape(8, 64, 8, 128).transpose(2, 1, 0, 3))
    sh["pvec"] = pv
    sh["odwin"] = _tile_w_in(inp["od_w_in"][0], 4096)
    sh["odwout"] = _tile_w_in(inp["od_w_out"][0], 1024)
    sh["mbin"] = _tile_w_in(wi[:, 1792:3328], 1536)
    sh["mbout"] = np.ascontiguousarray(wo_[512:1024].reshape(8, 64, 8, 128).transpose(2, 1, 0, 3))
    pos = np.arange(S, dtype=np.float32)
    kaug = np.zeros((10, S), np.float32)
    for j in range(8):
        kaug[j, j * 256:(j + 1) * 256] = 240000.0
    kaug[8] = pos
    kaug[9] = 1.0
    sh["mbkaug"] = kaug
    slopes = np.exp2(-np.arange(1, 9, dtype=np.float32))
    qc = np.zeros((8, 2, S), np.float32)
    qc[:, 0, :] = 8.0 * slopes[:, None]
    qc[:, 1, :] = -8.0 * slopes[:, None] * pos[None, :]
    sh["mbqc"] = qc
    return sh


_NC_CACHE = {}


def run(inputs, stage=ALL_STAGES, ncores=8, trace=False):
    stage = tuple(stage)
    import time
    t0 = time.time()
    inp = {k_: np.asarray(v, dtype=np.float32) for k_, v in inputs.items()}
    sh = _prep_shared(inp)
    t1 = time.time()
    if stage not in _NC_CACHE:
        _NC_CACHE[stage] = build(stage)
    t2 = time.time()
    print("[kernel] prep %.1fs build %.1fs" % (t1 - t0, t2 - t1), flush=True)
    nc = _NC_CACHE[stage]
    in_maps = []
    for b in range(ncores):
        m = dict(sh)
        m["xT"] = np.ascontiguousarray(inp["x"][b].T.reshape(8, 128, S).transpose(1, 0, 2))
        in_maps.append(m)
    t3 = time.time()
    res = run_bass_kernel_spmd(nc, in_maps, core_ids=list(range(ncores)), trace=trace)
    print("[kernel] run %.1fs" % (time.time() - t3), flush=True)
    outs = []
    for b in range(ncores):
        o = np.asarray(res.results[b]["outT"])
        outs.append(o.transpose(1, 0, 2).reshape(D, S).T)
    if "dbg" in stage:
        np.save("dbg_out.npy", np.asarray(res.results[0]["dbg"]))
    return np.stack(outs).astype(np.float32), res


def kernel(**inputs):
    out, _ = run(inputs)
    return out
```

```python
import numpy as np
from contextlib import ExitStack
import concourse.bass as bass
import concourse.mybir as mybir
from concourse.bass_utils import run_bass_kernel_spmd

F32 = mybir.dt.float32
F32R = mybir.dt.float32r
BF16 = mybir.dt.bfloat16
AF = mybir.ActivationFunctionType
ALU = mybir.AluOpType
AX = mybir.AxisListType

D = 1024
S = 2048
FF = 2816
NFC = 22
EPS = 1e-6


class Buf:
    __slots__ = ("lw", "rd", "name")

    def __init__(self, name=""):
        self.lw = None
        self.rd = []
        self.name = name


class Op:
    __slots__ = ("eng", "fn", "deps", "needed", "sem", "val", "is_dma", "prev_dma")

    def __init__(self, eng, fn):
        self.eng = eng
        self.fn = fn
        self.deps = []
        self.needed = False
        self.sem = None
        self.val = 0
        self.is_dma = False
        self.prev_dma = None


ENGS = ("pe", "dve", "act", "pool", "sp")


class Prog:
    NSLOT = 6

    def __init__(self):
        self.ops = {e: [] for e in ENGS}
        self.fence_deps = []
        self.last = {e: None for e in ENGS}
        self.dma_slots = {e: [] for e in ENGS}
        self.dma_count = {e: 0 for e in ENGS}
        self.all_dma_last = {}

    def _collect(self, op, R, W):
        deps = []
        for b in R:
            if b.lw is not None:
                deps.append(b.lw)
        for b in W:
            if b.lw is not None:
                deps.append(b.lw)
            deps.extend(b.rd)
        deps.extend(self.fence_deps)
        seen = set()
        for d in deps:
            if id(d) in seen or d is op:
                continue
            seen.add(id(d))
            if d.eng == "pe" and op.eng == "pe" and not d.is_dma and not op.is_dma:
                continue
            op.deps.append(d)
            d.needed = True
        for b in W:
            b.lw = op
            b.rd = []
        for b in R:
            b.rd.append(op)

    def op(self, eng, fn, R=(), W=()):
        o = Op(eng, fn)
        self._collect(o, R, W)
        self.ops[eng].append(o)
        self.last[eng] = o
        return o

    def dma(self, q, out, in_, R=(), W=()):
        o = Op(q, lambda e: e.dma_start(out=out, in_=in_))
        o.is_dma = True
        o.needed = True
        k = self.dma_count[q]
        self.dma_count[q] += 1
        slot = k % self.NSLOT
        o.sem = ("dma", q, slot)
        o.val = 16 * (k // self.NSLOT + 1)
        slots = self.dma_slots[q]
        if len(slots) <= slot:
            slots.append(None)
        o.prev_dma = slots[slot]
        slots[slot] = o
        self._collect(o, R, W)
        self.ops[q].append(o)
        self.all_dma_last[(q, slot)] = o
        return o

    def fence(self):
        deps = [o for o in self.last.values() if o is not None]
        deps += list(self.all_dma_last.values())
        for d in deps:
            d.needed = True
        self.fence_deps = deps

    def emit(self, nc, es, final_waits):
        sems = {}
        for e in ENGS:
            sems[("eng", e)] = es.enter_context(nc.semaphore("s_" + e))
            for sl in range(len(self.dma_slots[e])):
                sems[("dma", e, sl)] = es.enter_context(nc.semaphore("d_%s_%d" % (e, sl)))
        for e in ENGS:
            cnt = 0
            for o in self.ops[e]:
                if o.is_dma:
                    continue
                o.sem = ("eng", e)
                if o.needed:
                    cnt += 1
                    o.val = cnt
        handles = {"pe": "tensor", "dve": "vector", "act": "scalar", "pool": "gpsimd", "sp": "sync"}
        block = es.enter_context(nc.Block())

        def make(e):
            def body(eng):
                seen = {}
                for o in self.ops[e]:
                    waits = list(o.deps)
                    if o.is_dma and o.prev_dma is not None:
                        waits.append(o.prev_dma)
                    for d in waits:
                        if seen.get(d.sem, 0) < d.val:
                            eng.wait_ge(sems[d.sem], d.val)
                            seen[d.sem] = d.val
                    ins = o.fn(eng)
                    if o.is_dma:
                        ins.then_inc(sems[o.sem], 16)
                    elif o.needed:
                        ins.then_inc(sems[o.sem], 1)
                if e == "sp":
                    for d in final_waits:
                        if seen.get(d.sem, 0) < d.val:
                            eng.wait_ge(sems[d.sem], d.val)
                            seen[d.sem] = d.val
            return body

        for e in ENGS:
            getattr(block, handles[e])(make(e))


def r32(ap):
    return ap


class K:
    def __init__(self, stage):
        self.stage = stage
        self.nc = bass.Bass("TRN2", target_bir_lowering=False)
        self.P = Prog()
        self.es = ExitStack()
        self.wq = 0

    def dram_in(self, name, shape, dt=F32):
        return self.nc.dram_tensor(name, list(shape), dt, kind="ExternalInput").ap()

    def sb(self, name, shape, dt=F32):
        return self.es.enter_context(self.nc.sbuf_tensor(name, list(shape), dt))

    def ps(self, name, shape, dt=F32):
        return self.es.enter_context(self.nc.psum_tensor(name, list(shape), dt))


ALL_STAGES = ("f00", "rwkv", "moba", "f01", "f10", "hgrn", "f11", "final")


def build(stage=ALL_STAGES):
    k = K(stage)
    nc, P, es = k.nc, k.P, k.es
    with es:
        _build(k)
    return nc


def _build(k):
    nc, P = k.nc, k.P
    stage = k.stage
    xT_d = k.dram_in("xT", [128, 8, S])
    pv_d = k.dram_in("pvec", [128, NPV])
    wg_d = [[k.dram_in("wg%d%d" % (l, f), [NFC, 128, 8, 128]) for f in range(2)] for l in range(2)]
    wu_d = [[k.dram_in("wu%d%d" % (l, f), [NFC, 128, 8, 128]) for f in range(2)] for l in range(2)]
    wd_d = [[k.dram_in("wd%d%d" % (l, f), [8, 128, NFC, 128]) for f in range(2)] for l in range(2)]
    odwin_d = k.dram_in("odwin", [32, 128, 8, 128])
    odwout_d = k.dram_in("odwout", [8, 128, 8, 128])
    rwin_d = k.dram_in("rwin", [24, 128, 8, 64])
    rwlo_d = k.dram_in("rwlo", [2, 128, 8, 128])
    rww2_d = k.dram_in("rww2", [64, 512])
    rwa2_d = k.dram_in("rwa2", [64, 512])
    rwg2_d = k.dram_in("rwg2", [128, 512])
    rwout_d = k.dram_in("rwout", [8, 64, 8, 128])
    mbin_d = k.dram_in("mbin", [12, 128, 8, 128])
    mbout_d = k.dram_in("mbout", [8, 64, 8, 128])
    mbkaug_d = k.dram_in("mbkaug", [10, S])
    mbqc_d = k.dram_in("mbqc", [8, 2, S])
    mscr_d = nc.dram_tensor("mscr", [128, 8, S], F32).ap()
    mscr_b = [Buf() for g in range(8)]
    dbg_d = nc.dram_tensor("dbg", [64, 32, 1024], F32, kind="ExternalOutput").ap() if "dbg" in stage else None
    dbg_ops = []
    out_d = nc.dram_tensor("outT", [128, 8, S], F32, kind="ExternalOutput").ap()

    xT = k.sb("xT_sb", [128, 8, S])
    xb = [[Buf("x%d_%d" % (c, g)) for g in range(4)] for c in range(8)]
    pv = k.sb("pv_sb", [128, NPV])
    pvb = Buf("pv")
    ones = k.sb("ones", [128, 128])
    onesb = Buf("ones")
    epsb_t = k.sb("epsc", [128, 4])
    epsb = Buf("eps")
    ARENA = 32800
    arena = k.sb("arena", [128, ARENA])

    pbank = [k.ps("pb%d" % i, [128, 512]) for i in range(8)]
    pbb = [Buf("pb%d" % i) for i in range(8)]

    P.op("pool", lambda e: e.memset(ones[:], 1.0), W=[onesb])
    P.op("pool", lambda e: e.memset(epsb_t[:, 0:1], EPS), W=[epsb])
    P.dma("sp", pv[:], pv_d[:], W=[pvb])
    for c in range(8):
        for g in range(4):
            P.dma("sp", xT[:, c, g * 512:(g + 1) * 512], xT_d[:, c, g * 512:(g + 1) * 512], W=[xb[c][g]])

    ident = k.sb("ident", [128, 128])
    mask4t = k.sb("mask4", [128, 512])
    mask4 = mask4t[:].rearrange("p (c m) -> p c m", c=4)
    resetm = k.sb("resetm", [128, 512])
    lbt = k.sb("lbt", [128, 16])
    cb = Buf("consts")
    lbb = Buf("lb")

    def setup_consts(e):
        e.memset(ident[:], 0.0)
        e.affine_select(out=ident[:], in_=ones[:], pattern=[[1, 128]], compare_op=ALU.is_equal, fill=0.0, base=0, channel_multiplier=-1)
        for i in range(4):
            e.affine_select(out=mask4t[:, i * 128:(i + 1) * 128], in_=ones[:], pattern=[[1, 128]], compare_op=ALU.is_ge, fill=0.0,
                            base=0, channel_multiplier=-1)
            e.memset(mask4t[0:64, i * 128 + 64:(i + 1) * 128], 0.0)
        e.memset(resetm[:], 1.0)
        ins = None
        for i in range(8):
            ins = e.memset(resetm[:, i * 64:i * 64 + 1], 0.0)
        return ins
    P.op("pool", setup_consts, R=[onesb], W=[cb])
    P.op("dve", lambda e: e.tensor_tensor(out=lbt[:, 0:8], in0=pv[:, PV_LBZ + 8:PV_LBZ + 16], in1=pv[:, PV_LBZ:PV_LBZ + 8], op=ALU.subtract),
         R=[pvb], W=[lbb])
    P.op("act", lambda e: e.activation(out=lbt[:, 0:8], in_=lbt[:, 0:8], func=AF.Sigmoid), R=[lbb], W=[lbb])
    P.op("dve", lambda e: e.tensor_scalar(out=lbt[:, 8:16], in0=lbt[:, 0:8], scalar1=-1.0, scalar2=1.0, op0=ALU.mult, op1=ALU.add),
         R=[lbb], W=[lbb])

    mk = k.sb("rwmask", [64, 4 * 512])
    mLs = mk[:, 0:512].rearrange("p (h t) -> p h t", h=8)
    mUs = mk[:, 512:1024].rearrange("p (h t) -> p h t", h=8)
    mUi = mk[:, 1024:1536].rearrange("p (h t) -> p h t", h=8)
    id8 = mk[:, 1536:2048].rearrange("p (h t) -> p h t", h=8)
    omka = k.sb("omka", [64, 8])
    cb2 = Buf("consts2")

    def setup2(e):
        o3 = ones[0:64, :].rearrange("p (a b) -> p a b", a=2)
        e.memset(mk[:], 1.0)
        e.affine_select(out=mLs, in_=mLs, pattern=[[0, 8], [-1, 64]], compare_op=ALU.is_gt, fill=0.0, base=0, channel_multiplier=1)
        e.affine_select(out=mUs, in_=mUs, pattern=[[0, 8], [1, 64]], compare_op=ALU.is_gt, fill=0.0, base=0, channel_multiplier=-1)
        e.affine_select(out=mUi, in_=mUi, pattern=[[0, 8], [1, 64]], compare_op=ALU.is_ge, fill=0.0, base=0, channel_multiplier=-1)
        return e.affine_select(out=id8, in_=id8, pattern=[[0, 8], [1, 64]], compare_op=ALU.is_equal, fill=0.0, base=0, channel_multiplier=-1)
    P.op("pool", setup2, W=[cb2])
    P.op("dve", lambda e: e.tensor_scalar(out=omka[:], in0=pv[0:64, PV_RW + 48:PV_RW + 56], scalar1=-1.0, scalar2=1.0, op0=ALU.mult, op1=ALU.add),
         R=[pvb], W=[cb2])
    P.op("pool", lambda e: e.memset(epsb_t[:, 1:2], 64e-5), W=[epsb])

    def rstd_group(g, rstd_ap, rstd_buf, sq_ap, sq_buf, bank, ndiv=1024.0):
        P.op("act", lambda e: e.activation(out=sq_ap, in_=xT[:, :, g * 512:(g + 1) * 512], func=AF.Square),
             R=[xb[c][g] for c in range(8)], W=[sq_buf])

        def mm(e):
            ins = None
            for c in range(8):
                ins = e.matmul(pbank[bank][:], ones[:], sq_ap[:, c, :], start=(c == 0), stop=(c == 7))
            return ins
        P.op("pe", mm, R=[sq_buf, onesb], W=[pbb[bank]])
        P.op("act", lambda e: e.activation(out=rstd_ap, in_=pbank[bank][:], func=AF.Ln, bias=epsb_t[:, 0:1], scale=1.0 / ndiv),
             R=[pbb[bank], epsb], W=[rstd_buf])
        P.op("act", lambda e: e.activation(out=rstd_ap, in_=rstd_ap, func=AF.Exp, scale=-0.5),
             R=[rstd_buf], W=[rstd_buf])

    def ffn(l, which):
        f = 0 if which == 0 else 1
        gcol = PV_NORMG + (l * 3 + which) * 8
        A = Arena(arena, ARENA)
        hT = A.take(8192).bitcast(BF16).rearrange("p (c t) -> p c t", c=8)
        act = A.take(11264).bitcast(BF16).rearrange("p (c t) -> p c t", c=11)
        sq = arena[:, 8192:8192 + 4096].rearrange("p (c t) -> p c t", c=8)
        rstd = A.take(512)
        sg = [A.take(512) for i in range(2)]
        NW = 3
        wgb = [A.take(512).bitcast(BF16).rearrange("p (c m) -> p c m", c=8) for i in range(NW)]
        wub = [A.take(512).bitcast(BF16).rearrange("p (c m) -> p c m", c=8) for i in range(NW)]
        wdb = [A.take(704).bitcast(BF16).rearrange("p (c m) -> p c m", c=11) for i in range(2)]
        wgs = [A.take(1024).rearrange("p (c m) -> p c m", c=8) for i in range(2)]
        wus = [A.take(1024).rearrange("p (c m) -> p c m", c=8) for i in range(2)]
        wds = [A.take(1408).rearrange("p (c m) -> p c m", c=11) for i in range(2)]
        wgsb = [Buf(), Buf()]; wusb = [Buf(), Buf()]; wdsb = [Buf(), Buf()]
        hb = [[Buf() for g in range(4)] for c in range(8)]
        actb = [[Buf() for g in range(4)] for c in range(11)]
        sqb, rstdb = Buf(), Buf()
        sgb = [Buf(), Buf()]
        wgbb = [Buf() for i in range(NW)]
        wubb = [Buf() for i in range(NW)]
        wdbb = [Buf(), Buf()]
        cnt = {"w": 0, "wd": 0, "p": 0, "s": 0, "ws": 0}
        for g in range(4):
            rstd_group(g, rstd, rstdb, sq, sqb, 7)
            for c in range(8):
                P.op("dve", lambda e, c=c, g=g: e.scalar_tensor_tensor(
                    out=hT[:, c, g * 512:(g + 1) * 512], in0=xT[:, c, g * 512:(g + 1) * 512],
                    scalar=pv[:, gcol + c:gcol + c + 1], in1=rstd, op0=ALU.mult, op1=ALU.mult),
                    R=[xb[c][g], rstdb, pvb], W=[hb[c][g]])
        P.fence()

        def phase_a(fc, fl):
            s = cnt["w"] % NW
            cnt["w"] += 1
            ss_ = cnt["ws"] % 2
            cnt["ws"] += 1
            P.dma("sp", wgs[ss_], wg_d[l][f][fc], W=[wgsb[ss_]])
            P.dma("sp", wus[ss_], wu_d[l][f][fc], W=[wusb[ss_]])
            P.op("pool", lambda e: e.tensor_copy(out=wgb[s], in_=wgs[ss_]), R=[wgsb[ss_]], W=[wgbb[s]])
            P.op("pool", lambda e: e.tensor_copy(out=wub[s], in_=wus[ss_]), R=[wusb[ss_]], W=[wubb[s]])
            def a_group(g):
                bg, bu = (cnt["p"] % 2) * 2, (cnt["p"] % 2) * 2 + 1
                cnt["p"] += 1
                si = cnt["s"] % 2
                cnt["s"] += 1

                def mmg(e):
                    ins = None
                    for c in range(8):
                        ins = e.matmul(pbank[bg][:], wgb[s][:, c, :], hT[:, c, g * 512:(g + 1) * 512], start=(c == 0), stop=(c == 7))
                    return ins

                def mmu(e):
                    ins = None
                    for c in range(8):
                        ins = e.matmul(pbank[bu][:], wub[s][:, c, :], hT[:, c, g * 512:(g + 1) * 512], start=(c == 0), stop=(c == 7))
                    return ins
                P.op("pe", mmg, R=[wgbb[s]] + [hb[c][g] for c in range(8)], W=[pbb[bg]])
                P.op("pe", mmu, R=[wubb[s]] + [hb[c][g] for c in range(8)], W=[pbb[bu]])
                P.op("act", lambda e: e.activation(out=sg[si], in_=pbank[bg][:], func=AF.Silu), R=[pbb[bg]], W=[sgb[si]])
                P.op("dve", lambda e: e.tensor_tensor(out=act[:, fl, g * 512:(g + 1) * 512], in0=pbank[bu][:], in1=sg[si], op=ALU.mult),
                     R=[pbb[bu], sgb[si]], W=[actb[fl][g]])
            for g_ in range(4):
                a_group(g_)

        def phase_b(fh, dc):
            s = cnt["wd"] % 2
            cnt["wd"] += 1
            P.dma("sp", wds[s], wd_d[l][f][dc][:, fh * 11:(fh + 1) * 11, :], W=[wdsb[s]])
            P.op("pool", lambda e: e.tensor_copy(out=wdb[s], in_=wds[s]), R=[wdsb[s]], W=[wdbb[s]])
            def b_group(g):
                bo = 4 + (cnt["p"] % 2)
                cnt["p"] += 1

                def mmd(e):
                    ins = None
                    for fl in range(11):
                        ins = e.matmul(pbank[bo][:], wdb[s][:, fl, :], act[:, fl, g * 512:(g + 1) * 512], start=(fl == 0), stop=(fl == 10))
                    return ins
                P.op("pe", mmd, R=[wdbb[s]] + [actb[fl][g] for fl in range(11)], W=[pbb[bo]])
                P.op("dve", lambda e: e.scalar_tensor_tensor(
                    out=xT[:, dc, g * 512:(g + 1) * 512], in0=pbank[bo][:], scalar=0.5,
                    in1=xT[:, dc, g * 512:(g + 1) * 512], op0=ALU.mult, op1=ALU.add),
                    R=[pbb[bo], xb[dc][g]], W=[xb[dc][g]])
            for g_ in range(4):
                b_group(g_)
        for fh in range(2):
            for fl in range(11):
                phase_a(fh * 11 + fl, fl)
            for dc in range(8):
                phase_b(fh, dc)
        P.fence()

    def final_out(norm):
        o = 0
        sq = arena[:, o:o + 4096].rearrange("p (c t) -> p c t", c=8); o += 4096
        rstd = arena[:, o:o + 512]; o += 512
        ob = [arena[:, o + i * 4096:o + (i + 1) * 4096].rearrange("p (c t) -> p c t", c=8) for i in range(2)]; o += 8192
        sqb, rstdb = Buf(), Buf()
        obb = [Buf(), Buf()]
        outs = []
        for g in range(4):
            s = g % 2
            if norm:
                rstd_group(g, rstd, rstdb, sq, sqb, 7)
                for c in range(8):
                    P.op("dve", lambda e, c=c, g=g, s=s: e.scalar_tensor_tensor(
                        out=ob[s][:, c, :], in0=xT[:, c, g * 512:(g + 1) * 512],
                        scalar=pv[:, PV_FINALG + c:PV_FINALG + c + 1], in1=rstd, op0=ALU.mult, op1=ALU.mult),
                        R=[xb[c][g], rstdb, pvb], W=[obb[s]])
                outs.append(P.dma("sp", out_d[:, :, g * 512:(g + 1) * 512], ob[s], R=[obb[s]]))
            else:
                outs.append(P.dma("sp", out_d[:, :, g * 512:(g + 1) * 512], xT[:, :, g * 512:(g + 1) * 512],
                                  R=[xb[c][g] for c in range(8)]))
        return outs


    def hgrn():
        A = Arena(arena, ARENA)
        hT = A.take(2048).bitcast(BF16).rearrange("p (c t) -> p c t", c=8)
        hb = [Buf() for c in range(8)]
        wts = [A.take(1024).rearrange("p (c m) -> p c m", c=8) for j in range(4)]
        wtsb = [Buf() for j in range(4)]
        wt = [[A.take(512).bitcast(BF16).rearrange("p (c m) -> p c m", c=8) for j in range(4)] for s_ in range(2)]
        wtb = [[Buf() for j in range(4)] for s_ in range(2)]
        names = ["qT", "fT", "lf", "kT", "bT", "eb", "e2", "oTs", "sqo", "rs2", "tmp"]
        T = {n: A.take(512) for n in names}
        TB = {n: Buf(n) for n in names}
        DB = []
        for i in range(2):
            d = {}
            for n in ("qe", "ke", "sgT"):
                d[n] = A.take(512); d[n + "b"] = Buf()
            for n in ("ke2tm", "vtm", "scs"):
                d[n] = A.take(512).rearrange("p (c m) -> p c m", c=4); d[n + "b"] = Buf()
            d["ebl"] = A.take(8); d["eblb"] = Buf()
            DB.append(d)
        state = [A.take(1024).rearrange("p (h v) -> p h v", h=8) for i in range(2)]
        stb = [[Buf() for h in range(8)] for i in range(2)]
        yT = A.take(2048).bitcast(BF16).rearrange("p (c t) -> p c t", c=8)
        yb = [Buf() for h in range(8)]
        wos = [A.take(1024).rearrange("p (c m) -> p c m", c=8) for i in range(1)]
        wosb = [Buf()]
        wo = [A.take(512).bitcast(BF16).rearrange("p (c m) -> p c m", c=8) for i in range(2)]
        wob = [Buf(), Buf()]
        sq = A.take(4096).rearrange("p (c t) -> p c t", c=8); sqb = Buf()
        rstd = A.take(512); rstdb = Buf()
        gcol = PV_NORMG + (1 * 3 + 1) * 8
        scur = [0] * 8
        cnt = {"wo": 0}
        for h in range(8):
            P.op("pool", lambda e, h=h: e.memset(state[0][:, h, :], 0.0), W=[stb[0][h]])

        def norm(g):
            rstd_group(g, rstd, rstdb, sq, sqb, 7)

            def hn(e):
                ins = None
                for c in range(8):
                    ins = e.scalar_tensor_tensor(out=hT[:, c, :], in0=xT[:, c, g * 512:(g + 1) * 512],
                                                 scalar=pv[:, gcol + c:gcol + c + 1], in1=rstd, op0=ALU.mult, op1=ALU.mult)
                return ins
            P.op("dve", hn, R=[xb[c][g] for c in range(8)] + [rstdb, pvb], W=hb)

        def prep(g, h, s_):
            d = DB[s_]
            for j in range(4):
                P.dma("sp", wts[j], odwin_d[j * 8 + h], W=[wtsb[j]])
                P.op("pool", lambda e, j=j: e.tensor_copy(out=wt[s_][j], in_=wts[j]), R=[wtsb[j]], W=[wtb[s_][j]])
            yield

            def proj(j, bank):
                def mm(e):
                    ins = None
                    for c in range(8):
                        ins = e.matmul(pbank[bank][:], wt[s_][j][:, c, :], hT[:, c, :], start=(c == 0), stop=(c == 7))
                    return ins
                P.op("pe", mm, R=[wtb[s_][j]] + hb, W=[pbb[bank]])
            proj(0, 0)
            P.op("act", lambda e: e.activation(out=T["qT"], in_=pbank[0][:], func=AF.Copy), R=[pbb[0]], W=[TB["qT"]])
            yield
            proj(1, 1)
            P.op("act", lambda e: e.activation(out=T["fT"], in_=pbank[1][:], func=AF.Sigmoid), R=[pbb[1]], W=[TB["fT"]])
            yield
            P.op("dve", lambda e: e.tensor_scalar(out=T["fT"], in0=T["fT"], scalar1=lbt[:, 8 + h:9 + h], scalar2=lbt[:, h:h + 1],
                                                  op0=ALU.mult, op1=ALU.add), R=[TB["fT"], lbb], W=[TB["fT"]])
            yield
            P.op("act", lambda e: e.activation(out=T["lf"], in_=T["fT"], func=AF.Ln), R=[TB["fT"]], W=[TB["lf"]])
            P.op("dve", lambda e: e.tensor_scalar(out=T["kT"], in0=T["fT"], scalar1=-1.0, scalar2=1.0, op0=ALU.mult, op1=ALU.add),
                 R=[TB["fT"]], W=[TB["kT"]])
            yield
            P.op("dve", lambda e: e.tensor_tensor_scan(out=T["bT"], data0=resetm[:], data1=T["lf"], initial=0.0, op0=ALU.mult, op1=ALU.add),
                 R=[TB["lf"], cb], W=[TB["bT"]])
            yield
            P.op("act", lambda e: e.activation(out=T["eb"], in_=T["bT"], func=AF.Exp), R=[TB["bT"]], W=[TB["eb"]])
            yield
            P.op("dve", lambda e: e.tensor_tensor(out=d["qe"], in0=T["qT"], in1=T["eb"], op=ALU.mult), R=[TB["qT"], TB["eb"]], W=[d["qeb"]])
            yield
            P.op("act", lambda e: e.activation(out=T["eb"], in_=T["bT"], func=AF.Exp, scale=-1.0), R=[TB["bT"], d["qeb"]], W=[TB["eb"]])
            yield
            P.op("dve", lambda e: e.tensor_tensor(out=d["ke"], in0=T["kT"], in1=T["eb"], op=ALU.mult), R=[TB["kT"], TB["eb"]], W=[d["keb"]])
            yield

            def e2f(e):
                ins = None
                for ci in range(8):
                    ins = e.activation(out=T["e2"][:, ci * 64:(ci + 1) * 64], in_=T["bT"][:, ci * 64:(ci + 1) * 64], func=AF.Exp,
                                       scale=-1.0, bias=T["bT"][:, ci * 64 + 63:ci * 64 + 64])
                return ins
            P.op("act", e2f, R=[TB["bT"]], W=[TB["e2"]])
            yield
            P.op("dve", lambda e: e.tensor_tensor(out=T["e2"], in0=T["e2"], in1=T["kT"], op=ALU.mult), R=[TB["kT"], TB["e2"]], W=[TB["e2"]])
            P.op("act", lambda e: e.activation(out=d["ebl"], in_=T["bT"].rearrange("p (c t) -> p c t", t=64)[:, :, 63], func=AF.Exp),
                 R=[TB["bT"]], W=[d["eblb"]])
            yield

            def tr(e):
                ins = None
                for tt in range(4):
                    ins = e.transpose(pbank[2][:, tt * 128:(tt + 1) * 128], T["e2"][:, tt * 128:(tt + 1) * 128], ident[:])
                return ins
            P.op("pe", tr, R=[TB["e2"], cb], W=[pbb[2]])
            P.op("act", lambda e: e.activation(out=d["ke2tm"], in_=pbank[2][:].rearrange("p (c m) -> p c m", c=4), func=AF.Copy),
                 R=[pbb[2]], W=[d["ke2tmb"]])
            yield

            def vproj(e):
                ins = None
                for tt in range(4):
                    for c in range(8):
                        ins = e.matmul(pbank[3][:, tt * 128:(tt + 1) * 128], hT[:, c, tt * 128:(tt + 1) * 128], wt[s_][2][:, c, :],
                                       start=(c == 0), stop=(c == 7))
                return ins
            P.op("pe", vproj, R=[wtb[s_][2]] + hb, W=[pbb[3]])
            P.op("dve", lambda e: e.tensor_copy(out=d["vtm"], in_=pbank[3][:].rearrange("p (c m) -> p c m", c=4)), R=[pbb[3]], W=[d["vtmb"]])
            yield
            proj(3, 0)
            P.op("act", lambda e: e.activation(out=d["sgT"], in_=pbank[0][:], func=AF.Sigmoid), R=[pbb[0]], W=[d["sgTb"]])
            yield

            def scm(e):
                ins = None
                for p_ in range(4):
                    ins = e.matmul(pbank[4][:, p_ * 128:(p_ + 1) * 128], d["ke"][:, p_ * 128:(p_ + 1) * 128],
                                   d["qe"][:, p_ * 128:(p_ + 1) * 128], start=True, stop=True)
                return ins
            P.op("pe", scm, R=[d["keb"], d["qeb"]], W=[pbb[4]])
            P.op("dve", lambda e: e.tensor_tensor(out=d["scs"], in0=pbank[4][:].rearrange("p (c m) -> p c m", c=4), in1=mask4, op=ALU.mult),
                 R=[pbb[4], cb], W=[d["scsb"]])
            yield

        def chunks(g, h, s_):
            d = DB[s_]

            def one(p_, half):
                ci = p_ * 2 + half
                sc = scur[h]
                cs = p_ * 128 + half * 64
                r0 = half * 64

                def omm(e):
                    if half == 0:
                        e.matmul(pbank[5][:, p_ * 128:(p_ + 1) * 128], d["vtm"][:, p_, :], d["scs"][:, p_, :], start=True, stop=False)
                    return e.matmul(pbank[5][:, cs:cs + 64], state[sc][:, h, :], d["qe"][:, cs:cs + 64], start=False, stop=(half == 1))
                P.op("pe", omm, R=[d["vtmb"], d["scsb"], stb[sc][h], d["qeb"]], W=[pbb[5]])
                P.op("pe", lambda e: e.matmul(pbank[6][:, 0:128], d["ke2tm"][r0:r0 + 64, p_, :], d["vtm"][r0:r0 + 64, p_, :],
                                              start=True, stop=True), R=[d["ke2tmb"], d["vtmb"]], W=[pbb[6]])
                P.op("dve", lambda e: e.scalar_tensor_tensor(
                    out=state[1 - sc][:, h, :], in0=state[sc][:, h, :], scalar=d["ebl"][:, ci:ci + 1], in1=pbank[6][:, 0:128],
                    op0=ALU.mult, op1=ALU.add), R=[stb[sc][h], d["eblb"], pbb[6]], W=[stb[1 - sc][h]])
                scur[h] = 1 - sc
            for p_ in range(4):
                for half in range(2):
                    one(p_, half)
                    yield
            P.op("act", lambda e: e.activation(out=T["oTs"], in_=pbank[5][:], func=AF.Copy), R=[pbb[5]], W=[TB["oTs"]])
            P.op("act", lambda e: e.activation(out=T["sqo"], in_=pbank[5][:], func=AF.Square), R=[pbb[5]], W=[TB["sqo"]])
            yield
            P.op("pe", lambda e: e.matmul(pbank[7][:], ones[:], T["sqo"], start=True, stop=True), R=[TB["sqo"], onesb], W=[pbb[7]])
            P.op("act", lambda e: e.activation(out=T["rs2"], in_=pbank[7][:], func=AF.Ln, bias=epsb_t[:, 0:1], scale=1.0 / 128.0),
                 R=[pbb[7], epsb], W=[TB["rs2"]])
            yield
            P.op("act", lambda e: e.activation(out=T["rs2"], in_=T["rs2"], func=AF.Exp, scale=-0.5), R=[TB["rs2"]], W=[TB["rs2"]])
            yield
            P.op("dve", lambda e: e.scalar_tensor_tensor(out=T["tmp"], in0=T["oTs"], scalar=pv[:, PV_HGNW + h:PV_HGNW + h + 1],
                                                         in1=T["rs2"], op0=ALU.mult, op1=ALU.mult),
                 R=[TB["oTs"], TB["rs2"], pvb], W=[TB["tmp"]])
            yield
            P.op("dve", lambda e: e.tensor_tensor(out=yT[:, h, :], in0=T["tmp"], in1=d["sgT"], op=ALU.mult),
                 R=[TB["tmp"], d["sgTb"]], W=[yb[h]])
            yield

        def outproj(g):
            for dc in range(8):
                s2 = cnt["wo"] % 2
                cnt["wo"] += 1
                P.dma("sp", wos[0], odwout_d[dc], W=[wosb[0]])
                P.op("pool", lambda e, s2=s2: e.tensor_copy(out=wo[s2], in_=wos[0]), R=[wosb[0]], W=[wob[s2]])

                def mo(e, s2=s2):
                    ins = None
                    for h in range(8):
                        ins = e.matmul(pbank[7][:], wo[s2][:, h, :], yT[:, h, :], start=(h == 0), stop=(h == 7))
                    return ins
                P.op("pe", mo, R=[wob[s2]] + yb, W=[pbb[7]])
                P.op("dve", lambda e, dc=dc: e.tensor_tensor(out=xT[:, dc, g * 512:(g + 1) * 512], in0=pbank[7][:],
                                                             in1=xT[:, dc, g * 512:(g + 1) * 512], op=ALU.add),
                     R=[pbb[7], xb[dc][g]], W=[xb[dc][g]])

        def interleave(a_, b_):
            gens = [x for x in (a_, b_) if x is not None]
            while gens:
                for x in list(gens):
                    try:
                        next(x)
                    except StopIteration:
                        gens.remove(x)
        prev = None
        prev_g = None
        idx = 0
        for g in range(4):
            for h in range(8):
                if h == 0:
                    norm(g)
                interleave(prev, prep(g, h, idx % 2))
                if prev is not None and h == 0:
                    outproj(prev_g)
                prev = chunks(g, h, idx % 2)
                prev_g = g
                idx += 1
        interleave(prev, None)
        outproj(3)
        P.fence()

    def rwkv():
        G = 128
        NG = S // G
        A = Arena(arena, ARENA)
        hT = A.take(4 * (G + 2)).bitcast(BF16).rearrange("p (c t) -> p c t", c=8); hb = Buf()
        sq = A.take(8 * G).rearrange("p (c t) -> p c t", c=8); sqb = Buf()
        rstd = A.take(G); rstdb = Buf()
        wrkvs = [A.take(512).rearrange("p (c m) -> p c m", c=8) for i in range(3)]
        wrkvsb = [Buf() for i in range(3)]
        wrkv = [[A.take(256).bitcast(BF16).rearrange("p (c m) -> p c m", c=8) for i in range(3)] for s_ in range(2)]
        wrkvb = [[Buf() for i in range(3)] for s_ in range(2)]
        wlos = A.take(1024).rearrange("p (c m) -> p c m", c=8); wlosb = Buf()
        wlo = [A.take(512).bitcast(BF16).rearrange("p (c m) -> p c m", c=8) for i in range(2)]
        wlob = [Buf(), Buf()]
        yfh = A.take(512).bitcast(BF16).rearrange("p (h t) -> p h t", h=8); yfhb = Buf()
        w2a2 = A.take(512); g2 = A.take(512); wconst = Buf()
        praw = [A.take(G + 1) for i in range(2)]; prawb = [Buf(), Buf()]
        dtmp = [A.take(G) for i in range(2)]; dtmpb = [Buf(), Buf()]
        tw = A.take(G); twb = Buf()
        sg = A.take(G); sgb = Buf()
        QN = ["r", "k", "v", "lw", "a", "kk", "Lw", "e", "at", "bt", "kt", "BhT", "KhT"]
        Q = {n: A.take(8 * G).rearrange("p (h t) -> p h t", h=8) for n in QN}
        QB = {n: Buf(n) for n in QN}
        HO_ = ("TT", "AakT", "Vtm")
        CN = ["P0", "P1", "PT0", "PT1", "Xs", "Us", "H0", "H1", "ArbT", "ArkT", "Bhtm", "Khtm"] + [n + "0" for n in HO_] + [n + "1" for n in HO_]
        Cc = {n: A.take(512).rearrange("p (h t) -> p h t", h=8) for n in CN}
        CB = {n: Buf(n) for n in CN}
        gC = A.take(16).rearrange("p (h c) -> p h c", h=8); gCb = Buf()
        wos = wlos.rearrange("p c m -> p (c m)").rearrange("p (h m) -> p h m", h=8); wosb = wlosb
        wo = [A.take(512).bitcast(BF16).rearrange("p (h m) -> p h m", h=8) for i in range(2)]; wob = [Buf(), Buf()]
        gcol = PV_NORMG + (0 * 3 + 1) * 8
        MU, W0, A0, KK, KA, OMKA, RK, LNW, LNB = PV_RW, PV_RW + 24, PV_RW + 32, PV_RW + 40, PV_RW + 48, PV_RW + 56, PV_RW + 64, PV_RW + 72, PV_RW + 80
        MUWA, MUG = PV_RW + 88, PV_RW + 89
        o64 = ones[0:64, 0:64]

        P.dma("sp", w2a2[0:64, :], rww2_d[:, :], W=[wconst])
        P.dma("sp", w2a2[64:128, :], rwa2_d[:, :], W=[wconst])
        P.dma("sp", g2, rwg2_d[:, :], W=[wconst])
        P.op("pool", lambda e: e.memset(Cc["H0"][0:64], 0.0), W=[CB["H0"]])
        P.op("pool", lambda e: e.memset(hT[:, :, 0:1], 0.0), W=[hb])
        hcur = 0
        woi = 0
        pi = 0
        def do_group(g):
            nonlocal hcur, woi, pi
            t0 = g * G
            g4 = t0 // 512
            xg = [xb[c][g4] for c in range(8)]
            if g > 0:
                P.op("dve", lambda e: e.tensor_copy(out=hT[:, :, 0:1], in_=hT[:, :, G:G + 1]), R=[hb], W=[hb])
            P.op("act", lambda e, t0=t0: e.activation(out=sq, in_=xT[:, :, t0:t0 + G], func=AF.Square), R=xg, W=[sqb])

            def mmn(e):
                ins = None
                for c in range(8):
                    ins = e.matmul(pbank[7][:, 0:G], ones[:], sq[:, c, :], start=(c == 0), stop=(c == 7))
                return ins
            P.op("pe", mmn, R=[sqb, onesb], W=[pbb[7]])
            P.op("act", lambda e: e.activation(out=rstd, in_=pbank[7][:, 0:G], func=AF.Ln, bias=epsb_t[:, 0:1], scale=1.0 / 1024.0),
                 R=[pbb[7], epsb], W=[rstdb])
            P.op("act", lambda e: e.activation(out=rstd, in_=rstd, func=AF.Exp, scale=-0.5), R=[rstdb], W=[rstdb])

            def hnorm(e, t0=t0):
                ins = None
                for c in range(8):
                    ins = e.scalar_tensor_tensor(out=hT[:, c, 1:G + 1], in0=xT[:, c, t0:t0 + G], scalar=pv[:, gcol + c:gcol + c + 1],
                                                 in1=rstd, op0=ALU.mult, op1=ALU.mult)
                return ins
            P.op("dve", hnorm, R=xg + [rstdb, pvb, hb], W=[hb])
            for h in range(8):
                for qi, qn in enumerate(("r", "k", "v")):
                    P.dma("sp", wrkvs[qi], rwin_d[qi * 8 + h], W=[wrkvsb[qi]])
                    ws_ = h % 2
                    P.op("pool", lambda e, qi=qi, ws_=ws_: e.tensor_copy(out=wrkv[ws_][qi], in_=wrkvs[qi]), R=[wrkvsb[qi]], W=[wrkvb[ws_][qi]])
                    bank = pi % 2
                    sl = pi % 2
                    pi += 1

                    def mm(e, qi=qi, bank=bank, ws_=ws_):
                        ins = None
                        for c in range(8):
                            ins = e.matmul(pbank[bank][0:64, 0:G + 1], wrkv[ws_][qi][:, c, :], hT[:, c, 0:G + 1], start=(c == 0), stop=(c == 7))
                        return ins
                    P.op("pe", mm, R=[wrkvb[ws_][qi], hb], W=[pbb[bank]])
                    P.op("act", lambda e, bank=bank, sl=sl: e.activation(out=praw[sl][0:64, :], in_=pbank[bank][0:64, 0:G + 1], func=AF.Copy),
                         R=[pbb[bank]], W=[prawb[sl]])
                    P.op("dve", lambda e, sl=sl: e.tensor_tensor(out=dtmp[sl][0:64, :], in0=praw[sl][0:64, 0:G], in1=praw[sl][0:64, 1:G + 1],
                                                                 op=ALU.subtract), R=[prawb[sl]], W=[dtmpb[sl]])
                    P.op("dve", lambda e, sl=sl, qn=qn, qi=qi, h=h: e.scalar_tensor_tensor(
                        out=Q[qn][0:64, h, :], in0=dtmp[sl][0:64, :], scalar=pv[0:64, MU + qi * 8 + h:MU + qi * 8 + h + 1],
                        in1=praw[sl][0:64, 1:G + 1], op0=ALU.mult, op1=ALU.add), R=[dtmpb[sl], prawb[sl], pvb], W=[QB[qn]])
            for j in range(2):
                P.dma("sp", wlos, rwlo_d[j], W=[wlosb])
                P.op("pool", lambda e, j=j: e.tensor_copy(out=wlo[j], in_=wlos), R=[wlosb], W=[wlob[j]])
                bank = pi % 2
                sl = pi % 2
                pi += 1

                def mml(e, j=j, bank=bank):
                    ins = None
                    for c in range(8):
                        ins = e.matmul(pbank[bank][:, 0:G + 1], wlo[j][:, c, :], hT[:, c, 0:G + 1], start=(c == 0), stop=(c == 7))
                    return ins
                P.op("pe", mml, R=[wlob[j], hb], W=[pbb[bank]])
                P.op("act", lambda e, bank=bank, sl=sl: e.activation(out=praw[sl], in_=pbank[bank][:, 0:G + 1], func=AF.Copy),
                     R=[pbb[bank]], W=[prawb[sl]])
                P.op("dve", lambda e, sl=sl: e.tensor_tensor(out=dtmp[sl], in0=praw[sl][:, 0:G], in1=praw[sl][:, 1:G + 1], op=ALU.subtract),
                     R=[prawb[sl]], W=[dtmpb[sl]])
                dst, dstb = (tw, twb) if j == 0 else (sg, sgb)
                mcol = MUWA if j == 0 else MUG
                P.op("dve", lambda e, sl=sl, dst=dst, mcol=mcol: e.scalar_tensor_tensor(
                    out=dst, in0=dtmp[sl], scalar=pv[:, mcol:mcol + 1], in1=praw[sl][:, 1:G + 1], op0=ALU.mult, op1=ALU.add),
                    R=[dtmpb[sl], prawb[sl], pvb], W=[dstb])
            P.op("act", lambda e: e.activation(out=tw[0:64, :], in_=tw[0:64, :], func=AF.Tanh), R=[twb], W=[twb])
            P.op("act", lambda e: e.activation(out=sg, in_=sg, func=AF.Sigmoid), R=[sgb], W=[sgb])
            for half in range(2):
                def mmw(e, half=half):
                    ins = None
                    for hh in range(4):
                        h = half * 4 + hh
                        ins = e.matmul(pbank[2][0:64, hh * G:(hh + 1) * G], w2a2[0:64, h * 64:(h + 1) * 64], tw[0:64, :], start=True, stop=True)
                    return ins
                P.op("pe", mmw, R=[wconst, twb], W=[pbb[2]])

                def sw(e, half=half):
                    ins = None
                    for hh in range(4):
                        h = half * 4 + hh
                        ins = e.activation(out=Q["lw"][0:64, h, :], in_=pbank[2][0:64, hh * G:(hh + 1) * G], func=AF.Sigmoid,
                                           bias=pv[0:64, W0 + h:W0 + h + 1])
                    return ins
                P.op("act", sw, R=[pbb[2], pvb], W=[QB["lw"]])

                def mma(e, half=half):
                    ins = None
                    for hh in range(4):
                        h = half * 4 + hh
                        ins = e.matmul(pbank[3][0:64, hh * G:(hh + 1) * G], w2a2[64:128, h * 64:(h + 1) * 64], tw[64:128, :], start=True, stop=True)
                    return ins
                P.op("pe", mma, R=[wconst, twb], W=[pbb[3]])

                def sa(e, half=half):
                    ins = None
                    for hh in range(4):
                        h = half * 4 + hh
                        ins = e.activation(out=Q["a"][0:64, h, :], in_=pbank[3][0:64, hh * G:(hh + 1) * G], func=AF.Sigmoid,
                                           bias=pv[0:64, A0 + h:A0 + h + 1])
                    return ins
                P.op("act", sa, R=[pbb[3], pvb], W=[QB["a"]])
            P.op("dve", lambda e: e.tensor_scalar(out=Q["lw"][0:64], in0=Q["lw"][0:64], scalar1=-0.6065306597126334, scalar2=None, op0=ALU.mult),
                 R=[QB["lw"]], W=[QB["lw"]])
            def kk1(e):
                ins = None
                for h in range(8):
                    ins = e.tensor_scalar(out=Q["kk"][0:64, h, :], in0=Q["k"][0:64, h, :], scalar1=pv[0:64, KK + h:KK + h + 1], scalar2=None, op0=ALU.mult)
                return ins
            P.op("dve", kk1, R=[QB["k"], pvb], W=[QB["kk"]])
            P.op("act", lambda e: e.activation(out=Q["e"][0:64], in_=Q["kk"][0:64], func=AF.Square), R=[QB["kk"]], W=[QB["e"]])
            for half in range(2):
                P.op("pe", lambda e, half=half: e.matmul(pbank[2][0:64, :], o64, Q["e"][0:64, half * 4:(half + 1) * 4, :], start=True, stop=True),
                     R=[QB["e"], onesb], W=[pbb[2]])
                P.op("dve", lambda e, half=half: e.tensor_scalar(out=Q["e"][0:64, half * 4:(half + 1) * 4, :],
                                                                 in0=pbank[2][0:64, :].rearrange("p (h t) -> p h t", h=4),
                                                                 scalar1=1e-24, scalar2=None, op0=ALU.max), R=[pbb[2], QB["e"]], W=[QB["e"]])
            P.op("act", lambda e: e.activation(out=Q["e"][0:64], in_=Q["e"][0:64], func=AF.Ln), R=[QB["e"]], W=[QB["e"]])
            P.op("act", lambda e: e.activation(out=Q["e"][0:64], in_=Q["e"][0:64], func=AF.Exp, scale=-0.5), R=[QB["e"]], W=[QB["e"]])
            P.op("dve", lambda e: e.tensor_tensor(out=Q["kk"][0:64], in0=Q["kk"][0:64], in1=Q["e"][0:64], op=ALU.mult),
                 R=[QB["kk"], QB["e"]], W=[QB["kk"]])
            def km1(e):
                ins = None
                for h in range(8):
                    ins = e.tensor_scalar(out=Q["e"][0:64, h, :], in0=Q["a"][0:64, h, :], scalar1=pv[0:64, KA + h:KA + h + 1],
                                          scalar2=omka[0:64, h:h + 1], op0=ALU.mult, op1=ALU.add)
                return ins
            P.op("dve", km1, R=[QB["a"], pvb, cb2], W=[QB["e"]])
            P.op("dve", lambda e: e.tensor_tensor(out=Q["k"][0:64], in0=Q["k"][0:64], in1=Q["e"][0:64], op=ALU.mult),
                 R=[QB["k"], QB["e"]], W=[QB["k"]])
            P.op("dve", lambda e: e.tensor_tensor(out=Q["a"][0:64], in0=Q["a"][0:64], in1=Q["kk"][0:64], op=ALU.mult),
                 R=[QB["a"], QB["kk"]], W=[QB["a"]])
            def bn1(e):
                ins = None
                for h in range(8):
                    ins = e.scalar_tensor_tensor(out=Q["e"][0:64, h, :], in0=Q["r"][0:64, h, :], scalar=pv[0:64, RK + h:RK + h + 1],
                                                 in1=Q["k"][0:64, h, :], op0=ALU.mult, op1=ALU.mult)
                return ins
            P.op("dve", bn1, R=[QB["r"], QB["k"], pvb], W=[QB["e"]])
            def scn(e):
                ins = None
                for h in range(8):
                    ins = e.tensor_tensor_scan(out=Q["Lw"][0:64, h, :], data0=resetm[0:64, 0:G], data1=Q["lw"][0:64, h, :], initial=0.0,
                                               op0=ALU.mult, op1=ALU.add)
                return ins
            P.op("dve", scn, R=[QB["lw"], cb], W=[QB["Lw"]])
            P.op("dve", lambda e: e.tensor_tensor(out=Q["lw"][0:64], in0=Q["Lw"][0:64], in1=Q["lw"][0:64], op=ALU.subtract),
                 R=[QB["Lw"], QB["lw"]], W=[QB["lw"]])
            for half in range(2):
                P.op("pe", lambda e, half=half: e.matmul(pbank[3][0:64, :], o64, Q["e"][0:64, half * 4:(half + 1) * 4, :], start=True, stop=True),
                     R=[QB["e"], onesb], W=[pbb[3]])
                P.op("dve", lambda e, half=half: e.tensor_tensor(out=Q["BhT"][0:64, half * 4:(half + 1) * 4, :],
                                                                 in0=pbank[3][0:64, :].rearrange("p (h t) -> p h t", h=4),
                                                                 in1=Q["v"][0:64, half * 4:(half + 1) * 4, :], op=ALU.mult),
                     R=[pbb[3], QB["v"]], W=[QB["BhT"]])
            P.op("act", lambda e: e.activation(out=Q["e"][0:64], in_=Q["lw"][0:64], func=AF.Exp), R=[QB["lw"]], W=[QB["e"]])
            P.op("dve", lambda e: e.scalar_tensor_tensor(out=Q["at"][0:64], in0=Q["kk"][0:64], scalar=-1.0, in1=Q["e"][0:64], op0=ALU.mult, op1=ALU.mult),
                 R=[QB["kk"], QB["e"]], W=[QB["at"]])
            P.op("act", lambda e: e.activation(out=Q["lw"][0:64], in_=Q["BhT"][0:64], func=AF.Copy), R=[QB["BhT"], QB["e"]], W=[QB["lw"]])
            P.op("act", lambda e: e.activation(out=Q["e"][0:64], in_=Q["Lw"][0:64], func=AF.Exp), R=[QB["Lw"], QB["at"]], W=[QB["e"]])
            P.op("dve", lambda e: e.tensor_tensor(out=Q["r"][0:64], in0=Q["r"][0:64], in1=Q["e"][0:64], op=ALU.mult), R=[QB["r"], QB["e"]], W=[QB["r"]])
            P.op("act", lambda e: e.activation(out=Q["e"][0:64], in_=Q["Lw"][0:64], func=AF.Exp, scale=-1.0), R=[QB["Lw"], QB["r"]], W=[QB["e"]])
            P.op("dve", lambda e: e.tensor_tensor(out=Q["bt"][0:64], in0=Q["a"][0:64], in1=Q["e"][0:64], op=ALU.mult), R=[QB["a"], QB["e"]], W=[QB["bt"]])
            P.op("dve", lambda e: e.tensor_tensor(out=Q["kt"][0:64], in0=Q["k"][0:64], in1=Q["e"][0:64], op=ALU.mult), R=[QB["k"], QB["e"]], W=[QB["kt"]])
            def eld(e):
                ins = None
                for h in range(8):
                    for ci in range(G // 64):
                        ins = e.activation(out=Q["e"][0:64, h, ci * 64:(ci + 1) * 64], in_=Q["Lw"][0:64, h, ci * 64:(ci + 1) * 64], func=AF.Exp,
                                           scale=-1.0, bias=Q["Lw"][0:64, h, ci * 64 + 63:ci * 64 + 64])
                return ins
            P.op("act", eld, R=[QB["Lw"], QB["bt"], QB["kt"]], W=[QB["e"]])
            P.op("act", lambda e: e.activation(out=gC[0:64], in_=Q["Lw"][0:64].rearrange("p h (c t) -> p h c t", t=64)[:, :, :, 63], func=AF.Exp),
                 R=[QB["Lw"]], W=[gCb])
            P.op("dve", lambda e: e.tensor_tensor(out=Q["BhT"][0:64], in0=Q["a"][0:64], in1=Q["e"][0:64], op=ALU.mult),
                 R=[QB["a"], QB["e"], QB["lw"]], W=[QB["BhT"]])
            P.op("dve", lambda e: e.tensor_tensor(out=Q["KhT"][0:64], in0=Q["k"][0:64], in1=Q["e"][0:64], op=ALU.mult),
                 R=[QB["k"], QB["e"]], W=[QB["KhT"]])
            if dbg_d is not None and g == 0:
                for i_, n_ in enumerate(["r", "k", "v", "lw", "a", "kk", "Lw", "at", "bt", "kt", "BhT", "KhT"]):
                    dbg_ops.append(P.dma("sp", dbg_d[:, i_, :].rearrange("p (h t) -> p h t", h=8), Q[n_][0:64], R=[QB[n_]]))
            def chunk_indep(ci):
                cs = ci * 64
                sfx = str(ci % 2)
                N_ = lambda n: n + sfx if n in HO_ else n

                def amat(bank, ln, rn, dst, mask, eng):
                    def mm(e):
                        ins = None
                        for h in range(8):
                            ins = e.matmul(pbank[bank][0:64, h * 64:(h + 1) * 64], Q[ln][0:64, h, cs:cs + 64], Q[rn][0:64, h, cs:cs + 64],
                                           start=True, stop=True)
                        return ins
                    P.op("pe", mm, R=[QB[ln], QB[rn]], W=[pbb[bank]])
                    P.op(eng, lambda e: e.tensor_tensor(out=Cc[N_(dst)][0:64], in0=pbank[bank][0:64, :].rearrange("p (h t) -> p h t", h=8),
                                                        in1=mask, op=ALU.mult), R=[pbb[bank], cb2], W=[CB[N_(dst)]])
                amat(2, "at", "bt", "P0", mLs, "dve")
                yield
                amat(3, "bt", "at", "PT0", mUs, "dve")
                yield
                amat(2, "kt", "at", "AakT", mUs, "dve")
                yield
                P.op("dve", lambda e: e.tensor_tensor(out=Cc[N_("TT")][0:64], in0=Cc["PT0"][0:64], in1=id8, op=ALU.add), R=[CB["PT0"], cb2], W=[CB[N_("TT")]])
                def do_tr(pairs):
                    for src, dst in pairs:
                        def trp(e, src=src):
                            ins = None
                            for h in range(8):
                                ins = e.transpose(pbank[4][0:64, h * 64:(h + 1) * 64], Q[src][0:64, h, cs:cs + 64], ident[0:64, 0:64])
                            return ins
                        P.op("pe", trp, R=[QB[src], cb], W=[pbb[4]])
                        P.op("act", lambda e, dst=dst: e.activation(out=Cc[N_(dst)][0:64], in_=pbank[4][0:64, :].rearrange("p (h t) -> p h t", h=8), func=AF.Copy),
                             R=[pbb[4]], W=[CB[N_(dst)]])
                        yield

                yield from do_tr((("v", "Vtm"),))
                pc = 0
                for lev in range(1, 6):
                    Pn, Pp = "P%d" % (1 - pc), "P%d" % pc
                    PTn, PTp = "PT%d" % (1 - pc), "PT%d" % pc

                    def sqm(e, Pp=Pp, PTp=PTp):
                        ins = None
                        for h in range(8):
                            ins = e.matmul(pbank[2][0:64, h * 64:(h + 1) * 64], Cc[PTp][0:64, h, :], Cc[Pp][0:64, h, :], start=True, stop=True)
                        return ins
                    P.op("pe", sqm, R=[CB[Pp], CB[PTp]], W=[pbb[2]])
                    P.op("act", lambda e, Pn=Pn: e.activation(out=Cc[Pn][0:64], in_=pbank[2][0:64, :].rearrange("p (h t) -> p h t", h=8), func=AF.Copy),
                         R=[pbb[2]], W=[CB[Pn]])
                    yield
                    if lev < 5:
                        def sqt(e, Pp=Pp, PTp=PTp):
                            ins = None
                            for h in range(8):
                                ins = e.matmul(pbank[3][0:64, h * 64:(h + 1) * 64], Cc[Pp][0:64, h, :], Cc[PTp][0:64, h, :], start=True, stop=True)
                            return ins
                        P.op("pe", sqt, R=[CB[Pp], CB[PTp]], W=[pbb[3]])
                        P.op("dve", lambda e, PTn=PTn: e.tensor_copy(out=Cc[PTn][0:64], in_=pbank[3][0:64, :].rearrange("p (h t) -> p h t", h=8)),
                             R=[pbb[3]], W=[CB[PTn]])
                        yield

                    def ttm(e, Pn=Pn):
                        ins = None
                        for h in range(8):
                            ins = e.matmul(pbank[7][0:64, h * 64:(h + 1) * 64], Cc[Pn][0:64, h, :], Cc[N_("TT")][0:64, h, :], start=True, stop=True)
                        return ins
                    P.op("pe", ttm, R=[CB[Pn], CB[N_("TT")]], W=[pbb[7]])
                    P.op("dve", lambda e: e.tensor_tensor(out=Cc[N_("TT")][0:64], in0=pbank[7][0:64, :].rearrange("p (h t) -> p h t", h=8),
                                                          in1=Cc[N_("TT")][0:64], op=ALU.add), R=[pbb[7], CB[N_("TT")]], W=[CB[N_("TT")]])
                    yield
                    pc = 1 - pc
                amat(3, "bt", "r", "ArbT", mUi, "dve")
                yield
                amat(2, "kt", "r", "ArkT", mUi, "dve")
                yield
                yield from do_tr((("BhT", "Bhtm"), ("KhT", "Khtm")))
            def chunk_dep(ci):
                nonlocal hcur
                cs = ci * 64
                sfx = str(ci % 2)
                N_ = lambda n: n + sfx if n in HO_ else n
                Hc, Hn = "H%d" % hcur, "H%d" % (1 - hcur)

                def xmm(e, Hc=Hc):
                    ins = None
                    for h in range(8):
                        e.matmul(pbank[6][0:64, h * 64:(h + 1) * 64], Q["at"][0:64, h, cs:cs + 64], Cc[Hc][0:64, h, :], start=True, stop=False)
                        ins = e.matmul(pbank[6][0:64, h * 64:(h + 1) * 64], Cc[N_("AakT")][0:64, h, :], Cc[N_("Vtm")][0:64, h, :], start=False, stop=True)
                    return ins
                P.op("pe", xmm, R=[QB["at"], CB[Hc], CB[N_("AakT")], CB[N_("Vtm")]], W=[pbb[6]])
                P.op("act", lambda e: e.activation(out=Cc["Xs"][0:64], in_=pbank[6][0:64, :].rearrange("p (h t) -> p h t", h=8), func=AF.Copy),
                     R=[pbb[6]], W=[CB["Xs"]])
                yield
                yield
                yield
                yield

                def umm(e):
                    ins = None
                    for h in range(8):
                        ins = e.matmul(pbank[6][0:64, h * 64:(h + 1) * 64], Cc[N_("TT")][0:64, h, :], Cc["Xs"][0:64, h, :], start=True, stop=True)
                    return ins
                P.op("pe", umm, R=[CB[N_("TT")], CB["Xs"]], W=[pbb[6]])
                P.op("act", lambda e: e.activation(out=Cc["Us"][0:64], in_=pbank[6][0:64, :].rearrange("p (h t) -> p h t", h=8), func=AF.Copy),
                     R=[pbb[6]], W=[CB["Us"]])
                yield
                yield
                yield
                yield

                def ymm(e, Hc=Hc):
                    ins = None
                    for h in range(8):
                        e.matmul(pbank[0][0:64, h * 64:(h + 1) * 64], Cc[Hc][0:64, h, :], Q["r"][0:64, h, cs:cs + 64], start=True, stop=False)
                        e.matmul(pbank[0][0:64, h * 64:(h + 1) * 64], Cc["Us"][0:64, h, :], Cc[N_("ArbT")][0:64, h, :], start=False, stop=False)
                        ins = e.matmul(pbank[0][0:64, h * 64:(h + 1) * 64], Cc[N_("Vtm")][0:64, h, :], Cc[N_("ArkT")][0:64, h, :], start=False, stop=True)
                    return ins
                P.op("pe", ymm, R=[CB[Hc], QB["r"], CB["Us"], CB[N_("ArbT")], CB[N_("Vtm")], CB[N_("ArkT")]], W=[pbb[0]])
                P.op("act", lambda e: e.activation(out=Q["Lw"][0:64, :, cs:cs + 64], in_=pbank[0][0:64, :].rearrange("p (h t) -> p h t", h=8), func=AF.Copy),
                     R=[pbb[0], gCb], W=[QB["Lw"]])

                def hmm(e):
                    ins = None
                    for h in range(8):
                        e.matmul(pbank[1][0:64, h * 64:(h + 1) * 64], Cc[N_("Bhtm")][0:64, h, :], Cc["Us"][0:64, h, :], start=True, stop=False)
                        ins = e.matmul(pbank[1][0:64, h * 64:(h + 1) * 64], Cc[N_("Khtm")][0:64, h, :], Cc[N_("Vtm")][0:64, h, :], start=False, stop=True)
                    return ins
                P.op("pe", hmm, R=[CB[N_("Bhtm")], CB["Us"], CB[N_("Khtm")], CB[N_("Vtm")]], W=[pbb[1]])

                def hup(e, Hc=Hc, Hn=Hn, ci=ci):
                    ins = None
                    for h in range(8):
                        ins = e.scalar_tensor_tensor(out=Cc[Hn][0:64, h, :], in0=Cc[Hc][0:64, h, :], scalar=gC[0:64, h, ci:ci + 1],
                                                     in1=pbank[1][0:64, h * 64:(h + 1) * 64], op0=ALU.mult, op1=ALU.add)
                    return ins
                P.op("dve", hup, R=[CB[Hc], gCb, pbb[1]], W=[CB[Hn]])
                hcur = 1 - hcur
                yield
                if dbg_d is not None and g == 0 and ci == 0:
                    for i_, n_ in enumerate(["P0", "PT0", "TT0", "AakT0", "ArbT", "ArkT", "Vtm0", "Bhtm", "Khtm", "Xs", "Us", Hn]):
                        dbg_ops.append(P.dma("sp", dbg_d[:, 12 + i_, 0:512].rearrange("p (h t) -> p h t", h=8), Cc[n_][0:64], R=[CB[n_]]))
            def _il(a_, b_):
                gens = [x for x in (a_, b_) if x is not None]
                while gens:
                    for x in list(gens):
                        try:
                            next(x)
                        except StopIteration:
                            gens.remove(x)
            nch = G // 64
            _il(chunk_indep(0), None)
            for ci_ in range(nch):
                _il(chunk_dep(ci_), chunk_indep(ci_ + 1) if ci_ + 1 < nch else None)
            if dbg_d is not None and g == 0:
                dbg_ops.append(P.dma("sp", dbg_d[:, 24, :].rearrange("p (h t) -> p h t", h=8), Q["Lw"][0:64], R=[QB["Lw"]]))
            yr = Q["Lw"]; yrb = QB["Lw"]
            cen = Q["at"]; cenb = QB["at"]
            for half in range(2):
                P.op("pe", lambda e, half=half: e.matmul(pbank[2][0:64, :], o64, yr[0:64, half * 4:(half + 1) * 4, :], start=True, stop=True),
                     R=[yrb, onesb], W=[pbb[2]])
                P.op("dve", lambda e, half=half: e.scalar_tensor_tensor(out=cen[0:64, half * 4:(half + 1) * 4, :],
                                                                        in0=pbank[2][0:64, :].rearrange("p (h t) -> p h t", h=4), scalar=-1.0 / 64.0,
                                                                        in1=yr[0:64, half * 4:(half + 1) * 4, :], op0=ALU.mult, op1=ALU.add),
                     R=[pbb[2], yrb, CB["H0"], CB["H1"]], W=[cenb])
            P.op("act", lambda e: e.activation(out=Q["e"][0:64], in_=cen[0:64], func=AF.Square), R=[cenb, QB["BhT"], QB["KhT"]], W=[QB["e"]])
            for half in range(2):
                P.op("pe", lambda e, half=half: e.matmul(pbank[3][0:64, :], o64, Q["e"][0:64, half * 4:(half + 1) * 4, :], start=True, stop=True),
                     R=[QB["e"], onesb], W=[pbb[3]])
                P.op("act", lambda e, half=half: e.activation(out=Q["bt"][0:64, half * 4:(half + 1) * 4, :],
                                                               in_=pbank[3][0:64, :].rearrange("p (h t) -> p h t", h=4), func=AF.Ln,
                                                               bias=epsb_t[0:64, 1:2], scale=1.0 / 64.0), R=[pbb[3], epsb], W=[QB["bt"]])
            P.op("act", lambda e: e.activation(out=Q["bt"][0:64], in_=Q["bt"][0:64], func=AF.Exp, scale=-0.5), R=[QB["bt"]], W=[QB["bt"]])
            P.op("dve", lambda e: e.tensor_tensor(out=cen[0:64], in0=cen[0:64], in1=Q["bt"][0:64], op=ALU.mult), R=[cenb, QB["bt"]], W=[cenb])

            def lnx(e):
                ins = None
                for h in range(8):
                    ins = e.tensor_scalar(out=cen[0:64, h, :], in0=cen[0:64, h, :], scalar1=pv[0:64, LNW + h:LNW + h + 1],
                                          scalar2=pv[0:64, LNB + h:LNB + h + 1], op0=ALU.mult, op1=ALU.add)
                return ins
            P.op("dve", lnx, R=[cenb, pvb], W=[cenb])
            P.op("dve", lambda e: e.tensor_tensor(out=cen[0:64], in0=cen[0:64], in1=Q["lw"][0:64], op=ALU.add), R=[cenb, QB["lw"]], W=[cenb])
            for half in range(2):
                def gmm(e, half=half):
                    ins = None
                    for hh in range(4):
                        h = half * 4 + hh
                        ins = e.matmul(pbank[2][0:64, hh * G:(hh + 1) * G], g2[:, h * 64:(h + 1) * 64], sg, start=True, stop=True)
                    return ins
                P.op("pe", gmm, R=[wconst, sgb], W=[pbb[2]])
                P.op("dve", lambda e, half=half: e.tensor_tensor(out=yfh[0:64, half * 4:(half + 1) * 4, :], in0=pbank[2][0:64, :].rearrange("p (h t) -> p h t", h=4),
                                                                 in1=cen[0:64, half * 4:(half + 1) * 4, :], op=ALU.mult),
                     R=[pbb[2], cenb], W=[yfhb])
            yf = yfh; yfb = yfhb
            for dh in range(2):
                for dd in range(4):
                    dc = dh * 4 + dd
                    s2 = woi % 2
                    woi += 1
                    stg_ap, stg_b = (wos, wosb) if dc % 2 == 0 else (sq, sqb)
                    ob_ = 7 if dh == 0 else 4
                    P.dma("sp", stg_ap[0:64], rwout_d[dc], W=[stg_b])
                    P.op("pool", lambda e, s2=s2, stg_ap=stg_ap: e.tensor_copy(out=wo[s2][0:64], in_=stg_ap[0:64]), R=[stg_b], W=[wob[s2]])

                    def mo(e, s2=s2, dd=dd, dh=dh, ob_=ob_):
                        ins = None
                        for h in range(8):
                            ins = e.matmul(pbank[ob_][:, dd * G:(dd + 1) * G], wo[s2][0:64, h, :], yf[0:64, h, :], start=(h == 0), stop=(h == 7))
                        return ins
                    P.op("pe", mo, R=[wob[s2], yfb], W=[pbb[ob_]])
                P.op("dve", lambda e, dh=dh, t0=t0, ob_=ob_: e.tensor_tensor(out=xT[:, dh * 4:(dh + 1) * 4, t0:t0 + G],
                                                                    in0=pbank[ob_][:, 0:4 * G].rearrange("p (c t) -> p c t", c=4),
                                                                    in1=xT[:, dh * 4:(dh + 1) * 4, t0:t0 + G], op=ALU.add),
                     R=[pbb[ob_]] + [xb[c][g4] for c in range(dh * 4, dh * 4 + 4)], W=[xb[c][g4] for c in range(dh * 4, dh * 4 + 4)])
        for g_ in range(NG):
            do_group(g_)
        P.fence()


    def moba():
        G = 256
        NEG = 240000.0
        A = Arena(arena, ARENA)
        kT = A.take(8192).rearrange("p (c t) -> p c t", c=4)
        kTb = [[Buf() for g in range(8)] for c in range(4)]
        Va = A.take(16 * 8 * 65).rearrange("p (k h d) -> p k h d", k=16, h=8)
        Vab = [Buf() for kt in range(16)]
        hT = A.take(2048).rearrange("p (c t) -> p c t", c=8); hb = Buf()
        sq = A.take(2048).rearrange("p (c t) -> p c t", c=8); sqb = Buf()
        rstd = A.take(256); rstdb = Buf()
        wt = [A.take(1024).rearrange("p (c m) -> p c m", c=8) for i in range(2)]; wtb = [Buf(), Buf()]
        qT = A.take(1024).rearrange("p (c t) -> p c t", c=4); qTb = [Buf() for c in range(4)]
        ksum = A.take(32).rearrange("p (c j) -> p c j", c=4); ksb = Buf()
        gsel = A.take(16).rearrange("p (a j) -> p a j", a=2); gselb = Buf()
        m8 = A.take(16).rearrange("p (a j) -> p a j", a=2); m8b = Buf()
        negm = A.take(16).rearrange("p (a j) -> p a j", a=2); negmb = Buf()
        qaug = [A.take(256) for i in range(2)]; qaugb = [Buf(), Buf()]
        kaug = A.take(2048); kaugb = Buf()
        PT = [A.take(256) for i in range(2)]; PTb = [Buf() for i in range(2)]
        stmp = [A.take(256) for i in range(1)]; stmpb = [Buf()]
        oa = A.take(256); oab = Buf()
        rden = A.take(256); rdenb = Buf()
        oh = A.take(2048).rearrange("p (h t) -> p h t", h=8); ohb = [Buf() for h in range(8)]
        wo = [A.take(1024).rearrange("p (h m) -> p h m", h=8) for i in range(1)]; wob = [Buf()]
        stg = [A.take(512).rearrange("p (c t) -> p c t", c=2) for i in range(2)]; stgb = [Buf(), Buf()]
        ncA = A.take(256); ncB = A.take(256); sel65 = A.take(64); mcb = Buf()
        gcol = PV_NORMG + (0 * 3 + 1) * 8
        cnt = {"w": 0, "p": 0, "pt": 0, "st": 0, "wo": 0, "qa": 0}

        def setup(e):
            e.memset(ncA[:], 0.0)
            e.memset(ncB[:], 0.0)
            e.affine_select(out=ncA[:], in_=ncA[:], pattern=[[1, 256]], compare_op=ALU.is_ge, fill=-NEG, base=0, channel_multiplier=-1)
            e.affine_select(out=ncB[:], in_=ncB[:], pattern=[[1, 256]], compare_op=ALU.is_ge, fill=-NEG, base=-128, channel_multiplier=-1)
            e.memset(sel65[0:64, :], 0.0)
            e.memset(sel65[64:65, :], 1.0)
            return e.memset(Va[:, :, :, 64:65], 1.0)
        P.op("pool", setup, W=[mcb] + Vab)
        P.dma("sp", kaug[0:10, :], mbkaug_d[:, :], W=[kaugb])

        def proj_fm(widx, dst_ap, dst_bufs):
            s_ = cnt["w"] % 2
            cnt["w"] += 1
            bank = cnt["p"] % 2
            cnt["p"] += 1
            P.dma("sp", wt[s_], mbin_d[widx], W=[wtb[s_]])

            def mm(e):
                ins = None
                for c in range(8):
                    ins = e.matmul(pbank[bank][:, 0:G], wt[s_][:, c, :], hT[:, c, :], start=(c == 0), stop=(c == 7))
                return ins
            P.op("pe", mm, R=[wtb[s_], hb], W=[pbb[bank]])
            P.op("act", lambda e: e.activation(out=dst_ap, in_=pbank[bank][:, 0:G], func=AF.Copy), R=[pbb[bank]], W=dst_bufs)

        def proj_v(g, hp):
            s_ = cnt["w"] % 2
            cnt["w"] += 1
            bank = cnt["p"] % 2
            cnt["p"] += 1
            P.dma("sp", wt[s_], mbin_d[8 + hp], W=[wtb[s_]])

            def mm(e):
                ins = None
                for tt in range(2):
                    for c in range(8):
                        ins = e.matmul(pbank[bank][:, tt * 128:(tt + 1) * 128], hT[:, c, tt * 128:(tt + 1) * 128], wt[s_][:, c, :],
                                       start=(c == 0), stop=(c == 7))
                return ins
            P.op("pe", mm, R=[wtb[s_], hb], W=[pbb[bank]])
            P.op("dve", lambda e: e.tensor_copy(out=Va[:, 2 * g:2 * g + 2, 2 * hp:2 * hp + 2, 0:64],
                                                in_=pbank[bank][:, 0:256].rearrange("p (k h d) -> p k h d", k=2, h=2)),
                 R=[pbb[bank]], W=[Vab[2 * g], Vab[2 * g + 1]])

        def prep_head(g, h):
            hp, ph = h // 2, h % 2
            r0 = ph * 64
            ob = g
            qa = qaug[h % 2]
            qab = qaugb[h % 2]
            P.dma("sp", qa[8:10, :], mbqc_d[h, :, g * G:(g + 1) * G], W=[qab])
            if ob >= 4:
                def gmm(e):
                    ins = None
                    for tt in range(2):
                        ins = e.matmul(pbank[2][:, tt * 8:(tt + 1) * 8], qT[r0:r0 + 64, hp, tt * 128:(tt + 1) * 128], ksum[r0:r0 + 64, hp, :],
                                       start=True, stop=True)
                    return ins
                P.op("pe", gmm, R=[qTb[hp], ksb], W=[pbb[2]])
                P.op("pool", lambda e: e.memset(gsel, -1e30), W=[gselb])
                P.op("dve", lambda e: e.tensor_copy(out=gsel[:, :, 0:ob], in_=pbank[2][:, 0:16].rearrange("p (a j) -> p a j", a=2)[:, :, 0:ob]),
                     R=[pbb[2]], W=[gselb])

                def mx(e):
                    e.max(out=m8[:, 0, :], in_=gsel[:, 0, :])
                    return e.max(out=m8[:, 1, :], in_=gsel[:, 1, :])
                P.op("dve", mx, R=[gselb], W=[m8b])

                def ng(e):
                    ins = None
                    for tt in range(2):
                        ins = e.tensor_scalar(out=negm[:, tt, :], in0=gsel[:, tt, :], scalar1=m8[:, tt, 2:3], scalar2=1.0, op0=ALU.is_ge, op1=ALU.subtract)
                    return ins
                P.op("dve", ng, R=[gselb, m8b], W=[negmb])
                P.op("dve", lambda e: e.memset(negm[:, :, ob:8], 0.0), R=[], W=[negmb])

                def trn(e):
                    ins = None
                    for tt in range(2):
                        ins = e.transpose(pbank[2][0:8, 128 + tt * 128:128 + (tt + 1) * 128], negm[:, tt, :], ident[:])
                    return ins
                P.op("pe", trn, R=[negmb, cb], W=[pbb[2]])
                P.op("act", lambda e: e.activation(out=qa[0:8, :], in_=pbank[2][0:8, 128:384], func=AF.Copy), R=[pbb[2]], W=[qab])
            else:
                P.op("pool", lambda e: e.memset(qa[0:8, :], 0.0), W=[qab])

        def attn_head(g, h, pending_tail=None):
            hp, ph = h // 2, h % 2
            r0 = ph * 64
            ob = g
            qa = qaug[h % 2]
            qab = qaugb[h % 2]
            ob5 = 5 + (h % 2)
            nkt = 2 * ob + 2
            pend = []

            def emit_pv(kt, c0, pti):
                P.op("pe", lambda e: e.matmul(pbank[ob5][0:65, c0:G], Va[:, kt, h, :], PT[pti][:, c0:G], start=(kt == 0), stop=(kt == nkt - 1)),
                     R=[Vab[kt], PTb[pti]], W=[pbb[ob5]])
            for kt in range(nkt):
                diag = kt - 2 * ob
                c0 = 128 if diag == 1 else 0
                n = G - c0
                sb_ = 3 + (cnt["st"] % 2)
                cnt["st"] += 1
                pti = cnt["pt"] % 2
                cnt["pt"] += 1

                def smm(e, kt=kt, c0=c0, sb_=sb_):
                    e.matmul(pbank[sb_][:, c0:G], kT[r0:r0 + 64, hp, kt * 128:(kt + 1) * 128], qT[r0:r0 + 64, hp, c0:G], start=True, stop=False)
                    return e.matmul(pbank[sb_][:, c0:G], kaug[0:10, kt * 128:(kt + 1) * 128], qa[0:10, c0:G], start=False, stop=True)
                P.op("pe", smm, R=[kTb[hp][kt // 2], qTb[hp], kaugb, qab], W=[pbb[sb_]])
                if diag >= 0:
                    nc_ = ncA if diag == 0 else ncB
                    si = 0
                    P.op("dve", lambda e, c0=c0, sb_=sb_, nc_=nc_, si=si: e.tensor_tensor(out=stmp[si][:, c0:G], in0=pbank[sb_][:, c0:G], in1=nc_[:, c0:G], op=ALU.add),
                         R=[pbb[sb_], mcb], W=[stmpb[si]])
                    P.op("act", lambda e, c0=c0, pti=pti, si=si: e.activation(out=PT[pti][:, c0:G], in_=stmp[si][:, c0:G], func=AF.Exp, scale=0.125),
                         R=[stmpb[si]], W=[PTb[pti]])
                else:
                    P.op("act", lambda e, c0=c0, pti=pti, sb_=sb_: e.activation(out=PT[pti][:, c0:G], in_=pbank[sb_][:, c0:G], func=AF.Exp, scale=0.125),
                         R=[pbb[sb_]], W=[PTb[pti]])
                pend.append((kt, c0, pti))
                if len(pend) > 1:
                    emit_pv(*pend.pop(0))
                if pending_tail is not None and kt == min(1, nkt - 1):
                    pending_tail()
                    pending_tail = None
            while pend:
                emit_pv(*pend.pop(0))
            def tail():
                P.op("act", lambda e: e.activation(out=oa[0:65, :], in_=pbank[ob5][0:65, 0:G], func=AF.Copy), R=[pbb[ob5]], W=[oab])
                P.op("pe", lambda e: e.matmul(pbank[7][0:64, 0:G], sel65[0:65, :], oa[0:65, :], start=True, stop=True), R=[oab, mcb], W=[pbb[7]])
                P.op("dve", lambda e: e.reciprocal(out=rden[0:64, :], in_=pbank[7][0:64, 0:G]), R=[pbb[7]], W=[rdenb])
                P.op("dve", lambda e: e.tensor_tensor(out=oh[0:64, h, :], in0=oa[0:64, :], in1=rden[0:64, :], op=ALU.mult), R=[oab, rdenb], W=[ohb[h]])
            return tail

        def do_group(g):
            t0 = g * G
            g4 = t0 // 512
            xg = [xb[c][g4] for c in range(8)]
            P.op("act", lambda e: e.activation(out=sq, in_=xT[:, :, t0:t0 + G], func=AF.Square), R=xg, W=[sqb])

            def mmn(e):
                ins = None
                for c in range(8):
                    ins = e.matmul(pbank[7][:, 0:G], ones[:], sq[:, c, :], start=(c == 0), stop=(c == 7))
                return ins
            P.op("pe", mmn, R=[sqb, onesb], W=[pbb[7]])
            P.op("act", lambda e: e.activation(out=rstd, in_=pbank[7][:, 0:G], func=AF.Ln, bias=epsb_t[:, 0:1], scale=1.0 / 1024.0),
                 R=[pbb[7], epsb], W=[rstdb])
            P.op("act", lambda e: e.activation(out=rstd, in_=rstd, func=AF.Exp, scale=-0.5), R=[rstdb], W=[rstdb])

            def hnorm(e):
                ins = None
                for c in range(8):
                    ins = e.scalar_tensor_tensor(out=hT[:, c, :], in0=xT[:, c, t0:t0 + G], scalar=pv[:, gcol + c:gcol + c + 1],
                                                 in1=rstd, op0=ALU.mult, op1=ALU.mult)
                return ins
            P.op("dve", hnorm, R=xg + [rstdb, pvb], W=[hb])
            for hp in range(4):
                proj_fm(hp, qT[:, hp, :], [qTb[hp]])
                proj_fm(4 + hp, kT[:, hp, t0:t0 + G], [kTb[hp][g]])
                proj_v(g, hp)
            P.op("dve", lambda e: e.tensor_reduce(out=ksum[:, :, g], in_=kT[:, :, t0:t0 + G], axis=AX.X, op=ALU.add),
                 R=[kTb[hp][g] for hp in range(4)], W=[ksb])
            prep_head(g, 0)
            tl = None
            for h in range(8):
                if h + 1 < 8:
                    prep_head(g, h + 1)
                tl = attn_head(g, h, tl)
            tl()
            if dbg_d is not None and g == 0:
                dbg_ops.append(P.dma("sp", dbg_d[:, 0:2, :].rearrange("p a (h t) -> p (a h) t", h=4), oh[0:64], R=ohb))
                dbg_ops.append(P.dma("sp", dbg_d[:, 2, 0:256], qT[0:64, 0, :], R=qTb))
                dbg_ops.append(P.dma("sp", dbg_d[:, 3, 0:256], kT[0:64, 0, 0:256], R=[kTb[0][0]]))
                dbg_ops.append(P.dma("sp", dbg_d[:, 4:6, :].rearrange("p a (h d) -> p (a h) d", h=4)[:, :, 0:65], Va[0:64, 0, :, :], R=[Vab[0]]))
                dbg_ops.append(P.dma("sp", dbg_d[:, 6, 0:256], oa[0:64, :], R=[oab]))
                dbg_ops.append(P.dma("sp", dbg_d[:, 7, 0:256], rden[0:64, :], R=[rdenb]))
                dbg_ops.append(P.dma("sp", dbg_d[:, 8, 0:256], PT[0][0:64, :], R=[PTb[0]]))
                dbg_ops.append(P.dma("sp", dbg_d[:, 9, 0:256], PT[1][0:64, :], R=[PTb[1]]))
                dbg_ops.append(P.dma("sp", dbg_d[:, 10, 0:256], ncA[0:64, :], R=[mcb]))
                dbg_ops.append(P.dma("sp", dbg_d[0:10, 11, 0:256], qaug[0][0:10, :], R=[qaugb[0]]))
                dbg_ops.append(P.dma("sp", dbg_d[0:10, 12, 0:256], kaug[0:10, 0:256], R=[kaugb]))
            for dh in range(4):
                for dd in range(2):
                    dc = dh * 2 + dd
                    w_ap, w_b = (wo[0], wob[0]) if dc % 2 == 0 else (sq[:, 0:4, :].rearrange("p a (b m) -> p (a b) m", m=128), sqb)
                    P.dma("sp", w_ap[0:64], mbout_d[dc], W=[w_b])

                    def mo(e, w_ap=w_ap, dd=dd):
                        ins = None
                        for h in range(8):
                            ins = e.matmul(pbank[7][:, dd * G:(dd + 1) * G], w_ap[0:64, h, :], oh[0:64, h, :], start=(h == 0), stop=(h == 7))
                        return ins
                    P.op("pe", mo, R=[w_b] + ohb, W=[pbb[7]])
                si = cnt["wo"] % 2
                cnt["wo"] += 1
                P.op("act", lambda e, si=si: e.activation(out=stg[si], in_=pbank[7][:, 0:2 * G].rearrange("p (c t) -> p c t", c=2), func=AF.Copy),
                     R=[pbb[7]], W=[stgb[si]])
                P.dma("sp", mscr_d[:, dh * 2:(dh + 1) * 2, t0:t0 + G], stg[si], R=[stgb[si]], W=[mscr_b[g]])
        for g_ in range(8):
            do_group(g_)
        P.fence()

    def moba_add():
        A = Arena(arena, ARENA)
        tb = [A.take(4096).rearrange("p (c t) -> p c t", c=8) for i in range(2)]
        tbb = [Buf(), Buf()]
        for g in range(4):
            si = g % 2
            P.dma("sp", tb[si], mscr_d[:, :, g * 512:(g + 1) * 512], R=[mscr_b[2 * g], mscr_b[2 * g + 1]], W=[tbb[si]])
            P.op("dve", lambda e, g=g, si=si: e.tensor_tensor(out=xT[:, :, g * 512:(g + 1) * 512], in0=xT[:, :, g * 512:(g + 1) * 512],
                                                              in1=tb[si], op=ALU.add),
                 R=[tbb[si]] + [xb[c][g] for c in range(8)], W=[xb[c][g] for c in range(8)])
        P.fence()

    P.fence()
    st = stage
    if "f00" in st:
        ffn(0, 0)
    if "moba" in st:
        moba()
    if "rwkv" in st:
        rwkv()
    if "moba" in st:
        moba_add()
    if "f01" in st:
        ffn(0, 2)
    if "f10" in st:
        ffn(1, 0)
    if "hgrn" in st:
        hgrn()
    if "f11" in st:
        ffn(1, 2)
    outs = final_out("final" in st)
    P.emit(nc, k.es, outs + dbg_ops)


PV_NORMG = 0
PV_FINALG = 48
PV_HGNW = 56
PV_LBZ = 64
PV_RW = 80
NPV = 176


class Arena:
    def __init__(self, ap, size):
        self.ap, self.o, self.size = ap, 0, size

    def take(self, n):
        a = self.ap[:, self.o:self.o + n]
        self.o += n
        assert self.o <= self.size, self.o
        return a


def _tile_w_in(w, ncols):
    n = ncols // 128
    return np.ascontiguousarray(w.reshape(8, 128, n, 128).transpose(2, 1, 0, 3))


def _prep_shared(inp):
    sh = {}
    for l in range(2):
        for f, nm in enumerate(("ffn1", "ffn2")):
            sh["wg%d%d" % (l, f)] = _tile_w_in(inp[nm + "_wg"][l], FF)
            sh["wu%d%d" % (l, f)] = _tile_w_in(inp[nm + "_wu"][l], FF)
            wd = inp[nm + "_wd"][l]
            sh["wd%d%d" % (l, f)] = np.ascontiguousarray(wd.reshape(NFC, 128, 8, 128).transpose(2, 1, 0, 3))
    pv = np.zeros((128, NPV), np.float32)
    ng = inp["norm_g"].reshape(6, 8, 128)
    pv[:, PV_NORMG:PV_NORMG + 48] = ng.transpose(2, 0, 1).reshape(128, 48)
    pv[:, PV_FINALG:PV_FINALG + 8] = inp["final_g"].reshape(8, 128).T
    pv[:, PV_HGNW:PV_HGNW + 8] = inp["hg_norm_w"][0].reshape(8, 128).T
    pv[:, PV_LBZ:PV_LBZ + 16] = inp["hg_lb_logits"].reshape(2, 8, 128).transpose(2, 0, 1).reshape(128, 16)
    h64 = lambda v: np.asarray(v).reshape(8, 64).T
    mu = inp["rw_mu"][0]
    pv[0:64, PV_RW:PV_RW + 24] = mu[0:1536].reshape(24, 64).T
    pv[0:64, PV_RW + 24:PV_RW + 32] = h64(inp["rw_w0"][0])
    pv[0:64, PV_RW + 32:PV_RW + 40] = h64(inp["rw_a0"][0])
    pv[0:64, PV_RW + 40:PV_RW + 48] = h64(inp["rw_k_k"][0])
    pv[0:64, PV_RW + 48:PV_RW + 56] = h64(inp["rw_k_a"][0])
    pv[0:64, PV_RW + 64:PV_RW + 72] = h64(inp["rw_r_k"][0])
    pv[0:64, PV_RW + 72:PV_RW + 80] = h64(inp["rw_lnx_w"][0])
    pv[0:64, PV_RW + 80:PV_RW + 88] = h64(inp["rw_lnx_b"][0])
    pv[:, PV_RW + 88] = mu[1536:1664]
    pv[:, PV_RW + 89] = mu[1664:1792]
    wi = inp["ev_w_in"][0]
    sh["rwin"] = np.ascontiguousarray(wi[:, 0:1536].reshape(8, 128, 24, 64).transpose(2, 1, 0, 3))
    sh["rwlo"] = _tile_w_in(wi[:, 1536:1792], 256)
    sh["rww2"] = np.ascontiguousarray(inp["rw_w2"][0])
    sh["rwa2"] = np.ascontiguousarray(inp["rw_a2"][0])
    sh["rwg2"] = np.ascontiguousarray(inp["rw_g2"][0])
    wo_ = inp["ev_w_out"][0]
    sh["rwout"] = np.ascontiguousarray(wo_[0:512].reshape(8, 64, 8, 128).transpose(2, 1, 0, 3))
    sh["pvec"] = pv
    sh["odwin"] = _tile_w_in(inp["od_w_in"][0], 4096)
    sh["odwout"] = _tile_w_in(inp["od_w_out"][0], 1024)
    sh["mbin"] = _tile_w_in(wi[:, 1792:3328], 1536)
    sh["mbout"] = np.ascontiguousarray(wo_[512:1024].reshape(8, 64, 8, 128).transpose(2, 1, 0, 3))
    pos = np.arange(S, dtype=np.float32)
    kaug = np.zeros((10, S), np.float32)
    for j in range(8):
        kaug[j, j * 256:(j + 1) * 256] = 240000.0
    kaug[8] = pos
    kaug[9] = 1.0
    sh["mbkaug"] = kaug
    slopes = np.exp2(-np.arange(1, 9, dtype=np.float32))
    qc = np.zeros((8, 2, S), np.float32)
    qc[:, 0, :] = 8.0 * slopes[:, None]
    qc[:, 1, :] = -8.0 * slopes[:, None] * pos[None, :]
    sh["mbqc"] = qc
    return sh


_NC_CACHE = {}


def run(inputs, stage=ALL_STAGES, ncores=8, trace=False):
    stage = tuple(stage)
    import time
    t0 = time.time()
    inp = {k_: np.asarray(v, dtype=np.float32) for k_, v in inputs.items()}
    sh = _prep_shared(inp)
    t1 = time.time()
    if stage not in _NC_CACHE:
        _NC_CACHE[stage] = build(stage)
    t2 = time.time()
    print("[kernel] prep %.1fs build %.1fs" % (t1 - t0, t2 - t1), flush=True)
    nc = _NC_CACHE[stage]
    in_maps = []
    for b in range(ncores):
        m = dict(sh)
        m["xT"] = np.ascontiguousarray(inp["x"][b].T.reshape(8, 128, S).transpose(1, 0, 2))
        in_maps.append(m)
    t3 = time.time()
    res = run_bass_kernel_spmd(nc, in_maps, core_ids=list(range(ncores)), trace=trace)
    print("[kernel] run %.1fs" % (time.time() - t3), flush=True)
    outs = []
    for b in range(ncores):
        o = np.asarray(res.results[b]["outT"])
        outs.append(o.transpose(1, 0, 2).reshape(D, S).T)
    if "dbg" in stage:
        np.save("dbg_out.npy", np.asarray(res.results[0]["dbg"]))
    return np.stack(outs).astype(np.float32), res


def kernel(**inputs):
    out, _ = run(inputs)
    return out
```
